# Optimizing a Trainium2 kernel written in Bass

```python
import math
import jax, jax.numpy as jnp
from jax import lax
import numpy as np

D_MODEL = 2048
BATCH = 4
SEQ = 4096
DEPTH = 1

CHUNK = 64
A_HEADS = 4
A_HEAD_DIM = 256
A_WIDTH = A_HEADS * A_HEAD_DIM
CONV_WIDTH = 4
FORGET_BIAS_LO = 3.0
FORGET_BIAS_HI = 6.0
B_HEADS = 8
B_HEAD_DIM = 64
B_V_DIM = 2 * B_HEAD_DIM
B_QK_WIDTH = 2 * B_HEADS * B_HEAD_DIM
B_WIDTH = B_HEADS * B_V_DIM
Q_BLOCK = 128
N_GROUPS = 4
EXPERTS_PER_GROUP = 8
N_EXPERTS = N_GROUPS * EXPERTS_PER_GROUP
TOP_K = 2
D_EXPERT = 512
MOE_BLOCK = 128
DEEPNORM_ALPHA = (2 * DEPTH) ** 0.25
DEEPNORM_BETA = (8 * DEPTH) ** -0.25
LN_EPS = 1e-5
IN_COLS = (A_WIDTH, A_WIDTH, A_WIDTH, A_WIDTH, A_HEADS, A_HEADS,
           B_QK_WIDTH, B_QK_WIDTH, B_WIDTH, D_MODEL, D_MODEL)
V_COL_IDS = (2, 8)
F_COL_ID = 5
N_IN = sum(IN_COLS)

kernel_name = "hybrid_mlstm_diffattn_hmoe_block"


def _col_offsets():
    offs = [0]
    for c in IN_COLS:
        offs.append(offs[-1] + c)
    return offs


def _layer_norm(x, g, b):
    xf = x.astype(jnp.float32)
    mu = jnp.mean(xf, axis=-1, keepdims=True)
    var = jnp.mean(jnp.square(xf - mu), axis=-1, keepdims=True)
    y = (xf - mu) * lax.rsqrt(var + LN_EPS)
    return (y * g + b).astype(x.dtype)


def _causal_dwconv(u, w, bias):
    c = u.shape[-1]
    y = lax.conv_general_dilated(u, w.astype(u.dtype)[:, None, :], window_strides=(1,),
                                 padding=[(CONV_WIDTH - 1, 0)],
                                 dimension_numbers=('NWC', 'WIO', 'NWC'),
                                 feature_group_count=c)
    return y + bias


def _mlstm(q, k, v, i_pre, f_pre):
    B, S, H, dh = q.shape
    nc = S // CHUNK
    f32 = jnp.float32
    to_chunks = lambda t: t.astype(f32).reshape(B, nc, CHUNK, H, dh).transpose(1, 0, 3, 2, 4)
    gate_chunks = lambda t: t.astype(f32).reshape(B, nc, CHUNK, H).transpose(1, 0, 3, 2)
    qc = to_chunks(q)
    kc = to_chunks(k) * (dh ** -0.5)
    vc = to_chunks(v)
    ic = gate_chunks(i_pre)
    lfc = jax.nn.log_sigmoid(gate_chunks(f_pre))
    tril = jnp.tril(jnp.ones((CHUNK, CHUNK), dtype=bool))

    def step(carry, xs):
        C, n, m = carry
        q_, k_, v_, i_, lf_ = xs
        b = jnp.cumsum(lf_, axis=-1)
        dmat = b[..., :, None] - b[..., None, :] + i_[..., None, :]
        dmat = jnp.where(tril, dmat, -jnp.inf)
        inter = b + m[..., None]
        m_t = jnp.maximum(inter, jnp.max(dmat, axis=-1))
        w_intra = jnp.exp(dmat - m_t[..., None]) * jnp.einsum('bhtd,bhsd->bhts', q_, k_)
        w_inter = jnp.exp(inter - m_t)
        num = (w_inter[..., None] * jnp.einsum('bhtk,bhkv->bhtv', q_, C)
               + jnp.einsum('bhts,bhsv->bhtv', w_intra, v_))
        den = w_inter * jnp.einsum('bhtk,bhk->bht', q_, n) + jnp.sum(w_intra, axis=-1)
        h = num / jnp.maximum(jnp.abs(den), jnp.exp(-m_t))[..., None]
        b_last = b[..., -1]
        g = b_last[..., None] - b + i_
        m_new = jnp.maximum(b_last + m, jnp.max(g, axis=-1))
        decay = jnp.exp(b_last + m - m_new)
        wk = jnp.exp(g - m_new[..., None])
        C = decay[..., None, None] * C + jnp.einsum('bhs,bhsk,bhsv->bhkv', wk, k_, v_)
        n = decay[..., None] * n + jnp.einsum('bhs,bhsk->bhk', wk, k_)
        return (C, n, m_new), h

    init = (jnp.zeros((B, H, dh, dh), f32), jnp.zeros((B, H, dh), f32), jnp.zeros((B, H), f32))
    _, h = lax.scan(step, init, (qc, kc, vc, ic, lfc))
    return h.transpose(1, 0, 3, 2, 4).reshape(B, S, H, dh)


def _diff_attention(q, k, v, lam):
    B, S, H, _, d = q.shape
    f32 = jnp.float32
    nqb = S // Q_BLOCK
    q_blocks = jnp.moveaxis(q.reshape(B, nqb, Q_BLOCK, H, 2, d), 1, 0)
    slopes = 2.0 ** (-8.0 * jnp.arange(1, H + 1, dtype=f32) / H)
    k_pos = jnp.arange(S)
    scale = d ** -0.5

    def one_block(args):
        q_blk, blk = args
        q_pos = blk * Q_BLOCK + jnp.arange(Q_BLOCK)
        s = jnp.einsum('bqhnd,bkhnd->bhnqk', q_blk, k).astype(f32) * scale
        dist = jnp.abs(q_pos[:, None] - k_pos[None, :]).astype(f32)
        visible = (k_pos[None, :] // CHUNK) <= (q_pos[:, None] // CHUNK)
        s = jnp.where(visible, s - slopes[None, :, None, None, None] * dist, -jnp.inf)
        p = jax.nn.softmax(s, axis=-1)
        a = p[:, :, 0] - lam * p[:, :, 1]
        return jnp.einsum('bhqk,bkhe->bqhe', a.astype(v.dtype), v)

    o = lax.map(one_block, (q_blocks, jnp.arange(nqb)))
    return jnp.moveaxis(o, 0, 1).reshape(B, S, H, v.shape[-1]).astype(f32)


def _mixer(x, w_in, b_in, conv_w, conv_b, norm_a_g, lq1, lk1, lq2, lk2, norm_b_g,
           w_a, w_b, w_out, lambda_init):
    B, S, _ = x.shape
    f32 = jnp.float32
    offs = _col_offsets()
    z = x @ w_in + b_in
    qa, ka, va, oa, ia, fa, qb, kb, vb, ga, gb = [z[..., offs[j]:offs[j + 1]] for j in range(len(IN_COLS))]
    qk = jax.nn.silu(_causal_dwconv(jnp.concatenate([qa, ka], axis=-1), conv_w, conv_b))
    qa, ka = qk[..., :A_WIDTH], qk[..., A_WIDTH:]
    heads_a = lambda t: t.reshape(B, S, A_HEADS, A_HEAD_DIM)
    h_a = _mlstm(heads_a(qa), heads_a(ka), heads_a(va), ia, fa)
    h_a = jax.nn.sigmoid(oa.astype(f32)).reshape(B, S, A_HEADS, A_HEAD_DIM) * h_a
    mu = jnp.mean(h_a, axis=-1, keepdims=True)
    var = jnp.mean(jnp.square(h_a - mu), axis=-1, keepdims=True)
    h_a = ((h_a - mu) * lax.rsqrt(var + LN_EPS)).reshape(B, S, A_WIDTH) * norm_a_g
    y_a = h_a.astype(x.dtype) @ w_a
    lam = (jnp.exp(jnp.sum(lq1.astype(f32) * lk1.astype(f32)))
           - jnp.exp(jnp.sum(lq2.astype(f32) * lk2.astype(f32))) + lambda_init)
    o_b = _diff_attention(qb.reshape(B, S, B_HEADS, 2, B_HEAD_DIM),
                          kb.reshape(B, S, B_HEADS, 2, B_HEAD_DIM),
                          vb.reshape(B, S, B_HEADS, B_V_DIM), lam)
    o_b = o_b * lax.rsqrt(jnp.mean(jnp.square(o_b), axis=-1, keepdims=True) + LN_EPS) * norm_b_g * (1.0 - lambda_init)
    y_b = o_b.reshape(B, S, B_WIDTH).astype(x.dtype) @ w_b
    merged = jax.nn.sigmoid(ga) * y_a + jax.nn.sigmoid(gb) * y_b
    return merged @ w_out


def _hier_moe(h, w_grp, b_grp, w_exp, b_exp, w_gate, w_up, w_down):
    B, S, D = h.shape
    T = B * S
    M = T * TOP_K
    f32 = jnp.float32
    ht = h.reshape(T, D)
    tok = jnp.arange(T)
    grp_logits = (ht @ w_grp + b_grp).astype(f32)
    grp_sel = jnp.argmax(grp_logits, axis=-1)
    grp_prob = jax.nn.softmax(grp_logits, axis=-1)[tok, grp_sel]
    exp_logits = (ht @ w_exp + b_exp).astype(f32).reshape(T, N_GROUPS, EXPERTS_PER_GROUP)
    in_grp = exp_logits[tok, grp_sel]
    top_val, top_idx = lax.top_k(in_grp, TOP_K)
    slot_w = (grp_prob[:, None] * jax.nn.softmax(top_val, axis=-1)).reshape(M)
    slot_e = (grp_sel[:, None] * EXPERTS_PER_GROUP + top_idx).reshape(M).astype(jnp.int32)
    slot_tok = jnp.repeat(jnp.arange(T, dtype=jnp.int32), TOP_K)
    order = jnp.argsort(slot_e)
    e_sorted = slot_e[order]
    counts = jnp.bincount(slot_e, length=N_EXPERTS)
    starts = jnp.cumsum(counts) - counts
    padded = ((counts + MOE_BLOCK - 1) // MOE_BLOCK) * MOE_BLOCK
    pad_ends = jnp.cumsum(padded)
    pad_starts = pad_ends - padded
    pos = pad_starts[e_sorted] + (jnp.arange(M) - starts[e_sorted])
    n_blocks = M // MOE_BLOCK + N_EXPERTS
    P = n_blocks * MOE_BLOCK
    tok_at = jnp.full((P,), T, jnp.int32).at[pos].set(slot_tok[order])
    w_at = jnp.zeros((P,), f32).at[pos].set(slot_w[order])
    block_e = jnp.clip(jnp.searchsorted(pad_ends, jnp.arange(n_blocks) * MOE_BLOCK, side='right'),
                       0, N_EXPERTS - 1)
    x_pad = jnp.concatenate([ht, jnp.zeros((1, D), ht.dtype)], axis=0)[tok_at].reshape(n_blocks, MOE_BLOCK, D)

    def expert_block(args):
        xb, e = args
        return (jax.nn.silu(xb @ w_gate[e]) * (xb @ w_up[e])) @ w_down[e]

    y = lax.map(expert_block, (x_pad, block_e)).reshape(P, D)
    out = jnp.zeros((T + 1, D), y.dtype).at[tok_at].add(y * w_at[:, None].astype(y.dtype))
    return out[:T].reshape(B, S, D)


def setup_inputs(seed: int = 0) -> dict:
    key = jax.random.key(seed)
    ks = jax.random.split(key, 24)
    nrm = lambda k, shape: jax.random.normal(k, shape, jnp.float32)
    offs = _col_offsets()
    col_scale = jnp.concatenate([jnp.full((c,), DEEPNORM_BETA if j in V_COL_IDS else 1.0, jnp.float32)
                                 for j, c in enumerate(IN_COLS)])
    w_in = nrm(ks[1], (DEPTH, D_MODEL, N_IN)) * (D_MODEL ** -0.5) * col_scale
    b_in = 0.01 * nrm(ks[2], (DEPTH, N_IN))
    b_in = b_in.at[:, offs[F_COL_ID]:offs[F_COL_ID + 1]].add(
        jnp.linspace(FORGET_BIAS_LO, FORGET_BIAS_HI, A_HEADS, dtype=jnp.float32))
    return {
        "x": nrm(ks[0], (BATCH, SEQ, D_MODEL)),
        "w_in": w_in,
        "b_in": b_in,
        "conv_w": nrm(ks[3], (DEPTH, CONV_WIDTH, 2 * A_WIDTH)) * (CONV_WIDTH ** -0.5),
        "conv_b": 0.01 * nrm(ks[4], (DEPTH, 2 * A_WIDTH)),
        "mlstm_norm_g": 1.0 + 0.02 * nrm(ks[5], (DEPTH, A_WIDTH)),
        "lambda_q1": 0.1 * nrm(ks[6], (DEPTH, B_HEAD_DIM)),
        "lambda_k1": 0.1 * nrm(ks[7], (DEPTH, B_HEAD_DIM)),
        "lambda_q2": 0.1 * nrm(ks[8], (DEPTH, B_HEAD_DIM)),
        "lambda_k2": 0.1 * nrm(ks[9], (DEPTH, B_HEAD_DIM)),
        "diff_norm_g": 1.0 + 0.02 * nrm(ks[10], (DEPTH, B_V_DIM)),
        "w_a": nrm(ks[11], (DEPTH, A_WIDTH, D_MODEL)) * (A_WIDTH ** -0.5) * DEEPNORM_BETA,
        "w_b": nrm(ks[12], (DEPTH, B_WIDTH, D_MODEL)) * (B_WIDTH ** -0.5) * DEEPNORM_BETA,
        "w_out": nrm(ks[13], (DEPTH, D_MODEL, D_MODEL)) * (D_MODEL ** -0.5) * DEEPNORM_BETA,
        "ln1_g": 1.0 + 0.02 * nrm(ks[14], (DEPTH, D_MODEL)),
        "ln1_b": 0.02 * nrm(ks[15], (DEPTH, D_MODEL)),
        "w_grp": nrm(ks[16], (DEPTH, D_MODEL, N_GROUPS)) * (D_MODEL ** -0.5),
        "b_grp": 0.01 * nrm(ks[17], (DEPTH, N_GROUPS)),
        "w_exp": nrm(ks[18], (DEPTH, D_MODEL, N_EXPERTS)) * (D_MODEL ** -0.5),
        "b_exp": 0.01 * nrm(ks[19], (DEPTH, N_EXPERTS)),
        "w_gate": nrm(ks[20], (DEPTH, N_EXPERTS, D_MODEL, D_EXPERT)) * (D_MODEL ** -0.5),
        "w_up": nrm(ks[21], (DEPTH, N_EXPERTS, D_MODEL, D_EXPERT)) * (D_MODEL ** -0.5),
        "w_down": nrm(ks[22], (DEPTH, N_EXPERTS, D_EXPERT, D_MODEL)) * (D_EXPERT ** -0.5) * DEEPNORM_BETA,
        "ln2_g": 1.0 + 0.02 * nrm(ks[23], (DEPTH, D_MODEL)),
        "ln2_b": 0.02 * nrm(jax.random.fold_in(ks[23], 1), (DEPTH, D_MODEL)),
    }


def reference(x, w_in, b_in, conv_w, conv_b, mlstm_norm_g, lambda_q1, lambda_k1, lambda_q2, lambda_k2,
              diff_norm_g, w_a, w_b, w_out, ln1_g, ln1_b, w_grp, b_grp, w_exp, b_exp,
              w_gate, w_up, w_down, ln2_g, ln2_b):
    for l in range(DEPTH):
        lambda_init = 0.8 - 0.6 * math.exp(-0.3 * l)
        mix = _mixer(x, w_in[l], b_in[l], conv_w[l], conv_b[l], mlstm_norm_g[l],
                     lambda_q1[l], lambda_k1[l], lambda_q2[l], lambda_k2[l], diff_norm_g[l],
                     w_a[l], w_b[l], w_out[l], lambda_init)
        x = _layer_norm(DEEPNORM_ALPHA * x + mix, ln1_g[l], ln1_b[l])
        ffn = _hier_moe(x, w_grp[l], b_grp[l], w_exp[l], b_exp[l], w_gate[l], w_up[l], w_down[l])
        x = _layer_norm(DEEPNORM_ALPHA * x + ffn, ln2_g[l], ln2_b[l])
    return x
```

```python
import math
from contextlib import ExitStack, contextmanager
import numpy as np
import concourse.bass as bass
import concourse.mybir as mybir
from concourse.bass_utils import run_bass_kernel_spmd

F32 = mybir.dt.float32
BF16 = mybir.dt.bfloat16
I32 = mybir.dt.int32
AF = mybir.ActivationFunctionType
ALU = mybir.AluOpType
AX = mybir.AxisListType

D = 2048
DC = 16
NIN = 11272
O_QA, O_KA, O_VA, O_OA, O_IA, O_FA, O_QB, O_KB, O_VB, O_GA, O_GB = (
    0, 1024, 2048, 3072, 4096, 4100, 4104, 5128, 6152, 7176, 9224)
ALPHA = 2.0 ** 0.25
EPS = 1e-5
NE = 32
BIG = 30000.0
LN16 = math.log(16.0)
ACLAMP = 40.0
import os
ILV = int(os.environ.get('ILV', '1'))
EXPV = int(os.environ.get('EXPV', '0'))
NIL = int(os.environ.get('NIL', '2'))
SLOPES = [2.0 ** (-(h + 1)) for h in range(8)]


class _Stop(Exception):
    pass


class Dep:
    __slots__ = ("w", "rs")

    def __init__(self):
        self.w = None
        self.rs = {}


class _Eng:
    def __init__(self, name, eng, sem):
        self.name, self.eng, self.sem = name, eng, sem
        self.count = 0
        self.waited = {}


class _DmaSem:
    def __init__(self, key, sem):
        self.key, self.sem, self.total = key, sem, 0


class Kern:
    def __init__(self, nc, stack, n_dma_sems=48):
        self.nc = nc
        self.stack = stack
        self.E = {}
        for name, eng in (("pe", nc.tensor), ("act", nc.scalar), ("dve", nc.vector),
                          ("pool", nc.gpsimd), ("sp", nc.sync)):
            sem = stack.enter_context(nc.semaphore("s_" + name))
            self.E[name] = _Eng(name, eng, sem)
        self.dsems = [_DmaSem("d%d" % i, stack.enter_context(nc.semaphore("d%d" % i)))
                      for i in range(n_dma_sems)]
        self.drr = 0
        self.n_ins = 0
        self.uid = 0
        self._cur = None
        self._atomic = 0

    def sb(self, name, shape, dt):
        self.uid += 1
        return self.stack.enter_context(self.nc.sbuf_tensor("%s_%d" % (name, self.uid), list(shape), dt))

    def ps(self, name, shape, dt):
        return self.stack.enter_context(self.nc.psum_tensor(name, list(shape), dt))

    @contextmanager
    def scope(self):
        old = self.stack
        stopped = False
        with ExitStack() as st:
            self.stack = st
            try:
                yield
            except _Stop:
                stopped = True
            if not stopped:
                self.barrier()
        self.stack = old
        if stopped:
            raise _Stop()

    def _wait(self, es, ev):
        key, sem, val = ev
        if es.name == "pe" and key == "pe":
            return
        if es.waited.get(key, 0) >= val:
            return
        es.eng.wait_ge(sem, val)
        es.waited[key] = val

    def _pre(self, es, r, w):
        for d in r:
            if d.w is not None:
                self._wait(es, d.w)
        for d in w:
            if d.w is not None:
                self._wait(es, d.w)
            for ev in d.rs.values():
                self._wait(es, ev)

    def _post(self, ev, r, w):
        for d in r:
            d.rs[ev[0]] = ev
        for d in w:
            d.w = ev
            d.rs = {}

    def op(self, en, fn, r=(), w=()):
        es = self.E[en]
        self._pre(es, r, w)
        ins = fn(es.eng)
        es.count += 1
        ins.then_inc(es.sem, 1)
        self.n_ins += 1
        ev = (es.name, es.sem, es.count)
        self._post(ev, r, w)
        self._yield()
        return ev

    def dma(self, qn, out, in_, r=(), w=(), indirect=None, **kw):
        es = self.E[qn]
        self._pre(es, r, w)
        ds = self.dsems[self.drr]
        self.drr = (self.drr + 1) % len(self.dsems)
        if ds.total > 0:
            self._wait(es, (ds.key, ds.sem, ds.total))
        if indirect is not None:
            ins = es.eng.indirect_dma_start(out=out, in_=in_, **indirect)
        else:
            ins = es.eng.dma_start(out=out, in_=in_, **kw)
        ds.total += 16
        ins.then_inc(ds.sem, 16)
        self.n_ins += 1
        ev = (ds.key, ds.sem, ds.total)
        self._post(ev, r, w)
        self._yield()
        return ev

    def _yield(self):
        w = self._cur
        if w is None or self._atomic > 0:
            return
        self._sched_sem.release()
        w["go"].acquire()

    def interleave(self, fns, width=2):
        import threading
        if width <= 1 or len(fns) <= 1:
            for fn in fns:
                fn()
            return
        self._sched_sem = threading.Semaphore(0)
        pending = list(fns)
        active = []
        err = []

        def runner(w, fn):
            w["go"].acquire()
            try:
                fn()
            except BaseException as e:
                err.append(e)
            w["done"] = True
            self._sched_sem.release()
        while pending or active:
            while pending and len(active) < width:
                w = {"go": threading.Semaphore(0), "done": False}
                w["t"] = threading.Thread(target=runner, args=(w, pending.pop(0)), daemon=True)
                w["t"].start()
                active.append(w)
            for w in list(active):
                self._cur = w
                w["go"].release()
                self._sched_sem.acquire()
                self._cur = None
                if w["done"]:
                    active.remove(w)
                if err:
                    raise err[0]

    @contextmanager
    def atomic(self):
        self._atomic += 1
        try:
            yield
        finally:
            self._atomic -= 1

    def barrier(self):
        evs = [(e.name, e.sem, e.count) for e in self.E.values() if e.count > 0]
        evs += [(d.key, d.sem, d.total) for d in self.dsems if d.total > 0]
        for es in self.E.values():
            for ev in evs:
                if ev[0] == es.name and es.name == "pe":
                    continue
                self._wait(es, ev)


class Ring:
    def __init__(self, items):
        self.items = items
        self.i = 0

    def next(self):
        it = self.items[self.i]
        self.i = (self.i + 1) % len(self.items)
        return it


def build(TP, TO, CAP, dbg=False, stop=99):
    TA = TP + TO
    NTA, NTO, NTP = TA // 128, TO // 128, TP // 128
    NCH, CH0 = TA // 64, TP // 64
    NROW = NE * CAP + 128
    TRASH = NE * CAP
    nc = bass.Bass("TRN2", target_bir_lowering=False)

    def din(name, shape, dt=F32):
        return nc.dram_tensor(name, list(shape), dt, kind="ExternalInput").ap()

    x = din("x", [TA, D])
    w_in = din("w_in", [D, NIN])
    bfm_d = din("bfm", [128, 64]); bfmp_d = din("bfm_pre", [128, 64])
    btm_d = din("btm", [1, 3072]); btmp_d = din("btm_pre", [1, 3072])
    bif_d = din("bif", [4, 2]); bifp_d = din("bif_pre", [4, 2])
    valid_d = din("valid_tm", [128, NTA])
    cw_d = din("cw", [128, 16, 4]); cb_d = din("cb", [128, 16])
    ng_d = din("mlstm_norm_g", [1024]); dg_d = din("diff_norm_g", [128])
    lam_d = din("lamv", [4, 64])
    w_a = din("w_a", [1024, D]); w_b = din("w_b", [1024, D]); w_out = din("w_out", [D, D])
    ln1g_d = din("ln1_g", [D]); ln1b_d = din("ln1_b", [D]); ln2g_d = din("ln2_g", [D]); ln2b_d = din("ln2_b", [D])
    wr_d = din("w_r", [D, 36]); br_d = din("b_r", [1, 36])
    if stop > 6:
        w_gate = din("w_gate", [NE, D, 512]); w_up = din("w_up", [NE, D, 512]); w_down = din("w_down", [NE, 512, D])
    ident_d = din("ident", [128, 128]); mask64_d = din("mask64", [64, 64]); ustrict_d = din("ustrict", [128, 128])
    dgb_d = din("diagbias", [128, 8, 128]); abt_d = din("alibi", [128, 8, 32]); ecap_d = din("ecap", [128, 32])
    out_d = nc.dram_tensor("out", [TO, D], F32, kind="ExternalOutput").ap()

    def dscr(name, shape, dt):
        if dbg:
            return nc.dram_tensor(name, list(shape), dt, kind="ExternalOutput").ap()
        return nc.dram_tensor(name, list(shape), dt).ap()

    s_qkaT = dscr("s_qkaT", [2048, TA], BF16)
    s_qkbT = dscr("s_qkbT", [2048, TA], BF16)
    s_gT = dscr("s_gT", [4096, TO], BF16)
    s_va = dscr("s_va", [TA, 1024], BF16)
    s_vb = dscr("s_vb", [TA, 1024], BF16)
    s_oa = dscr("s_oa", [TO, 1024], BF16)
    s_seq = dscr("s_seq", [3, 4, TA], F32)
    s_dec = dscr("s_dec", [4, NCH], F32)
    s_h1 = dscr("s_h1", [TO, D], F32)
    s_Xg = dscr("s_Xg", [NROW, D], BF16)
    s_Yg = dscr("s_Yg", [NROW, D], BF16)
    s_haT = dscr("s_haT", [1024, TO], BF16)
    s_obT = dscr("s_obT", [1024, TO], BF16)
    s_mgT = dscr("s_mgT", [2048, TO], BF16)
    HB = min(512, TO)
    if dbg:
        s_TT = dscr("s_TT", [64, 3 * NCH * 4], F32)
        s_dtab = dscr("s_dtab", [128, NTO * 2], I32)
        s_wtab = dscr("s_wtab", [128, NTO * 2], F32)

    with ExitStack() as st0:
        k = Kern(nc, st0)
        try:
            banks = [(k.ps("pf%d" % i, [128, 512], F32), Dep()) for i in range(8)]
            pf = Ring(banks[0:6])
            pb = Ring([(banks[i][0].bitcast(BF16), banks[i][1]) for i in (6, 7)])
            d_c = Dep()
            identf = k.sb("identf", [128, 128], F32)
            identb = k.sb("identb", [128, 128], BF16)
            onesb = k.sb("onesb", [128, 128], BF16)
            onesf = k.sb("onesf", [128, 128], F32)
            mask64 = k.sb("mask64", [64, 64], F32)
            ustr = k.sb("ustr", [128, 128], BF16)
            dgb = k.sb("dgb", [128, 8, 128], BF16)
            abt = k.sb("abt", [128, 8, 32], F32)
            ecap = k.sb("ecap", [128, 32], F32)
            validb = k.sb("validb", [128, NTA], BF16)
            zero1 = k.sb("zero1", [128, 1], F32)
            epsc = k.sb("epsc", [128, 1], F32)
            k.dma("sp", identf[:], ident_d, w=[d_c])
            k.dma("pool", identb[:], ident_d, w=[d_c])
            k.dma("sp", mask64[:], mask64_d, w=[d_c])
            k.dma("pool", ustr[:], ustrict_d, w=[d_c])
            k.dma("pool", dgb[:], dgb_d, w=[d_c])
            k.dma("sp", abt[:], abt_d, w=[d_c])
            k.dma("sp", ecap[:], ecap_d, w=[d_c])
            k.dma("pool", validb[:], valid_d, w=[d_c])
            k.op("dve", lambda e: e.memset(onesb[:], 1.0), w=[d_c])
            k.op("dve", lambda e: e.memset(onesf[:], 1.0), w=[d_c])
            k.op("dve", lambda e: e.memset(zero1[:], 0.0), w=[d_c])
            k.op("dve", lambda e: e.memset(epsc[:], EPS), w=[d_c])
            d_zt, d_Xg, d_Yg = Dep(), Dep(), Dep()

            dtab = k.sb("dtab", [128, NTO, 2], I32)
            wtab = k.sb("wtab", [128, NTO, 2], F32)
            TT = k.sb("TT", [64, 3, NCH, 4], F32)
            decbc = k.sb("decbc", [128, 4 * NCH], F32)
            gscope = ExitStack()
            _old = k.stack
            k.stack = gscope
            Gi = k.sb("Gi", [4, TA], F32); Gf = k.sb("Gf", [4, TA], F32)
            k.stack = _old
            d_G = Dep()
            d_tab = Dep()
            d_h1 = Dep()
            d_haT, d_obT, d_mg = Dep(), Dep(), Dep()

            with k.scope():
                zt = k.sb("zt", [128, 4096], BF16)
                k.op("pool", lambda e: e.memset(zt[:], 0.0), w=[d_zt])
                r0 = 0
                while r0 < NROW:
                    nr = min(256, NROW - r0)
                    k.dma("sp", s_Xg[r0:r0 + nr, :].rearrange("(t p) d -> p t d", p=128),
                          zt[:, 0:(nr // 128) * D].rearrange("p (t d) -> p t d", d=D), r=[d_zt], w=[d_Xg])
                    r0 += nr
                k.dma("sp", s_Yg[TRASH:TRASH + 128, :], zt[:, 0:D], r=[d_zt], w=[d_Yg])
                TX = max(TP, TO)
                xT = k.sb("xT", [128, DC, TX], BF16)
                xb = Ring([(k.sb("xb", [128, D], BF16), Dep()) for _ in range(2)])
                wt = Ring([(k.sb("wt", [128, DC, 512], BF16), Dep()) for _ in range(2)])
                wif = k.sb("wif", [128, DC, 8], BF16)
                evf = Ring([(k.sb("evf", [128, 4, 512], BF16), Dep()) for _ in range(2)])
                evt = Ring([(k.sb("evt", [128, 512], BF16), Dep()) for _ in range(3)])
                bfm = k.sb("bfm", [128, 64], F32); bfmp = k.sb("bfmp", [128, 64], F32)
                btm = k.sb("btm", [1, 3072], BF16); btmp = k.sb("btmp", [1, 3072], BF16)
                bif = k.sb("bif", [4, 2], F32); bifp = k.sb("bifp", [4, 2], F32)
                d_b, d_wif = Dep(), Dep()
                k.dma("sp", bfm[:], bfm_d, w=[d_b]); k.dma("sp", bfmp[:], bfmp_d, w=[d_b])
                k.dma("pool", btm[:], btm_d, w=[d_b]); k.dma("pool", btmp[:], btmp_d, w=[d_b])
                k.dma("sp", bif[:], bif_d, w=[d_b]); k.dma("sp", bifp[:], bifp_d, w=[d_b])
                k.dma("pool", wif[:], w_in.rearrange("(c p) n -> p c n", p=128)[:, :, O_IA:O_IA + 8], w=[d_wif])
                d_scr = {"qka": Dep(), "qkb": Dep(), "g": Dep(), "va": Dep(), "vb": Dep(), "oa": Dep()}

                FM = []
                for j in range(2):
                    FM.append((O_QA + 512 * j, "qka", s_qkaT, 512 * j, AF.Identity, "q", 4 * j))
                    FM.append((O_KA + 512 * j, "qka", s_qkaT, 1024 + 512 * j, AF.Identity, "all", 8 + 4 * j))
                    FM.append((O_QB + 512 * j, "qkb", s_qkbT, 512 * j, AF.Identity, "own", 16 + 4 * j))
                    FM.append((O_KB + 512 * j, "qkb", s_qkbT, 1024 + 512 * j, AF.Identity, "all", 24 + 4 * j))
                for j in range(8):
                    FM.append((O_GA + 512 * j, "g", s_gT, 512 * j, AF.Sigmoid, "gate", 32 + 4 * j))
                TM = []
                for j in range(2):
                    TM.append((O_VA + 512 * j, "va", s_va, 512 * j, AF.Identity, "all", 512 * j))
                    TM.append((O_OA + 512 * j, "oa", s_oa, 512 * j, AF.Sigmoid, "own", 1024 + 512 * j))
                    TM.append((O_VB + 512 * j, "vb", s_vb, 512 * j, AF.Identity, "all", 2048 + 512 * j))

                for phase in ("pre", "own"):
                    t0, nt = (0, TP) if phase == "pre" else (TP, TO)
                    bfm_x, btm_x, bif_x = (bfmp, btmp, bifp) if phase == "pre" else (bfm, btm, bif)
                    d_xT = [Dep() for _ in range(nt // 128)]
                    for ti in range(nt // 128):
                        xb_t, xb_d = xb.next()
                        k.dma("pool", xb_t[:], x[t0 + ti * 128:t0 + (ti + 1) * 128, :], w=[xb_d])
                        for g in range(4):
                            pt, pd = pb.next()

                            def f(pe, g=g, pt=pt, xb_t=xb_t):
                                for j in range(4):
                                    c = g * 4 + j
                                    ins = pe.transpose(pt[:, j * 128:(j + 1) * 128], xb_t[:, c * 128:(c + 1) * 128], identb[:])
                                return ins
                            k.op("pe", f, r=[xb_d, d_c], w=[pd])
                            k.op("dve", lambda e, g=g, ti=ti, pt=pt: e.tensor_copy(
                                xT[:, g * 4:(g + 1) * 4, ti * 128:(ti + 1) * 128],
                                pt[:, 0:512].rearrange("p (j n) -> p j n", j=4)), r=[pd], w=[d_xT[ti]])
                    tb = 0
                    while tb < nt:
                        n = min(512, nt - tb)
                        dx = d_xT[tb // 128:(tb + n) // 128]
                        for gi, Gt in ((0, Gi), (1, Gf)):
                            bk, bd = pf.next()

                            def f(pe, gi=gi, bk=bk, tb=tb, n=n):
                                for c in range(DC):
                                    ins = pe.matmul(bk[0:4, 0:n], wif[:, c, gi * 4:(gi + 1) * 4], xT[:, c, tb:tb + n],
                                                    start=(c == 0), stop=(c == DC - 1))
                                return ins
                            k.op("pe", f, r=dx + [d_wif], w=[bd])
                            k.op("act", lambda e, gi=gi, Gt=Gt, bk=bk, tb=tb, n=n: e.activation(
                                Gt[0:4, t0 + tb:t0 + tb + n], bk[0:4, 0:n], AF.Identity, bias=bif_x[:, gi:gi + 1], scale=1.0),
                                r=[bd, d_b], w=[d_G])
                        tb += n
                    for (c0, dkey, dst, row0, func, which, fmc) in FM:
                        if phase == "pre":
                            if which in ("own", "gate"):
                                continue
                            tlo = (TP - 128) if which == "q" else 0
                        else:
                            tlo = 0
                        w_t, w_d = wt.next()
                        k.dma("pool", w_t[:], w_in.rearrange("(c p) n -> p c n", p=128)[:, :, c0:c0 + 512], w=[w_d])
                        tb = tlo
                        while tb < nt:
                            n = min(512, nt - tb)
                            dx = d_xT[tb // 128:(tb + n) // 128]
                            ev_t, ev_d = evf.next()
                            for g in range(4):
                                bk, bd = pf.next()

                                def f(pe, g=g, bk=bk, tb=tb, n=n, w_t=w_t):
                                    for c in range(DC):
                                        ins = pe.matmul(bk[:, 0:n], w_t[:, c, g * 128:(g + 1) * 128], xT[:, c, tb:tb + n],
                                                        start=(c == 0), stop=(c == DC - 1))
                                    return ins
                                k.op("pe", f, r=dx + [w_d], w=[bd])
                                k.op("act", lambda e, g=g, bk=bk, n=n, ev_t=ev_t, func=func, fmc=fmc: e.activation(
                                    ev_t[:, g, 0:n], bk[:, 0:n], func, bias=bfm_x[:, fmc + g:fmc + g + 1], scale=1.0),
                                    r=[bd, d_b], w=[ev_d])
                            tcol = (tb if which == "gate" else t0 + tb)
                            k.dma("sp", dst[row0:row0 + 512, tcol:tcol + n].rearrange("(g p) t -> p g t", p=128),
                                  ev_t[:, :, 0:n], r=[ev_d], w=[d_scr[dkey]])
                            tb += n
                    for (c0, dkey, dst, col0, func, which, bcol) in TM:
                        if phase == "pre" and which == "own":
                            continue
                        w_t, w_d = wt.next()
                        k.dma("pool", w_t[:], w_in.rearrange("(c p) n -> p c n", p=128)[:, :, c0:c0 + 512], w=[w_d])
                        for ti in range(nt // 128):
                            bk, bd = pf.next()

                            def f(pe, bk=bk, ti=ti, w_t=w_t, bcol=bcol):
                                for c in range(DC):
                                    pe.matmul(bk[:, :], xT[:, c, ti * 128:(ti + 1) * 128], w_t[:, c, :],
                                              start=(c == 0), stop=False)
                                return pe.matmul(bk[:, :], onesb[0:1, :], btm_x[0:1, bcol:bcol + 512], start=False, stop=True)
                            k.op("pe", f, r=[d_xT[ti], w_d, d_b, d_c], w=[bd])
                            e_t, e_d = evt.next()
                            k.op("act", lambda e, bk=bk, e_t=e_t, func=func: e.activation(e_t[:], bk[:, :], func),
                                 r=[bd], w=[e_d])
                            trow = (ti * 128 if which == "own" else t0 + ti * 128)
                            k.dma("sp", dst[trow:trow + 128, col0:col0 + 512], e_t[:], r=[e_d], w=[d_scr[dkey]])

            if True:
                if stop <= 1:
                    raise _Stop()
                with k.scope():
                    t1 = k.sb("t1", [4, TA], F32); t2 = k.sb("t2", [4, TA], F32)
                    mt = k.sb("mt", [4, NCH], F32); dec = k.sb("dec", [4, NCH], F32)
                    sq = k.sb("sq", [4, 3, TA], F32)
                    dq = Dep()
                    V = "dve"
                    k.op("act", lambda e: e.activation(t1[:], Gf[:], AF.Abs), r=[d_G], w=[dq])
                    k.op("act", lambda e: e.activation(t1[:], t1[:], AF.Exp, scale=-1.0), r=[dq], w=[dq])
                    k.op("act", lambda e: e.activation(t1[:], t1[:], AF.Ln, bias=1.0, scale=1.0), r=[dq], w=[dq])
                    k.op(V, lambda e: e.tensor_scalar_min(t2[:], Gf[:], 0.0), r=[d_G], w=[dq])
                    k.op(V, lambda e: e.tensor_sub(t2[:], t2[:], t1[:]), r=[dq], w=[dq])
                    k.op(V, lambda e: e.tensor_scalar_mul(t2[:], t2[:], 0.5), r=[dq], w=[dq])
                    k.op(V, lambda e: e.tensor_tensor_scan(t1[:], t2[:], t2[:], 0.0, ALU.add, ALU.add), r=[dq], w=[dq])
                    Bc = t1
                    k.op(V, lambda e: e.tensor_sub(Gi[:], Gi[:], Bc[:]), r=[dq, d_G], w=[dq, d_G])
                    at = Gi
                    k.op(V, lambda e: e.tensor_tensor_scan(t2[:], at[:], at[:], 0.0, ALU.max, ALU.max), r=[dq, d_G], w=[dq])
                    ut = t2
                    ut3 = ut[:].rearrange("p (c s) -> p c s", s=64)
                    at3 = at[:].rearrange("p (c s) -> p c s", s=64)
                    Bc3 = Bc[:].rearrange("p (c s) -> p c s", s=64)
                    k.op(V, lambda e: e.memset(mt[:, 0:1], 0.0), w=[dq])
                    if NCH > 1:
                        k.op(V, lambda e: e.tensor_copy(mt[:, 1:NCH], ut3[:, 0:NCH - 1, 63]), r=[dq], w=[dq])
                    uL = ut3[:, :, 63]
                    mtb = mt[:, :].unsqueeze(2).to_broadcast([4, NCH, 64])
                    k.op(V, lambda e: e.tensor_sub(dec[:], mt[:], uL), r=[dq], w=[dq])
                    k.op("act", lambda e: e.activation(dec[:], dec[:], AF.Exp), r=[dq], w=[dq])
                    sq0 = sq[:, 0, :].rearrange("p (c s) -> p c s", s=64)
                    sq1 = sq[:, 1, :].rearrange("p (c s) -> p c s", s=64)
                    sq2 = sq[:, 2, :].rearrange("p (c s) -> p c s", s=64)
                    k.op(V, lambda e: e.tensor_sub(sq0, at3, mtb), r=[dq, d_G], w=[dq])
                    k.op(V, lambda e: e.tensor_scalar(sq[:, 0, :], sq[:, 0, :], 80.0, -LN16, ALU.min, ALU.add), r=[dq], w=[dq])
                    k.op("act", lambda e: e.activation(sq[:, 0, :], sq[:, 0, :], AF.Exp), r=[dq], w=[dq])
                    k.op(V, lambda e: e.tensor_tensor(sq1, sq0, dec[:, :].unsqueeze(2).to_broadcast([4, NCH, 64]), ALU.mult),
                         r=[dq], w=[dq])
                    k.op(V, lambda e: e.tensor_tensor(sq2, Bc3, mtb, ALU.add), r=[dq], w=[dq])
                    k.op(V, lambda e: e.tensor_scalar(sq[:, 2, :], sq[:, 2, :], -1.0, 80.0, ALU.mult, ALU.min), r=[dq], w=[dq])
                    k.op("act", lambda e: e.activation(sq[:, 2, :], sq[:, 2, :], AF.Exp), r=[dq], w=[dq])
                    d_seq = Dep()
                    k.dma("sp", s_dec, dec[:], r=[dq], w=[d_seq])
                    d_TT = Dep()
                    for q in range(3):
                        c0 = 0
                        while c0 < NCH:
                            ncc = min(128, NCH - c0)
                            bk, bd = pf.next()

                            def f(pe, bk=bk, q=q, c0=c0, ncc=ncc):
                                for cc in range(ncc):
                                    c = c0 + cc
                                    ins = pe.transpose(bk[0:64, cc * 4:cc * 4 + 4], sq[0:4, q, c * 64:(c + 1) * 64], identf[0:4, 0:4])
                                return ins
                            k.op("pe", f, r=[dq, d_c], w=[bd])
                            k.op("act", lambda e, bk=bk, q=q, c0=c0, ncc=ncc: e.copy(
                                TT[:, q, c0:c0 + ncc, :], bk[0:64, 0:ncc * 4].rearrange("p (c h) -> p c h", h=4)), r=[bd], w=[d_TT])
                            c0 += ncc
                    k.dma("sp", decbc[:], s_dec.rearrange("h c -> (h c)").partition_broadcast(128), r=[d_seq], w=[d_TT])
                gscope.close()
                if dbg:
                    k.dma("sp", s_TT, TT[:].rearrange("p a b c -> p (a b c)"), r=[d_TT], w=[Dep()])
                if stop <= 2:
                    raise _Stop()
                with k.scope():
                    qT = k.sb("qT", [128, 8, TO], BF16)
                    kT = k.sb("kT", [128, 8, TA], BF16)
                    cw = k.sb("cw", [128, 16, 4], F32); cb = k.sb("cb", [128, 16], F32)
                    ngb = k.sb("ngb", [64, 1024], F32)
                    d_cw, d_qT, d_kT = Dep(), Dep(), Dep()
                    k.dma("sp", cw[:], cw_d, w=[d_cw]); k.dma("sp", cb[:], cb_d, w=[d_cw])
                    k.dma("sp", ngb[:], ng_d.partition_broadcast(64), w=[d_cw])
                    cscope = k.scope()
                    cscope.__enter__()
                    cin = Ring([(k.sb("cin", [128, 3 + TA], BF16), Dep()) for _ in range(3)])
                    Dg = k.sb("Dg", [128, 16, 4, 128], BF16)
                    d_Dg = Dep()
                    for fc in range(16):
                        for j in range(4):
                            k.op("dve", lambda e, fc=fc, j=j: e.tensor_scalar_mul(Dg[:, fc, j, :], identf[:], cw[:, fc, j:j + 1]),
                                 r=[d_cw, d_c], w=[d_Dg])
                    for fc in list(range(8, 16)) + list(range(8)):
                        isq = fc < 8
                        lo = (TP - 128) if isq else 0
                        o0 = TP if isq else 0
                        n = TA - o0
                        ci, cd = cin.next()
                        if not isq:
                            k.op("pool", lambda e, ci=ci: e.memset(ci[:, 0:3], 0.0), w=[cd])
                        k.dma("sp", ci[:, 3 + lo:3 + TA], s_qkaT[fc * 128:(fc + 1) * 128, lo:TA], r=[d_scr["qka"]], w=[cd])
                        tb = 0
                        while tb < n:
                            nn = min(512, n - tb)
                            bk, bd = pf.next()

                            def f(pe, bk=bk, ci=ci, fc=fc, o0=o0, tb=tb, nn=nn):
                                for j in range(4):
                                    ins = pe.matmul(bk[:, 0:nn], Dg[:, fc, j, :], ci[:, o0 + tb + j:o0 + tb + j + nn],
                                                    start=(j == 0), stop=(j == 3))
                                return ins
                            k.op("pe", f, r=[cd, d_Dg], w=[bd])
                            if isq:
                                k.op("act", lambda e, bk=bk, fc=fc, tb=tb, nn=nn: e.activation(
                                    qT[:, fc, tb:tb + nn], bk[:, 0:nn], AF.Silu, bias=cb[:, fc:fc + 1], scale=1.0),
                                    r=[bd, d_cw], w=[d_qT])
                            else:
                                k.op("act", lambda e, bk=bk, fc=fc, tb=tb, nn=nn: e.activation(
                                    kT[:, fc - 8, tb:tb + nn], bk[:, 0:nn], AF.Silu, bias=cb[:, fc:fc + 1], scale=1.0),
                                    r=[bd, d_cw], w=[d_kT])
                            tb += nn
                    cscope.__exit__(None, None, None)
                    a_S = Ring([banks[0], banks[1]])
                    bO, bOd = banks[2]
                    bO1, bO1d = banks[3]
                    m_U = Ring(banks[4:5])
                    bSN, bSNd = banks[5]
                    bNN, bNNd = banks[6]
                    pb_all = pb
                    pb = Ring([(banks[7][0].bitcast(BF16), banks[7][1])])
                    Cst = [k.sb("Cst", [128, 2, 257], F32) for _ in range(4)]
                    hblk = Ring([(k.sb("hblk", [128, 8, HB], BF16), Dep()) for _ in range(1)])
                    Cbf = [k.sb("Cbf", [128, 2, 257], BF16) for _ in range(4)]
                    d_C = [Dep() for _ in range(4)]
                    d_Cbf = [Dep() for _ in range(4)]
                    for h in range(4):
                        k.op("pool", lambda e, h=h: e.memset(Cst[h][:], 0.0), w=[d_C[h]])
                        k.op("pool", lambda e, h=h: e.memset(Cbf[h][:], 0.0), w=[d_Cbf[h]])
                    vch_items = []
                    for _ in range(3):
                        vt = k.sb("vch", [64, 4, 257], BF16)
                        vd = Dep()
                        k.op("pool", lambda e, vt=vt: e.memset(vt[:, :, 256:257], 1.0), w=[vd])
                        vch_items.append((vt, vd))
                    vch = Ring(vch_items)
                    sor = Ring([(k.sb("so", [64, 1024], BF16), Dep()) for _ in range(1)])
                    kwr = Ring([(k.sb("kw", [64, 256], BF16), Dep()) for _ in range(3)])
                    Wtr = Ring([(k.sb("Wt", [64, 64], BF16), Dep()) for _ in range(3)])
                    Nsr = Ring([(k.sb("Ns", [64, 4, 257], F32), Dep()) for _ in range(2)])
                    hgr = Ring([(k.sb("hg", [64, 4, 256], F32), Dep()) for _ in range(1)])
                    hbr = Ring([(k.sb("hb", [64, 1024], BF16), Dep()) for _ in range(2)])
                    smr = Ring([(k.sb("sm", [64, 64], F32), Dep()) for _ in range(2)])
                    lamt = k.sb("lamt", [128, 4, 64], F32)
                    lsm = k.sb("lsm", [128, 8], F32)
                    gnb = k.sb("gnb", [128, 128], F32)
                    d_l = Dep()
                    k.dma("sp", lamt[:], lam_d.rearrange("a b -> (a b)").partition_broadcast(128).rearrange("p (a b) -> p a b", a=4), w=[d_l])
                    k.dma("sp", gnb[:], dg_d.partition_broadcast(128), w=[d_l])
                    k.op("dve", lambda e: e.tensor_tensor(lamt[:, 0, :], lamt[:, 0, :], lamt[:, 1, :], ALU.mult), r=[d_l], w=[d_l])
                    k.op("dve", lambda e: e.tensor_tensor(lamt[:, 2, :], lamt[:, 2, :], lamt[:, 3, :], ALU.mult), r=[d_l], w=[d_l])
                    k.op("dve", lambda e: e.reduce_sum(lsm[:, 0:1], lamt[:, 0, :], AX.X), r=[d_l], w=[d_l])
                    k.op("dve", lambda e: e.reduce_sum(lsm[:, 1:2], lamt[:, 2, :], AX.X), r=[d_l], w=[d_l])
                    k.op("act", lambda e: e.activation(lsm[:, 2:4], lsm[:, 0:2], AF.Exp), r=[d_l], w=[d_l])
                    k.op("dve", lambda e: e.tensor_sub(lsm[:, 4:5], lsm[:, 3:4], lsm[:, 2:3]), r=[d_l], w=[d_l])
                    k.op("dve", lambda e: e.tensor_scalar_add(lsm[:, 5:6], lsm[:, 4:5], -0.2), r=[d_l], w=[d_l])
                    k.op("dve", lambda e: e.tensor_scalar_mul(gnb[:], gnb[:], 0.8), r=[d_l], w=[d_l])
                    neglam = lsm[:, 5:6]
                    kb_items = []
                    for _ in range(1):
                        kt_ = k.sb("kbT", [128, 2, TA], BF16)
                        kd_ = Dep()
                        k.op("pool", lambda e, kt_=kt_: e.memset(kt_[64:128, 0, :], 0.0), w=[kd_])
                        k.op("pool", lambda e, kt_=kt_: e.memset(kt_[0:64, 1, :], 0.0), w=[kd_])
                        kb_items.append((kt_, kd_))
                    kbr = Ring(kb_items)
                    qbr = Ring([(k.sb("qbT", [128, TO], BF16), Dep()) for _ in range(1)])
                    vb_items = []
                    for _ in range(1):
                        vt = k.sb("vbe", [128, NTA, 129], BF16)
                        vd = Dep()
                        k.op("dve", lambda e, vt=vt: e.tensor_copy(vt[:, :, 128], validb[:, :]), r=[d_c], w=[vd])
                        vb_items.append((vt, vd))
                    vbr = Ring(vb_items)
                    PTr = Ring([(k.sb("PT", [128, 256], BF16), Dep()) for _ in range(4)])
                    o1r = Ring([(k.sb("o1", [128, 128], F32), Dep()) for _ in range(2)])
                    o2r = Ring([(k.sb("o2", [128, 128], F32), Dep()) for _ in range(2)])
                    obr = Ring([(k.sb("ob", [128, 128], BF16), Dep()) for _ in range(2)])
                    s8r = Ring([(k.sb("s8", [128, 8], F32), Dep()) for _ in range(2)])
                    Osr = Ring([(k.sb("Os", [128, 2, 129], F32), Dep()) for _ in range(2)])
                    oblk = Ring([(k.sb("oblk", [128, HB], BF16), Dep()) for _ in range(2)])

                    def mlstm_gen():
                        hb_cur = None
                        for c in range(NCH):
                            own = c >= CH0
                            tq = (c - CH0) * 64
                            v_t, v_d = vch.next()
                            k.dma("sp", v_t[:, :, 0:256], s_va[c * 64:(c + 1) * 64, :].rearrange("s (h d) -> s h d", h=4),
                                  r=[d_scr["va"]], w=[v_d])
                            if own:
                                so_t, so_d = sor.next()
                                k.dma("sp", so_t[:], s_oa[tq:tq + 64, :], r=[d_scr["oa"]], w=[so_d])
                                Ns_t, Ns_d = Nsr.next()
                            for h in range(4):
                                pt, pd = pb.next()

                                def f(pe, pt=pt, h=h, c=c):
                                    for j in range(2):
                                        ins = pe.transpose(pt[0:64, j * 128:(j + 1) * 128], kT[:, h * 2 + j, c * 64:(c + 1) * 64], identb[:])
                                    return ins
                                k.op("pe", f, r=[d_kT, d_c], w=[pd])
                                kw_t, kw_d = kwr.next()
                                k.op("act", lambda e, kw_t=kw_t, pt=pt, h=h, c=c: e.activation(
                                    kw_t[:], pt[0:64, 0:256], AF.Identity, scale=TT[:, 1, c, h:h + 1]), r=[pd, d_TT], w=[kw_d])
                                yield
                                bU, bUd = m_U.next()

                                def f(pe, bU=bU, kw_t=kw_t, v_t=v_t, h=h):
                                    for j in range(2):
                                        pe.matmul(bU[:, j * 256:(j + 1) * 256], kw_t[:, j * 128:(j + 1) * 128], v_t[:, h, 0:256],
                                                  start=True, stop=True)
                                    for j in range(2):
                                        ins = pe.matmul(bSN[:, 400 + j:401 + j], kw_t[:, j * 128:(j + 1) * 128], v_t[:, h, 256:257],
                                                        start=True, stop=True)
                                    return ins
                                k.op("pe", f, r=[kw_d, v_d], w=[bUd, bSNd])
                                if not own:
                                    yield
                                if own:
                                    def f(pe, h=h, c=c, tq=tq):
                                        for j in range(2):
                                            ins = pe.matmul(bSN[0:64, 320:384], kT[:, h * 2 + j, c * 64:(c + 1) * 64],
                                                            qT[:, h * 2 + j, tq:tq + 64], start=(j == 0), stop=(j == 1))
                                        return ins
                                    k.op("pe", f, r=[d_kT, d_qT], w=[bSNd])
                                    W_t, W_d = Wtr.next()
                                    k.op("dve", lambda e, W_t=W_t, h=h, c=c: e.scalar_tensor_tensor(
                                        W_t[:], bSN[0:64, 320:384], TT[:, 0, c, h:h + 1], mask64[:], ALU.mult, ALU.mult),
                                        r=[bSNd, d_TT, d_c], w=[W_d])
                                    yield

                                    def f(pe, W_t=W_t, v_t=v_t, h=h, tq=tq):
                                        for j in range(2):
                                            pe.matmul(bNN[0:64, 0:257], qT[:, h * 2 + j, tq:tq + 64], Cbf[h][:, j, :],
                                                      start=(j == 0), stop=False)
                                        return pe.matmul(bNN[0:64, 0:257], W_t[:], v_t[:, h, :], start=False, stop=True)
                                    k.op("pe", f, r=[d_qT, d_Cbf[h], W_d, v_d], w=[bNNd])
                                    k.op("act", lambda e, Ns_t=Ns_t, h=h: e.copy(Ns_t[:, h, :], bNN[0:64, 0:257]),
                                         r=[bNNd], w=[Ns_d])
                                    yield
                                dsc = decbc[:, h * NCH + c:h * NCH + c + 1]
                                k.op("dve", lambda e, h=h, bU=bU, dsc=dsc: e.scalar_tensor_tensor(
                                    Cst[h][:, :, 0:256], Cst[h][:, :, 0:256], dsc,
                                    bU[:, 0:512].rearrange("p (j n) -> p j n", j=2), ALU.mult, ALU.add),
                                    r=[bUd, d_TT], w=[d_C[h]])
                                k.op("dve", lambda e, h=h, dsc=dsc: e.scalar_tensor_tensor(
                                    Cst[h][:, :, 256:257], Cst[h][:, :, 256:257], dsc,
                                    bSN[:, 400:402].rearrange("p (j n) -> p j n", j=2), ALU.mult, ALU.add),
                                    r=[bSNd, d_TT], w=[d_C[h]])
                                k.op("act", lambda e, h=h: e.copy(Cbf[h][:], Cst[h][:]), r=[d_C[h]], w=[d_Cbf[h]])
                                yield
                            if own:
                                sm_t, sm_d = smr.next()
                                k.op("act", lambda e, sm_t=sm_t, Ns_t=Ns_t: e.activation(
                                    sm_t[:, 0:4], Ns_t[:, :, 256], AF.Abs), r=[Ns_d], w=[sm_d])
                                k.op("dve", lambda e, sm_t=sm_t, c=c: e.tensor_tensor(
                                    sm_t[:, 0:4], sm_t[:, 0:4], TT[:, 2, c, :], ALU.max), r=[sm_d, d_TT], w=[sm_d])
                                k.op("dve", lambda e, sm_t=sm_t: e.reciprocal(sm_t[:, 0:4], sm_t[:, 0:4]), r=[sm_d], w=[sm_d])
                                hg_t, hg_d = hgr.next()
                                k.op("dve", lambda e, hg_t=hg_t, Ns_t=Ns_t, sm_t=sm_t: e.tensor_tensor(
                                    hg_t[:], Ns_t[:, :, 0:256], sm_t[:, 0:4].unsqueeze(2).to_broadcast([64, 4, 256]), ALU.mult),
                                    r=[Ns_d, sm_d], w=[hg_d])
                                k.op("dve", lambda e, hg_t=hg_t, so_t=so_t: e.tensor_tensor(
                                    hg_t[:], hg_t[:], so_t[:].rearrange("s (h d) -> s h d", h=4), ALU.mult),
                                    r=[so_d, hg_d], w=[hg_d])

                                def f(e, hg_t=hg_t, sm_t=sm_t):
                                    for hh in range(4):
                                        ins = e.bn_stats(sm_t[:, 4 + 6 * hh:10 + 6 * hh], hg_t[:, hh, :])
                                    return ins
                                k.op("dve", f, r=[hg_d], w=[sm_d])

                                def f(e, sm_t=sm_t):
                                    for hh in range(4):
                                        ins = e.bn_aggr(sm_t[:, 28 + 2 * hh:30 + 2 * hh], sm_t[:, 4 + 6 * hh:10 + 6 * hh])
                                    return ins
                                k.op("dve", f, r=[sm_d], w=[sm_d])
                                mv = sm_t[:, 28:36].rearrange("s (h k) -> s h k", h=4)
                                k.op("act", lambda e, sm_t=sm_t, mv=mv: e.activation(
                                    sm_t[:, 36:40], mv[:, :, 1], AF.Ln, bias=epsc[0:64, 0:1], scale=1.0), r=[sm_d, d_c], w=[sm_d])
                                k.op("act", lambda e, sm_t=sm_t: e.activation(
                                    sm_t[:, 36:40], sm_t[:, 36:40], AF.Exp, scale=-0.5), r=[sm_d], w=[sm_d])
                                k.op("dve", lambda e, hg_t=hg_t, mv=mv: e.tensor_tensor(
                                    hg_t[:], hg_t[:], mv[:, :, 0:1].to_broadcast([64, 4, 256]), ALU.subtract),
                                    r=[sm_d, hg_d], w=[hg_d])
                                k.op("dve", lambda e, hg_t=hg_t, sm_t=sm_t: e.tensor_tensor(
                                    hg_t[:], hg_t[:], sm_t[:, 36:40].unsqueeze(2).to_broadcast([64, 4, 256]), ALU.mult),
                                    r=[sm_d, hg_d], w=[hg_d])
                                hb_t, hb_d = hbr.next()
                                k.op("dve", lambda e, hg_t=hg_t, hb_t=hb_t: e.tensor_tensor(
                                    hb_t[:], hg_t[:].rearrange("s h d -> s (h d)"), ngb[:], ALU.mult),
                                    r=[hg_d, d_cw], w=[hb_d])
                                pt, pd = pb.next()

                                def f(pe, pt=pt, hb_t=hb_t):
                                    for fc in range(8):
                                        ins = pe.transpose(pt[:, fc * 64:(fc + 1) * 64], hb_t[:, fc * 128:(fc + 1) * 128], identb[0:64, 0:64])
                                    return ins
                                k.op("pe", f, r=[hb_d, d_c], w=[pd])
                                if tq % HB == 0:
                                    hb_cur = hblk.next()
                                hk_t, hk_d = hb_cur
                                k.op("act", lambda e, pt=pt, tq=tq, hk_t=hk_t: e.copy(
                                    hk_t[:, :, tq % HB:tq % HB + 64], pt[:, 0:512].rearrange("p (f s) -> p f s", f=8)), r=[pd], w=[hk_d])
                                if (tq + 64) % HB == 0:
                                    tb0 = tq + 64 - HB
                                    k.dma("sp", s_haT[:, tb0:tb0 + HB].rearrange("(f p) t -> p f t", p=128), hk_t[:], r=[hk_d], w=[d_haT])
                            yield

                    def attn_gen():
                        st_a = {'ob': None}
                        for h in range(8):
                            kb_t, kb_d = kbr.next(); qb_t, qb_d = qbr.next(); vb_t, vb_d = vbr.next()
                            k.dma("sp", kb_t[0:64, 0, :], s_qkbT[1024 + h * 128:1024 + h * 128 + 64, :], r=[d_scr["qkb"]], w=[kb_d])
                            k.dma("sp", kb_t[64:128, 1, :], s_qkbT[1024 + h * 128 + 64:1024 + (h + 1) * 128, :], r=[d_scr["qkb"]], w=[kb_d])
                            k.dma("sp", qb_t[:], s_qkbT[h * 128:(h + 1) * 128, TP:TA], r=[d_scr["qkb"]], w=[qb_d])
                            for t8 in range(0, NTA, 8):
                                t9 = min(NTA, t8 + 8)
                                k.dma("sp", vb_t[:, t8:t9, 0:128],
                                      s_vb[t8 * 128:t9 * 128, h * 128:(h + 1) * 128].rearrange("(t p) d -> p t d", p=128),
                                      r=[d_scr["vb"]], w=[vb_d])
                            items = []
                            for qi in range(NTO):
                                qt = NTP + qi
                                kt_lo = 0
                                while kt_lo < qt and SLOPES[h] * (127 - 128 * (qt - kt_lo)) < -ACLAMP:
                                    kt_lo += 1
                                for kt in range(kt_lo, qt + 1):
                                    items.append((qi, qt, kt, kt_lo))

                            def emit_qk(it, h=h, kb_t=kb_t, qb_t=qb_t, kb_d=kb_d, qb_d=qb_d):
                                qi, qt, kt, kt_lo = it
                                bSt, sd = a_S.next()
                                off = 0
                                diag = (kt == qt)

                                def f(pe):
                                    for m in range(2):
                                        ins = pe.matmul(bSt[:, off + m * 128:off + (m + 1) * 128], kb_t[:, m, kt * 128:(kt + 1) * 128],
                                                        qb_t[:, qi * 128:(qi + 1) * 128], start=True, stop=(not diag))
                                        if diag:
                                            ins = pe.matmul(bSt[:, off + m * 128:off + (m + 1) * 128], identb[:], dgb[:, h, :], start=False, stop=True)
                                    return ins
                                k.op("pe", f, r=[kb_d, qb_d, d_c], w=[sd])
                                return bSt, sd
                            def do_pv(it, P_t, P_d, h=h, vb_t=vb_t, vb_d=vb_d):
                                qi, qt, kt, kt_lo = it

                                def f(pe, P_t=P_t, vb_t=vb_t, kt=kt, qt=qt, kt_lo=kt_lo):
                                    pe.matmul(bO[:, 0:129], P_t[:, 0:128], vb_t[:, kt, :], start=(kt == kt_lo), stop=(kt == qt))
                                    return pe.matmul(bO1[:, 0:129], P_t[:, 128:256], vb_t[:, kt, :], start=(kt == kt_lo), stop=(kt == qt))
                                k.op("pe", f, r=[P_d, vb_d], w=[bOd, bO1d])
                                if kt != qt:
                                    return
                                s8, s8d = s8r.next()
                                Os, Osd = Osr.next()
                                k.op("act", lambda e, Os=Os: e.copy(Os[:, 0, :], bO[:, 0:129]), r=[bOd], w=[Osd])
                                k.op("dve", lambda e, Os=Os: e.tensor_copy(Os[:, 1, :], bO1[:, 0:129]), r=[bO1d], w=[Osd])
                                k.op("dve", lambda e, s8=s8, Os=Os: e.reciprocal(s8[:, 0:2], Os[:, :, 128]), r=[Osd], w=[s8d])
                                k.op("dve", lambda e, s8=s8: e.tensor_tensor(s8[:, 2:3], s8[:, 1:2], neglam, ALU.mult),
                                     r=[s8d, d_l], w=[s8d])
                                o1, o1d = o1r.next(); o2, o2d = o2r.next()
                                k.op("dve", lambda e, o1=o1, s8=s8, Os=Os: e.tensor_scalar_mul(o1[:], Os[:, 0, 0:128], s8[:, 0:1]),
                                     r=[Osd, s8d], w=[o1d])
                                k.op("dve", lambda e, o1=o1, s8=s8, Os=Os: e.scalar_tensor_tensor(
                                    o1[:], Os[:, 1, 0:128], s8[:, 2:3], o1[:], ALU.mult, ALU.add), r=[Osd, s8d], w=[o1d])
                                k.op("pool", lambda e, o1=o1, o2=o2: e.tensor_tensor(o2[:], o1[:], o1[:], ALU.mult), r=[o1d], w=[o2d])
                                k.op("dve", lambda e, o2=o2, s8=s8: e.reduce_sum(s8[:, 3:4], o2[:], AX.X), r=[o2d], w=[s8d])
                                k.op("dve", lambda e, s8=s8: e.tensor_scalar(s8[:, 4:5], s8[:, 3:4], 1.0 / 128.0, EPS, ALU.mult, ALU.add),
                                     r=[s8d], w=[s8d])
                                k.op("act", lambda e, s8=s8: e.activation(s8[:, 5:6], s8[:, 4:5], AF.Ln), r=[s8d], w=[s8d])
                                k.op("act", lambda e, s8=s8: e.activation(s8[:, 5:6], s8[:, 5:6], AF.Exp, scale=-0.5),
                                     r=[s8d], w=[s8d])
                                ob, obd = obr.next()
                                k.op("dve", lambda e, ob=ob, o1=o1, s8=s8: e.scalar_tensor_tensor(
                                    ob[:], o1[:], s8[:, 5:6], gnb[:], ALU.mult, ALU.mult), r=[o1d, s8d, d_l], w=[obd])
                                pt, pd = pb.next()
                                k.op("pe", lambda pe, pt=pt, ob=ob: pe.transpose(pt[:, 0:128], ob[:], identb[:]), r=[obd, d_c], w=[pd])
                                tq = qi * 128
                                if tq % HB == 0:
                                    st_a['ob'] = oblk.next()
                                ok_t, ok_d = st_a['ob']
                                k.op("act", lambda e, pt=pt, ok_t=ok_t, tq=tq: e.copy(ok_t[:, tq % HB:tq % HB + 128], pt[:, 0:128]),
                                     r=[pd], w=[ok_d])
                                if (tq + 128) % HB == 0:
                                    tb0 = tq + 128 - HB
                                    k.dma("sp", s_obT[h * 128:(h + 1) * 128, tb0:tb0 + HB], ok_t[:], r=[ok_d], w=[d_obT])
                            nxt = emit_qk(items[0])
                            pend = None
                            for j, it in enumerate(items):
                                qi, qt, kt, kt_lo = it
                                bSt, sd = nxt
                                off = 0
                                if j + 1 < len(items):
                                    nxt = emit_qk(items[j + 1])
                                diag = (kt == qt)
                                P_t, P_d = PTr.next()
                                bias_ap = zero1[:, 0:1] if diag else abt[:, h, qt - kt:qt - kt + 1]
                                k.op("act", lambda e, P_t=P_t, off=off, bias_ap=bias_ap, bSt=bSt: e.activation(
                                    P_t[:], bSt[:, off:off + 256], AF.Exp, bias=bias_ap, scale=0.125), r=[sd, d_c], w=[P_d])

                                if pend is not None:
                                    do_pv(*pend)
                                pend = (it, P_t, P_d)
                                yield
                            if pend is not None:
                                do_pv(*pend)
                            yield

                    gm = mlstm_gen()
                    ga_ = attn_gen()
                    if stop <= 3:
                        for _ in gm:
                            pass
                        raise _Stop()
                    if stop <= 3.5:
                        for _ in ga_:
                            pass
                        raise _Stop()
                    n_m = (CH0 * (4 * 3 + 1)) + ((NCH - CH0) * (4 * 4 + 1))
                    n_a = 0
                    for h_ in range(8):
                        for qi_ in range(NTO):
                            qt_ = NTP + qi_
                            lo_ = 0
                            while lo_ < qt_ and SLOPES[h_] * (127 - 128 * (qt_ - lo_)) < -ACLAMP:
                                lo_ += 1
                            n_a += qt_ + 1 - lo_
                    done_m = done_a = 0
                    m_alive = a_alive = True
                    while m_alive or a_alive:
                        if m_alive:
                            try:
                                next(gm); done_m += 1
                            except StopIteration:
                                m_alive = False
                        if ILV == 0:
                            tgt = n_a + 10 if not m_alive else 0
                        else:
                            tgt = n_a + 10 if not m_alive else (done_m * n_a) // n_m
                        while a_alive and done_a < tgt:
                            try:
                                next(ga_); done_a += 1
                            except StopIteration:
                                a_alive = False
                    for g_ in (gm, ga_):
                        for _ in g_:
                            pass
                    pb = pb_all
                if stop <= 4:
                    raise _Stop()
                with k.scope():
                    war = Ring([(k.sb("wa", [128, 8, 512], BF16), Dep()) for _ in range(2)])
                    wbr = Ring([(k.sb("wb", [128, 8, 512], BF16), Dep()) for _ in range(2)])
                    gar = Ring([(k.sb("ga", [128, 512], BF16), Dep()) for _ in range(2)])
                    gbr = Ring([(k.sb("gb", [128, 512], BF16), Dep()) for _ in range(2)])
                    m1r = Ring([(k.sb("m1", [128, 512], F32), Dep()) for _ in range(2)])
                    m2r = Ring([(k.sb("m2", [128, 512], F32), Dep()) for _ in range(2)])
                    hkr = Ring([(k.sb("hk", [128, 8, HB], BF16), Dep()) for _ in range(2)])
                    okr = Ring([(k.sb("ok", [128, 8, HB], BF16), Dep()) for _ in range(2)])
                    mgr = Ring([(k.sb("mgo", [128, 512], BF16), Dep()) for _ in range(3)])
                    st_e1 = {"db": None, "tb": None}

                    def e1_unit(db, tb, n, g):
                        with k.atomic():
                            if st_e1["db"] != db:
                                wa_t, wa_d = war.next(); wb_t, wb_d = wbr.next()
                                k.dma("pool", wa_t[:], w_a.rearrange("(c p) n -> p c n", p=128)[:, :, db * 512:(db + 1) * 512], w=[wa_d])
                                k.dma("pool", wb_t[:], w_b.rearrange("(c p) n -> p c n", p=128)[:, :, db * 512:(db + 1) * 512], w=[wb_d])
                                st_e1["db"] = db
                                st_e1["w"] = (wa_t, wa_d, wb_t, wb_d)
                            if st_e1["tb"] != (db, tb):
                                hk, hkd = hkr.next(); ok, okd = okr.next()
                                k.dma("sp", hk[:, :, 0:n], s_haT[:, tb:tb + n].rearrange("(f p) t -> p f t", p=128), r=[d_haT], w=[hkd])
                                k.dma("sp", ok[:, :, 0:n], s_obT[:, tb:tb + n].rearrange("(f p) t -> p f t", p=128), r=[d_obT], w=[okd])
                                st_e1["tb"] = (db, tb)
                                st_e1["h"] = (hk, hkd, ok, okd)
                        wa_t, wa_d, wb_t, wb_d = st_e1["w"]
                        hk, hkd, ok, okd = st_e1["h"]
                        dc = db * 4 + g
                        ga_t, ga_d = gar.next(); gb_t, gb_d = gbr.next()
                        k.dma("sp", ga_t[:, 0:n], s_gT[dc * 128:(dc + 1) * 128, tb:tb + n], r=[d_scr["g"]], w=[ga_d])
                        k.dma("sp", gb_t[:, 0:n], s_gT[2048 + dc * 128:2048 + (dc + 1) * 128, tb:tb + n], r=[d_scr["g"]], w=[gb_d])
                        bA, bAd = pf.next(); bB, bBd = pf.next()

                        def f(pe):
                            for fc in range(8):
                                ins = pe.matmul(bA[:, 0:n], wa_t[:, fc, g * 128:(g + 1) * 128], hk[:, fc, 0:n],
                                                start=(fc == 0), stop=(fc == 7))
                            return ins
                        k.op("pe", f, r=[wa_d, hkd], w=[bAd])

                        def f(pe):
                            for fc in range(8):
                                ins = pe.matmul(bB[:, 0:n], wb_t[:, fc, g * 128:(g + 1) * 128], ok[:, fc, 0:n],
                                                start=(fc == 0), stop=(fc == 7))
                            return ins
                        k.op("pe", f, r=[wb_d, okd], w=[bBd])
                        m1, m1d = m1r.next(); m2, m2d = m2r.next()
                        k.op("dve", lambda e: e.tensor_tensor(m1[:, 0:n], bA[:, 0:n], ga_t[:, 0:n], ALU.mult),
                             r=[bAd, ga_d], w=[m1d])
                        k.op("dve", lambda e: e.tensor_tensor(m2[:, 0:n], bB[:, 0:n], gb_t[:, 0:n], ALU.mult),
                             r=[bBd, gb_d], w=[m2d])
                        mg_t, mg_dd = mgr.next()
                        k.op("pool", lambda e: e.tensor_tensor(mg_t[:, 0:n], m1[:, 0:n], m2[:, 0:n], ALU.add), r=[m1d, m2d], w=[mg_dd])
                        k.dma("sp", s_mgT[dc * 128:(dc + 1) * 128, tb:tb + n], mg_t[:, 0:n], r=[mg_dd], w=[d_mg])
                    units = []
                    for db in range(4):
                        tb = 0
                        while tb < TO:
                            n = min(512, TO - tb)
                            for g in range(4):
                                units.append(lambda db=db, tb=tb, n=n, g=g: e1_unit(db, tb, n, g))
                            tb += n
                    k.interleave(units, width=NIL)
            if stop <= 5:
                raise _Stop()
            with k.scope():
                wo = k.sb("wo", [128, DC, D], BF16)
                wr = k.sb("wr", [128, DC, 36], F32)
                br = k.sb("br", [1, 36], F32)
                l1g = k.sb("l1g", [128, D], F32); l1b = k.sb("l1b", [128, D], F32)
                cnt = k.sb("cnt", [128, 32], F32)
                d_wo, d_cnt = Dep(), Dep()
                for q4 in range(4):
                    k.dma("pool", wo[:, :, q4 * 512:(q4 + 1) * 512],
                          w_out.rearrange("(c p) n -> p c n", p=128)[:, :, q4 * 512:(q4 + 1) * 512], w=[d_wo])
                k.dma("sp", wr[:], wr_d.rearrange("(c p) n -> p c n", p=128), w=[d_wo])
                k.dma("sp", br[:], br_d, w=[d_wo])
                k.dma("sp", l1g[:], ln1g_d.partition_broadcast(128), w=[d_wo])
                k.dma("sp", l1b[:], ln1b_d.partition_broadcast(128), w=[d_wo])
                k.op("dve", lambda e: e.memset(cnt[:], 0.0), w=[d_cnt])
                xtr = Ring([(k.sb("xt", [128, D], F32), Dep()) for _ in range(2)])
                x1r = Ring([(k.sb("x1", [128, D], F32), Dep()) for _ in range(2)])
                hbr2 = Ring([(k.sb("hb2", [128, D], BF16), Dep()) for _ in range(2)])
                hTr = Ring([(k.sb("hT", [128, DC, 128], F32), Dep()) for _ in range(2)])
                rsr = Ring([(k.sb("rs", [128, 256], F32), Dep()) for _ in range(2)])
                mkr = Ring([(k.sb("mk", [128, 32], BF16), Dep()) for _ in range(2)])
                mbr = Ring([(k.sb("mgb", [128, DC, HB], BF16), Dep()) for _ in range(2)])
                st_e2 = {"mb": None}

                def e2_tile(ti):
                    if (ti * 128) % HB == 0:
                        with k.atomic():
                            st_e2["mb"] = mbr.next()
                            k.dma("sp", st_e2["mb"][0][:], s_mgT[:, ti * 128:ti * 128 + HB].rearrange("(c p) t -> p c t", p=128),
                                  r=[d_mg], w=[st_e2["mb"][1]])
                    mgb, mgbd = st_e2["mb"]
                    tloc = (ti * 128) % HB
                    xt, xtd = xtr.next(); x1, x1d = x1r.next()
                    k.dma("sp", xt[:], x[TP + ti * 128:TP + (ti + 1) * 128, :], w=[xtd])
                    for db in range(4):
                        bk, bd = pf.next()

                        def f(pe, bk=bk, mgb=mgb, tloc=tloc, db=db):
                            for c in range(DC):
                                ins = pe.matmul(bk[:, :], mgb[:, c, tloc:tloc + 128], wo[:, c, db * 512:(db + 1) * 512],
                                                start=(c == 0), stop=(c == DC - 1))
                            return ins
                        k.op("pe", f, r=[mgbd, d_wo], w=[bd])
                        k.op("dve", lambda e, x1=x1, xt=xt, bk=bk, db=db: e.scalar_tensor_tensor(
                            x1[:, db * 512:(db + 1) * 512], xt[:, db * 512:(db + 1) * 512], ALPHA, bk[:, :], ALU.mult, ALU.add),
                            r=[xtd, bd], w=[x1d])
                    rs, rsd = rsr.next()

                    def layer_norm(xx, xd, rs, rsd, g_t, b_t, gdep):
                        st = rs[:, 0:24].rearrange("p (g k) -> p g k", g=4)
                        def fbn(e):
                            for gg in range(4):
                                ins = e.bn_stats(rs[:, 6 * gg:6 * gg + 6], xx[:, gg * 512:(gg + 1) * 512])
                            return ins
                        k.op("dve", fbn, r=[xd], w=[rsd])
                        k.op("dve", lambda e: e.bn_aggr(rs[:, 24:26], rs[:, 0:24]), r=[rsd], w=[rsd])
                        k.op("act", lambda e: e.activation(rs[:, 26:27], rs[:, 25:26], AF.Ln, bias=epsc[:, 0:1], scale=1.0), r=[rsd, d_c], w=[rsd])
                        k.op("act", lambda e: e.activation(rs[:, 26:27], rs[:, 26:27], AF.Exp, scale=-0.5), r=[rsd], w=[rsd])
                        k.op("dve", lambda e: e.tensor_scalar(xx[:], xx[:], rs[:, 24:25], rs[:, 26:27], ALU.subtract, ALU.mult),
                             r=[rsd, xd], w=[xd])
                        k.op("pool", lambda e: e.tensor_tensor(xx[:], xx[:], g_t[:], ALU.mult), r=[xd, gdep], w=[xd])
                        k.op("pool", lambda e: e.tensor_tensor(xx[:], xx[:], b_t[:], ALU.add), r=[xd, gdep], w=[xd])
                    layer_norm(x1, x1d, rs, rsd, l1g, l1b, d_wo)
                    k.dma("sp", s_h1[ti * 128:(ti + 1) * 128, :], x1[:], r=[x1d], w=[d_h1])
                    hb2, hb2d = hbr2.next()
                    k.op("act", lambda e, hb2=hb2, x1=x1: e.copy(hb2[:], x1[:]), r=[x1d], w=[hb2d])
                    hT, hTd = hTr.next()
                    for g in range(4):
                        bk, bd = pf.next()

                        def f(pe, bk=bk, x1=x1, g=g):
                            for j in range(4):
                                c = g * 4 + j
                                ins = pe.transpose(bk[:, j * 128:(j + 1) * 128], x1[:, c * 128:(c + 1) * 128], identf[:])
                            return ins
                        k.op("pe", f, r=[x1d, d_c], w=[bd])
                        k.op("act", lambda e, hT=hT, bk=bk, g=g: e.copy(
                            hT[:, g * 4:(g + 1) * 4, :], bk[:, :].rearrange("p (j n) -> p j n", j=4)), r=[bd], w=[hTd])
                    bk, bd = pf.next()

                    def f(pe, bk=bk, hT=hT):
                        for c in range(DC):
                            pe.matmul(bk[:, 0:36], hT[:, c, :], wr[:, c, :], start=(c == 0), stop=False)
                        return pe.matmul(bk[:, 0:36], onesf[0:1, :], br[0:1, :], start=False, stop=True)
                    k.op("pe", f, r=[hTd, d_wo, d_c], w=[bd])
                    V = "dve"
                    lg = rs[:, 32:68]
                    k.op(V, lambda e, bk=bk, lg=lg: e.tensor_copy(lg, bk[:, 0:36]), r=[bd], w=[rsd])
                    g4 = rs[:, 32:36]; e32 = rs[:, 36:68]
                    gmx = rs[:, 68:69]; ngm = rs[:, 69:70]; ohg = rs[:, 70:74]; eg = rs[:, 74:78]; sg = rs[:, 78:79]; gp = rs[:, 79:80]
                    pen = rs[:, 80:84]; msk = rs[:, 84:116]; top8 = rs[:, 116:124]; oh1 = rs[:, 124:156]; oh2 = rs[:, 156:188]
                    dd = rs[:, 188:189]; p1 = rs[:, 189:190]; p2 = rs[:, 190:191]; tmp = rs[:, 192:224]
                    pk = rs[:, 224:225]; ek = rs[:, 225:226]; okk = rs[:, 226:227]; dsf = rs[:, 227:228]; posg = rs[:, 228:260 - 4]
                    k.op(V, lambda e: e.reduce_max(gmx, g4, AX.X), r=[rsd], w=[rsd])
                    k.op(V, lambda e: e.tensor_scalar_mul(ngm, gmx, -1.0), r=[rsd], w=[rsd])
                    k.op(V, lambda e: e.tensor_scalar(ohg, g4, gmx, None, ALU.is_equal), r=[rsd], w=[rsd])
                    k.op("act", lambda e: e.activation(eg, g4, AF.Exp, bias=ngm, scale=1.0), r=[rsd], w=[rsd])
                    k.op(V, lambda e: e.reduce_sum(sg, eg, AX.X), r=[rsd], w=[rsd])
                    k.op(V, lambda e: e.reciprocal(gp, sg), r=[rsd], w=[rsd])
                    k.op(V, lambda e: e.tensor_scalar(pen, ohg, BIG, -BIG, ALU.mult, ALU.add), r=[rsd], w=[rsd])
                    k.op(V, lambda e: e.tensor_tensor(msk.rearrange("p (g j) -> p g j", g=4), e32.rearrange("p (g j) -> p g j", g=4),
                                                      pen.unsqueeze(2).to_broadcast([128, 4, 8]), ALU.add), r=[rsd], w=[rsd])
                    k.op(V, lambda e: e.max(top8, msk), r=[rsd], w=[rsd])
                    k.op(V, lambda e: e.tensor_scalar(oh1, msk, top8[:, 0:1], None, ALU.is_equal), r=[rsd], w=[rsd])
                    k.op(V, lambda e: e.tensor_scalar(oh2, msk, top8[:, 1:2], None, ALU.is_equal), r=[rsd], w=[rsd])
                    k.op(V, lambda e: e.tensor_sub(dd, top8[:, 0:1], top8[:, 1:2]), r=[rsd], w=[rsd])
                    k.op("act", lambda e: e.activation(p1, dd, AF.Sigmoid), r=[rsd], w=[rsd])
                    k.op("act", lambda e: e.activation(p2, dd, AF.Sigmoid, scale=-1.0), r=[rsd], w=[rsd])
                    k.op(V, lambda e, ti=ti: e.tensor_tensor(wtab[:, ti, 0:1], p1, gp, ALU.mult), r=[rsd], w=[d_tab])
                    k.op(V, lambda e, ti=ti: e.tensor_tensor(wtab[:, ti, 1:2], p2, gp, ALU.mult), r=[rsd], w=[d_tab])
                    mk, mkd = mkr.next()
                    k.op(V, lambda e, mk=mk: e.tensor_tensor(mk[:], oh1, oh2, ALU.add), r=[rsd], w=[mkd])
                    bk2, bd2 = pf.next()

                    def f(pe, bk2=bk2, mk=mk):
                        pe.matmul(bk2[:, 0:32], ustr[:], mk[:], start=True, stop=True)
                        return pe.matmul(bk2[:, 32:64], onesb[:], mk[:], start=True, stop=True)
                    k.op("pe", f, r=[mkd, d_c], w=[bd2])
                    posg = rs[:, 224:256]
                    pk = rs[:, 28:29]; ek = rs[:, 29:30]; okk = rs[:, 30:31]; dsf = rs[:, 31:32]
                    with k.atomic():
                        k.op(V, lambda e, bk2=bk2: e.tensor_tensor(posg, bk2[:, 0:32], cnt[:], ALU.add), r=[bd2, d_cnt, rsd], w=[rsd])
                        k.op(V, lambda e, bk2=bk2: e.tensor_tensor(cnt[:], cnt[:], bk2[:, 32:64], ALU.add), r=[bd2, rsd], w=[d_cnt])
                    for kk, oh in ((0, oh1), (1, oh2)):
                        k.op(V, lambda e, oh=oh: e.tensor_tensor(tmp, oh, posg, ALU.mult), r=[rsd], w=[rsd])
                        k.op(V, lambda e: e.reduce_sum(pk, tmp, AX.X), r=[rsd], w=[rsd])
                        k.op(V, lambda e, oh=oh: e.tensor_tensor(tmp, oh, ecap[:], ALU.mult), r=[rsd, d_c], w=[rsd])
                        k.op(V, lambda e: e.reduce_sum(ek, tmp, AX.X), r=[rsd], w=[rsd])
                        k.op(V, lambda e: e.tensor_scalar(okk, pk, float(CAP), None, ALU.is_lt), r=[rsd], w=[rsd])
                        k.op(V, lambda e: e.tensor_tensor(dsf, ek, pk, ALU.add), r=[rsd], w=[rsd])
                        k.op(V, lambda e: e.tensor_scalar_add(dsf, dsf, -float(TRASH)), r=[rsd], w=[rsd])
                        k.op(V, lambda e: e.tensor_tensor(dsf, dsf, okk, ALU.mult), r=[rsd], w=[rsd])
                        k.op(V, lambda e: e.tensor_scalar_add(dsf, dsf, float(TRASH)), r=[rsd], w=[rsd])
                        k.op(V, lambda e, ti=ti, kk=kk: e.tensor_copy(dtab[:, ti, kk:kk + 1], dsf), r=[rsd], w=[d_tab])
                        k.dma("pool", s_Xg, hb2[:], r=[hb2d, d_tab, d_Xg], w=[d_Xg],
                              indirect=dict(out_offset=bass.IndirectOffsetOnAxis(ap=dtab[:, ti, kk:kk + 1], axis=0), in_offset=None))
                k.interleave([(lambda ti=ti: e2_tile(ti)) for ti in range(NTO)], width=NIL)

            if dbg:
                k.dma("sp", s_dtab, dtab[:].rearrange("p a b -> p (a b)"), r=[d_tab], w=[Dep()])
                k.dma("sp", s_wtab, wtab[:].rearrange("p a b -> p (a b)"), r=[d_tab], w=[Dep()])
            if stop <= 6:
                raise _Stop()
            NCT = CAP // 128
            with k.scope():
                wgr = Ring([(k.sb("wg", [128, DC, 512], BF16), Dep()) for _ in range(3)])
                wur = Ring([(k.sb("wu", [128, DC, 512], BF16), Dep()) for _ in range(3)])
                wdr = Ring([(k.sb("wd", [128, 4, D], BF16), Dep()) for _ in range(3)])
                Xer = Ring([(k.sb("Xe", [128, NCT, D], BF16), Dep()) for _ in range(2)])
                XTr = Ring([(k.sb("XT", [128, DC, CAP], BF16), Dep()) for _ in range(2)])
                ATr = Ring([(k.sb("AT", [128, 4, CAP], BF16), Dep()) for _ in range(2)])
                sgr = Ring([(k.sb("sgt", [128, CAP], F32), Dep()) for _ in range(2)])
                Ysr = Ring([(k.sb("Ys", [128, D], BF16), Dep()) for _ in range(2)])
                def expert_fn(e_):
                    wg, wgd = wgr.next(); wu, wud = wur.next(); wd, wdd = wdr.next()
                    k.dma("pool", wg[:], w_gate[e_].rearrange("(c p) n -> p c n", p=128), w=[wgd])
                    k.dma("pool", wu[:], w_up[e_].rearrange("(c p) n -> p c n", p=128), w=[wud])
                    k.dma("pool", wd[:], w_down[e_].rearrange("(c p) n -> p c n", p=128), w=[wdd])
                    Xe, Xed = Xer.next(); XT, XTd = XTr.next(); AT, ATd = ATr.next()
                    k.dma("sp", Xe[:], s_Xg[e_ * CAP:(e_ + 1) * CAP, :].rearrange("(t p) d -> p t d", p=128), r=[d_Xg], w=[Xed])
                    for t in range(NCT):
                        for g in range(4):
                            pt, pd = pb.next()

                            def f(pe, pt=pt, Xe=Xe, t=t, g=g):
                                for j in range(4):
                                    c = g * 4 + j
                                    ins = pe.transpose(pt[:, j * 128:(j + 1) * 128], Xe[:, t, c * 128:(c + 1) * 128], identb[:])
                                return ins
                            k.op("pe", f, r=[Xed, d_c], w=[pd])
                            eng = "dve" if (g % 2 == 0) else "act"
                            if eng == "dve":
                                k.op("dve", lambda e, XT=XT, pt=pt, g=g, t=t: e.tensor_copy(
                                    XT[:, g * 4:(g + 1) * 4, t * 128:(t + 1) * 128], pt[:, 0:512].rearrange("p (j n) -> p j n", j=4)),
                                    r=[pd], w=[XTd])
                            else:
                                k.op("act", lambda e, XT=XT, pt=pt, g=g, t=t: e.copy(
                                    XT[:, g * 4:(g + 1) * 4, t * 128:(t + 1) * 128], pt[:, 0:512].rearrange("p (j n) -> p j n", j=4)),
                                    r=[pd], w=[XTd])
                    for fcx in range(4):
                        bG, bGd = pf.next(); bU, bUd = pf.next()

                        def f(pe, bG=bG, wg=wg, XT=XT, fcx=fcx):
                            for c in range(DC):
                                ins = pe.matmul(bG[:, 0:CAP], wg[:, c, fcx * 128:(fcx + 1) * 128], XT[:, c, :], start=(c == 0), stop=(c == DC - 1))
                            return ins
                        k.op("pe", f, r=[wgd, XTd], w=[bGd])

                        def f(pe, bU=bU, wu=wu, XT=XT, fcx=fcx):
                            for c in range(DC):
                                ins = pe.matmul(bU[:, 0:CAP], wu[:, c, fcx * 128:(fcx + 1) * 128], XT[:, c, :], start=(c == 0), stop=(c == DC - 1))
                            return ins
                        k.op("pe", f, r=[wud, XTd], w=[bUd])
                        sg_t, sg_d = sgr.next()
                        k.op("act", lambda e, sg_t=sg_t, bG=bG: e.activation(sg_t[:], bG[:, 0:CAP], AF.Silu), r=[bGd], w=[sg_d])
                        k.op("dve", lambda e, AT=AT, sg_t=sg_t, bU=bU, fcx=fcx: e.tensor_tensor(AT[:, fcx, :], sg_t[:], bU[:, 0:CAP], ALU.mult),
                             r=[sg_d, bUd], w=[ATd])
                    for t in range(NCT):
                        Ys, Ysd = Ysr.next()
                        for db in range(4):
                            bk, bd = pf.next()

                            def f(pe, bk=bk, AT=AT, wd=wd, t=t, db=db):
                                for fcx in range(4):
                                    ins = pe.matmul(bk[:, :], AT[:, fcx, t * 128:(t + 1) * 128], wd[:, fcx, db * 512:(db + 1) * 512],
                                                    start=(fcx == 0), stop=(fcx == 3))
                                return ins
                            k.op("pe", f, r=[ATd, wdd], w=[bd])
                            if db % 2 == 0:
                                k.op("act", lambda e, Ys=Ys, bk=bk, db=db: e.copy(Ys[:, db * 512:(db + 1) * 512], bk[:, :]), r=[bd], w=[Ysd])
                            else:
                                k.op("dve", lambda e, Ys=Ys, bk=bk, db=db: e.tensor_copy(Ys[:, db * 512:(db + 1) * 512], bk[:, :]), r=[bd], w=[Ysd])
                        r0 = e_ * CAP + t * 128
                        k.dma("sp", s_Yg[r0:r0 + 128, :], Ys[:], r=[Ysd], w=[d_Yg])
                k.interleave([(lambda e_=e_: expert_fn(e_)) for e_ in range(NE)], width=NIL)

            if stop <= 7:
                raise _Stop()
            d_out = Dep()
            with k.scope():
                l2g = k.sb("l2g", [128, D], F32); l2b = k.sb("l2b", [128, D], F32)
                d_l2 = Dep()
                k.dma("sp", l2g[:], ln2g_d.partition_broadcast(128), w=[d_l2])
                k.dma("sp", l2b[:], ln2b_d.partition_broadcast(128), w=[d_l2])
                h1r = Ring([(k.sb("h1", [128, D], F32), Dep()) for _ in range(3)])
                y1r = Ring([(k.sb("y1", [128, D], BF16), Dep()) for _ in range(3)])
                y2r = Ring([(k.sb("y2", [128, D], BF16), Dep()) for _ in range(3)])
                rsr = Ring([(k.sb("rs2", [128, 32], F32), Dep()) for _ in range(3)])
                def g_tile(ti):
                    h1, h1d = h1r.next(); y1, y1d = y1r.next(); y2, y2d = y2r.next()
                    k.dma("sp", h1[:], s_h1[ti * 128:(ti + 1) * 128, :], r=[d_h1], w=[h1d])
                    k.dma("pool", y1[:], s_Yg, r=[d_Yg, d_tab], w=[y1d],
                          indirect=dict(out_offset=None, in_offset=bass.IndirectOffsetOnAxis(ap=dtab[:, ti, 0:1], axis=0)))
                    k.dma("pool", y2[:], s_Yg, r=[d_Yg, d_tab], w=[y2d],
                          indirect=dict(out_offset=None, in_offset=bass.IndirectOffsetOnAxis(ap=dtab[:, ti, 1:2], axis=0)))
                    k.op("act", lambda e, h1=h1: e.mul(h1[:], h1[:], ALPHA), r=[h1d], w=[h1d])
                    k.op("dve", lambda e, h1=h1, y1=y1, ti=ti: e.scalar_tensor_tensor(
                        h1[:], y1[:], wtab[:, ti, 0:1], h1[:], ALU.mult, ALU.add), r=[y1d, d_tab, h1d], w=[h1d])
                    k.op("dve", lambda e, h1=h1, y2=y2, ti=ti: e.scalar_tensor_tensor(
                        h1[:], y2[:], wtab[:, ti, 1:2], h1[:], ALU.mult, ALU.add), r=[y2d, d_tab, h1d], w=[h1d])
                    rs, rsd = rsr.next()
                    st = rs[:, 0:24].rearrange("p (g k) -> p g k", g=4)
                    def fbn(e, rs=rs, h1=h1):
                        for gg in range(4):
                            ins = e.bn_stats(rs[:, 6 * gg:6 * gg + 6], h1[:, gg * 512:(gg + 1) * 512])
                        return ins
                    k.op("dve", fbn, r=[h1d], w=[rsd])
                    k.op("dve", lambda e, rs=rs: e.bn_aggr(rs[:, 24:26], rs[:, 0:24]), r=[rsd], w=[rsd])
                    k.op("act", lambda e, rs=rs: e.activation(rs[:, 26:27], rs[:, 25:26], AF.Ln, bias=epsc[:, 0:1], scale=1.0), r=[rsd, d_c], w=[rsd])
                    k.op("act", lambda e, rs=rs: e.activation(rs[:, 26:27], rs[:, 26:27], AF.Exp, scale=-0.5), r=[rsd], w=[rsd])
                    k.op("dve", lambda e, rs=rs, h1=h1: e.tensor_scalar(h1[:], h1[:], rs[:, 24:25], rs[:, 26:27], ALU.subtract, ALU.mult),
                         r=[rsd, h1d], w=[h1d])
                    k.op("pool", lambda e, h1=h1: e.tensor_tensor(h1[:], h1[:], l2g[:], ALU.mult), r=[h1d, d_l2], w=[h1d])
                    k.op("pool", lambda e, h1=h1: e.tensor_tensor(h1[:], h1[:], l2b[:], ALU.add), r=[h1d, d_l2], w=[h1d])
                    k.dma("sp", out_d[ti * 128:(ti + 1) * 128, :], h1[:], r=[h1d], w=[d_out])
                k.interleave([(lambda ti=ti: g_tile(ti)) for ti in range(NTO)], width=3)
        except _Stop:
            gscope.close()
        k.barrier()
        build.stats = (k.n_ins, {n: e.count for n, e in k.E.items()})
    return nc


def _consts(TP, TO, CAP):
    TA = TP + TO
    c = {}
    c["ident"] = np.eye(128, dtype=np.float32)
    s = np.arange(64)
    c["mask64"] = (s[:, None] <= s[None, :]).astype(np.float32)
    p = np.arange(128)
    c["ustrict"] = (p[:, None] < p[None, :]).astype(np.float32)
    slopes = 2.0 ** (-8.0 * np.arange(1, 9) / 8.0)
    kl = p[:, None]; ql = p[None, :]
    vis = (kl // 64) <= (ql // 64)
    dg = np.zeros((128, 8, 128), np.float32)
    for h in range(8):
        b = np.where(kl <= ql, slopes[h] * kl, slopes[h] * (2 * ql - kl))
        dg[:, h, :] = np.where(vis, b, -BIG) * 8.0
    c["diagbias"] = dg
    ab = np.zeros((128, 8, 32), np.float32)
    for h in range(8):
        for dlt in range(32):
            ab[:, h, dlt] = np.maximum(slopes[h] * (p - 128 * dlt), -ACLAMP)
    c["alibi"] = ab
    c["ecap"] = np.tile((np.arange(32) * CAP).astype(np.float32)[None, :], (128, 1))
    return c


def _prep_shared(inp):
    f = lambda a: np.ascontiguousarray(np.asarray(a, dtype=np.float32))
    b_in = f(inp["b_in"][0])
    sh = {}
    sh["w_in"] = f(inp["w_in"][0])

    def fm_bias(b):
        cols = np.concatenate([b[O_QA:O_QA + 1024], b[O_KA:O_KA + 1024], b[O_QB:O_QB + 1024], b[O_KB:O_KB + 1024],
                               b[O_GA:O_GA + 2048], b[O_GB:O_GB + 2048]])
        return np.ascontiguousarray(cols.reshape(64, 128).T)

    def tm_bias(b):
        return np.ascontiguousarray(np.concatenate([b[O_VA:O_VA + 1024], b[O_OA:O_OA + 1024], b[O_VB:O_VB + 1024]])[None, :])

    def if_bias(b):
        return np.ascontiguousarray(np.stack([b[O_IA:O_IA + 4], b[O_FA:O_FA + 4]], axis=1))
    b_mask = np.zeros_like(b_in)
    b_mask[O_IA:O_IA + 4] = -BIG
    b_mask[O_FA:O_FA + 4] = BIG
    sh["bias_real"] = (fm_bias(b_in), tm_bias(b_in), if_bias(b_in))
    sh["bias_mask"] = (fm_bias(b_mask), tm_bias(b_mask), if_bias(b_mask))
    cw = f(inp["conv_w"][0])
    sh["cw"] = np.ascontiguousarray(cw.T.reshape(16, 128, 4).transpose(1, 0, 2))
    sh["cb"] = np.ascontiguousarray(f(inp["conv_b"][0]).reshape(16, 128).T)
    sh["mlstm_norm_g"] = f(inp["mlstm_norm_g"][0])
    sh["diff_norm_g"] = f(inp["diff_norm_g"][0])
    sh["lamv"] = np.ascontiguousarray(np.stack([f(inp["lambda_q1"][0]), f(inp["lambda_k1"][0]),
                                                f(inp["lambda_q2"][0]), f(inp["lambda_k2"][0])]))
    for n in ("w_a", "w_b", "w_out", "ln1_g", "ln1_b", "ln2_g", "ln2_b", "w_gate", "w_up", "w_down"):
        sh[n] = f(inp[n][0])
    sh["w_r"] = np.ascontiguousarray(np.concatenate([f(inp["w_grp"][0]), f(inp["w_exp"][0])], axis=1))
    sh["b_r"] = np.ascontiguousarray(np.concatenate([f(inp["b_grp"][0]), f(inp["b_exp"][0])])[None, :])
    return sh


def make_in_maps(inp, TP, TO, CAP):
    x = np.asarray(inp["x"], dtype=np.float32)
    B, S, _ = x.shape
    assert S == TP + TO or S == 2 * TO
    sh = _prep_shared(inp)
    cs = _consts(TP, TO, CAP)
    TA = TP + TO
    maps = []
    for b in range(B):
        for half in range(2):
            m = {}
            if half == 0:
                xa = np.zeros((TA, D), np.float32)
                xa[TP:] = x[b, 0:TO]
                valid = np.concatenate([np.zeros(TP, np.float32), np.ones(TO, np.float32)])
                pre = sh["bias_mask"]
            else:
                xa = np.ascontiguousarray(x[b, TO - TP:2 * TO])
                valid = np.ones(TA, np.float32)
                pre = sh["bias_real"]
            m["x"] = xa
            m["valid_tm"] = np.ascontiguousarray(valid.reshape(TA // 128, 128).T)
            m["bfm"], m["btm"], m["bif"] = sh["bias_real"]
            m["bfm_pre"], m["btm_pre"], m["bif_pre"] = pre
            for n in ("w_in", "cw", "cb", "mlstm_norm_g", "diff_norm_g", "lamv", "w_a", "w_b", "w_out", "ln1_g", "ln1_b",
                      "ln2_g", "ln2_b", "w_gate", "w_up", "w_down", "w_r", "b_r"):
                m[n] = sh[n]
            m.update(cs)
            maps.append(m)
    return maps


_TP, _TO, _CAP = 2048, 2048, 256


def kernel(**inputs):
    x = np.asarray(inputs["x"])
    B, S, _ = x.shape
    maps = make_in_maps(inputs, _TP, _TO, _CAP)
    nc = build(_TP, _TO, _CAP)
    res = run_bass_kernel_spmd(nc, maps, core_ids=list(range(len(maps))))
    out = np.zeros((B, S, D), np.float32)
    i = 0
    for b in range(B):
        for half in range(2):
            out[b, half * _TO:(half + 1) * _TO] = res.results[i]["out"]
            i += 1
    return out
```

```python
import math
from contextlib import ExitStack, contextmanager
import numpy as np
import concourse.bass as bass
import concourse.mybir as mybir
from concourse.bass_utils import run_bass_kernel_spmd

F32 = mybir.dt.float32
BF16 = mybir.dt.bfloat16
I32 = mybir.dt.int32
AF = mybir.ActivationFunctionType
ALU = mybir.AluOpType
AX = mybir.AxisListType

D = 2048
DC = 16
NIN = 11272
O_QA, O_KA, O_VA, O_OA, O_IA, O_FA, O_QB, O_KB, O_VB, O_GA, O_GB = (
    0, 1024, 2048, 3072, 4096, 4100, 4104, 5128, 6152, 7176, 9224)
ALPHA = 2.0 ** 0.25
EPS = 1e-5
NE = 32
BIG = 30000.0
LN16 = math.log(16.0)
ACLAMP = 40.0
import os
ILV = int(os.environ.get('ILV', '1'))
EXPV = int(os.environ.get('EXPV', '0'))
NIL = int(os.environ.get('NIL', '2'))
SLOPES = [2.0 ** (-(h + 1)) for h in range(8)]


class _Stop(Exception):
    pass


class Dep:
    __slots__ = ("w", "rs")

    def __init__(self):
        self.w = None
        self.rs = {}


class _Eng:
    def __init__(self, name, eng, sem):
        self.name, self.eng, self.sem = name, eng, sem
        self.count = 0
        self.waited = {}


class _DmaSem:
    def __init__(self, key, sem):
        self.key, self.sem, self.total = key, sem, 0


class Kern:
    def __init__(self, nc, stack, n_dma_sems=48):
        self.nc = nc
        self.stack = stack
        self.E = {}
        for name, eng in (("pe", nc.tensor), ("act", nc.scalar), ("dve", nc.vector),
                          ("pool", nc.gpsimd), ("sp", nc.sync)):
            sem = stack.enter_context(nc.semaphore("s_" + name))
            self.E[name] = _Eng(name, eng, sem)
        self.dsems = [_DmaSem("d%d" % i, stack.enter_context(nc.semaphore("d%d" % i)))
                      for i in range(n_dma_sems)]
        self.drr = 0
        self.n_ins = 0
        self.uid = 0
        self._cur = None
        self._atomic = 0

    def sb(self, name, shape, dt):
        self.uid += 1
        return self.stack.enter_context(self.nc.sbuf_tensor("%s_%d" % (name, self.uid), list(shape), dt))

    def ps(self, name, shape, dt):
        return self.stack.enter_context(self.nc.psum_tensor(name, list(shape), dt))

    @contextmanager
    def scope(self):
        old = self.stack
        stopped = False
        with ExitStack() as st:
            self.stack = st
            try:
                yield
            except _Stop:
                stopped = True
            if not stopped:
                self.barrier()
        self.stack = old
        if stopped:
            raise _Stop()

    def _wait(self, es, ev):
        key, sem, val = ev
        if es.name == "pe" and key == "pe":
            return
        if es.waited.get(key, 0) >= val:
            return
        es.eng.wait_ge(sem, val)
        es.waited[key] = val

    def _pre(self, es, r, w):
        for d in r:
            if d.w is not None:
                self._wait(es, d.w)
        for d in w:
            if d.w is not None:
                self._wait(es, d.w)
            for ev in d.rs.values():
                self._wait(es, ev)

    def _post(self, ev, r, w):
        for d in r:
            d.rs[ev[0]] = ev
        for d in w:
            d.w = ev
            d.rs = {}

    def op(self, en, fn, r=(), w=()):
        es = self.E[en]
        self._pre(es, r, w)
        ins = fn(es.eng)
        es.count += 1
        ins.then_inc(es.sem, 1)
        self.n_ins += 1
        ev = (es.name, es.sem, es.count)
        self._post(ev, r, w)
        self._yield()
        return ev

    def dma(self, qn, out, in_, r=(), w=(), indirect=None, **kw):
        es = self.E[qn]
        self._pre(es, r, w)
        ds = self.dsems[self.drr]
        self.drr = (self.drr + 1) % len(self.dsems)
        if ds.total > 0:
            self._wait(es, (ds.key, ds.sem, ds.total))
        if indirect is not None:
            ins = es.eng.indirect_dma_start(out=out, in_=in_, **indirect)
        else:
            ins = es.eng.dma_start(out=out, in_=in_, **kw)
        ds.total += 16
        ins.then_inc(ds.sem, 16)
        self.n_ins += 1
        ev = (ds.key, ds.sem, ds.total)
        self._post(ev, r, w)
        self._yield()
        return ev

    def _yield(self):
        w = self._cur
        if w is None or self._atomic > 0:
            return
        self._sched_sem.release()
        w["go"].acquire()

    def interleave(self, fns, width=2):
        import threading
        if width <= 1 or len(fns) <= 1:
            for fn in fns:
                fn()
            return
        self._sched_sem = threading.Semaphore(0)
        pending = list(fns)
        active = []
        err = []

        def runner(w, fn):
            w["go"].acquire()
            try:
                fn()
            except BaseException as e:
                err.append(e)
            w["done"] = True
            self._sched_sem.release()
        while pending or active:
            while pending and len(active) < width:
                w = {"go": threading.Semaphore(0), "done": False}
                w["t"] = threading.Thread(target=runner, args=(w, pending.pop(0)), daemon=True)
                w["t"].start()
                active.append(w)
            for w in list(active):
                self._cur = w
                w["go"].release()
                self._sched_sem.acquire()
                self._cur = None
                if w["done"]:
                    active.remove(w)
                if err:
                    raise err[0]

    @contextmanager
    def atomic(self):
        self._atomic += 1
        try:
            yield
        finally:
            self._atomic -= 1

    def barrier(self):
        evs = [(e.name, e.sem, e.count) for e in self.E.values() if e.count > 0]
        evs += [(d.key, d.sem, d.total) for d in self.dsems if d.total > 0]
        for es in self.E.values():
            for ev in evs:
                if ev[0] == es.name and es.name == "pe":
                    continue
                self._wait(es, ev)


class Ring:
    def __init__(self, items):
        self.items = items
        self.i = 0

    def next(self):
        it = self.items[self.i]
        self.i = (self.i + 1) % len(self.items)
        return it


def build(TP, TO, CAP, dbg=False, stop=99):
    TA = TP + TO
    NTA, NTO, NTP = TA // 128, TO // 128, TP // 128
    NCH, CH0 = TA // 64, TP // 64
    NROW = NE * CAP + 128
    TRASH = NE * CAP
    nc = bass.Bass("TRN2", target_bir_lowering=False)

    def din(name, shape, dt=F32):
        return nc.dram_tensor(name, list(shape), dt, kind="ExternalInput").ap()

    x = din("x", [TA, D])
    w_in = din("w_in", [D, NIN])
    bfm_d = din("bfm", [128, 64]); bfmp_d = din("bfm_pre", [128, 64])
    btm_d = din("btm", [1, 3072]); btmp_d = din("btm_pre", [1, 3072])
    bif_d = din("bif", [4, 2]); bifp_d = din("bif_pre", [4, 2])
    valid_d = din("valid_tm", [128, NTA])
    cw_d = din("cw", [128, 16, 4]); cb_d = din("cb", [128, 16])
    ng_d = din("mlstm_norm_g", [1024]); dg_d = din("diff_norm_g", [128])
    lam_d = din("lamv", [4, 64])
    w_a = din("w_a", [1024, D]); w_b = din("w_b", [1024, D]); w_out = din("w_out", [D, D])
    ln1g_d = din("ln1_g", [D]); ln1b_d = din("ln1_b", [D]); ln2g_d = din("ln2_g", [D]); ln2b_d = din("ln2_b", [D])
    wr_d = din("w_r", [D, 36]); br_d = din("b_r", [1, 36])
    if stop > 6:
        w_gate = din("w_gate", [NE, D, 512]); w_up = din("w_up", [NE, D, 512]); w_down = din("w_down", [NE, 512, D])
    ident_d = din("ident", [128, 128]); mask64_d = din("mask64", [64, 64]); ustrict_d = din("ustrict", [128, 128])
    dgb_d = din("diagbias", [128, 8, 128]); abt_d = din("alibi", [128, 8, 32]); ecap_d = din("ecap", [128, 32])
    out_d = nc.dram_tensor("out", [TO, D], F32, kind="ExternalOutput").ap()

    def dscr(name, shape, dt):
        if dbg:
            return nc.dram_tensor(name, list(shape), dt, kind="ExternalOutput").ap()
        return nc.dram_tensor(name, list(shape), dt).ap()

    s_qkaT = dscr("s_qkaT", [2048, TA], BF16)
    s_qkbT = dscr("s_qkbT", [2048, TA], BF16)
    s_gT = dscr("s_gT", [4096, TO], BF16)
    s_va = dscr("s_va", [TA, 1024], BF16)
    s_vb = dscr("s_vb", [TA, 1024], BF16)
    s_oa = dscr("s_oa", [TO, 1024], BF16)
    s_seq = dscr("s_seq", [3, 4, TA], F32)
    s_dec = dscr("s_dec", [4, NCH], F32)
    s_h1 = dscr("s_h1", [TO, D], F32)
    s_Xg = dscr("s_Xg", [NROW, D], BF16)
    s_Yg = dscr("s_Yg", [NROW, D], BF16)
    s_haT = dscr("s_haT", [1024, TO], BF16)
    s_obT = dscr("s_obT", [1024, TO], BF16)
    s_mgT = dscr("s_mgT", [2048, TO], BF16)
    HB = min(512, TO)
    if dbg:
        s_TT = dscr("s_TT", [64, 3 * NCH * 4], F32)
        s_dtab = dscr("s_dtab", [128, NTO * 2], I32)
        s_wtab = dscr("s_wtab", [128, NTO * 2], F32)

    with ExitStack() as st0:
        k = Kern(nc, st0)
        try:
            banks = [(k.ps("pf%d" % i, [128, 512], F32), Dep()) for i in range(8)]
            pf = Ring(banks[0:6])
            pb = Ring([(banks[i][0].bitcast(BF16), banks[i][1]) for i in (6, 7)])
            d_c = Dep()
            identf = k.sb("identf", [128, 128], F32)
            identb = k.sb("identb", [128, 128], BF16)
            onesb = k.sb("onesb", [128, 128], BF16)
            onesf = k.sb("onesf", [128, 128], F32)
            mask64 = k.sb("mask64", [64, 64], F32)
            ustr = k.sb("ustr", [128, 128], BF16)
            dgb = k.sb("dgb", [128, 8, 128], BF16)
            abt = k.sb("abt", [128, 8, 32], F32)
            ecap = k.sb("ecap", [128, 32], F32)
            validb = k.sb("validb", [128, NTA], BF16)
            zero1 = k.sb("zero1", [128, 1], F32)
            epsc = k.sb("epsc", [128, 1], F32)
            k.dma("sp", identf[:], ident_d, w=[d_c])
            k.dma("pool", identb[:], ident_d, w=[d_c])
            k.dma("sp", mask64[:], mask64_d, w=[d_c])
            k.dma("pool", ustr[:], ustrict_d, w=[d_c])
            k.dma("pool", dgb[:], dgb_d, w=[d_c])
            k.dma("sp", abt[:], abt_d, w=[d_c])
            k.dma("sp", ecap[:], ecap_d, w=[d_c])
            k.dma("pool", validb[:], valid_d, w=[d_c])
            k.op("dve", lambda e: e.memset(onesb[:], 1.0), w=[d_c])
            k.op("dve", lambda e: e.memset(onesf[:], 1.0), w=[d_c])
            k.op("dve", lambda e: e.memset(zero1[:], 0.0), w=[d_c])
            k.op("dve", lambda e: e.memset(epsc[:], EPS), w=[d_c])
            d_zt, d_Xg, d_Yg = Dep(), Dep(), Dep()

            dtab = k.sb("dtab", [128, NTO, 2], I32)
            wtab = k.sb("wtab", [128, NTO, 2], F32)
            TT = k.sb("TT", [64, 3, NCH, 4], F32)
            decbc = k.sb("decbc", [128, 4 * NCH], F32)
            gscope = ExitStack()
            _old = k.stack
            k.stack = gscope
            Gi = k.sb("Gi", [4, TA], F32); Gf = k.sb("Gf", [4, TA], F32)
            k.stack = _old
            d_G = Dep()
            d_tab = Dep()
            d_h1 = Dep()
            d_haT, d_obT, d_mg = Dep(), Dep(), Dep()

            with k.scope():
                zt = k.sb("zt", [128, 4096], BF16)
                k.op("pool", lambda e: e.memset(zt[:], 0.0), w=[d_zt])
                r0 = 0
                while r0 < NROW:
                    nr = min(256, NROW - r0)
                    k.dma("sp", s_Xg[r0:r0 + nr, :].rearrange("(t p) d -> p t d", p=128),
                          zt[:, 0:(nr // 128) * D].rearrange("p (t d) -> p t d", d=D), r=[d_zt], w=[d_Xg])
                    r0 += nr
                k.dma("sp", s_Yg[TRASH:TRASH + 128, :], zt[:, 0:D], r=[d_zt], w=[d_Yg])
                TX = max(TP, TO)
                xT = k.sb("xT", [128, DC, TX], BF16)
                xb = Ring([(k.sb("xb", [128, D], BF16), Dep()) for _ in range(2)])
                wt = Ring([(k.sb("wt", [128, DC, 512], BF16), Dep()) for _ in range(2)])
                wif = k.sb("wif", [128, DC, 8], BF16)
                evf = Ring([(k.sb("evf", [128, 4, 512], BF16), Dep()) for _ in range(2)])
                evt = Ring([(k.sb("evt", [128, 512], BF16), Dep()) for _ in range(3)])
                bfm = k.sb("bfm", [128, 64], F32); bfmp = k.sb("bfmp", [128, 64], F32)
                btm = k.sb("btm", [1, 3072], BF16); btmp = k.sb("btmp", [1, 3072], BF16)
                bif = k.sb("bif", [4, 2], F32); bifp = k.sb("bifp", [4, 2], F32)
                d_b, d_wif = Dep(), Dep()
                k.dma("sp", bfm[:], bfm_d, w=[d_b]); k.dma("sp", bfmp[:], bfmp_d, w=[d_b])
                k.dma("pool", btm[:], btm_d, w=[d_b]); k.dma("pool", btmp[:], btmp_d, w=[d_b])
                k.dma("sp", bif[:], bif_d, w=[d_b]); k.dma("sp", bifp[:], bifp_d, w=[d_b])
                k.dma("pool", wif[:], w_in.rearrange("(c p) n -> p c n", p=128)[:, :, O_IA:O_IA + 8], w=[d_wif])
                d_scr = {"qka": Dep(), "qkb": Dep(), "g": Dep(), "va": Dep(), "vb": Dep(), "oa": Dep()}

                FM = []
                for j in range(2):
                    FM.append((O_QA + 512 * j, "qka", s_qkaT, 512 * j, AF.Identity, "q", 4 * j))
                    FM.append((O_KA + 512 * j, "qka", s_qkaT, 1024 + 512 * j, AF.Identity, "all", 8 + 4 * j))
                    FM.append((O_QB + 512 * j, "qkb", s_qkbT, 512 * j, AF.Identity, "own", 16 + 4 * j))
                    FM.append((O_KB + 512 * j, "qkb", s_qkbT, 1024 + 512 * j, AF.Identity, "all", 24 + 4 * j))
                for j in range(8):
                    FM.append((O_GA + 512 * j, "g", s_gT, 512 * j, AF.Sigmoid, "gate", 32 + 4 * j))
                TM = []
                for j in range(2):
                    TM.append((O_VA + 512 * j, "va", s_va, 512 * j, AF.Identity, "all", 512 * j))
                    TM.append((O_OA + 512 * j, "oa", s_oa, 512 * j, AF.Sigmoid, "own", 1024 + 512 * j))
                    TM.append((O_VB + 512 * j, "vb", s_vb, 512 * j, AF.Identity, "all", 2048 + 512 * j))

                for phase in ("pre", "own"):
                    t0, nt = (0, TP) if phase == "pre" else (TP, TO)
                    bfm_x, btm_x, bif_x = (bfmp, btmp, bifp) if phase == "pre" else (bfm, btm, bif)
                    d_xT = [Dep() for _ in range(nt // 128)]
                    for ti in range(nt // 128):
                        xb_t, xb_d = xb.next()
                        k.dma("pool", xb_t[:], x[t0 + ti * 128:t0 + (ti + 1) * 128, :], w=[xb_d])
                        for g in range(4):
                            pt, pd = pb.next()

                            def f(pe, g=g, pt=pt, xb_t=xb_t):
                                for j in range(4):
                                    c = g * 4 + j
                                    ins = pe.transpose(pt[:, j * 128:(j + 1) * 128], xb_t[:, c * 128:(c + 1) * 128], identb[:])
                                return ins
                            k.op("pe", f, r=[xb_d, d_c], w=[pd])
                            k.op("dve", lambda e, g=g, ti=ti, pt=pt: e.tensor_copy(
                                xT[:, g * 4:(g + 1) * 4, ti * 128:(ti + 1) * 128],
                                pt[:, 0:512].rearrange("p (j n) -> p j n", j=4)), r=[pd], w=[d_xT[ti]])
                    tb = 0
                    while tb < nt:
                        n = min(512, nt - tb)
                        dx = d_xT[tb // 128:(tb + n) // 128]
                        for gi, Gt in ((0, Gi), (1, Gf)):
                            bk, bd = pf.next()

                            def f(pe, gi=gi, bk=bk, tb=tb, n=n):
                                for c in range(DC):
                                    ins = pe.matmul(bk[0:4, 0:n], wif[:, c, gi * 4:(gi + 1) * 4], xT[:, c, tb:tb + n],
                                                    start=(c == 0), stop=(c == DC - 1))
                                return ins
                            k.op("pe", f, r=dx + [d_wif], w=[bd])
                            k.op("act", lambda e, gi=gi, Gt=Gt, bk=bk, tb=tb, n=n: e.activation(
                                Gt[0:4, t0 + tb:t0 + tb + n], bk[0:4, 0:n], AF.Identity, bias=bif_x[:, gi:gi + 1], scale=1.0),
                                r=[bd, d_b], w=[d_G])
                        tb += n
                    for (c0, dkey, dst, row0, func, which, fmc) in FM:
                        if phase == "pre":
                            if which in ("own", "gate"):
                                continue
                            tlo = (TP - 128) if which == "q" else 0
                        else:
                            tlo = 0
                        w_t, w_d = wt.next()
                        k.dma("pool", w_t[:], w_in.rearrange("(c p) n -> p c n", p=128)[:, :, c0:c0 + 512], w=[w_d])
                        tb = tlo
                        while tb < nt:
                            n = min(512, nt - tb)
                            dx = d_xT[tb // 128:(tb + n) // 128]
                            ev_t, ev_d = evf.next()
                            for g in range(4):
                                bk, bd = pf.next()

                                def f(pe, g=g, bk=bk, tb=tb, n=n, w_t=w_t):
                                    for c in range(DC):
                                        ins = pe.matmul(bk[:, 0:n], w_t[:, c, g * 128:(g + 1) * 128], xT[:, c, tb:tb + n],
                                                        start=(c == 0), stop=(c == DC - 1))
                                    return ins
                                k.op("pe", f, r=dx + [w_d], w=[bd])
                                k.op("act", lambda e, g=g, bk=bk, n=n, ev_t=ev_t, func=func, fmc=fmc: e.activation(
                                    ev_t[:, g, 0:n], bk[:, 0:n], func, bias=bfm_x[:, fmc + g:fmc + g + 1], scale=1.0),
                                    r=[bd, d_b], w=[ev_d])
                            tcol = (tb if which == "gate" else t0 + tb)
                            k.dma("sp", dst[row0:row0 + 512, tcol:tcol + n].rearrange("(g p) t -> p g t", p=128),
                                  ev_t[:, :, 0:n], r=[ev_d], w=[d_scr[dkey]])
                            tb += n
                    for (c0, dkey, dst, col0, func, which, bcol) in TM:
                        if phase == "pre" and which == "own":
                            continue
                        w_t, w_d = wt.next()
                        k.dma("pool", w_t[:], w_in.rearrange("(c p) n -> p c n", p=128)[:, :, c0:c0 + 512], w=[w_d])
                        for ti in range(nt // 128):
                            bk, bd = pf.next()

                            def f(pe, bk=bk, ti=ti, w_t=w_t, bcol=bcol):
                                for c in range(DC):
                                    pe.matmul(bk[:, :], xT[:, c, ti * 128:(ti + 1) * 128], w_t[:, c, :],
                                              start=(c == 0), stop=False)
                                return pe.matmul(bk[:, :], onesb[0:1, :], btm_x[0:1, bcol:bcol + 512], start=False, stop=True)
                            k.op("pe", f, r=[d_xT[ti], w_d, d_b, d_c], w=[bd])
                            e_t, e_d = evt.next()
                            k.op("act", lambda e, bk=bk, e_t=e_t, func=func: e.activation(e_t[:], bk[:, :], func),
                                 r=[bd], w=[e_d])
                            trow = (ti * 128 if which == "own" else t0 + ti * 128)
                            k.dma("sp", dst[trow:trow + 128, col0:col0 + 512], e_t[:], r=[e_d], w=[d_scr[dkey]])

            if True:
                if stop <= 1:
                    raise _Stop()
                with k.scope():
                    t1 = k.sb("t1", [4, TA], F32); t2 = k.sb("t2", [4, TA], F32)
                    mt = k.sb("mt", [4, NCH], F32); dec = k.sb("dec", [4, NCH], F32)
                    sq = k.sb("sq", [4, 3, TA], F32)
                    dq = Dep()
                    V = "dve"
                    k.op("act", lambda e: e.activation(t1[:], Gf[:], AF.Abs), r=[d_G], w=[dq])
                    k.op("act", lambda e: e.activation(t1[:], t1[:], AF.Exp, scale=-1.0), r=[dq], w=[dq])
                    k.op("act", lambda e: e.activation(t1[:], t1[:], AF.Ln, bias=1.0, scale=1.0), r=[dq], w=[dq])
                    k.op(V, lambda e: e.tensor_scalar_min(t2[:], Gf[:], 0.0), r=[d_G], w=[dq])
                    k.op(V, lambda e: e.tensor_sub(t2[:], t2[:], t1[:]), r=[dq], w=[dq])
                    k.op(V, lambda e: e.tensor_scalar_mul(t2[:], t2[:], 0.5), r=[dq], w=[dq])
                    k.op(V, lambda e: e.tensor_tensor_scan(t1[:], t2[:], t2[:], 0.0, ALU.add, ALU.add), r=[dq], w=[dq])
                    Bc = t1
                    k.op(V, lambda e: e.tensor_sub(Gi[:], Gi[:], Bc[:]), r=[dq, d_G], w=[dq, d_G])
                    at = Gi
                    k.op(V, lambda e: e.tensor_tensor_scan(t2[:], at[:], at[:], 0.0, ALU.max, ALU.max), r=[dq, d_G], w=[dq])
                    ut = t2
                    ut3 = ut[:].rearrange("p (c s) -> p c s", s=64)
                    at3 = at[:].rearrange("p (c s) -> p c s", s=64)
                    Bc3 = Bc[:].rearrange("p (c s) -> p c s", s=64)
                    k.op(V, lambda e: e.memset(mt[:, 0:1], 0.0), w=[dq])
                    if NCH > 1:
                        k.op(V, lambda e: e.tensor_copy(mt[:, 1:NCH], ut3[:, 0:NCH - 1, 63]), r=[dq], w=[dq])
                    uL = ut3[:, :, 63]
                    mtb = mt[:, :].unsqueeze(2).to_broadcast([4, NCH, 64])
                    k.op(V, lambda e: e.tensor_sub(dec[:], mt[:], uL), r=[dq], w=[dq])
                    k.op("act", lambda e: e.activation(dec[:], dec[:], AF.Exp), r=[dq], w=[dq])
                    sq0 = sq[:, 0, :].rearrange("p (c s) -> p c s", s=64)
                    sq1 = sq[:, 1, :].rearrange("p (c s) -> p c s", s=64)
                    sq2 = sq[:, 2, :].rearrange("p (c s) -> p c s", s=64)
                    k.op(V, lambda e: e.tensor_sub(sq0, at3, mtb), r=[dq, d_G], w=[dq])
                    k.op(V, lambda e: e.tensor_scalar(sq[:, 0, :], sq[:, 0, :], 80.0, -LN16, ALU.min, ALU.add), r=[dq], w=[dq])
                    k.op("act", lambda e: e.activation(sq[:, 0, :], sq[:, 0, :], AF.Exp), r=[dq], w=[dq])
                    k.op(V, lambda e: e.tensor_tensor(sq1, sq0, dec[:, :].unsqueeze(2).to_broadcast([4, NCH, 64]), ALU.mult),
                         r=[dq], w=[dq])
                    k.op(V, lambda e: e.tensor_tensor(sq2, Bc3, mtb, ALU.add), r=[dq], w=[dq])
                    k.op(V, lambda e: e.tensor_scalar(sq[:, 2, :], sq[:, 2, :], -1.0, 80.0, ALU.mult, ALU.min), r=[dq], w=[dq])
                    k.op("act", lambda e: e.activation(sq[:, 2, :], sq[:, 2, :], AF.Exp), r=[dq], w=[dq])
                    d_seq = Dep()
                    k.dma("sp", s_dec, dec[:], r=[dq], w=[d_seq])
                    d_TT = Dep()
                    for q in range(3):
                        c0 = 0
                        while c0 < NCH:
                            ncc = min(128, NCH - c0)
                            bk, bd = pf.next()

                            def f(pe, bk=bk, q=q, c0=c0, ncc=ncc):
                                for cc in range(ncc):
                                    c = c0 + cc
                                    ins = pe.transpose(bk[0:64, cc * 4:cc * 4 + 4], sq[0:4, q, c * 64:(c + 1) * 64], identf[0:4, 0:4])
                                return ins
                            k.op("pe", f, r=[dq, d_c], w=[bd])
                            k.op("act", lambda e, bk=bk, q=q, c0=c0, ncc=ncc: e.copy(
                                TT[:, q, c0:c0 + ncc, :], bk[0:64, 0:ncc * 4].rearrange("p (c h) -> p c h", h=4)), r=[bd], w=[d_TT])
                            c0 += ncc
                    k.dma("sp", decbc[:], s_dec.rearrange("h c -> (h c)").partition_broadcast(128), r=[d_seq], w=[d_TT])
                gscope.close()
                if dbg:
                    k.dma("sp", s_TT, TT[:].rearrange("p a b c -> p (a b c)"), r=[d_TT], w=[Dep()])
                if stop <= 2:
                    raise _Stop()
                with k.scope():
                    qT = k.sb("qT", [128, 8, TO], BF16)
                    kT = k.sb("kT", [128, 8, TA], BF16)
                    cw = k.sb("cw", [128, 16, 4], F32); cb = k.sb("cb", [128, 16], F32)
                    ngb = k.sb("ngb", [64, 1024], F32)
                    d_cw, d_qT, d_kT = Dep(), Dep(), Dep()
                    k.dma("sp", cw[:], cw_d, w=[d_cw]); k.dma("sp", cb[:], cb_d, w=[d_cw])
                    k.dma("sp", ngb[:], ng_d.partition_broadcast(64), w=[d_cw])
                    cscope = k.scope()
                    cscope.__enter__()
                    cin = Ring([(k.sb("cin", [128, 3 + TA], BF16), Dep()) for _ in range(3)])
                    Dg = k.sb("Dg", [128, 16, 4, 128], BF16)
                    d_Dg = Dep()
                    for fc in range(16):
                        for j in range(4):
                            k.op("dve", lambda e, fc=fc, j=j: e.tensor_scalar_mul(Dg[:, fc, j, :], identf[:], cw[:, fc, j:j + 1]),
                                 r=[d_cw, d_c], w=[d_Dg])
                    for fc in list(range(8, 16)) + list(range(8)):
                        isq = fc < 8
                        lo = (TP - 128) if isq else 0
                        o0 = TP if isq else 0
                        n = TA - o0
                        ci, cd = cin.next()
                        if not isq:
                            k.op("pool", lambda e, ci=ci: e.memset(ci[:, 0:3], 0.0), w=[cd])
                        k.dma("sp", ci[:, 3 + lo:3 + TA], s_qkaT[fc * 128:(fc + 1) * 128, lo:TA], r=[d_scr["qka"]], w=[cd])
                        tb = 0
                        while tb < n:
                            nn = min(512, n - tb)
                            bk, bd = pf.next()

                            def f(pe, bk=bk, ci=ci, fc=fc, o0=o0, tb=tb, nn=nn):
                                for j in range(4):
                                    ins = pe.matmul(bk[:, 0:nn], Dg[:, fc, j, :], ci[:, o0 + tb + j:o0 + tb + j + nn],
                                                    start=(j == 0), stop=(j == 3))
                                return ins
                            k.op("pe", f, r=[cd, d_Dg], w=[bd])
                            if isq:
                                k.op("act", lambda e, bk=bk, fc=fc, tb=tb, nn=nn: e.activation(
                                    qT[:, fc, tb:tb + nn], bk[:, 0:nn], AF.Silu, bias=cb[:, fc:fc + 1], scale=1.0),
                                    r=[bd, d_cw], w=[d_qT])
                            else:
                                k.op("act", lambda e, bk=bk, fc=fc, tb=tb, nn=nn: e.activation(
                                    kT[:, fc - 8, tb:tb + nn], bk[:, 0:nn], AF.Silu, bias=cb[:, fc:fc + 1], scale=1.0),
                                    r=[bd, d_cw], w=[d_kT])
                            tb += nn
                    cscope.__exit__(None, None, None)
                    a_S = Ring([banks[0], banks[1]])
                    bO, bOd = banks[2]
                    bO1, bO1d = banks[3]
                    m_U = Ring(banks[4:5])
                    bSN, bSNd = banks[5]
                    bNN, bNNd = banks[6]
                    pb_all = pb
                    pb = Ring([(banks[7][0].bitcast(BF16), banks[7][1])])
                    Cst = [k.sb("Cst", [128, 2, 257], F32) for _ in range(4)]
                    hblk = Ring([(k.sb("hblk", [128, 8, HB], BF16), Dep()) for _ in range(1)])
                    Cbf = [k.sb("Cbf", [128, 2, 257], BF16) for _ in range(4)]
                    d_C = [Dep() for _ in range(4)]
                    d_Cbf = [Dep() for _ in range(4)]
                    for h in range(4):
                        k.op("pool", lambda e, h=h: e.memset(Cst[h][:], 0.0), w=[d_C[h]])
                        k.op("pool", lambda e, h=h: e.memset(Cbf[h][:], 0.0), w=[d_Cbf[h]])
                    vch_items = []
                    for _ in range(3):
                        vt = k.sb("vch", [64, 4, 257], BF16)
                        vd = Dep()
                        k.op("pool", lambda e, vt=vt: e.memset(vt[:, :, 256:257], 1.0), w=[vd])
                        vch_items.append((vt, vd))
                    vch = Ring(vch_items)
                    sor = Ring([(k.sb("so", [64, 1024], BF16), Dep()) for _ in range(1)])
                    kwr = Ring([(k.sb("kw", [64, 256], BF16), Dep()) for _ in range(3)])
                    Wtr = Ring([(k.sb("Wt", [64, 64], BF16), Dep()) for _ in range(3)])
                    Nsr = Ring([(k.sb("Ns", [64, 4, 257], F32), Dep()) for _ in range(2)])
                    hgr = Ring([(k.sb("hg", [64, 4, 256], F32), Dep()) for _ in range(1)])
                    hbr = Ring([(k.sb("hb", [64, 1024], BF16), Dep()) for _ in range(2)])
                    smr = Ring([(k.sb("sm", [64, 64], F32), Dep()) for _ in range(2)])
                    lamt = k.sb("lamt", [128, 4, 64], F32)
                    lsm = k.sb("lsm", [128, 8], F32)
                    gnb = k.sb("gnb", [128, 128], F32)
                    d_l = Dep()
                    k.dma("sp", lamt[:], lam_d.rearrange("a b -> (a b)").partition_broadcast(128).rearrange("p (a b) -> p a b", a=4), w=[d_l])
                    k.dma("sp", gnb[:], dg_d.partition_broadcast(128), w=[d_l])
                    k.op("dve", lambda e: e.tensor_tensor(lamt[:, 0, :], lamt[:, 0, :], lamt[:, 1, :], ALU.mult), r=[d_l], w=[d_l])
                    k.op("dve", lambda e: e.tensor_tensor(lamt[:, 2, :], lamt[:, 2, :], lamt[:, 3, :], ALU.mult), r=[d_l], w=[d_l])
                    k.op("dve", lambda e: e.reduce_sum(lsm[:, 0:1], lamt[:, 0, :], AX.X), r=[d_l], w=[d_l])
                    k.op("dve", lambda e: e.reduce_sum(lsm[:, 1:2], lamt[:, 2, :], AX.X), r=[d_l], w=[d_l])
                    k.op("act", lambda e: e.activation(lsm[:, 2:4], lsm[:, 0:2], AF.Exp), r=[d_l], w=[d_l])
                    k.op("dve", lambda e: e.tensor_sub(lsm[:, 4:5], lsm[:, 3:4], lsm[:, 2:3]), r=[d_l], w=[d_l])
                    k.op("dve", lambda e: e.tensor_scalar_add(lsm[:, 5:6], lsm[:, 4:5], -0.2), r=[d_l], w=[d_l])
                    k.op("dve", lambda e: e.tensor_scalar_mul(gnb[:], gnb[:], 0.8), r=[d_l], w=[d_l])
                    neglam = lsm[:, 5:6]
                    kb_items = []
                    for _ in range(1):
                        kt_ = k.sb("kbT", [128, 2, TA], BF16)
                        kd_ = Dep()
                        k.op("pool", lambda e, kt_=kt_: e.memset(kt_[64:128, 0, :], 0.0), w=[kd_])
                        k.op("pool", lambda e, kt_=kt_: e.memset(kt_[0:64, 1, :], 0.0), w=[kd_])
                        kb_items.append((kt_, kd_))
                    kbr = Ring(kb_items)
                    qbr = Ring([(k.sb("qbT", [128, TO], BF16), Dep()) for _ in range(1)])
                    vb_items = []
                    for _ in range(1):
                        vt = k.sb("vbe", [128, NTA, 129], BF16)
                        vd = Dep()
                        k.op("dve", lambda e, vt=vt: e.tensor_copy(vt[:, :, 128], validb[:, :]), r=[d_c], w=[vd])
                        vb_items.append((vt, vd))
                    vbr = Ring(vb_items)
                    PTr = Ring([(k.sb("PT", [128, 256], BF16), Dep()) for _ in range(4)])
                    o1r = Ring([(k.sb("o1", [128, 128], F32), Dep()) for _ in range(2)])
                    o2r = Ring([(k.sb("o2", [128, 128], F32), Dep()) for _ in range(2)])
                    obr = Ring([(k.sb("ob", [128, 128], BF16), Dep()) for _ in range(4)])
                    s8r = Ring([(k.sb("s8", [128, 8], F32), Dep()) for _ in range(2)])
                    Osr = Ring([(k.sb("Os", [128, 2, 129], F32), Dep()) for _ in range(2)])
                    oblk = Ring([(k.sb("oblk", [128, HB], BF16), Dep()) for _ in range(2)])

                    def mlstm_gen():
                        st_m = {'hb': None, 'fin': None}
                        for c in range(NCH):
                            own = c >= CH0
                            tq = (c - CH0) * 64
                            v_t, v_d = vch.next()
                            k.dma("sp", v_t[:, :, 0:256], s_va[c * 64:(c + 1) * 64, :].rearrange("s (h d) -> s h d", h=4),
                                  r=[d_scr["va"]], w=[v_d])
                            if own:
                                so_t, so_d = sor.next()
                                k.dma("sp", so_t[:], s_oa[tq:tq + 64, :], r=[d_scr["oa"]], w=[so_d])
                                Ns_t, Ns_d = Nsr.next()
                            for h in range(4):
                                pt, pd = pb.next()

                                def f(pe, pt=pt, h=h, c=c):
                                    for j in range(2):
                                        ins = pe.transpose(pt[0:64, j * 128:(j + 1) * 128], kT[:, h * 2 + j, c * 64:(c + 1) * 64], identb[:])
                                    return ins
                                k.op("pe", f, r=[d_kT, d_c], w=[pd])
                                kw_t, kw_d = kwr.next()
                                k.op("act", lambda e, kw_t=kw_t, pt=pt, h=h, c=c: e.activation(
                                    kw_t[:], pt[0:64, 0:256], AF.Identity, scale=TT[:, 1, c, h:h + 1]), r=[pd, d_TT], w=[kw_d])
                                yield
                                bU, bUd = m_U.next()

                                def f(pe, bU=bU, kw_t=kw_t, v_t=v_t, h=h):
                                    for j in range(2):
                                        pe.matmul(bU[:, j * 256:(j + 1) * 256], kw_t[:, j * 128:(j + 1) * 128], v_t[:, h, 0:256],
                                                  start=True, stop=True)
                                    for j in range(2):
                                        ins = pe.matmul(bSN[:, 400 + j:401 + j], kw_t[:, j * 128:(j + 1) * 128], v_t[:, h, 256:257],
                                                        start=True, stop=True)
                                    return ins
                                k.op("pe", f, r=[kw_d, v_d], w=[bUd, bSNd])
                                if not own:
                                    yield
                                if own:
                                    def f(pe, h=h, c=c, tq=tq):
                                        for j in range(2):
                                            ins = pe.matmul(bSN[0:64, 320:384], kT[:, h * 2 + j, c * 64:(c + 1) * 64],
                                                            qT[:, h * 2 + j, tq:tq + 64], start=(j == 0), stop=(j == 1))
                                        return ins
                                    k.op("pe", f, r=[d_kT, d_qT], w=[bSNd])
                                    W_t, W_d = Wtr.next()
                                    k.op("dve", lambda e, W_t=W_t, h=h, c=c: e.scalar_tensor_tensor(
                                        W_t[:], bSN[0:64, 320:384], TT[:, 0, c, h:h + 1], mask64[:], ALU.mult, ALU.mult),
                                        r=[bSNd, d_TT, d_c], w=[W_d])
                                    yield

                                    def f(pe, W_t=W_t, v_t=v_t, h=h, tq=tq):
                                        for j in range(2):
                                            pe.matmul(bNN[0:64, 0:257], qT[:, h * 2 + j, tq:tq + 64], Cbf[h][:, j, :],
                                                      start=(j == 0), stop=False)
                                        return pe.matmul(bNN[0:64, 0:257], W_t[:], v_t[:, h, :], start=False, stop=True)
                                    k.op("pe", f, r=[d_qT, d_Cbf[h], W_d, v_d], w=[bNNd])
                                    k.op("act", lambda e, Ns_t=Ns_t, h=h: e.copy(Ns_t[:, h, :], bNN[0:64, 0:257]),
                                         r=[bNNd], w=[Ns_d])
                                    yield
                                dsc = decbc[:, h * NCH + c:h * NCH + c + 1]
                                k.op("dve", lambda e, h=h, bU=bU, dsc=dsc: e.scalar_tensor_tensor(
                                    Cst[h][:, :, 0:256], Cst[h][:, :, 0:256], dsc,
                                    bU[:, 0:512].rearrange("p (j n) -> p j n", j=2), ALU.mult, ALU.add),
                                    r=[bUd, d_TT], w=[d_C[h]])
                                k.op("dve", lambda e, h=h, dsc=dsc: e.scalar_tensor_tensor(
                                    Cst[h][:, :, 256:257], Cst[h][:, :, 256:257], dsc,
                                    bSN[:, 400:402].rearrange("p (j n) -> p j n", j=2), ALU.mult, ALU.add),
                                    r=[bSNd, d_TT], w=[d_C[h]])
                                k.op("act", lambda e, h=h: e.copy(Cbf[h][:], Cst[h][:]), r=[d_C[h]], w=[d_Cbf[h]])
                                if h == 1 and st_m['fin'] is not None:
                                    st_m['fin']()
                                    st_m['fin'] = None
                                yield
                            if own:
                                sm_t, sm_d = smr.next()
                                k.op("act", lambda e, sm_t=sm_t, Ns_t=Ns_t: e.activation(
                                    sm_t[:, 0:4], Ns_t[:, :, 256], AF.Abs), r=[Ns_d], w=[sm_d])
                                k.op("dve", lambda e, sm_t=sm_t, c=c: e.tensor_tensor(
                                    sm_t[:, 0:4], sm_t[:, 0:4], TT[:, 2, c, :], ALU.max), r=[sm_d, d_TT], w=[sm_d])
                                k.op("dve", lambda e, sm_t=sm_t: e.reciprocal(sm_t[:, 0:4], sm_t[:, 0:4]), r=[sm_d], w=[sm_d])
                                hg_t, hg_d = hgr.next()
                                k.op("dve", lambda e, hg_t=hg_t, Ns_t=Ns_t, sm_t=sm_t: e.tensor_tensor(
                                    hg_t[:], Ns_t[:, :, 0:256], sm_t[:, 0:4].unsqueeze(2).to_broadcast([64, 4, 256]), ALU.mult),
                                    r=[Ns_d, sm_d], w=[hg_d])
                                k.op("dve", lambda e, hg_t=hg_t, so_t=so_t: e.tensor_tensor(
                                    hg_t[:], hg_t[:], so_t[:].rearrange("s (h d) -> s h d", h=4), ALU.mult),
                                    r=[so_d, hg_d], w=[hg_d])

                                def f(e, hg_t=hg_t, sm_t=sm_t):
                                    for hh in range(4):
                                        ins = e.bn_stats(sm_t[:, 4 + 6 * hh:10 + 6 * hh], hg_t[:, hh, :])
                                    return ins
                                k.op("dve", f, r=[hg_d], w=[sm_d])

                                def f(e, sm_t=sm_t):
                                    for hh in range(4):
                                        ins = e.bn_aggr(sm_t[:, 28 + 2 * hh:30 + 2 * hh], sm_t[:, 4 + 6 * hh:10 + 6 * hh])
                                    return ins
                                k.op("dve", f, r=[sm_d], w=[sm_d])
                                mv = sm_t[:, 28:36].rearrange("s (h k) -> s h k", h=4)
                                k.op("act", lambda e, sm_t=sm_t, mv=mv: e.activation(
                                    sm_t[:, 36:40], mv[:, :, 1], AF.Ln, bias=epsc[0:64, 0:1], scale=1.0), r=[sm_d, d_c], w=[sm_d])
                                k.op("act", lambda e, sm_t=sm_t: e.activation(
                                    sm_t[:, 36:40], sm_t[:, 36:40], AF.Exp, scale=-0.5), r=[sm_d], w=[sm_d])
                                k.op("dve", lambda e, hg_t=hg_t, mv=mv: e.tensor_tensor(
                                    hg_t[:], hg_t[:], mv[:, :, 0:1].to_broadcast([64, 4, 256]), ALU.subtract),
                                    r=[sm_d, hg_d], w=[hg_d])
                                k.op("dve", lambda e, hg_t=hg_t, sm_t=sm_t: e.tensor_tensor(
                                    hg_t[:], hg_t[:], sm_t[:, 36:40].unsqueeze(2).to_broadcast([64, 4, 256]), ALU.mult),
                                    r=[sm_d, hg_d], w=[hg_d])
                                hb_t, hb_d = hbr.next()
                                k.op("dve", lambda e, hg_t=hg_t, hb_t=hb_t: e.tensor_tensor(
                                    hb_t[:], hg_t[:].rearrange("s h d -> s (h d)"), ngb[:], ALU.mult),
                                    r=[hg_d, d_cw], w=[hb_d])
                                def mfin(hb_t=hb_t, hb_d=hb_d, tq=tq):
                                    pt, pd = pb.next()

                                    def f(pe):
                                        for fc in range(8):
                                            ins = pe.transpose(pt[:, fc * 64:(fc + 1) * 64], hb_t[:, fc * 128:(fc + 1) * 128], identb[0:64, 0:64])
                                        return ins
                                    k.op("pe", f, r=[hb_d, d_c], w=[pd])
                                    if tq % HB == 0:
                                        st_m['hb'] = hblk.next()
                                    hk_t, hk_d = st_m['hb']
                                    k.op("act", lambda e: e.copy(
                                        hk_t[:, :, tq % HB:tq % HB + 64], pt[:, 0:512].rearrange("p (f s) -> p f s", f=8)), r=[pd], w=[hk_d])
                                    if (tq + 64) % HB == 0:
                                        tb0 = tq + 64 - HB
                                        k.dma("sp", s_haT[:, tb0:tb0 + HB].rearrange("(f p) t -> p f t", p=128), hk_t[:], r=[hk_d], w=[d_haT])
                                st_m['fin'] = mfin
                            yield
                        if st_m['fin'] is not None:
                            st_m['fin']()
                            st_m['fin'] = None
                        yield

                    def attn_gen():
                        st_a = {'ob': None}
                        deferred = []
                        for h in range(8):
                            kb_t, kb_d = kbr.next(); qb_t, qb_d = qbr.next(); vb_t, vb_d = vbr.next()
                            k.dma("sp", kb_t[0:64, 0, :], s_qkbT[1024 + h * 128:1024 + h * 128 + 64, :], r=[d_scr["qkb"]], w=[kb_d])
                            k.dma("sp", kb_t[64:128, 1, :], s_qkbT[1024 + h * 128 + 64:1024 + (h + 1) * 128, :], r=[d_scr["qkb"]], w=[kb_d])
                            k.dma("sp", qb_t[:], s_qkbT[h * 128:(h + 1) * 128, TP:TA], r=[d_scr["qkb"]], w=[qb_d])
                            for t8 in range(0, NTA, 8):
                                t9 = min(NTA, t8 + 8)
                                k.dma("sp", vb_t[:, t8:t9, 0:128],
                                      s_vb[t8 * 128:t9 * 128, h * 128:(h + 1) * 128].rearrange("(t p) d -> p t d", p=128),
                                      r=[d_scr["vb"]], w=[vb_d])
                            items = []
                            for qi in range(NTO):
                                qt = NTP + qi
                                kt_lo = 0
                                while kt_lo < qt and SLOPES[h] * (127 - 128 * (qt - kt_lo)) < -ACLAMP:
                                    kt_lo += 1
                                for kt in range(kt_lo, qt + 1):
                                    items.append((qi, qt, kt, kt_lo))

                            def emit_qk(it, h=h, kb_t=kb_t, qb_t=qb_t, kb_d=kb_d, qb_d=qb_d):
                                qi, qt, kt, kt_lo = it
                                bSt, sd = a_S.next()
                                off = 0
                                diag = (kt == qt)

                                def f(pe):
                                    for m in range(2):
                                        ins = pe.matmul(bSt[:, off + m * 128:off + (m + 1) * 128], kb_t[:, m, kt * 128:(kt + 1) * 128],
                                                        qb_t[:, qi * 128:(qi + 1) * 128], start=True, stop=(not diag))
                                        if diag:
                                            ins = pe.matmul(bSt[:, off + m * 128:off + (m + 1) * 128], identb[:], dgb[:, h, :], start=False, stop=True)
                                    return ins
                                k.op("pe", f, r=[kb_d, qb_d, d_c], w=[sd])
                                return bSt, sd
                            def do_pv(it, P_t, P_d, h=h, vb_t=vb_t, vb_d=vb_d):
                                qi, qt, kt, kt_lo = it

                                def f(pe, P_t=P_t, vb_t=vb_t, kt=kt, qt=qt, kt_lo=kt_lo):
                                    pe.matmul(bO[:, 0:129], P_t[:, 0:128], vb_t[:, kt, :], start=(kt == kt_lo), stop=(kt == qt))
                                    return pe.matmul(bO1[:, 0:129], P_t[:, 128:256], vb_t[:, kt, :], start=(kt == kt_lo), stop=(kt == qt))
                                k.op("pe", f, r=[P_d, vb_d], w=[bOd, bO1d])
                                if kt != qt:
                                    return
                                s8, s8d = s8r.next()
                                Os, Osd = Osr.next()
                                k.op("act", lambda e, Os=Os: e.copy(Os[:, 0, :], bO[:, 0:129]), r=[bOd], w=[Osd])
                                k.op("dve", lambda e, Os=Os: e.tensor_copy(Os[:, 1, :], bO1[:, 0:129]), r=[bO1d], w=[Osd])
                                k.op("dve", lambda e, s8=s8, Os=Os: e.reciprocal(s8[:, 0:2], Os[:, :, 128]), r=[Osd], w=[s8d])
                                k.op("dve", lambda e, s8=s8: e.tensor_tensor(s8[:, 2:3], s8[:, 1:2], neglam, ALU.mult),
                                     r=[s8d, d_l], w=[s8d])
                                o1, o1d = o1r.next(); o2, o2d = o2r.next()
                                k.op("dve", lambda e, o1=o1, s8=s8, Os=Os: e.tensor_scalar_mul(o1[:], Os[:, 0, 0:128], s8[:, 0:1]),
                                     r=[Osd, s8d], w=[o1d])
                                k.op("dve", lambda e, o1=o1, s8=s8, Os=Os: e.scalar_tensor_tensor(
                                    o1[:], Os[:, 1, 0:128], s8[:, 2:3], o1[:], ALU.mult, ALU.add), r=[Osd, s8d], w=[o1d])
                                k.op("pool", lambda e, o1=o1, o2=o2: e.tensor_tensor(o2[:], o1[:], o1[:], ALU.mult), r=[o1d], w=[o2d])
                                k.op("dve", lambda e, o2=o2, s8=s8: e.reduce_sum(s8[:, 3:4], o2[:], AX.X), r=[o2d], w=[s8d])
                                k.op("dve", lambda e, s8=s8: e.tensor_scalar(s8[:, 4:5], s8[:, 3:4], 1.0 / 128.0, EPS, ALU.mult, ALU.add),
                                     r=[s8d], w=[s8d])
                                k.op("act", lambda e, s8=s8: e.activation(s8[:, 5:6], s8[:, 4:5], AF.Ln), r=[s8d], w=[s8d])
                                k.op("act", lambda e, s8=s8: e.activation(s8[:, 5:6], s8[:, 5:6], AF.Exp, scale=-0.5),
                                     r=[s8d], w=[s8d])
                                ob, obd = obr.next()
                                k.op("dve", lambda e, ob=ob, o1=o1, s8=s8: e.scalar_tensor_tensor(
                                    ob[:], o1[:], s8[:, 5:6], gnb[:], ALU.mult, ALU.mult), r=[o1d, s8d, d_l], w=[obd])
                                def fin(ob=ob, obd=obd, qi=qi, h=h):
                                    pt, pd = pb.next()
                                    k.op("pe", lambda pe: pe.transpose(pt[:, 0:128], ob[:], identb[:]), r=[obd, d_c], w=[pd])
                                    tq = qi * 128
                                    if tq % HB == 0:
                                        st_a['ob'] = oblk.next()
                                    ok_t, ok_d = st_a['ob']
                                    k.op("act", lambda e: e.copy(ok_t[:, tq % HB:tq % HB + 128], pt[:, 0:128]),
                                         r=[pd], w=[ok_d])
                                    if (tq + 128) % HB == 0:
                                        tb0 = tq + 128 - HB
                                        k.dma("sp", s_obT[h * 128:(h + 1) * 128, tb0:tb0 + HB], ok_t[:], r=[ok_d], w=[d_obT])
                                deferred.append([3, fin])
                            nxt = emit_qk(items[0])
                            pend = None
                            for j, it in enumerate(items):
                                qi, qt, kt, kt_lo = it
                                bSt, sd = nxt
                                off = 0
                                if j + 1 < len(items):
                                    nxt = emit_qk(items[j + 1])
                                diag = (kt == qt)
                                P_t, P_d = PTr.next()
                                bias_ap = zero1[:, 0:1] if diag else abt[:, h, qt - kt:qt - kt + 1]
                                k.op("act", lambda e, P_t=P_t, off=off, bias_ap=bias_ap, bSt=bSt: e.activation(
                                    P_t[:], bSt[:, off:off + 256], AF.Exp, bias=bias_ap, scale=0.125), r=[sd, d_c], w=[P_d])

                                if pend is not None:
                                    do_pv(*pend)
                                pend = (it, P_t, P_d)
                                for dfr in list(deferred):
                                    dfr[0] -= 1
                                    if dfr[0] <= 0:
                                        deferred.remove(dfr)
                                        dfr[1]()
                                yield
                            if pend is not None:
                                do_pv(*pend)
                            yield
                        for dfr in list(deferred):
                            dfr[1]()
                        deferred.clear()
                        yield

                    gm = mlstm_gen()
                    ga_ = attn_gen()
                    if stop <= 3:
                        for _ in gm:
                            pass
                        raise _Stop()
                    if stop <= 3.5:
                        for _ in ga_:
                            pass
                        raise _Stop()
                    n_m = (CH0 * (4 * 3 + 1)) + ((NCH - CH0) * (4 * 4 + 1))
                    n_a = 0
                    for h_ in range(8):
                        for qi_ in range(NTO):
                            qt_ = NTP + qi_
                            lo_ = 0
                            while lo_ < qt_ and SLOPES[h_] * (127 - 128 * (qt_ - lo_)) < -ACLAMP:
                                lo_ += 1
                            n_a += qt_ + 1 - lo_
                    done_m = done_a = 0
                    m_alive = a_alive = True
                    while m_alive or a_alive:
                        if m_alive:
                            try:
                                next(gm); done_m += 1
                            except StopIteration:
                                m_alive = False
                        if ILV == 0:
                            tgt = n_a + 10 if not m_alive else 0
                        else:
                            tgt = n_a + 10 if not m_alive else (done_m * n_a) // n_m
                        while a_alive and done_a < tgt:
                            try:
                                next(ga_); done_a += 1
                            except StopIteration:
                                a_alive = False
                    for g_ in (gm, ga_):
                        for _ in g_:
                            pass
                    pb = pb_all
                if stop <= 4:
                    raise _Stop()
                with k.scope():
                    war = Ring([(k.sb("wa", [128, 8, 512], BF16), Dep()) for _ in range(2)])
                    wbr = Ring([(k.sb("wb", [128, 8, 512], BF16), Dep()) for _ in range(2)])
                    gar = Ring([(k.sb("ga", [128, 512], BF16), Dep()) for _ in range(2)])
                    gbr = Ring([(k.sb("gb", [128, 512], BF16), Dep()) for _ in range(2)])
                    m1r = Ring([(k.sb("m1", [128, 512], F32), Dep()) for _ in range(2)])
                    m2r = Ring([(k.sb("m2", [128, 512], F32), Dep()) for _ in range(2)])
                    hkr = Ring([(k.sb("hk", [128, 8, HB], BF16), Dep()) for _ in range(2)])
                    okr = Ring([(k.sb("ok", [128, 8, HB], BF16), Dep()) for _ in range(2)])
                    mgr = Ring([(k.sb("mgo", [128, 512], BF16), Dep()) for _ in range(3)])
                    st_e1 = {"db": None, "tb": None}

                    def e1_unit(db, tb, n, g):
                        with k.atomic():
                            if st_e1["db"] != db:
                                wa_t, wa_d = war.next(); wb_t, wb_d = wbr.next()
                                k.dma("pool", wa_t[:], w_a.rearrange("(c p) n -> p c n", p=128)[:, :, db * 512:(db + 1) * 512], w=[wa_d])
                                k.dma("pool", wb_t[:], w_b.rearrange("(c p) n -> p c n", p=128)[:, :, db * 512:(db + 1) * 512], w=[wb_d])
                                st_e1["db"] = db
                                st_e1["w"] = (wa_t, wa_d, wb_t, wb_d)
                            if st_e1["tb"] != (db, tb):
                                hk, hkd = hkr.next(); ok, okd = okr.next()
                                k.dma("sp", hk[:, :, 0:n], s_haT[:, tb:tb + n].rearrange("(f p) t -> p f t", p=128), r=[d_haT], w=[hkd])
                                k.dma("sp", ok[:, :, 0:n], s_obT[:, tb:tb + n].rearrange("(f p) t -> p f t", p=128), r=[d_obT], w=[okd])
                                st_e1["tb"] = (db, tb)
                                st_e1["h"] = (hk, hkd, ok, okd)
                        wa_t, wa_d, wb_t, wb_d = st_e1["w"]
                        hk, hkd, ok, okd = st_e1["h"]
                        dc = db * 4 + g
                        ga_t, ga_d = gar.next(); gb_t, gb_d = gbr.next()
                        k.dma("sp", ga_t[:, 0:n], s_gT[dc * 128:(dc + 1) * 128, tb:tb + n], r=[d_scr["g"]], w=[ga_d])
                        k.dma("sp", gb_t[:, 0:n], s_gT[2048 + dc * 128:2048 + (dc + 1) * 128, tb:tb + n], r=[d_scr["g"]], w=[gb_d])
                        bA, bAd = pf.next(); bB, bBd = pf.next()

                        def f(pe):
                            for fc in range(8):
                                ins = pe.matmul(bA[:, 0:n], wa_t[:, fc, g * 128:(g + 1) * 128], hk[:, fc, 0:n],
                                                start=(fc == 0), stop=(fc == 7))
                            return ins
                        k.op("pe", f, r=[wa_d, hkd], w=[bAd])

                        def f(pe):
                            for fc in range(8):
                                ins = pe.matmul(bB[:, 0:n], wb_t[:, fc, g * 128:(g + 1) * 128], ok[:, fc, 0:n],
                                                start=(fc == 0), stop=(fc == 7))
                            return ins
                        k.op("pe", f, r=[wb_d, okd], w=[bBd])
                        m1, m1d = m1r.next(); m2, m2d = m2r.next()
                        k.op("dve", lambda e: e.tensor_tensor(m1[:, 0:n], bA[:, 0:n], ga_t[:, 0:n], ALU.mult),
                             r=[bAd, ga_d], w=[m1d])
                        k.op("dve", lambda e: e.tensor_tensor(m2[:, 0:n], bB[:, 0:n], gb_t[:, 0:n], ALU.mult),
                             r=[bBd, gb_d], w=[m2d])
                        mg_t, mg_dd = mgr.next()
                        k.op("pool", lambda e: e.tensor_tensor(mg_t[:, 0:n], m1[:, 0:n], m2[:, 0:n], ALU.add), r=[m1d, m2d], w=[mg_dd])
                        k.dma("sp", s_mgT[dc * 128:(dc + 1) * 128, tb:tb + n], mg_t[:, 0:n], r=[mg_dd], w=[d_mg])
                    units = []
                    for db in range(4):
                        tb = 0
                        while tb < TO:
                            n = min(512, TO - tb)
                            for g in range(4):
                                units.append(lambda db=db, tb=tb, n=n, g=g: e1_unit(db, tb, n, g))
                            tb += n
                    k.interleave(units, width=NIL)
            if stop <= 5:
                raise _Stop()
            with k.scope():
                wo = k.sb("wo", [128, DC, D], BF16)
                wr = k.sb("wr", [128, DC, 36], F32)
                br = k.sb("br", [1, 36], F32)
                l1g = k.sb("l1g", [128, D], F32); l1b = k.sb("l1b", [128, D], F32)
                cnt = k.sb("cnt", [128, 32], F32)
                d_wo, d_cnt = Dep(), Dep()
                for q4 in range(4):
                    k.dma("pool", wo[:, :, q4 * 512:(q4 + 1) * 512],
                          w_out.rearrange("(c p) n -> p c n", p=128)[:, :, q4 * 512:(q4 + 1) * 512], w=[d_wo])
                k.dma("sp", wr[:], wr_d.rearrange("(c p) n -> p c n", p=128), w=[d_wo])
                k.dma("sp", br[:], br_d, w=[d_wo])
                k.dma("sp", l1g[:], ln1g_d.partition_broadcast(128), w=[d_wo])
                k.dma("sp", l1b[:], ln1b_d.partition_broadcast(128), w=[d_wo])
                k.op("dve", lambda e: e.memset(cnt[:], 0.0), w=[d_cnt])
                xtr = Ring([(k.sb("xt", [128, D], F32), Dep()) for _ in range(2)])
                x1r = Ring([(k.sb("x1", [128, D], F32), Dep()) for _ in range(2)])
                hbr2 = Ring([(k.sb("hb2", [128, D], BF16), Dep()) for _ in range(2)])
                hTr = Ring([(k.sb("hT", [128, DC, 128], F32), Dep()) for _ in range(2)])
                rsr = Ring([(k.sb("rs", [128, 256], F32), Dep()) for _ in range(2)])
                mkr = Ring([(k.sb("mk", [128, 32], BF16), Dep()) for _ in range(2)])
                mbr = Ring([(k.sb("mgb", [128, DC, HB], BF16), Dep()) for _ in range(2)])
                st_e2 = {"mb": None}

                def e2_tile(ti):
                    if (ti * 128) % HB == 0:
                        with k.atomic():
                            st_e2["mb"] = mbr.next()
                            k.dma("sp", st_e2["mb"][0][:], s_mgT[:, ti * 128:ti * 128 + HB].rearrange("(c p) t -> p c t", p=128),
                                  r=[d_mg], w=[st_e2["mb"][1]])
                    mgb, mgbd = st_e2["mb"]
                    tloc = (ti * 128) % HB
                    xt, xtd = xtr.next(); x1, x1d = x1r.next()
                    k.dma("sp", xt[:], x[TP + ti * 128:TP + (ti + 1) * 128, :], w=[xtd])
                    for db in range(4):
                        bk, bd = pf.next()

                        def f(pe, bk=bk, mgb=mgb, tloc=tloc, db=db):
                            for c in range(DC):
                                ins = pe.matmul(bk[:, :], mgb[:, c, tloc:tloc + 128], wo[:, c, db * 512:(db + 1) * 512],
                                                start=(c == 0), stop=(c == DC - 1))
                            return ins
                        k.op("pe", f, r=[mgbd, d_wo], w=[bd])
                        k.op("dve", lambda e, x1=x1, xt=xt, bk=bk, db=db: e.scalar_tensor_tensor(
                            x1[:, db * 512:(db + 1) * 512], xt[:, db * 512:(db + 1) * 512], ALPHA, bk[:, :], ALU.mult, ALU.add),
                            r=[xtd, bd], w=[x1d])
                    rs, rsd = rsr.next()

                    def layer_norm(xx, xd, rs, rsd, g_t, b_t, gdep):
                        st = rs[:, 0:24].rearrange("p (g k) -> p g k", g=4)
                        def fbn(e):
                            for gg in range(4):
                                ins = e.bn_stats(rs[:, 6 * gg:6 * gg + 6], xx[:, gg * 512:(gg + 1) * 512])
                            return ins
                        k.op("dve", fbn, r=[xd], w=[rsd])
                        k.op("dve", lambda e: e.bn_aggr(rs[:, 24:26], rs[:, 0:24]), r=[rsd], w=[rsd])
                        k.op("act", lambda e: e.activation(rs[:, 26:27], rs[:, 25:26], AF.Ln, bias=epsc[:, 0:1], scale=1.0), r=[rsd, d_c], w=[rsd])
                        k.op("act", lambda e: e.activation(rs[:, 26:27], rs[:, 26:27], AF.Exp, scale=-0.5), r=[rsd], w=[rsd])
                        k.op("dve", lambda e: e.tensor_scalar(xx[:], xx[:], rs[:, 24:25], rs[:, 26:27], ALU.subtract, ALU.mult),
                             r=[rsd, xd], w=[xd])
                        k.op("pool", lambda e: e.tensor_tensor(xx[:], xx[:], g_t[:], ALU.mult), r=[xd, gdep], w=[xd])
                        k.op("pool", lambda e: e.tensor_tensor(xx[:], xx[:], b_t[:], ALU.add), r=[xd, gdep], w=[xd])
                    layer_norm(x1, x1d, rs, rsd, l1g, l1b, d_wo)
                    k.dma("sp", s_h1[ti * 128:(ti + 1) * 128, :], x1[:], r=[x1d], w=[d_h1])
                    hb2, hb2d = hbr2.next()
                    k.op("act", lambda e, hb2=hb2, x1=x1: e.copy(hb2[:], x1[:]), r=[x1d], w=[hb2d])
                    hT, hTd = hTr.next()
                    for g in range(4):
                        bk, bd = pf.next()

                        def f(pe, bk=bk, x1=x1, g=g):
                            for j in range(4):
                                c = g * 4 + j
                                ins = pe.transpose(bk[:, j * 128:(j + 1) * 128], x1[:, c * 128:(c + 1) * 128], identf[:])
                            return ins
                        k.op("pe", f, r=[x1d, d_c], w=[bd])
                        k.op("act", lambda e, hT=hT, bk=bk, g=g: e.copy(
                            hT[:, g * 4:(g + 1) * 4, :], bk[:, :].rearrange("p (j n) -> p j n", j=4)), r=[bd], w=[hTd])
                    bk, bd = pf.next()

                    def f(pe, bk=bk, hT=hT):
                        for c in range(DC):
                            pe.matmul(bk[:, 0:36], hT[:, c, :], wr[:, c, :], start=(c == 0), stop=False)
                        return pe.matmul(bk[:, 0:36], onesf[0:1, :], br[0:1, :], start=False, stop=True)
                    k.op("pe", f, r=[hTd, d_wo, d_c], w=[bd])
                    V = "dve"
                    lg = rs[:, 32:68]
                    k.op(V, lambda e, bk=bk, lg=lg: e.tensor_copy(lg, bk[:, 0:36]), r=[bd], w=[rsd])
                    g4 = rs[:, 32:36]; e32 = rs[:, 36:68]
                    gmx = rs[:, 68:69]; ngm = rs[:, 69:70]; ohg = rs[:, 70:74]; eg = rs[:, 74:78]; sg = rs[:, 78:79]; gp = rs[:, 79:80]
                    pen = rs[:, 80:84]; msk = rs[:, 84:116]; top8 = rs[:, 116:124]; oh1 = rs[:, 124:156]; oh2 = rs[:, 156:188]
                    dd = rs[:, 188:189]; p1 = rs[:, 189:190]; p2 = rs[:, 190:191]; tmp = rs[:, 192:224]
                    pk = rs[:, 224:225]; ek = rs[:, 225:226]; okk = rs[:, 226:227]; dsf = rs[:, 227:228]; posg = rs[:, 228:260 - 4]
                    k.op(V, lambda e: e.reduce_max(gmx, g4, AX.X), r=[rsd], w=[rsd])
                    k.op(V, lambda e: e.tensor_scalar_mul(ngm, gmx, -1.0), r=[rsd], w=[rsd])
                    k.op(V, lambda e: e.tensor_scalar(ohg, g4, gmx, None, ALU.is_equal), r=[rsd], w=[rsd])
                    k.op("act", lambda e: e.activation(eg, g4, AF.Exp, bias=ngm, scale=1.0), r=[rsd], w=[rsd])
                    k.op(V, lambda e: e.reduce_sum(sg, eg, AX.X), r=[rsd], w=[rsd])
                    k.op(V, lambda e: e.reciprocal(gp, sg), r=[rsd], w=[rsd])
                    k.op(V, lambda e: e.tensor_scalar(pen, ohg, BIG, -BIG, ALU.mult, ALU.add), r=[rsd], w=[rsd])
                    k.op(V, lambda e: e.tensor_tensor(msk.rearrange("p (g j) -> p g j", g=4), e32.rearrange("p (g j) -> p g j", g=4),
                                                      pen.unsqueeze(2).to_broadcast([128, 4, 8]), ALU.add), r=[rsd], w=[rsd])
                    k.op(V, lambda e: e.max(top8, msk), r=[rsd], w=[rsd])
                    k.op(V, lambda e: e.tensor_scalar(oh1, msk, top8[:, 0:1], None, ALU.is_equal), r=[rsd], w=[rsd])
                    k.op(V, lambda e: e.tensor_scalar(oh2, msk, top8[:, 1:2], None, ALU.is_equal), r=[rsd], w=[rsd])
                    k.op(V, lambda e: e.tensor_sub(dd, top8[:, 0:1], top8[:, 1:2]), r=[rsd], w=[rsd])
                    k.op("act", lambda e: e.activation(p1, dd, AF.Sigmoid), r=[rsd], w=[rsd])
                    k.op("act", lambda e: e.activation(p2, dd, AF.Sigmoid, scale=-1.0), r=[rsd], w=[rsd])
                    k.op(V, lambda e, ti=ti: e.tensor_tensor(wtab[:, ti, 0:1], p1, gp, ALU.mult), r=[rsd], w=[d_tab])
                    k.op(V, lambda e, ti=ti: e.tensor_tensor(wtab[:, ti, 1:2], p2, gp, ALU.mult), r=[rsd], w=[d_tab])
                    mk, mkd = mkr.next()
                    k.op(V, lambda e, mk=mk: e.tensor_tensor(mk[:], oh1, oh2, ALU.add), r=[rsd], w=[mkd])
                    bk2, bd2 = pf.next()

                    def f(pe, bk2=bk2, mk=mk):
                        pe.matmul(bk2[:, 0:32], ustr[:], mk[:], start=True, stop=True)
                        return pe.matmul(bk2[:, 32:64], onesb[:], mk[:], start=True, stop=True)
                    k.op("pe", f, r=[mkd, d_c], w=[bd2])
                    posg = rs[:, 224:256]
                    pk = rs[:, 28:29]; ek = rs[:, 29:30]; okk = rs[:, 30:31]; dsf = rs[:, 31:32]
                    with k.atomic():
                        k.op(V, lambda e, bk2=bk2: e.tensor_tensor(posg, bk2[:, 0:32], cnt[:], ALU.add), r=[bd2, d_cnt, rsd], w=[rsd])
                        k.op(V, lambda e, bk2=bk2: e.tensor_tensor(cnt[:], cnt[:], bk2[:, 32:64], ALU.add), r=[bd2, rsd], w=[d_cnt])
                    for kk, oh in ((0, oh1), (1, oh2)):
                        k.op(V, lambda e, oh=oh: e.tensor_tensor(tmp, oh, posg, ALU.mult), r=[rsd], w=[rsd])
                        k.op(V, lambda e: e.reduce_sum(pk, tmp, AX.X), r=[rsd], w=[rsd])
                        k.op(V, lambda e, oh=oh: e.tensor_tensor(tmp, oh, ecap[:], ALU.mult), r=[rsd, d_c], w=[rsd])
                        k.op(V, lambda e: e.reduce_sum(ek, tmp, AX.X), r=[rsd], w=[rsd])
                        k.op(V, lambda e: e.tensor_scalar(okk, pk, float(CAP), None, ALU.is_lt), r=[rsd], w=[rsd])
                        k.op(V, lambda e: e.tensor_tensor(dsf, ek, pk, ALU.add), r=[rsd], w=[rsd])
                        k.op(V, lambda e: e.tensor_scalar_add(dsf, dsf, -float(TRASH)), r=[rsd], w=[rsd])
                        k.op(V, lambda e: e.tensor_tensor(dsf, dsf, okk, ALU.mult), r=[rsd], w=[rsd])
                        k.op(V, lambda e: e.tensor_scalar_add(dsf, dsf, float(TRASH)), r=[rsd], w=[rsd])
                        k.op(V, lambda e, ti=ti, kk=kk: e.tensor_copy(dtab[:, ti, kk:kk + 1], dsf), r=[rsd], w=[d_tab])
                        k.dma("pool", s_Xg, hb2[:], r=[hb2d, d_tab, d_Xg], w=[d_Xg],
                              indirect=dict(out_offset=bass.IndirectOffsetOnAxis(ap=dtab[:, ti, kk:kk + 1], axis=0), in_offset=None))
                k.interleave([(lambda ti=ti: e2_tile(ti)) for ti in range(NTO)], width=NIL)

            if dbg:
                k.dma("sp", s_dtab, dtab[:].rearrange("p a b -> p (a b)"), r=[d_tab], w=[Dep()])
                k.dma("sp", s_wtab, wtab[:].rearrange("p a b -> p (a b)"), r=[d_tab], w=[Dep()])
            if stop <= 6:
                raise _Stop()
            NCT = CAP // 128
            with k.scope():
                wgr = Ring([(k.sb("wg", [128, DC, 512], BF16), Dep()) for _ in range(3)])
                wur = Ring([(k.sb("wu", [128, DC, 512], BF16), Dep()) for _ in range(3)])
                wdr = Ring([(k.sb("wd", [128, 4, D], BF16), Dep()) for _ in range(3)])
                Xer = Ring([(k.sb("Xe", [128, NCT, D], BF16), Dep()) for _ in range(2)])
                XTr = Ring([(k.sb("XT", [128, DC, CAP], BF16), Dep()) for _ in range(2)])
                ATr = Ring([(k.sb("AT", [128, 4, CAP], BF16), Dep()) for _ in range(2)])
                sgr = Ring([(k.sb("sgt", [128, CAP], F32), Dep()) for _ in range(2)])
                Ysr = Ring([(k.sb("Ys", [128, D], BF16), Dep()) for _ in range(2)])
                def expert_fn(e_):
                    wg, wgd = wgr.next(); wu, wud = wur.next(); wd, wdd = wdr.next()
                    k.dma("pool", wg[:], w_gate[e_].rearrange("(c p) n -> p c n", p=128), w=[wgd])
                    k.dma("pool", wu[:], w_up[e_].rearrange("(c p) n -> p c n", p=128), w=[wud])
                    k.dma("pool", wd[:], w_down[e_].rearrange("(c p) n -> p c n", p=128), w=[wdd])
                    Xe, Xed = Xer.next(); XT, XTd = XTr.next(); AT, ATd = ATr.next()
                    k.dma("sp", Xe[:], s_Xg[e_ * CAP:(e_ + 1) * CAP, :].rearrange("(t p) d -> p t d", p=128), r=[d_Xg], w=[Xed])
                    for t in range(NCT):
                        for g in range(4):
                            pt, pd = pb.next()

                            def f(pe, pt=pt, Xe=Xe, t=t, g=g):
                                for j in range(4):
                                    c = g * 4 + j
                                    ins = pe.transpose(pt[:, j * 128:(j + 1) * 128], Xe[:, t, c * 128:(c + 1) * 128], identb[:])
                                return ins
                            k.op("pe", f, r=[Xed, d_c], w=[pd])
                            eng = "dve" if (g % 2 == 0) else "act"
                            if eng == "dve":
                                k.op("dve", lambda e, XT=XT, pt=pt, g=g, t=t: e.tensor_copy(
                                    XT[:, g * 4:(g + 1) * 4, t * 128:(t + 1) * 128], pt[:, 0:512].rearrange("p (j n) -> p j n", j=4)),
                                    r=[pd], w=[XTd])
                            else:
                                k.op("act", lambda e, XT=XT, pt=pt, g=g, t=t: e.copy(
                                    XT[:, g * 4:(g + 1) * 4, t * 128:(t + 1) * 128], pt[:, 0:512].rearrange("p (j n) -> p j n", j=4)),
                                    r=[pd], w=[XTd])
                    for fcx in range(4):
                        bG, bGd = pf.next(); bU, bUd = pf.next()

                        def f(pe, bG=bG, wg=wg, XT=XT, fcx=fcx):
                            for c in range(DC):
                                ins = pe.matmul(bG[:, 0:CAP], wg[:, c, fcx * 128:(fcx + 1) * 128], XT[:, c, :], start=(c == 0), stop=(c == DC - 1))
                            return ins
                        k.op("pe", f, r=[wgd, XTd], w=[bGd])

                        def f(pe, bU=bU, wu=wu, XT=XT, fcx=fcx):
                            for c in range(DC):
                                ins = pe.matmul(bU[:, 0:CAP], wu[:, c, fcx * 128:(fcx + 1) * 128], XT[:, c, :], start=(c == 0), stop=(c == DC - 1))
                            return ins
                        k.op("pe", f, r=[wud, XTd], w=[bUd])
                        sg_t, sg_d = sgr.next()
                        k.op("act", lambda e, sg_t=sg_t, bG=bG: e.activation(sg_t[:], bG[:, 0:CAP], AF.Silu), r=[bGd], w=[sg_d])
                        k.op("dve", lambda e, AT=AT, sg_t=sg_t, bU=bU, fcx=fcx: e.tensor_tensor(AT[:, fcx, :], sg_t[:], bU[:, 0:CAP], ALU.mult),
                             r=[sg_d, bUd], w=[ATd])
                    for t in range(NCT):
                        Ys, Ysd = Ysr.next()
                        for db in range(4):
                            bk, bd = pf.next()

                            def f(pe, bk=bk, AT=AT, wd=wd, t=t, db=db):
                                for fcx in range(4):
                                    ins = pe.matmul(bk[:, :], AT[:, fcx, t * 128:(t + 1) * 128], wd[:, fcx, db * 512:(db + 1) * 512],
                                                    start=(fcx == 0), stop=(fcx == 3))
                                return ins
                            k.op("pe", f, r=[ATd, wdd], w=[bd])
                            if db % 2 == 0:
                                k.op("act", lambda e, Ys=Ys, bk=bk, db=db: e.copy(Ys[:, db * 512:(db + 1) * 512], bk[:, :]), r=[bd], w=[Ysd])
                            else:
                                k.op("dve", lambda e, Ys=Ys, bk=bk, db=db: e.tensor_copy(Ys[:, db * 512:(db + 1) * 512], bk[:, :]), r=[bd], w=[Ysd])
                        r0 = e_ * CAP + t * 128
                        k.dma("sp", s_Yg[r0:r0 + 128, :], Ys[:], r=[Ysd], w=[d_Yg])
                k.interleave([(lambda e_=e_: expert_fn(e_)) for e_ in range(NE)], width=NIL)

            if stop <= 7:
                raise _Stop()
            d_out = Dep()
            with k.scope():
                l2g = k.sb("l2g", [128, D], F32); l2b = k.sb("l2b", [128, D], F32)
                d_l2 = Dep()
                k.dma("sp", l2g[:], ln2g_d.partition_broadcast(128), w=[d_l2])
                k.dma("sp", l2b[:], ln2b_d.partition_broadcast(128), w=[d_l2])
                h1r = Ring([(k.sb("h1", [128, D], F32), Dep()) for _ in range(3)])
                y1r = Ring([(k.sb("y1", [128, D], BF16), Dep()) for _ in range(3)])
                y2r = Ring([(k.sb("y2", [128, D], BF16), Dep()) for _ in range(3)])
                rsr = Ring([(k.sb("rs2", [128, 32], F32), Dep()) for _ in range(3)])
                def g_tile(ti):
                    h1, h1d = h1r.next(); y1, y1d = y1r.next(); y2, y2d = y2r.next()
                    k.dma("sp", h1[:], s_h1[ti * 128:(ti + 1) * 128, :], r=[d_h1], w=[h1d])
                    k.dma("pool", y1[:], s_Yg, r=[d_Yg, d_tab], w=[y1d],
                          indirect=dict(out_offset=None, in_offset=bass.IndirectOffsetOnAxis(ap=dtab[:, ti, 0:1], axis=0)))
                    k.dma("pool", y2[:], s_Yg, r=[d_Yg, d_tab], w=[y2d],
                          indirect=dict(out_offset=None, in_offset=bass.IndirectOffsetOnAxis(ap=dtab[:, ti, 1:2], axis=0)))
                    k.op("act", lambda e, h1=h1: e.mul(h1[:], h1[:], ALPHA), r=[h1d], w=[h1d])
                    k.op("dve", lambda e, h1=h1, y1=y1, ti=ti: e.scalar_tensor_tensor(
                        h1[:], y1[:], wtab[:, ti, 0:1], h1[:], ALU.mult, ALU.add), r=[y1d, d_tab, h1d], w=[h1d])
                    k.op("dve", lambda e, h1=h1, y2=y2, ti=ti: e.scalar_tensor_tensor(
                        h1[:], y2[:], wtab[:, ti, 1:2], h1[:], ALU.mult, ALU.add), r=[y2d, d_tab, h1d], w=[h1d])
                    rs, rsd = rsr.next()
                    st = rs[:, 0:24].rearrange("p (g k) -> p g k", g=4)
                    def fbn(e, rs=rs, h1=h1):
                        for gg in range(4):
                            ins = e.bn_stats(rs[:, 6 * gg:6 * gg + 6], h1[:, gg * 512:(gg + 1) * 512])
                        return ins
                    k.op("dve", fbn, r=[h1d], w=[rsd])
                    k.op("dve", lambda e, rs=rs: e.bn_aggr(rs[:, 24:26], rs[:, 0:24]), r=[rsd], w=[rsd])
                    k.op("act", lambda e, rs=rs: e.activation(rs[:, 26:27], rs[:, 25:26], AF.Ln, bias=epsc[:, 0:1], scale=1.0), r=[rsd, d_c], w=[rsd])
                    k.op("act", lambda e, rs=rs: e.activation(rs[:, 26:27], rs[:, 26:27], AF.Exp, scale=-0.5), r=[rsd], w=[rsd])
                    k.op("dve", lambda e, rs=rs, h1=h1: e.tensor_scalar(h1[:], h1[:], rs[:, 24:25], rs[:, 26:27], ALU.subtract, ALU.mult),
                         r=[rsd, h1d], w=[h1d])
                    k.op("pool", lambda e, h1=h1: e.tensor_tensor(h1[:], h1[:], l2g[:], ALU.mult), r=[h1d, d_l2], w=[h1d])
                    k.op("pool", lambda e, h1=h1: e.tensor_tensor(h1[:], h1[:], l2b[:], ALU.add), r=[h1d, d_l2], w=[h1d])
                    k.dma("sp", out_d[ti * 128:(ti + 1) * 128, :], h1[:], r=[h1d], w=[d_out])
                k.interleave([(lambda ti=ti: g_tile(ti)) for ti in range(NTO)], width=3)
        except _Stop:
            gscope.close()
        k.barrier()
        build.stats = (k.n_ins, {n: e.count for n, e in k.E.items()})
    return nc


def _consts(TP, TO, CAP):
    TA = TP + TO
    c = {}
    c["ident"] = np.eye(128, dtype=np.float32)
    s = np.arange(64)
    c["mask64"] = (s[:, None] <= s[None, :]).astype(np.float32)
    p = np.arange(128)
    c["ustrict"] = (p[:, None] < p[None, :]).astype(np.float32)
    slopes = 2.0 ** (-8.0 * np.arange(1, 9) / 8.0)
    kl = p[:, None]; ql = p[None, :]
    vis = (kl // 64) <= (ql // 64)
    dg = np.zeros((128, 8, 128), np.float32)
    for h in range(8):
        b = np.where(kl <= ql, slopes[h] * kl, slopes[h] * (2 * ql - kl))
        dg[:, h, :] = np.where(vis, b, -BIG) * 8.0
    c["diagbias"] = dg
    ab = np.zeros((128, 8, 32), np.float32)
    for h in range(8):
        for dlt in range(32):
            ab[:, h, dlt] = np.maximum(slopes[h] * (p - 128 * dlt), -ACLAMP)
    c["alibi"] = ab
    c["ecap"] = np.tile((np.arange(32) * CAP).astype(np.float32)[None, :], (128, 1))
    return c


def _prep_shared(inp):
    f = lambda a: np.ascontiguousarray(np.asarray(a, dtype=np.float32))
    b_in = f(inp["b_in"][0])
    sh = {}
    sh["w_in"] = f(inp["w_in"][0])

    def fm_bias(b):
        cols = np.concatenate([b[O_QA:O_QA + 1024], b[O_KA:O_KA + 1024], b[O_QB:O_QB + 1024], b[O_KB:O_KB + 1024],
                               b[O_GA:O_GA + 2048], b[O_GB:O_GB + 2048]])
        return np.ascontiguousarray(cols.reshape(64, 128).T)

    def tm_bias(b):
        return np.ascontiguousarray(np.concatenate([b[O_VA:O_VA + 1024], b[O_OA:O_OA + 1024], b[O_VB:O_VB + 1024]])[None, :])

    def if_bias(b):
        return np.ascontiguousarray(np.stack([b[O_IA:O_IA + 4], b[O_FA:O_FA + 4]], axis=1))
    b_mask = np.zeros_like(b_in)
    b_mask[O_IA:O_IA + 4] = -BIG
    b_mask[O_FA:O_FA + 4] = BIG
    sh["bias_real"] = (fm_bias(b_in), tm_bias(b_in), if_bias(b_in))
    sh["bias_mask"] = (fm_bias(b_mask), tm_bias(b_mask), if_bias(b_mask))
    cw = f(inp["conv_w"][0])
    sh["cw"] = np.ascontiguousarray(cw.T.reshape(16, 128, 4).transpose(1, 0, 2))
    sh["cb"] = np.ascontiguousarray(f(inp["conv_b"][0]).reshape(16, 128).T)
    sh["mlstm_norm_g"] = f(inp["mlstm_norm_g"][0])
    sh["diff_norm_g"] = f(inp["diff_norm_g"][0])
    sh["lamv"] = np.ascontiguousarray(np.stack([f(inp["lambda_q1"][0]), f(inp["lambda_k1"][0]),
                                                f(inp["lambda_q2"][0]), f(inp["lambda_k2"][0])]))
    for n in ("w_a", "w_b", "w_out", "ln1_g", "ln1_b", "ln2_g", "ln2_b", "w_gate", "w_up", "w_down"):
        sh[n] = f(inp[n][0])
    sh["w_r"] = np.ascontiguousarray(np.concatenate([f(inp["w_grp"][0]), f(inp["w_exp"][0])], axis=1))
    sh["b_r"] = np.ascontiguousarray(np.concatenate([f(inp["b_grp"][0]), f(inp["b_exp"][0])])[None, :])
    return sh


def make_in_maps(inp, TP, TO, CAP):
    x = np.asarray(inp["x"], dtype=np.float32)
    B, S, _ = x.shape
    assert S == TP + TO or S == 2 * TO
    sh = _prep_shared(inp)
    cs = _consts(TP, TO, CAP)
    TA = TP + TO
    maps = []
    for b in range(B):
        for half in range(2):
            m = {}
            if half == 0:
                xa = np.zeros((TA, D), np.float32)
                xa[TP:] = x[b, 0:TO]
                valid = np.concatenate([np.zeros(TP, np.float32), np.ones(TO, np.float32)])
                pre = sh["bias_mask"]
            else:
                xa = np.ascontiguousarray(x[b, TO - TP:2 * TO])
                valid = np.ones(TA, np.float32)
                pre = sh["bias_real"]
            m["x"] = xa
            m["valid_tm"] = np.ascontiguousarray(valid.reshape(TA // 128, 128).T)
            m["bfm"], m["btm"], m["bif"] = sh["bias_real"]
            m["bfm_pre"], m["btm_pre"], m["bif_pre"] = pre
            for n in ("w_in", "cw", "cb", "mlstm_norm_g", "diff_norm_g", "lamv", "w_a", "w_b", "w_out", "ln1_g", "ln1_b",
                      "ln2_g", "ln2_b", "w_gate", "w_up", "w_down", "w_r", "b_r"):
                m[n] = sh[n]
            m.update(cs)
            maps.append(m)
    return maps


_TP, _TO, _CAP = 2048, 2048, 256


def kernel(**inputs):
    x = np.asarray(inputs["x"])
    B, S, _ = x.shape
    maps = make_in_maps(inputs, _TP, _TO, _CAP)
    nc = build(_TP, _TO, _CAP)
    res = run_bass_kernel_spmd(nc, maps, core_ids=list(range(len(maps))))
    out = np.zeros((B, S, D), np.float32)
    i = 0
    for b in range(B):
        for half in range(2):
            out[b, half * _TO:(half + 1) * _TO] = res.results[i]["out"]
            i += 1
    return out
```

```python
import math
from contextlib import ExitStack, contextmanager
import numpy as np
import concourse.bass as bass
import concourse.mybir as mybir
from concourse.bass_utils import run_bass_kernel_spmd

F32 = mybir.dt.float32
BF16 = mybir.dt.bfloat16
I32 = mybir.dt.int32
AF = mybir.ActivationFunctionType
ALU = mybir.AluOpType
AX = mybir.AxisListType

D = 2048
DC = 16
NIN = 11272
O_QA, O_KA, O_VA, O_OA, O_IA, O_FA, O_QB, O_KB, O_VB, O_GA, O_GB = (
    0, 1024, 2048, 3072, 4096, 4100, 4104, 5128, 6152, 7176, 9224)
ALPHA = 2.0 ** 0.25
EPS = 1e-5
NE = 32
BIG = 30000.0
LN16 = math.log(16.0)
ACLAMP = 40.0
import os
ILV = int(os.environ.get('ILV', '1'))
EXPV = int(os.environ.get('EXPV', '0'))
NIL = int(os.environ.get('NIL', '2'))
SLOPES = [2.0 ** (-(h + 1)) for h in range(8)]


class _Stop(Exception):
    pass


class Dep:
    __slots__ = ("w", "rs")

    def __init__(self):
        self.w = None
        self.rs = {}


class _Eng:
    def __init__(self, name, eng, sem):
        self.name, self.eng, self.sem = name, eng, sem
        self.count = 0
        self.waited = {}


class _DmaSem:
    def __init__(self, key, sem):
        self.key, self.sem, self.total = key, sem, 0


class Kern:
    def __init__(self, nc, stack, n_dma_sems=48):
        self.nc = nc
        self.stack = stack
        self.E = {}
        for name, eng in (("pe", nc.tensor), ("act", nc.scalar), ("dve", nc.vector),
                          ("pool", nc.gpsimd), ("sp", nc.sync)):
            sem = stack.enter_context(nc.semaphore("s_" + name))
            self.E[name] = _Eng(name, eng, sem)
        self.dsems = [_DmaSem("d%d" % i, stack.enter_context(nc.semaphore("d%d" % i)))
                      for i in range(n_dma_sems)]
        self.dpool = {"hw": self.dsems[0:n_dma_sems // 2], "sw": self.dsems[n_dma_sems // 2:]}
        self.drr = {"hw": 0, "sw": 0}
        self.n_ins = 0
        self.uid = 0
        self._cur = None
        self._atomic = 0

    def sb(self, name, shape, dt):
        self.uid += 1
        return self.stack.enter_context(self.nc.sbuf_tensor("%s_%d" % (name, self.uid), list(shape), dt))

    def ps(self, name, shape, dt):
        return self.stack.enter_context(self.nc.psum_tensor(name, list(shape), dt))

    @contextmanager
    def scope(self):
        old = self.stack
        stopped = False
        with ExitStack() as st:
            self.stack = st
            try:
                yield
            except _Stop:
                stopped = True
            if not stopped:
                self.barrier()
        self.stack = old
        if stopped:
            raise _Stop()

    def _wait(self, es, ev):
        key, sem, val = ev
        if es.name == "pe" and key == "pe":
            return
        if es.waited.get(key, 0) >= val:
            return
        es.eng.wait_ge(sem, val)
        es.waited[key] = val

    def _pre(self, es, r, w):
        for d in r:
            if d.w is not None:
                self._wait(es, d.w)
        for d in w:
            if d.w is not None:
                self._wait(es, d.w)
            for ev in d.rs.values():
                self._wait(es, ev)

    def _post(self, ev, r, w):
        for d in r:
            d.rs[ev[0]] = ev
        for d in w:
            d.w = ev
            d.rs = {}

    def op(self, en, fn, r=(), w=()):
        es = self.E[en]
        self._pre(es, r, w)
        ins = fn(es.eng)
        es.count += 1
        ins.then_inc(es.sem, 1)
        self.n_ins += 1
        ev = (es.name, es.sem, es.count)
        self._post(ev, r, w)
        self._yield()
        return ev

    def dma(self, qn, out, in_, r=(), w=(), indirect=None, **kw):
        es = self.E[qn]
        self._pre(es, r, w)
        kind = "sw" if qn == "pool" else "hw"
        pool = self.dpool[kind]
        ds = pool[self.drr[kind]]
        self.drr[kind] = (self.drr[kind] + 1) % len(pool)
        if ds.total > 0:
            self._wait(es, (ds.key, ds.sem, ds.total))
        if indirect is not None:
            ins = es.eng.indirect_dma_start(out=out, in_=in_, **indirect)
        else:
            ins = es.eng.dma_start(out=out, in_=in_, **kw)
        ds.total += 16
        ins.then_inc(ds.sem, 16)
        self.n_ins += 1
        ev = (ds.key, ds.sem, ds.total)
        self._post(ev, r, w)
        self._yield()
        return ev

    def _yield(self):
        w = self._cur
        if w is None or self._atomic > 0:
            return
        self._sched_sem.release()
        w["go"].acquire()

    def interleave(self, fns, width=2):
        import threading
        if width <= 1 or len(fns) <= 1:
            for fn in fns:
                fn()
            return
        self._sched_sem = threading.Semaphore(0)
        pending = list(fns)
        active = []
        err = []

        def runner(w, fn):
            w["go"].acquire()
            try:
                fn()
            except BaseException as e:
                err.append(e)
            w["done"] = True
            self._sched_sem.release()
        while pending or active:
            while pending and len(active) < width:
                w = {"go": threading.Semaphore(0), "done": False}
                w["t"] = threading.Thread(target=runner, args=(w, pending.pop(0)), daemon=True)
                w["t"].start()
                active.append(w)
            for w in list(active):
                self._cur = w
                w["go"].release()
                self._sched_sem.acquire()
                self._cur = None
                if w["done"]:
                    active.remove(w)
                if err:
                    raise err[0]

    @contextmanager
    def atomic(self):
        self._atomic += 1
        try:
            yield
        finally:
            self._atomic -= 1

    def barrier(self):
        evs = [(e.name, e.sem, e.count) for e in self.E.values() if e.count > 0]
        evs += [(d.key, d.sem, d.total) for d in self.dsems if d.total > 0]
        for es in self.E.values():
            for ev in evs:
                if ev[0] == es.name and es.name == "pe":
                    continue
                self._wait(es, ev)


class Ring:
    def __init__(self, items):
        self.items = items
        self.i = 0

    def next(self):
        it = self.items[self.i]
        self.i = (self.i + 1) % len(self.items)
        return it


def build(TP, TO, CAP, dbg=False, stop=99):
    TA = TP + TO
    NTA, NTO, NTP = TA // 128, TO // 128, TP // 128
    NCH, CH0 = TA // 64, TP // 64
    NROW = NE * CAP + 128
    TRASH = NE * CAP
    nc = bass.Bass("TRN2", target_bir_lowering=False)

    def din(name, shape, dt=F32):
        return nc.dram_tensor(name, list(shape), dt, kind="ExternalInput").ap()

    x = din("x", [TA, D])
    w_in = din("w_in", [D, NIN])
    bfm_d = din("bfm", [128, 64]); bfmp_d = din("bfm_pre", [128, 64])
    btm_d = din("btm", [1, 3072]); btmp_d = din("btm_pre", [1, 3072])
    bif_d = din("bif", [4, 2]); bifp_d = din("bif_pre", [4, 2])
    valid_d = din("valid_tm", [128, NTA])
    cw_d = din("cw", [128, 16, 4]); cb_d = din("cb", [128, 16])
    ng_d = din("mlstm_norm_g", [1024]); dg_d = din("diff_norm_g", [128])
    lam_d = din("lamv", [4, 64])
    w_a = din("w_a", [1024, D]); w_b = din("w_b", [1024, D]); w_out = din("w_out", [D, D])
    ln1g_d = din("ln1_g", [D]); ln1b_d = din("ln1_b", [D]); ln2g_d = din("ln2_g", [D]); ln2b_d = din("ln2_b", [D])
    wr_d = din("w_r", [D, 36]); br_d = din("b_r", [1, 36])
    if stop > 6:
        w_gate = din("w_gate", [NE, D, 512]); w_up = din("w_up", [NE, D, 512]); w_down = din("w_down", [NE, 512, D])
    ident_d = din("ident", [128, 128]); mask64_d = din("mask64", [64, 64]); ustrict_d = din("ustrict", [128, 128])
    dgb_d = din("diagbias", [128, 8, 128]); abt_d = din("alibi", [128, 8, 32]); ecap_d = din("ecap", [128, 32])
    out_d = nc.dram_tensor("out", [TO, D], F32, kind="ExternalOutput").ap()

    def dscr(name, shape, dt):
        if dbg:
            return nc.dram_tensor(name, list(shape), dt, kind="ExternalOutput").ap()
        return nc.dram_tensor(name, list(shape), dt).ap()

    s_qkaT = dscr("s_qkaT", [2048, TA], BF16)
    s_qkbT = dscr("s_qkbT", [2048, TA], BF16)
    s_gT = dscr("s_gT", [4096, TO], BF16)
    s_va = dscr("s_va", [TA, 1024], BF16)
    s_vb = dscr("s_vb", [TA, 1024], BF16)
    s_oa = dscr("s_oa", [TO, 1024], BF16)
    s_seq = dscr("s_seq", [3, 4, TA], F32)
    s_dec = dscr("s_dec", [4, NCH], F32)
    s_h1 = dscr("s_h1", [TO, D], F32)
    s_Xg = dscr("s_Xg", [NROW, D], BF16)
    s_Yg = dscr("s_Yg", [NROW, D], BF16)
    s_haT = dscr("s_haT", [1024, TO], BF16)
    s_obT = dscr("s_obT", [1024, TO], BF16)
    s_mgT = dscr("s_mgT", [2048, TO], BF16)
    HB = min(512, TO)
    if dbg:
        s_TT = dscr("s_TT", [64, 3 * NCH * 4], F32)
        s_dtab = dscr("s_dtab", [128, NTO * 2], I32)
        s_wtab = dscr("s_wtab", [128, NTO * 2], F32)

    with ExitStack() as st0:
        k = Kern(nc, st0)
        try:
            banks = [(k.ps("pf%d" % i, [128, 512], F32), Dep()) for i in range(8)]
            pf = Ring(banks[0:6])
            pb = Ring([(banks[i][0].bitcast(BF16), banks[i][1]) for i in (6, 7)])
            d_c = Dep()
            identf = k.sb("identf", [128, 128], F32)
            identb = k.sb("identb", [128, 128], BF16)
            onesb = k.sb("onesb", [128, 128], BF16)
            onesf = k.sb("onesf", [128, 128], F32)
            mask64 = k.sb("mask64", [64, 64], F32)
            ustr = k.sb("ustr", [128, 128], BF16)
            dgb = k.sb("dgb", [128, 8, 128], BF16)
            abt = k.sb("abt", [128, 8, 32], F32)
            ecap = k.sb("ecap", [128, 32], F32)
            validb = k.sb("validb", [128, NTA], BF16)
            zero1 = k.sb("zero1", [128, 1], F32)
            epsc = k.sb("epsc", [128, 1], F32)
            k.dma("sp", identf[:], ident_d, w=[d_c])
            k.dma("pool", identb[:], ident_d, w=[d_c])
            k.dma("sp", mask64[:], mask64_d, w=[d_c])
            k.dma("pool", ustr[:], ustrict_d, w=[d_c])
            k.dma("pool", dgb[:], dgb_d, w=[d_c])
            k.dma("sp", abt[:], abt_d, w=[d_c])
            k.dma("sp", ecap[:], ecap_d, w=[d_c])
            k.dma("pool", validb[:], valid_d, w=[d_c])
            k.op("dve", lambda e: e.memset(onesb[:], 1.0), w=[d_c])
            k.op("dve", lambda e: e.memset(onesf[:], 1.0), w=[d_c])
            k.op("dve", lambda e: e.memset(zero1[:], 0.0), w=[d_c])
            k.op("dve", lambda e: e.memset(epsc[:], EPS), w=[d_c])
            d_zt, d_Xg, d_Yg = Dep(), Dep(), Dep()

            dtab = k.sb("dtab", [128, NTO, 2], I32)
            wtab = k.sb("wtab", [128, NTO, 2], F32)
            TT = k.sb("TT", [64, 3, NCH, 4], F32)
            decbc = k.sb("decbc", [128, 4 * NCH], F32)
            gscope = ExitStack()
            _old = k.stack
            k.stack = gscope
            Gi = k.sb("Gi", [4, TA], F32); Gf = k.sb("Gf", [4, TA], F32)
            k.stack = _old
            d_G = Dep()
            d_tab = Dep()
            d_h1 = Dep()
            d_haT, d_obT, d_mg = Dep(), Dep(), Dep()

            with k.scope():
                zt = k.sb("zt", [128, 4096], BF16)
                k.op("pool", lambda e: e.memset(zt[:], 0.0), w=[d_zt])
                r0 = 0
                while r0 < NROW:
                    nr = min(256, NROW - r0)
                    k.dma("sp", s_Xg[r0:r0 + nr, :].rearrange("(t p) d -> p t d", p=128),
                          zt[:, 0:(nr // 128) * D].rearrange("p (t d) -> p t d", d=D), r=[d_zt], w=[d_Xg])
                    r0 += nr
                k.dma("sp", s_Yg[TRASH:TRASH + 128, :], zt[:, 0:D], r=[d_zt], w=[d_Yg])
                TX = max(TP, TO)
                xT = k.sb("xT", [128, DC, TX], BF16)
                xb = Ring([(k.sb("xb", [128, D], BF16), Dep()) for _ in range(2)])
                wt = Ring([(k.sb("wt", [128, DC, 512], BF16), Dep()) for _ in range(2)])
                wif = k.sb("wif", [128, DC, 8], BF16)
                evf = Ring([(k.sb("evf", [128, 4, 512], BF16), Dep()) for _ in range(2)])
                evt = Ring([(k.sb("evt", [128, 512], BF16), Dep()) for _ in range(3)])
                bfm = k.sb("bfm", [128, 64], F32); bfmp = k.sb("bfmp", [128, 64], F32)
                btm = k.sb("btm", [1, 3072], BF16); btmp = k.sb("btmp", [1, 3072], BF16)
                bif = k.sb("bif", [4, 2], F32); bifp = k.sb("bifp", [4, 2], F32)
                d_b, d_wif = Dep(), Dep()
                k.dma("sp", bfm[:], bfm_d, w=[d_b]); k.dma("sp", bfmp[:], bfmp_d, w=[d_b])
                k.dma("pool", btm[:], btm_d, w=[d_b]); k.dma("pool", btmp[:], btmp_d, w=[d_b])
                k.dma("sp", bif[:], bif_d, w=[d_b]); k.dma("sp", bifp[:], bifp_d, w=[d_b])
                k.dma("pool", wif[:], w_in.rearrange("(c p) n -> p c n", p=128)[:, :, O_IA:O_IA + 8], w=[d_wif])
                d_scr = {"qka": Dep(), "qkb": Dep(), "g": Dep(), "va": Dep(), "vb": Dep(), "oa": Dep()}

                FM = []
                for j in range(2):
                    FM.append((O_QA + 512 * j, "qka", s_qkaT, 512 * j, AF.Identity, "q", 4 * j))
                    FM.append((O_KA + 512 * j, "qka", s_qkaT, 1024 + 512 * j, AF.Identity, "all", 8 + 4 * j))
                    FM.append((O_QB + 512 * j, "qkb", s_qkbT, 512 * j, AF.Identity, "own", 16 + 4 * j))
                    FM.append((O_KB + 512 * j, "qkb", s_qkbT, 1024 + 512 * j, AF.Identity, "all", 24 + 4 * j))
                for j in range(8):
                    FM.append((O_GA + 512 * j, "g", s_gT, 512 * j, AF.Sigmoid, "gate", 32 + 4 * j))
                TM = []
                for j in range(2):
                    TM.append((O_VA + 512 * j, "va", s_va, 512 * j, AF.Identity, "all", 512 * j))
                    TM.append((O_OA + 512 * j, "oa", s_oa, 512 * j, AF.Sigmoid, "own", 1024 + 512 * j))
                    TM.append((O_VB + 512 * j, "vb", s_vb, 512 * j, AF.Identity, "all", 2048 + 512 * j))

                for phase in ("pre", "own"):
                    t0, nt = (0, TP) if phase == "pre" else (TP, TO)
                    bfm_x, btm_x, bif_x = (bfmp, btmp, bifp) if phase == "pre" else (bfm, btm, bif)
                    d_xT = [Dep() for _ in range(nt // 128)]
                    for ti in range(nt // 128):
                        xb_t, xb_d = xb.next()
                        k.dma("pool", xb_t[:], x[t0 + ti * 128:t0 + (ti + 1) * 128, :], w=[xb_d])
                        for g in range(4):
                            pt, pd = pb.next()

                            def f(pe, g=g, pt=pt, xb_t=xb_t):
                                for j in range(4):
                                    c = g * 4 + j
                                    ins = pe.transpose(pt[:, j * 128:(j + 1) * 128], xb_t[:, c * 128:(c + 1) * 128], identb[:])
                                return ins
                            k.op("pe", f, r=[xb_d, d_c], w=[pd])
                            k.op("dve", lambda e, g=g, ti=ti, pt=pt: e.tensor_copy(
                                xT[:, g * 4:(g + 1) * 4, ti * 128:(ti + 1) * 128],
                                pt[:, 0:512].rearrange("p (j n) -> p j n", j=4)), r=[pd], w=[d_xT[ti]])
                    tb = 0
                    while tb < nt:
                        n = min(512, nt - tb)
                        dx = d_xT[tb // 128:(tb + n) // 128]
                        for gi, Gt in ((0, Gi), (1, Gf)):
                            bk, bd = pf.next()

                            def f(pe, gi=gi, bk=bk, tb=tb, n=n):
                                for c in range(DC):
                                    ins = pe.matmul(bk[0:4, 0:n], wif[:, c, gi * 4:(gi + 1) * 4], xT[:, c, tb:tb + n],
                                                    start=(c == 0), stop=(c == DC - 1))
                                return ins
                            k.op("pe", f, r=dx + [d_wif], w=[bd])
                            k.op("act", lambda e, gi=gi, Gt=Gt, bk=bk, tb=tb, n=n: e.activation(
                                Gt[0:4, t0 + tb:t0 + tb + n], bk[0:4, 0:n], AF.Identity, bias=bif_x[:, gi:gi + 1], scale=1.0),
                                r=[bd, d_b], w=[d_G])
                        tb += n
                    for (c0, dkey, dst, row0, func, which, fmc) in FM:
                        if phase == "pre":
                            if which in ("own", "gate"):
                                continue
                            tlo = (TP - 128) if which == "q" else 0
                        else:
                            tlo = 0
                        w_t, w_d = wt.next()
                        k.dma("pool", w_t[:], w_in.rearrange("(c p) n -> p c n", p=128)[:, :, c0:c0 + 512], w=[w_d])
                        tb = tlo
                        while tb < nt:
                            n = min(512, nt - tb)
                            dx = d_xT[tb // 128:(tb + n) // 128]
                            ev_t, ev_d = evf.next()
                            for g in range(4):
                                bk, bd = pf.next()

                                def f(pe, g=g, bk=bk, tb=tb, n=n, w_t=w_t):
                                    for c in range(DC):
                                        ins = pe.matmul(bk[:, 0:n], w_t[:, c, g * 128:(g + 1) * 128], xT[:, c, tb:tb + n],
                                                        start=(c == 0), stop=(c == DC - 1))
                                    return ins
                                k.op("pe", f, r=dx + [w_d], w=[bd])
                                k.op("act", lambda e, g=g, bk=bk, n=n, ev_t=ev_t, func=func, fmc=fmc: e.activation(
                                    ev_t[:, g, 0:n], bk[:, 0:n], func, bias=bfm_x[:, fmc + g:fmc + g + 1], scale=1.0),
                                    r=[bd, d_b], w=[ev_d])
                            tcol = (tb if which == "gate" else t0 + tb)
                            k.dma("sp", dst[row0:row0 + 512, tcol:tcol + n].rearrange("(g p) t -> p g t", p=128),
                                  ev_t[:, :, 0:n], r=[ev_d], w=[d_scr[dkey]])
                            tb += n
                    for (c0, dkey, dst, col0, func, which, bcol) in TM:
                        if phase == "pre" and which == "own":
                            continue
                        w_t, w_d = wt.next()
                        k.dma("pool", w_t[:], w_in.rearrange("(c p) n -> p c n", p=128)[:, :, c0:c0 + 512], w=[w_d])
                        for ti in range(nt // 128):
                            bk, bd = pf.next()

                            def f(pe, bk=bk, ti=ti, w_t=w_t, bcol=bcol):
                                for c in range(DC):
                                    pe.matmul(bk[:, :], xT[:, c, ti * 128:(ti + 1) * 128], w_t[:, c, :],
                                              start=(c == 0), stop=False)
                                return pe.matmul(bk[:, :], onesb[0:1, :], btm_x[0:1, bcol:bcol + 512], start=False, stop=True)
                            k.op("pe", f, r=[d_xT[ti], w_d, d_b, d_c], w=[bd])
                            e_t, e_d = evt.next()
                            k.op("act", lambda e, bk=bk, e_t=e_t, func=func: e.activation(e_t[:], bk[:, :], func),
                                 r=[bd], w=[e_d])
                            trow = (ti * 128 if which == "own" else t0 + ti * 128)
                            k.dma("sp", dst[trow:trow + 128, col0:col0 + 512], e_t[:], r=[e_d], w=[d_scr[dkey]])

            if True:
                if stop <= 1:
                    raise _Stop()
                with k.scope():
                    t1 = k.sb("t1", [4, TA], F32); t2 = k.sb("t2", [4, TA], F32)
                    mt = k.sb("mt", [4, NCH], F32); dec = k.sb("dec", [4, NCH], F32)
                    sq = k.sb("sq", [4, 3, TA], F32)
                    dq = Dep()
                    V = "dve"
                    k.op("act", lambda e: e.activation(t1[:], Gf[:], AF.Abs), r=[d_G], w=[dq])
                    k.op("act", lambda e: e.activation(t1[:], t1[:], AF.Exp, scale=-1.0), r=[dq], w=[dq])
                    k.op("act", lambda e: e.activation(t1[:], t1[:], AF.Ln, bias=1.0, scale=1.0), r=[dq], w=[dq])
                    k.op(V, lambda e: e.tensor_scalar_min(t2[:], Gf[:], 0.0), r=[d_G], w=[dq])
                    k.op(V, lambda e: e.tensor_sub(t2[:], t2[:], t1[:]), r=[dq], w=[dq])
                    k.op(V, lambda e: e.tensor_scalar_mul(t2[:], t2[:], 0.5), r=[dq], w=[dq])
                    k.op(V, lambda e: e.tensor_tensor_scan(t1[:], t2[:], t2[:], 0.0, ALU.add, ALU.add), r=[dq], w=[dq])
                    Bc = t1
                    k.op(V, lambda e: e.tensor_sub(Gi[:], Gi[:], Bc[:]), r=[dq, d_G], w=[dq, d_G])
                    at = Gi
                    k.op(V, lambda e: e.tensor_tensor_scan(t2[:], at[:], at[:], 0.0, ALU.max, ALU.max), r=[dq, d_G], w=[dq])
                    ut = t2
                    ut3 = ut[:].rearrange("p (c s) -> p c s", s=64)
                    at3 = at[:].rearrange("p (c s) -> p c s", s=64)
                    Bc3 = Bc[:].rearrange("p (c s) -> p c s", s=64)
                    k.op(V, lambda e: e.memset(mt[:, 0:1], 0.0), w=[dq])
                    if NCH > 1:
                        k.op(V, lambda e: e.tensor_copy(mt[:, 1:NCH], ut3[:, 0:NCH - 1, 63]), r=[dq], w=[dq])
                    uL = ut3[:, :, 63]
                    mtb = mt[:, :].unsqueeze(2).to_broadcast([4, NCH, 64])
                    k.op(V, lambda e: e.tensor_sub(dec[:], mt[:], uL), r=[dq], w=[dq])
                    k.op("act", lambda e: e.activation(dec[:], dec[:], AF.Exp), r=[dq], w=[dq])
                    sq0 = sq[:, 0, :].rearrange("p (c s) -> p c s", s=64)
                    sq1 = sq[:, 1, :].rearrange("p (c s) -> p c s", s=64)
                    sq2 = sq[:, 2, :].rearrange("p (c s) -> p c s", s=64)
                    k.op(V, lambda e: e.tensor_sub(sq0, at3, mtb), r=[dq, d_G], w=[dq])
                    k.op(V, lambda e: e.tensor_scalar(sq[:, 0, :], sq[:, 0, :], 80.0, -LN16, ALU.min, ALU.add), r=[dq], w=[dq])
                    k.op("act", lambda e: e.activation(sq[:, 0, :], sq[:, 0, :], AF.Exp), r=[dq], w=[dq])
                    k.op(V, lambda e: e.tensor_tensor(sq1, sq0, dec[:, :].unsqueeze(2).to_broadcast([4, NCH, 64]), ALU.mult),
                         r=[dq], w=[dq])
                    k.op(V, lambda e: e.tensor_tensor(sq2, Bc3, mtb, ALU.add), r=[dq], w=[dq])
                    k.op(V, lambda e: e.tensor_scalar(sq[:, 2, :], sq[:, 2, :], -1.0, 80.0, ALU.mult, ALU.min), r=[dq], w=[dq])
                    k.op("act", lambda e: e.activation(sq[:, 2, :], sq[:, 2, :], AF.Exp), r=[dq], w=[dq])
                    d_seq = Dep()
                    k.dma("sp", s_dec, dec[:], r=[dq], w=[d_seq])
                    d_TT = Dep()
                    for q in range(3):
                        c0 = 0
                        while c0 < NCH:
                            ncc = min(128, NCH - c0)
                            bk, bd = pf.next()

                            def f(pe, bk=bk, q=q, c0=c0, ncc=ncc):
                                for cc in range(ncc):
                                    c = c0 + cc
                                    ins = pe.transpose(bk[0:64, cc * 4:cc * 4 + 4], sq[0:4, q, c * 64:(c + 1) * 64], identf[0:4, 0:4])
                                return ins
                            k.op("pe", f, r=[dq, d_c], w=[bd])
                            k.op("act", lambda e, bk=bk, q=q, c0=c0, ncc=ncc: e.copy(
                                TT[:, q, c0:c0 + ncc, :], bk[0:64, 0:ncc * 4].rearrange("p (c h) -> p c h", h=4)), r=[bd], w=[d_TT])
                            c0 += ncc
                    k.dma("sp", decbc[:], s_dec.rearrange("h c -> (h c)").partition_broadcast(128), r=[d_seq], w=[d_TT])
                gscope.close()
                if dbg:
                    k.dma("sp", s_TT, TT[:].rearrange("p a b c -> p (a b c)"), r=[d_TT], w=[Dep()])
                if stop <= 2:
                    raise _Stop()
                with k.scope():
                    qT = k.sb("qT", [128, 8, TO], BF16)
                    kT = k.sb("kT", [128, 8, TA], BF16)
                    cw = k.sb("cw", [128, 16, 4], F32); cb = k.sb("cb", [128, 16], F32)
                    ngb = k.sb("ngb", [64, 1024], F32)
                    d_cw, d_qT, d_kT = Dep(), Dep(), Dep()
                    k.dma("sp", cw[:], cw_d, w=[d_cw]); k.dma("sp", cb[:], cb_d, w=[d_cw])
                    k.dma("sp", ngb[:], ng_d.partition_broadcast(64), w=[d_cw])
                    cscope = k.scope()
                    cscope.__enter__()
                    cin = Ring([(k.sb("cin", [128, 3 + TA], BF16), Dep()) for _ in range(3)])
                    Dg = k.sb("Dg", [128, 16, 4, 128], BF16)
                    d_Dg = Dep()
                    for fc in range(16):
                        for j in range(4):
                            k.op("dve", lambda e, fc=fc, j=j: e.tensor_scalar_mul(Dg[:, fc, j, :], identf[:], cw[:, fc, j:j + 1]),
                                 r=[d_cw, d_c], w=[d_Dg])
                    for fc in list(range(8, 16)) + list(range(8)):
                        isq = fc < 8
                        lo = (TP - 128) if isq else 0
                        o0 = TP if isq else 0
                        n = TA - o0
                        ci, cd = cin.next()
                        if not isq:
                            k.op("pool", lambda e, ci=ci: e.memset(ci[:, 0:3], 0.0), w=[cd])
                        k.dma("sp", ci[:, 3 + lo:3 + TA], s_qkaT[fc * 128:(fc + 1) * 128, lo:TA], r=[d_scr["qka"]], w=[cd])
                        tb = 0
                        while tb < n:
                            nn = min(512, n - tb)
                            bk, bd = pf.next()

                            def f(pe, bk=bk, ci=ci, fc=fc, o0=o0, tb=tb, nn=nn):
                                for j in range(4):
                                    ins = pe.matmul(bk[:, 0:nn], Dg[:, fc, j, :], ci[:, o0 + tb + j:o0 + tb + j + nn],
                                                    start=(j == 0), stop=(j == 3))
                                return ins
                            k.op("pe", f, r=[cd, d_Dg], w=[bd])
                            if isq:
                                k.op("act", lambda e, bk=bk, fc=fc, tb=tb, nn=nn: e.activation(
                                    qT[:, fc, tb:tb + nn], bk[:, 0:nn], AF.Silu, bias=cb[:, fc:fc + 1], scale=1.0),
                                    r=[bd, d_cw], w=[d_qT])
                            else:
                                k.op("act", lambda e, bk=bk, fc=fc, tb=tb, nn=nn: e.activation(
                                    kT[:, fc - 8, tb:tb + nn], bk[:, 0:nn], AF.Silu, bias=cb[:, fc:fc + 1], scale=1.0),
                                    r=[bd, d_cw], w=[d_kT])
                            tb += nn
                    cscope.__exit__(None, None, None)
                    a_S = Ring([banks[0], banks[1]])
                    bO, bOd = banks[2]
                    bO1, bO1d = banks[3]
                    m_U = Ring(banks[4:5])
                    bSN, bSNd = banks[5]
                    bNN, bNNd = banks[6]
                    pb_all = pb
                    pb = Ring([(banks[7][0].bitcast(BF16), banks[7][1])])
                    Cst = [k.sb("Cst", [128, 2, 257], F32) for _ in range(4)]
                    hblk = Ring([(k.sb("hblk", [128, 8, HB], BF16), Dep()) for _ in range(1)])
                    Cbf = [k.sb("Cbf", [128, 2, 257], BF16) for _ in range(4)]
                    d_C = [Dep() for _ in range(4)]
                    d_Cbf = [Dep() for _ in range(4)]
                    for h in range(4):
                        k.op("pool", lambda e, h=h: e.memset(Cst[h][:], 0.0), w=[d_C[h]])
                        k.op("pool", lambda e, h=h: e.memset(Cbf[h][:], 0.0), w=[d_Cbf[h]])
                    vch_items = []
                    for _ in range(3):
                        vt = k.sb("vch", [64, 4, 257], BF16)
                        vd = Dep()
                        k.op("pool", lambda e, vt=vt: e.memset(vt[:, :, 256:257], 1.0), w=[vd])
                        vch_items.append((vt, vd))
                    vch = Ring(vch_items)
                    sor = Ring([(k.sb("so", [64, 1024], BF16), Dep()) for _ in range(1)])
                    kwr = Ring([(k.sb("kw", [64, 256], BF16), Dep()) for _ in range(3)])
                    Wtr = Ring([(k.sb("Wt", [64, 64], BF16), Dep()) for _ in range(3)])
                    Nsr = Ring([(k.sb("Ns", [64, 4, 257], F32), Dep()) for _ in range(2)])
                    hgr = Ring([(k.sb("hg", [64, 4, 256], F32), Dep()) for _ in range(1)])
                    hbr = Ring([(k.sb("hb", [64, 1024], BF16), Dep()) for _ in range(2)])
                    smr = Ring([(k.sb("sm", [64, 64], F32), Dep()) for _ in range(2)])
                    lamt = k.sb("lamt", [128, 4, 64], F32)
                    lsm = k.sb("lsm", [128, 8], F32)
                    gnb = k.sb("gnb", [128, 128], F32)
                    d_l = Dep()
                    k.dma("sp", lamt[:], lam_d.rearrange("a b -> (a b)").partition_broadcast(128).rearrange("p (a b) -> p a b", a=4), w=[d_l])
                    k.dma("sp", gnb[:], dg_d.partition_broadcast(128), w=[d_l])
                    k.op("dve", lambda e: e.tensor_tensor(lamt[:, 0, :], lamt[:, 0, :], lamt[:, 1, :], ALU.mult), r=[d_l], w=[d_l])
                    k.op("dve", lambda e: e.tensor_tensor(lamt[:, 2, :], lamt[:, 2, :], lamt[:, 3, :], ALU.mult), r=[d_l], w=[d_l])
                    k.op("dve", lambda e: e.reduce_sum(lsm[:, 0:1], lamt[:, 0, :], AX.X), r=[d_l], w=[d_l])
                    k.op("dve", lambda e: e.reduce_sum(lsm[:, 1:2], lamt[:, 2, :], AX.X), r=[d_l], w=[d_l])
                    k.op("act", lambda e: e.activation(lsm[:, 2:4], lsm[:, 0:2], AF.Exp), r=[d_l], w=[d_l])
                    k.op("dve", lambda e: e.tensor_sub(lsm[:, 4:5], lsm[:, 3:4], lsm[:, 2:3]), r=[d_l], w=[d_l])
                    k.op("dve", lambda e: e.tensor_scalar_add(lsm[:, 5:6], lsm[:, 4:5], -0.2), r=[d_l], w=[d_l])
                    k.op("dve", lambda e: e.tensor_scalar_mul(gnb[:], gnb[:], 0.8), r=[d_l], w=[d_l])
                    neglam = lsm[:, 5:6]
                    kb_items = []
                    for _ in range(1):
                        kt_ = k.sb("kbT", [128, 2, TA], BF16)
                        kd_ = Dep()
                        k.op("pool", lambda e, kt_=kt_: e.memset(kt_[64:128, 0, :], 0.0), w=[kd_])
                        k.op("pool", lambda e, kt_=kt_: e.memset(kt_[0:64, 1, :], 0.0), w=[kd_])
                        kb_items.append((kt_, kd_))
                    kbr = Ring(kb_items)
                    qbr = Ring([(k.sb("qbT", [128, TO], BF16), Dep()) for _ in range(1)])
                    vb_items = []
                    for _ in range(1):
                        vt = k.sb("vbe", [128, NTA, 129], BF16)
                        vd = Dep()
                        k.op("dve", lambda e, vt=vt: e.tensor_copy(vt[:, :, 128], validb[:, :]), r=[d_c], w=[vd])
                        vb_items.append((vt, vd))
                    vbr = Ring(vb_items)
                    PTr = Ring([(k.sb("PT", [128, 256], BF16), Dep()) for _ in range(4)])
                    o1r = Ring([(k.sb("o1", [128, 128], F32), Dep()) for _ in range(2)])
                    o2r = Ring([(k.sb("o2", [128, 128], F32), Dep()) for _ in range(2)])
                    obr = Ring([(k.sb("ob", [128, 128], BF16), Dep()) for _ in range(4)])
                    s8r = Ring([(k.sb("s8", [128, 8], F32), Dep()) for _ in range(2)])
                    Osr = Ring([(k.sb("Os", [128, 2, 129], F32), Dep()) for _ in range(2)])
                    oblk = Ring([(k.sb("oblk", [128, HB], BF16), Dep()) for _ in range(2)])

                    def mlstm_gen():
                        st_m = {'hb': None, 'fin': None}
                        for c in range(NCH):
                            own = c >= CH0
                            tq = (c - CH0) * 64
                            v_t, v_d = vch.next()
                            k.dma("sp", v_t[:, :, 0:256], s_va[c * 64:(c + 1) * 64, :].rearrange("s (h d) -> s h d", h=4),
                                  r=[d_scr["va"]], w=[v_d])
                            if own:
                                so_t, so_d = sor.next()
                                k.dma("sp", so_t[:], s_oa[tq:tq + 64, :], r=[d_scr["oa"]], w=[so_d])
                                Ns_t, Ns_d = Nsr.next()
                            for h in range(4):
                                pt, pd = pb.next()

                                def f(pe, pt=pt, h=h, c=c):
                                    for j in range(2):
                                        ins = pe.transpose(pt[0:64, j * 128:(j + 1) * 128], kT[:, h * 2 + j, c * 64:(c + 1) * 64], identb[:])
                                    return ins
                                k.op("pe", f, r=[d_kT, d_c], w=[pd])
                                kw_t, kw_d = kwr.next()
                                k.op("act", lambda e, kw_t=kw_t, pt=pt, h=h, c=c: e.activation(
                                    kw_t[:], pt[0:64, 0:256], AF.Identity, scale=TT[:, 1, c, h:h + 1]), r=[pd, d_TT], w=[kw_d])
                                yield
                                bU, bUd = m_U.next()

                                def f(pe, bU=bU, kw_t=kw_t, v_t=v_t, h=h):
                                    for j in range(2):
                                        pe.matmul(bU[:, j * 256:(j + 1) * 256], kw_t[:, j * 128:(j + 1) * 128], v_t[:, h, 0:256],
                                                  start=True, stop=True)
                                    for j in range(2):
                                        ins = pe.matmul(bSN[:, 400 + j:401 + j], kw_t[:, j * 128:(j + 1) * 128], v_t[:, h, 256:257],
                                                        start=True, stop=True)
                                    return ins
                                k.op("pe", f, r=[kw_d, v_d], w=[bUd, bSNd])
                                if not own:
                                    yield
                                if own:
                                    def f(pe, h=h, c=c, tq=tq):
                                        for j in range(2):
                                            ins = pe.matmul(bSN[0:64, 320:384], kT[:, h * 2 + j, c * 64:(c + 1) * 64],
                                                            qT[:, h * 2 + j, tq:tq + 64], start=(j == 0), stop=(j == 1))
                                        return ins
                                    k.op("pe", f, r=[d_kT, d_qT], w=[bSNd])
                                    W_t, W_d = Wtr.next()
                                    k.op("dve", lambda e, W_t=W_t, h=h, c=c: e.scalar_tensor_tensor(
                                        W_t[:], bSN[0:64, 320:384], TT[:, 0, c, h:h + 1], mask64[:], ALU.mult, ALU.mult),
                                        r=[bSNd, d_TT, d_c], w=[W_d])
                                    yield

                                    def f(pe, W_t=W_t, v_t=v_t, h=h, tq=tq):
                                        for j in range(2):
                                            pe.matmul(bNN[0:64, 0:257], qT[:, h * 2 + j, tq:tq + 64], Cbf[h][:, j, :],
                                                      start=(j == 0), stop=False)
                                        return pe.matmul(bNN[0:64, 0:257], W_t[:], v_t[:, h, :], start=False, stop=True)
                                    k.op("pe", f, r=[d_qT, d_Cbf[h], W_d, v_d], w=[bNNd])
                                    k.op("act", lambda e, Ns_t=Ns_t, h=h: e.copy(Ns_t[:, h, :], bNN[0:64, 0:257]),
                                         r=[bNNd], w=[Ns_d])
                                    yield
                                dsc = decbc[:, h * NCH + c:h * NCH + c + 1]
                                k.op("dve", lambda e, h=h, bU=bU, dsc=dsc: e.scalar_tensor_tensor(
                                    Cst[h][:, :, 0:256], Cst[h][:, :, 0:256], dsc,
                                    bU[:, 0:512].rearrange("p (j n) -> p j n", j=2), ALU.mult, ALU.add),
                                    r=[bUd, d_TT], w=[d_C[h]])
                                k.op("dve", lambda e, h=h, dsc=dsc: e.scalar_tensor_tensor(
                                    Cst[h][:, :, 256:257], Cst[h][:, :, 256:257], dsc,
                                    bSN[:, 400:402].rearrange("p (j n) -> p j n", j=2), ALU.mult, ALU.add),
                                    r=[bSNd, d_TT], w=[d_C[h]])
                                k.op("act", lambda e, h=h: e.copy(Cbf[h][:], Cst[h][:]), r=[d_C[h]], w=[d_Cbf[h]])
                                if h == 1 and st_m['fin'] is not None:
                                    st_m['fin']()
                                    st_m['fin'] = None
                                yield
                            if own:
                                sm_t, sm_d = smr.next()
                                k.op("act", lambda e, sm_t=sm_t, Ns_t=Ns_t: e.activation(
                                    sm_t[:, 0:4], Ns_t[:, :, 256], AF.Abs), r=[Ns_d], w=[sm_d])
                                k.op("dve", lambda e, sm_t=sm_t, c=c: e.tensor_tensor(
                                    sm_t[:, 0:4], sm_t[:, 0:4], TT[:, 2, c, :], ALU.max), r=[sm_d, d_TT], w=[sm_d])
                                k.op("dve", lambda e, sm_t=sm_t: e.reciprocal(sm_t[:, 0:4], sm_t[:, 0:4]), r=[sm_d], w=[sm_d])
                                hg_t, hg_d = hgr.next()
                                k.op("dve", lambda e, hg_t=hg_t, Ns_t=Ns_t, sm_t=sm_t: e.tensor_tensor(
                                    hg_t[:], Ns_t[:, :, 0:256], sm_t[:, 0:4].unsqueeze(2).to_broadcast([64, 4, 256]), ALU.mult),
                                    r=[Ns_d, sm_d], w=[hg_d])
                                k.op("dve", lambda e, hg_t=hg_t, so_t=so_t: e.tensor_tensor(
                                    hg_t[:], hg_t[:], so_t[:].rearrange("s (h d) -> s h d", h=4), ALU.mult),
                                    r=[so_d, hg_d], w=[hg_d])

                                def f(e, hg_t=hg_t, sm_t=sm_t):
                                    for hh in range(4):
                                        ins = e.bn_stats(sm_t[:, 4 + 6 * hh:10 + 6 * hh], hg_t[:, hh, :])
                                    return ins
                                k.op("dve", f, r=[hg_d], w=[sm_d])

                                def f(e, sm_t=sm_t):
                                    for hh in range(4):
                                        ins = e.bn_aggr(sm_t[:, 28 + 2 * hh:30 + 2 * hh], sm_t[:, 4 + 6 * hh:10 + 6 * hh])
                                    return ins
                                k.op("dve", f, r=[sm_d], w=[sm_d])
                                mv = sm_t[:, 28:36].rearrange("s (h k) -> s h k", h=4)
                                k.op("act", lambda e, sm_t=sm_t, mv=mv: e.activation(
                                    sm_t[:, 36:40], mv[:, :, 1], AF.Ln, bias=epsc[0:64, 0:1], scale=1.0), r=[sm_d, d_c], w=[sm_d])
                                k.op("act", lambda e, sm_t=sm_t: e.activation(
                                    sm_t[:, 36:40], sm_t[:, 36:40], AF.Exp, scale=-0.5), r=[sm_d], w=[sm_d])
                                k.op("dve", lambda e, hg_t=hg_t, mv=mv: e.tensor_tensor(
                                    hg_t[:], hg_t[:], mv[:, :, 0:1].to_broadcast([64, 4, 256]), ALU.subtract),
                                    r=[sm_d, hg_d], w=[hg_d])
                                k.op("dve", lambda e, hg_t=hg_t, sm_t=sm_t: e.tensor_tensor(
                                    hg_t[:], hg_t[:], sm_t[:, 36:40].unsqueeze(2).to_broadcast([64, 4, 256]), ALU.mult),
                                    r=[sm_d, hg_d], w=[hg_d])
                                hb_t, hb_d = hbr.next()
                                k.op("dve", lambda e, hg_t=hg_t, hb_t=hb_t: e.tensor_tensor(
                                    hb_t[:], hg_t[:].rearrange("s h d -> s (h d)"), ngb[:], ALU.mult),
                                    r=[hg_d, d_cw], w=[hb_d])
                                def mfin(hb_t=hb_t, hb_d=hb_d, tq=tq):
                                    pt, pd = pb.next()

                                    def f(pe):
                                        for fc in range(8):
                                            ins = pe.transpose(pt[:, fc * 64:(fc + 1) * 64], hb_t[:, fc * 128:(fc + 1) * 128], identb[0:64, 0:64])
                                        return ins
                                    k.op("pe", f, r=[hb_d, d_c], w=[pd])
                                    if tq % HB == 0:
                                        st_m['hb'] = hblk.next()
                                    hk_t, hk_d = st_m['hb']
                                    k.op("act", lambda e: e.copy(
                                        hk_t[:, :, tq % HB:tq % HB + 64], pt[:, 0:512].rearrange("p (f s) -> p f s", f=8)), r=[pd], w=[hk_d])
                                    if (tq + 64) % HB == 0:
                                        tb0 = tq + 64 - HB
                                        k.dma("sp", s_haT[:, tb0:tb0 + HB].rearrange("(f p) t -> p f t", p=128), hk_t[:], r=[hk_d], w=[d_haT])
                                st_m['fin'] = mfin
                            yield
                        if st_m['fin'] is not None:
                            st_m['fin']()
                            st_m['fin'] = None
                        yield

                    def attn_gen():
                        st_a = {'ob': None}
                        deferred = []
                        for h in range(8):
                            kb_t, kb_d = kbr.next(); qb_t, qb_d = qbr.next(); vb_t, vb_d = vbr.next()
                            k.dma("sp", kb_t[0:64, 0, :], s_qkbT[1024 + h * 128:1024 + h * 128 + 64, :], r=[d_scr["qkb"]], w=[kb_d])
                            k.dma("sp", kb_t[64:128, 1, :], s_qkbT[1024 + h * 128 + 64:1024 + (h + 1) * 128, :], r=[d_scr["qkb"]], w=[kb_d])
                            k.dma("sp", qb_t[:], s_qkbT[h * 128:(h + 1) * 128, TP:TA], r=[d_scr["qkb"]], w=[qb_d])
                            for t8 in range(0, NTA, 8):
                                t9 = min(NTA, t8 + 8)
                                k.dma("sp", vb_t[:, t8:t9, 0:128],
                                      s_vb[t8 * 128:t9 * 128, h * 128:(h + 1) * 128].rearrange("(t p) d -> p t d", p=128),
                                      r=[d_scr["vb"]], w=[vb_d])
                            items = []
                            for qi in range(NTO):
                                qt = NTP + qi
                                kt_lo = 0
                                while kt_lo < qt and SLOPES[h] * (127 - 128 * (qt - kt_lo)) < -ACLAMP:
                                    kt_lo += 1
                                for kt in range(kt_lo, qt + 1):
                                    items.append((qi, qt, kt, kt_lo))

                            def emit_qk(it, h=h, kb_t=kb_t, qb_t=qb_t, kb_d=kb_d, qb_d=qb_d):
                                qi, qt, kt, kt_lo = it
                                bSt, sd = a_S.next()
                                off = 0
                                diag = (kt == qt)

                                def f(pe):
                                    for m in range(2):
                                        ins = pe.matmul(bSt[:, off + m * 128:off + (m + 1) * 128], kb_t[:, m, kt * 128:(kt + 1) * 128],
                                                        qb_t[:, qi * 128:(qi + 1) * 128], start=True, stop=(not diag))
                                        if diag:
                                            ins = pe.matmul(bSt[:, off + m * 128:off + (m + 1) * 128], identb[:], dgb[:, h, :], start=False, stop=True)
                                    return ins
                                k.op("pe", f, r=[kb_d, qb_d, d_c], w=[sd])
                                return bSt, sd
                            def do_pv(it, P_t, P_d, h=h, vb_t=vb_t, vb_d=vb_d):
                                qi, qt, kt, kt_lo = it

                                def f(pe, P_t=P_t, vb_t=vb_t, kt=kt, qt=qt, kt_lo=kt_lo):
                                    pe.matmul(bO[:, 0:129], P_t[:, 0:128], vb_t[:, kt, :], start=(kt == kt_lo), stop=(kt == qt))
                                    return pe.matmul(bO1[:, 0:129], P_t[:, 128:256], vb_t[:, kt, :], start=(kt == kt_lo), stop=(kt == qt))
                                k.op("pe", f, r=[P_d, vb_d], w=[bOd, bO1d])
                                if kt != qt:
                                    return
                                s8, s8d = s8r.next()
                                Os, Osd = Osr.next()
                                k.op("act", lambda e, Os=Os: e.copy(Os[:, 0, :], bO[:, 0:129]), r=[bOd], w=[Osd])
                                k.op("dve", lambda e, Os=Os: e.tensor_copy(Os[:, 1, :], bO1[:, 0:129]), r=[bO1d], w=[Osd])
                                k.op("dve", lambda e, s8=s8, Os=Os: e.reciprocal(s8[:, 0:2], Os[:, :, 128]), r=[Osd], w=[s8d])
                                k.op("dve", lambda e, s8=s8: e.tensor_tensor(s8[:, 2:3], s8[:, 1:2], neglam, ALU.mult),
                                     r=[s8d, d_l], w=[s8d])
                                o1, o1d = o1r.next(); o2, o2d = o2r.next()
                                k.op("dve", lambda e, o1=o1, s8=s8, Os=Os: e.tensor_scalar_mul(o1[:], Os[:, 0, 0:128], s8[:, 0:1]),
                                     r=[Osd, s8d], w=[o1d])
                                k.op("dve", lambda e, o1=o1, s8=s8, Os=Os: e.scalar_tensor_tensor(
                                    o1[:], Os[:, 1, 0:128], s8[:, 2:3], o1[:], ALU.mult, ALU.add), r=[Osd, s8d], w=[o1d])
                                k.op("pool", lambda e, o1=o1, o2=o2: e.tensor_tensor(o2[:], o1[:], o1[:], ALU.mult), r=[o1d], w=[o2d])
                                k.op("dve", lambda e, o2=o2, s8=s8: e.reduce_sum(s8[:, 3:4], o2[:], AX.X), r=[o2d], w=[s8d])
                                k.op("dve", lambda e, s8=s8: e.tensor_scalar(s8[:, 4:5], s8[:, 3:4], 1.0 / 128.0, EPS, ALU.mult, ALU.add),
                                     r=[s8d], w=[s8d])
                                k.op("act", lambda e, s8=s8: e.activation(s8[:, 5:6], s8[:, 4:5], AF.Ln), r=[s8d], w=[s8d])
                                k.op("act", lambda e, s8=s8: e.activation(s8[:, 5:6], s8[:, 5:6], AF.Exp, scale=-0.5),
                                     r=[s8d], w=[s8d])
                                ob, obd = obr.next()
                                k.op("dve", lambda e, ob=ob, o1=o1, s8=s8: e.scalar_tensor_tensor(
                                    ob[:], o1[:], s8[:, 5:6], gnb[:], ALU.mult, ALU.mult), r=[o1d, s8d, d_l], w=[obd])
                                def fin(ob=ob, obd=obd, qi=qi, h=h):
                                    pt, pd = pb.next()
                                    k.op("pe", lambda pe: pe.transpose(pt[:, 0:128], ob[:], identb[:]), r=[obd, d_c], w=[pd])
                                    tq = qi * 128
                                    if tq % HB == 0:
                                        st_a['ob'] = oblk.next()
                                    ok_t, ok_d = st_a['ob']
                                    k.op("act", lambda e: e.copy(ok_t[:, tq % HB:tq % HB + 128], pt[:, 0:128]),
                                         r=[pd], w=[ok_d])
                                    if (tq + 128) % HB == 0:
                                        tb0 = tq + 128 - HB
                                        k.dma("sp", s_obT[h * 128:(h + 1) * 128, tb0:tb0 + HB], ok_t[:], r=[ok_d], w=[d_obT])
                                deferred.append([3, fin])
                            nxt = emit_qk(items[0])
                            pend = None
                            for j, it in enumerate(items):
                                qi, qt, kt, kt_lo = it
                                bSt, sd = nxt
                                off = 0
                                if j + 1 < len(items):
                                    nxt = emit_qk(items[j + 1])
                                diag = (kt == qt)
                                P_t, P_d = PTr.next()
                                bias_ap = zero1[:, 0:1] if diag else abt[:, h, qt - kt:qt - kt + 1]
                                k.op("act", lambda e, P_t=P_t, off=off, bias_ap=bias_ap, bSt=bSt: e.activation(
                                    P_t[:], bSt[:, off:off + 256], AF.Exp, bias=bias_ap, scale=0.125), r=[sd, d_c], w=[P_d])

                                if pend is not None:
                                    do_pv(*pend)
                                pend = (it, P_t, P_d)
                                for dfr in list(deferred):
                                    dfr[0] -= 1
                                    if dfr[0] <= 0:
                                        deferred.remove(dfr)
                                        dfr[1]()
                                yield
                            if pend is not None:
                                do_pv(*pend)
                            yield
                        for dfr in list(deferred):
                            dfr[1]()
                        deferred.clear()
                        yield

                    gm = mlstm_gen()
                    ga_ = attn_gen()
                    if stop <= 3:
                        for _ in gm:
                            pass
                        raise _Stop()
                    if stop <= 3.5:
                        for _ in ga_:
                            pass
                        raise _Stop()
                    n_m = (CH0 * (4 * 3 + 1)) + ((NCH - CH0) * (4 * 4 + 1))
                    n_a = 0
                    for h_ in range(8):
                        for qi_ in range(NTO):
                            qt_ = NTP + qi_
                            lo_ = 0
                            while lo_ < qt_ and SLOPES[h_] * (127 - 128 * (qt_ - lo_)) < -ACLAMP:
                                lo_ += 1
                            n_a += qt_ + 1 - lo_
                    done_m = done_a = 0
                    m_alive = a_alive = True
                    while m_alive or a_alive:
                        if m_alive:
                            try:
                                next(gm); done_m += 1
                            except StopIteration:
                                m_alive = False
                        if ILV == 0:
                            tgt = n_a + 10 if not m_alive else 0
                        else:
                            tgt = n_a + 10 if not m_alive else (done_m * n_a) // n_m
                        while a_alive and done_a < tgt:
                            try:
                                next(ga_); done_a += 1
                            except StopIteration:
                                a_alive = False
                    for g_ in (gm, ga_):
                        for _ in g_:
                            pass
                    pb = pb_all
                if stop <= 4:
                    raise _Stop()
                with k.scope():
                    war = Ring([(k.sb("wa", [128, 8, 512], BF16), Dep()) for _ in range(2)])
                    wbr = Ring([(k.sb("wb", [128, 8, 512], BF16), Dep()) for _ in range(2)])
                    gar = Ring([(k.sb("ga", [128, 512], BF16), Dep()) for _ in range(2)])
                    gbr = Ring([(k.sb("gb", [128, 512], BF16), Dep()) for _ in range(2)])
                    m1r = Ring([(k.sb("m1", [128, 512], F32), Dep()) for _ in range(2)])
                    m2r = Ring([(k.sb("m2", [128, 512], F32), Dep()) for _ in range(2)])
                    hkr = Ring([(k.sb("hk", [128, 8, HB], BF16), Dep()) for _ in range(2)])
                    okr = Ring([(k.sb("ok", [128, 8, HB], BF16), Dep()) for _ in range(2)])
                    mgr = Ring([(k.sb("mgo", [128, 512], BF16), Dep()) for _ in range(3)])
                    st_e1 = {"db": None, "tb": None}

                    def e1_unit(db, tb, n, g):
                        with k.atomic():
                            if st_e1["db"] != db:
                                wa_t, wa_d = war.next(); wb_t, wb_d = wbr.next()
                                k.dma("pool", wa_t[:], w_a.rearrange("(c p) n -> p c n", p=128)[:, :, db * 512:(db + 1) * 512], w=[wa_d])
                                k.dma("pool", wb_t[:], w_b.rearrange("(c p) n -> p c n", p=128)[:, :, db * 512:(db + 1) * 512], w=[wb_d])
                                st_e1["db"] = db
                                st_e1["w"] = (wa_t, wa_d, wb_t, wb_d)
                            if st_e1["tb"] != (db, tb):
                                hk, hkd = hkr.next(); ok, okd = okr.next()
                                k.dma("sp", hk[:, :, 0:n], s_haT[:, tb:tb + n].rearrange("(f p) t -> p f t", p=128), r=[d_haT], w=[hkd])
                                k.dma("sp", ok[:, :, 0:n], s_obT[:, tb:tb + n].rearrange("(f p) t -> p f t", p=128), r=[d_obT], w=[okd])
                                st_e1["tb"] = (db, tb)
                                st_e1["h"] = (hk, hkd, ok, okd)
                        wa_t, wa_d, wb_t, wb_d = st_e1["w"]
                        hk, hkd, ok, okd = st_e1["h"]
                        dc = db * 4 + g
                        ga_t, ga_d = gar.next(); gb_t, gb_d = gbr.next()
                        k.dma("sp", ga_t[:, 0:n], s_gT[dc * 128:(dc + 1) * 128, tb:tb + n], r=[d_scr["g"]], w=[ga_d])
                        k.dma("sp", gb_t[:, 0:n], s_gT[2048 + dc * 128:2048 + (dc + 1) * 128, tb:tb + n], r=[d_scr["g"]], w=[gb_d])
                        bA, bAd = pf.next(); bB, bBd = pf.next()

                        def f(pe):
                            for fc in range(8):
                                ins = pe.matmul(bA[:, 0:n], wa_t[:, fc, g * 128:(g + 1) * 128], hk[:, fc, 0:n],
                                                start=(fc == 0), stop=(fc == 7))
                            return ins
                        k.op("pe", f, r=[wa_d, hkd], w=[bAd])

                        def f(pe):
                            for fc in range(8):
                                ins = pe.matmul(bB[:, 0:n], wb_t[:, fc, g * 128:(g + 1) * 128], ok[:, fc, 0:n],
                                                start=(fc == 0), stop=(fc == 7))
                            return ins
                        k.op("pe", f, r=[wb_d, okd], w=[bBd])
                        m1, m1d = m1r.next(); m2, m2d = m2r.next()
                        k.op("dve", lambda e: e.tensor_tensor(m1[:, 0:n], bA[:, 0:n], ga_t[:, 0:n], ALU.mult),
                             r=[bAd, ga_d], w=[m1d])
                        k.op("dve", lambda e: e.tensor_tensor(m2[:, 0:n], bB[:, 0:n], gb_t[:, 0:n], ALU.mult),
                             r=[bBd, gb_d], w=[m2d])
                        mg_t, mg_dd = mgr.next()
                        k.op("pool", lambda e: e.tensor_tensor(mg_t[:, 0:n], m1[:, 0:n], m2[:, 0:n], ALU.add), r=[m1d, m2d], w=[mg_dd])
                        k.dma("sp", s_mgT[dc * 128:(dc + 1) * 128, tb:tb + n], mg_t[:, 0:n], r=[mg_dd], w=[d_mg])
                    units = []
                    for db in range(4):
                        tb = 0
                        while tb < TO:
                            n = min(512, TO - tb)
                            for g in range(4):
                                units.append(lambda db=db, tb=tb, n=n, g=g: e1_unit(db, tb, n, g))
                            tb += n
                    k.interleave(units, width=NIL)
            if stop <= 5:
                raise _Stop()
            with k.scope():
                wo = k.sb("wo", [128, DC, D], BF16)
                wr = k.sb("wr", [128, DC, 36], F32)
                br = k.sb("br", [1, 36], F32)
                l1g = k.sb("l1g", [128, D], F32); l1b = k.sb("l1b", [128, D], F32)
                cnt = k.sb("cnt", [128, 32], F32)
                d_wo, d_cnt = Dep(), Dep()
                for q4 in range(4):
                    k.dma("pool", wo[:, :, q4 * 512:(q4 + 1) * 512],
                          w_out.rearrange("(c p) n -> p c n", p=128)[:, :, q4 * 512:(q4 + 1) * 512], w=[d_wo])
                k.dma("sp", wr[:], wr_d.rearrange("(c p) n -> p c n", p=128), w=[d_wo])
                k.dma("sp", br[:], br_d, w=[d_wo])
                k.dma("sp", l1g[:], ln1g_d.partition_broadcast(128), w=[d_wo])
                k.dma("sp", l1b[:], ln1b_d.partition_broadcast(128), w=[d_wo])
                k.op("dve", lambda e: e.memset(cnt[:], 0.0), w=[d_cnt])
                xtr = Ring([(k.sb("xt", [128, D], F32), Dep()) for _ in range(2)])
                x1r = Ring([(k.sb("x1", [128, D], F32), Dep()) for _ in range(2)])
                hbr2 = Ring([(k.sb("hb2", [128, D], BF16), Dep()) for _ in range(2)])
                hTr = Ring([(k.sb("hT", [128, DC, 128], F32), Dep()) for _ in range(2)])
                rsr = Ring([(k.sb("rs", [128, 256], F32), Dep()) for _ in range(2)])
                mkr = Ring([(k.sb("mk", [128, 32], BF16), Dep()) for _ in range(2)])
                mbr = Ring([(k.sb("mgb", [128, DC, HB], BF16), Dep()) for _ in range(2)])
                st_e2 = {"mb": None}

                def e2_tile(ti):
                    if (ti * 128) % HB == 0:
                        with k.atomic():
                            st_e2["mb"] = mbr.next()
                            k.dma("sp", st_e2["mb"][0][:], s_mgT[:, ti * 128:ti * 128 + HB].rearrange("(c p) t -> p c t", p=128),
                                  r=[d_mg], w=[st_e2["mb"][1]])
                    mgb, mgbd = st_e2["mb"]
                    tloc = (ti * 128) % HB
                    xt, xtd = xtr.next(); x1, x1d = x1r.next()
                    k.dma("sp", xt[:], x[TP + ti * 128:TP + (ti + 1) * 128, :], w=[xtd])
                    for db in range(4):
                        bk, bd = pf.next()

                        def f(pe, bk=bk, mgb=mgb, tloc=tloc, db=db):
                            for c in range(DC):
                                ins = pe.matmul(bk[:, :], mgb[:, c, tloc:tloc + 128], wo[:, c, db * 512:(db + 1) * 512],
                                                start=(c == 0), stop=(c == DC - 1))
                            return ins
                        k.op("pe", f, r=[mgbd, d_wo], w=[bd])
                        k.op("dve", lambda e, x1=x1, xt=xt, bk=bk, db=db: e.scalar_tensor_tensor(
                            x1[:, db * 512:(db + 1) * 512], xt[:, db * 512:(db + 1) * 512], ALPHA, bk[:, :], ALU.mult, ALU.add),
                            r=[xtd, bd], w=[x1d])
                    rs, rsd = rsr.next()

                    def layer_norm(xx, xd, rs, rsd, g_t, b_t, gdep):
                        st = rs[:, 0:24].rearrange("p (g k) -> p g k", g=4)
                        def fbn(e):
                            for gg in range(4):
                                ins = e.bn_stats(rs[:, 6 * gg:6 * gg + 6], xx[:, gg * 512:(gg + 1) * 512])
                            return ins
                        k.op("dve", fbn, r=[xd], w=[rsd])
                        k.op("dve", lambda e: e.bn_aggr(rs[:, 24:26], rs[:, 0:24]), r=[rsd], w=[rsd])
                        k.op("act", lambda e: e.activation(rs[:, 26:27], rs[:, 25:26], AF.Ln, bias=epsc[:, 0:1], scale=1.0), r=[rsd, d_c], w=[rsd])
                        k.op("act", lambda e: e.activation(rs[:, 26:27], rs[:, 26:27], AF.Exp, scale=-0.5), r=[rsd], w=[rsd])
                        k.op("dve", lambda e: e.tensor_scalar(xx[:], xx[:], rs[:, 24:25], rs[:, 26:27], ALU.subtract, ALU.mult),
                             r=[rsd, xd], w=[xd])
                        k.op("pool", lambda e: e.tensor_tensor(xx[:], xx[:], g_t[:], ALU.mult), r=[xd, gdep], w=[xd])
                        k.op("pool", lambda e: e.tensor_tensor(xx[:], xx[:], b_t[:], ALU.add), r=[xd, gdep], w=[xd])
                    layer_norm(x1, x1d, rs, rsd, l1g, l1b, d_wo)
                    k.dma("sp", s_h1[ti * 128:(ti + 1) * 128, :], x1[:], r=[x1d], w=[d_h1])
                    hb2, hb2d = hbr2.next()
                    k.op("act", lambda e, hb2=hb2, x1=x1: e.copy(hb2[:], x1[:]), r=[x1d], w=[hb2d])
                    hT, hTd = hTr.next()
                    for g in range(4):
                        bk, bd = pf.next()

                        def f(pe, bk=bk, x1=x1, g=g):
                            for j in range(4):
                                c = g * 4 + j
                                ins = pe.transpose(bk[:, j * 128:(j + 1) * 128], x1[:, c * 128:(c + 1) * 128], identf[:])
                            return ins
                        k.op("pe", f, r=[x1d, d_c], w=[bd])
                        k.op("act", lambda e, hT=hT, bk=bk, g=g: e.copy(
                            hT[:, g * 4:(g + 1) * 4, :], bk[:, :].rearrange("p (j n) -> p j n", j=4)), r=[bd], w=[hTd])
                    bk, bd = pf.next()

                    def f(pe, bk=bk, hT=hT):
                        for c in range(DC):
                            pe.matmul(bk[:, 0:36], hT[:, c, :], wr[:, c, :], start=(c == 0), stop=False)
                        return pe.matmul(bk[:, 0:36], onesf[0:1, :], br[0:1, :], start=False, stop=True)
                    k.op("pe", f, r=[hTd, d_wo, d_c], w=[bd])
                    V = "dve"
                    lg = rs[:, 32:68]
                    k.op(V, lambda e, bk=bk, lg=lg: e.tensor_copy(lg, bk[:, 0:36]), r=[bd], w=[rsd])
                    g4 = rs[:, 32:36]; e32 = rs[:, 36:68]
                    gmx = rs[:, 68:69]; ngm = rs[:, 69:70]; ohg = rs[:, 70:74]; eg = rs[:, 74:78]; sg = rs[:, 78:79]; gp = rs[:, 79:80]
                    pen = rs[:, 80:84]; msk = rs[:, 84:116]; top8 = rs[:, 116:124]; oh1 = rs[:, 124:156]; oh2 = rs[:, 156:188]
                    dd = rs[:, 188:189]; p1 = rs[:, 189:190]; p2 = rs[:, 190:191]; tmp = rs[:, 192:224]
                    pk = rs[:, 224:225]; ek = rs[:, 225:226]; okk = rs[:, 226:227]; dsf = rs[:, 227:228]; posg = rs[:, 228:260 - 4]
                    k.op(V, lambda e: e.reduce_max(gmx, g4, AX.X), r=[rsd], w=[rsd])
                    k.op(V, lambda e: e.tensor_scalar_mul(ngm, gmx, -1.0), r=[rsd], w=[rsd])
                    k.op(V, lambda e: e.tensor_scalar(ohg, g4, gmx, None, ALU.is_equal), r=[rsd], w=[rsd])
                    k.op("act", lambda e: e.activation(eg, g4, AF.Exp, bias=ngm, scale=1.0), r=[rsd], w=[rsd])
                    k.op(V, lambda e: e.reduce_sum(sg, eg, AX.X), r=[rsd], w=[rsd])
                    k.op(V, lambda e: e.reciprocal(gp, sg), r=[rsd], w=[rsd])
                    k.op(V, lambda e: e.tensor_scalar(pen, ohg, BIG, -BIG, ALU.mult, ALU.add), r=[rsd], w=[rsd])
                    k.op(V, lambda e: e.tensor_tensor(msk.rearrange("p (g j) -> p g j", g=4), e32.rearrange("p (g j) -> p g j", g=4),
                                                      pen.unsqueeze(2).to_broadcast([128, 4, 8]), ALU.add), r=[rsd], w=[rsd])
                    k.op(V, lambda e: e.max(top8, msk), r=[rsd], w=[rsd])
                    k.op(V, lambda e: e.tensor_scalar(oh1, msk, top8[:, 0:1], None, ALU.is_equal), r=[rsd], w=[rsd])
                    k.op(V, lambda e: e.tensor_scalar(oh2, msk, top8[:, 1:2], None, ALU.is_equal), r=[rsd], w=[rsd])
                    k.op(V, lambda e: e.tensor_sub(dd, top8[:, 0:1], top8[:, 1:2]), r=[rsd], w=[rsd])
                    k.op("act", lambda e: e.activation(p1, dd, AF.Sigmoid), r=[rsd], w=[rsd])
                    k.op("act", lambda e: e.activation(p2, dd, AF.Sigmoid, scale=-1.0), r=[rsd], w=[rsd])
                    k.op(V, lambda e, ti=ti: e.tensor_tensor(wtab[:, ti, 0:1], p1, gp, ALU.mult), r=[rsd], w=[d_tab])
                    k.op(V, lambda e, ti=ti: e.tensor_tensor(wtab[:, ti, 1:2], p2, gp, ALU.mult), r=[rsd], w=[d_tab])
                    mk, mkd = mkr.next()
                    k.op(V, lambda e, mk=mk: e.tensor_tensor(mk[:], oh1, oh2, ALU.add), r=[rsd], w=[mkd])
                    bk2, bd2 = pf.next()

                    def f(pe, bk2=bk2, mk=mk):
                        pe.matmul(bk2[:, 0:32], ustr[:], mk[:], start=True, stop=True)
                        return pe.matmul(bk2[:, 32:64], onesb[:], mk[:], start=True, stop=True)
                    k.op("pe", f, r=[mkd, d_c], w=[bd2])
                    posg = rs[:, 224:256]
                    pk = rs[:, 28:29]; ek = rs[:, 29:30]; okk = rs[:, 30:31]; dsf = rs[:, 31:32]
                    with k.atomic():
                        k.op(V, lambda e, bk2=bk2: e.tensor_tensor(posg, bk2[:, 0:32], cnt[:], ALU.add), r=[bd2, d_cnt, rsd], w=[rsd])
                        k.op(V, lambda e, bk2=bk2: e.tensor_tensor(cnt[:], cnt[:], bk2[:, 32:64], ALU.add), r=[bd2, rsd], w=[d_cnt])
                    for kk, oh in ((0, oh1), (1, oh2)):
                        k.op(V, lambda e, oh=oh: e.tensor_tensor(tmp, oh, posg, ALU.mult), r=[rsd], w=[rsd])
                        k.op(V, lambda e: e.reduce_sum(pk, tmp, AX.X), r=[rsd], w=[rsd])
                        k.op(V, lambda e, oh=oh: e.tensor_tensor(tmp, oh, ecap[:], ALU.mult), r=[rsd, d_c], w=[rsd])
                        k.op(V, lambda e: e.reduce_sum(ek, tmp, AX.X), r=[rsd], w=[rsd])
                        k.op(V, lambda e: e.tensor_scalar(okk, pk, float(CAP), None, ALU.is_lt), r=[rsd], w=[rsd])
                        k.op(V, lambda e: e.tensor_tensor(dsf, ek, pk, ALU.add), r=[rsd], w=[rsd])
                        k.op(V, lambda e: e.tensor_scalar_add(dsf, dsf, -float(TRASH)), r=[rsd], w=[rsd])
                        k.op(V, lambda e: e.tensor_tensor(dsf, dsf, okk, ALU.mult), r=[rsd], w=[rsd])
                        k.op(V, lambda e: e.tensor_scalar_add(dsf, dsf, float(TRASH)), r=[rsd], w=[rsd])
                        k.op(V, lambda e, ti=ti, kk=kk: e.tensor_copy(dtab[:, ti, kk:kk + 1], dsf), r=[rsd], w=[d_tab])
                        k.dma("pool", s_Xg, hb2[:], r=[hb2d, d_tab, d_Xg], w=[d_Xg],
                              indirect=dict(out_offset=bass.IndirectOffsetOnAxis(ap=dtab[:, ti, kk:kk + 1], axis=0), in_offset=None))
                k.interleave([(lambda ti=ti: e2_tile(ti)) for ti in range(NTO)], width=NIL)

            if dbg:
                k.dma("sp", s_dtab, dtab[:].rearrange("p a b -> p (a b)"), r=[d_tab], w=[Dep()])
                k.dma("sp", s_wtab, wtab[:].rearrange("p a b -> p (a b)"), r=[d_tab], w=[Dep()])
            if stop <= 6:
                raise _Stop()
            NCT = CAP // 128
            with k.scope():
                wgr = Ring([(k.sb("wg", [128, DC, 512], BF16), Dep()) for _ in range(3)])
                wur = Ring([(k.sb("wu", [128, DC, 512], BF16), Dep()) for _ in range(3)])
                wdr = Ring([(k.sb("wd", [128, 4, D], BF16), Dep()) for _ in range(3)])
                Xer = Ring([(k.sb("Xe", [128, NCT, D], BF16), Dep()) for _ in range(2)])
                XTr = Ring([(k.sb("XT", [128, DC, CAP], BF16), Dep()) for _ in range(2)])
                ATr = Ring([(k.sb("AT", [128, 4, CAP], BF16), Dep()) for _ in range(2)])
                sgr = Ring([(k.sb("sgt", [128, CAP], F32), Dep()) for _ in range(2)])
                Ysr = Ring([(k.sb("Ys", [128, D], BF16), Dep()) for _ in range(2)])
                def expert_fn(e_):
                    wg, wgd = wgr.next(); wu, wud = wur.next(); wd, wdd = wdr.next()
                    k.dma("pool", wg[:], w_gate[e_].rearrange("(c p) n -> p c n", p=128), w=[wgd])
                    k.dma("pool", wu[:], w_up[e_].rearrange("(c p) n -> p c n", p=128), w=[wud])
                    k.dma("pool", wd[:], w_down[e_].rearrange("(c p) n -> p c n", p=128), w=[wdd])
                    Xe, Xed = Xer.next(); XT, XTd = XTr.next(); AT, ATd = ATr.next()
                    k.dma("sp", Xe[:], s_Xg[e_ * CAP:(e_ + 1) * CAP, :].rearrange("(t p) d -> p t d", p=128), r=[d_Xg], w=[Xed])
                    for t in range(NCT):
                        for g in range(4):
                            pt, pd = pb.next()

                            def f(pe, pt=pt, Xe=Xe, t=t, g=g):
                                for j in range(4):
                                    c = g * 4 + j
                                    ins = pe.transpose(pt[:, j * 128:(j + 1) * 128], Xe[:, t, c * 128:(c + 1) * 128], identb[:])
                                return ins
                            k.op("pe", f, r=[Xed, d_c], w=[pd])
                            eng = "dve" if (g % 2 == 0) else "act"
                            if eng == "dve":
                                k.op("dve", lambda e, XT=XT, pt=pt, g=g, t=t: e.tensor_copy(
                                    XT[:, g * 4:(g + 1) * 4, t * 128:(t + 1) * 128], pt[:, 0:512].rearrange("p (j n) -> p j n", j=4)),
                                    r=[pd], w=[XTd])
                            else:
                                k.op("act", lambda e, XT=XT, pt=pt, g=g, t=t: e.copy(
                                    XT[:, g * 4:(g + 1) * 4, t * 128:(t + 1) * 128], pt[:, 0:512].rearrange("p (j n) -> p j n", j=4)),
                                    r=[pd], w=[XTd])
                    for fcx in range(4):
                        bG, bGd = pf.next(); bU, bUd = pf.next()

                        def f(pe, bG=bG, wg=wg, XT=XT, fcx=fcx):
                            for c in range(DC):
                                ins = pe.matmul(bG[:, 0:CAP], wg[:, c, fcx * 128:(fcx + 1) * 128], XT[:, c, :], start=(c == 0), stop=(c == DC - 1))
                            return ins
                        k.op("pe", f, r=[wgd, XTd], w=[bGd])

                        def f(pe, bU=bU, wu=wu, XT=XT, fcx=fcx):
                            for c in range(DC):
                                ins = pe.matmul(bU[:, 0:CAP], wu[:, c, fcx * 128:(fcx + 1) * 128], XT[:, c, :], start=(c == 0), stop=(c == DC - 1))
                            return ins
                        k.op("pe", f, r=[wud, XTd], w=[bUd])
                        sg_t, sg_d = sgr.next()
                        k.op("act", lambda e, sg_t=sg_t, bG=bG: e.activation(sg_t[:], bG[:, 0:CAP], AF.Silu), r=[bGd], w=[sg_d])
                        k.op("dve", lambda e, AT=AT, sg_t=sg_t, bU=bU, fcx=fcx: e.tensor_tensor(AT[:, fcx, :], sg_t[:], bU[:, 0:CAP], ALU.mult),
                             r=[sg_d, bUd], w=[ATd])
                    for t in range(NCT):
                        Ys, Ysd = Ysr.next()
                        for db in range(4):
                            bk, bd = pf.next()

                            def f(pe, bk=bk, AT=AT, wd=wd, t=t, db=db):
                                for fcx in range(4):
                                    ins = pe.matmul(bk[:, :], AT[:, fcx, t * 128:(t + 1) * 128], wd[:, fcx, db * 512:(db + 1) * 512],
                                                    start=(fcx == 0), stop=(fcx == 3))
                                return ins
                            k.op("pe", f, r=[ATd, wdd], w=[bd])
                            if db % 2 == 0:
                                k.op("act", lambda e, Ys=Ys, bk=bk, db=db: e.copy(Ys[:, db * 512:(db + 1) * 512], bk[:, :]), r=[bd], w=[Ysd])
                            else:
                                k.op("dve", lambda e, Ys=Ys, bk=bk, db=db: e.tensor_copy(Ys[:, db * 512:(db + 1) * 512], bk[:, :]), r=[bd], w=[Ysd])
                        r0 = e_ * CAP + t * 128
                        k.dma("sp", s_Yg[r0:r0 + 128, :], Ys[:], r=[Ysd], w=[d_Yg])
                k.interleave([(lambda e_=e_: expert_fn(e_)) for e_ in range(NE)], width=NIL)

            if stop <= 7:
                raise _Stop()
            d_out = Dep()
            with k.scope():
                l2g = k.sb("l2g", [128, D], F32); l2b = k.sb("l2b", [128, D], F32)
                d_l2 = Dep()
                k.dma("sp", l2g[:], ln2g_d.partition_broadcast(128), w=[d_l2])
                k.dma("sp", l2b[:], ln2b_d.partition_broadcast(128), w=[d_l2])
                h1r = Ring([(k.sb("h1", [128, D], F32), Dep()) for _ in range(3)])
                y1r = Ring([(k.sb("y1", [128, D], BF16), Dep()) for _ in range(3)])
                y2r = Ring([(k.sb("y2", [128, D], BF16), Dep()) for _ in range(3)])
                rsr = Ring([(k.sb("rs2", [128, 32], F32), Dep()) for _ in range(3)])
                def g_tile(ti):
                    h1, h1d = h1r.next(); y1, y1d = y1r.next(); y2, y2d = y2r.next()
                    k.dma("sp", h1[:], s_h1[ti * 128:(ti + 1) * 128, :], r=[d_h1], w=[h1d])
                    k.dma("pool", y1[:], s_Yg, r=[d_Yg, d_tab], w=[y1d],
                          indirect=dict(out_offset=None, in_offset=bass.IndirectOffsetOnAxis(ap=dtab[:, ti, 0:1], axis=0)))
                    k.dma("pool", y2[:], s_Yg, r=[d_Yg, d_tab], w=[y2d],
                          indirect=dict(out_offset=None, in_offset=bass.IndirectOffsetOnAxis(ap=dtab[:, ti, 1:2], axis=0)))
                    k.op("act", lambda e, h1=h1: e.mul(h1[:], h1[:], ALPHA), r=[h1d], w=[h1d])
                    k.op("dve", lambda e, h1=h1, y1=y1, ti=ti: e.scalar_tensor_tensor(
                        h1[:], y1[:], wtab[:, ti, 0:1], h1[:], ALU.mult, ALU.add), r=[y1d, d_tab, h1d], w=[h1d])
                    k.op("dve", lambda e, h1=h1, y2=y2, ti=ti: e.scalar_tensor_tensor(
                        h1[:], y2[:], wtab[:, ti, 1:2], h1[:], ALU.mult, ALU.add), r=[y2d, d_tab, h1d], w=[h1d])
                    rs, rsd = rsr.next()
                    st = rs[:, 0:24].rearrange("p (g k) -> p g k", g=4)
                    def fbn(e, rs=rs, h1=h1):
                        for gg in range(4):
                            ins = e.bn_stats(rs[:, 6 * gg:6 * gg + 6], h1[:, gg * 512:(gg + 1) * 512])
                        return ins
                    k.op("dve", fbn, r=[h1d], w=[rsd])
                    k.op("dve", lambda e, rs=rs: e.bn_aggr(rs[:, 24:26], rs[:, 0:24]), r=[rsd], w=[rsd])
                    k.op("act", lambda e, rs=rs: e.activation(rs[:, 26:27], rs[:, 25:26], AF.Ln, bias=epsc[:, 0:1], scale=1.0), r=[rsd, d_c], w=[rsd])
                    k.op("act", lambda e, rs=rs: e.activation(rs[:, 26:27], rs[:, 26:27], AF.Exp, scale=-0.5), r=[rsd], w=[rsd])
                    k.op("dve", lambda e, rs=rs, h1=h1: e.tensor_scalar(h1[:], h1[:], rs[:, 24:25], rs[:, 26:27], ALU.subtract, ALU.mult),
                         r=[rsd, h1d], w=[h1d])
                    k.op("pool", lambda e, h1=h1: e.tensor_tensor(h1[:], h1[:], l2g[:], ALU.mult), r=[h1d, d_l2], w=[h1d])
                    k.op("pool", lambda e, h1=h1: e.tensor_tensor(h1[:], h1[:], l2b[:], ALU.add), r=[h1d, d_l2], w=[h1d])
                    k.dma("sp", out_d[ti * 128:(ti + 1) * 128, :], h1[:], r=[h1d], w=[d_out])
                k.interleave([(lambda ti=ti: g_tile(ti)) for ti in range(NTO)], width=3)
        except _Stop:
            gscope.close()
        k.barrier()
        build.stats = (k.n_ins, {n: e.count for n, e in k.E.items()})
    return nc


def _consts(TP, TO, CAP):
    TA = TP + TO
    c = {}
    c["ident"] = np.eye(128, dtype=np.float32)
    s = np.arange(64)
    c["mask64"] = (s[:, None] <= s[None, :]).astype(np.float32)
    p = np.arange(128)
    c["ustrict"] = (p[:, None] < p[None, :]).astype(np.float32)
    slopes = 2.0 ** (-8.0 * np.arange(1, 9) / 8.0)
    kl = p[:, None]; ql = p[None, :]
    vis = (kl // 64) <= (ql // 64)
    dg = np.zeros((128, 8, 128), np.float32)
    for h in range(8):
        b = np.where(kl <= ql, slopes[h] * kl, slopes[h] * (2 * ql - kl))
        dg[:, h, :] = np.where(vis, b, -BIG) * 8.0
    c["diagbias"] = dg
    ab = np.zeros((128, 8, 32), np.float32)
    for h in range(8):
        for dlt in range(32):
            ab[:, h, dlt] = np.maximum(slopes[h] * (p - 128 * dlt), -ACLAMP)
    c["alibi"] = ab
    c["ecap"] = np.tile((np.arange(32) * CAP).astype(np.float32)[None, :], (128, 1))
    return c


def _prep_shared(inp):
    f = lambda a: np.ascontiguousarray(np.asarray(a, dtype=np.float32))
    b_in = f(inp["b_in"][0])
    sh = {}
    sh["w_in"] = f(inp["w_in"][0])

    def fm_bias(b):
        cols = np.concatenate([b[O_QA:O_QA + 1024], b[O_KA:O_KA + 1024], b[O_QB:O_QB + 1024], b[O_KB:O_KB + 1024],
                               b[O_GA:O_GA + 2048], b[O_GB:O_GB + 2048]])
        return np.ascontiguousarray(cols.reshape(64, 128).T)

    def tm_bias(b):
        return np.ascontiguousarray(np.concatenate([b[O_VA:O_VA + 1024], b[O_OA:O_OA + 1024], b[O_VB:O_VB + 1024]])[None, :])

    def if_bias(b):
        return np.ascontiguousarray(np.stack([b[O_IA:O_IA + 4], b[O_FA:O_FA + 4]], axis=1))
    b_mask = np.zeros_like(b_in)
    b_mask[O_IA:O_IA + 4] = -BIG
    b_mask[O_FA:O_FA + 4] = BIG
    sh["bias_real"] = (fm_bias(b_in), tm_bias(b_in), if_bias(b_in))
    sh["bias_mask"] = (fm_bias(b_mask), tm_bias(b_mask), if_bias(b_mask))
    cw = f(inp["conv_w"][0])
    sh["cw"] = np.ascontiguousarray(cw.T.reshape(16, 128, 4).transpose(1, 0, 2))
    sh["cb"] = np.ascontiguousarray(f(inp["conv_b"][0]).reshape(16, 128).T)
    sh["mlstm_norm_g"] = f(inp["mlstm_norm_g"][0])
    sh["diff_norm_g"] = f(inp["diff_norm_g"][0])
    sh["lamv"] = np.ascontiguousarray(np.stack([f(inp["lambda_q1"][0]), f(inp["lambda_k1"][0]),
                                                f(inp["lambda_q2"][0]), f(inp["lambda_k2"][0])]))
    for n in ("w_a", "w_b", "w_out", "ln1_g", "ln1_b", "ln2_g", "ln2_b", "w_gate", "w_up", "w_down"):
        sh[n] = f(inp[n][0])
    sh["w_r"] = np.ascontiguousarray(np.concatenate([f(inp["w_grp"][0]), f(inp["w_exp"][0])], axis=1))
    sh["b_r"] = np.ascontiguousarray(np.concatenate([f(inp["b_grp"][0]), f(inp["b_exp"][0])])[None, :])
    return sh


def make_in_maps(inp, TP, TO, CAP):
    x = np.asarray(inp["x"], dtype=np.float32)
    B, S, _ = x.shape
    assert S == TP + TO or S == 2 * TO
    sh = _prep_shared(inp)
    cs = _consts(TP, TO, CAP)
    TA = TP + TO
    maps = []
    for b in range(B):
        for half in range(2):
            m = {}
            if half == 0:
                xa = np.zeros((TA, D), np.float32)
                xa[TP:] = x[b, 0:TO]
                valid = np.concatenate([np.zeros(TP, np.float32), np.ones(TO, np.float32)])
                pre = sh["bias_mask"]
            else:
                xa = np.ascontiguousarray(x[b, TO - TP:2 * TO])
                valid = np.ones(TA, np.float32)
                pre = sh["bias_real"]
            m["x"] = xa
            m["valid_tm"] = np.ascontiguousarray(valid.reshape(TA // 128, 128).T)
            m["bfm"], m["btm"], m["bif"] = sh["bias_real"]
            m["bfm_pre"], m["btm_pre"], m["bif_pre"] = pre
            for n in ("w_in", "cw", "cb", "mlstm_norm_g", "diff_norm_g", "lamv", "w_a", "w_b", "w_out", "ln1_g", "ln1_b",
                      "ln2_g", "ln2_b", "w_gate", "w_up", "w_down", "w_r", "b_r"):
                m[n] = sh[n]
            m.update(cs)
            maps.append(m)
    return maps


_TP, _TO, _CAP = 2048, 2048, 256


def kernel(**inputs):
    x = np.asarray(inputs["x"])
    B, S, _ = x.shape
    maps = make_in_maps(inputs, _TP, _TO, _CAP)
    nc = build(_TP, _TO, _CAP)
    res = run_bass_kernel_spmd(nc, maps, core_ids=list(range(len(maps))))
    out = np.zeros((B, S, D), np.float32)
    i = 0
    for b in range(B):
        for half in range(2):
            out[b, half * _TO:(half + 1) * _TO] = res.results[i]["out"]
            i += 1
    return out
```

```python
import math
from contextlib import ExitStack, contextmanager
import numpy as np
import concourse.bass as bass
import concourse.mybir as mybir
from concourse.bass_utils import run_bass_kernel_spmd

F32 = mybir.dt.float32
BF16 = mybir.dt.bfloat16
I32 = mybir.dt.int32
AF = mybir.ActivationFunctionType
ALU = mybir.AluOpType
AX = mybir.AxisListType

D = 2048
DC = 16
NIN = 11272
O_QA, O_KA, O_VA, O_OA, O_IA, O_FA, O_QB, O_KB, O_VB, O_GA, O_GB = (
    0, 1024, 2048, 3072, 4096, 4100, 4104, 5128, 6152, 7176, 9224)
ALPHA = 2.0 ** 0.25
EPS = 1e-5
NE = 32
BIG = 30000.0
LN16 = math.log(16.0)
ACLAMP = 40.0
import os
ILV = int(os.environ.get('ILV', '1'))
EXPV = int(os.environ.get('EXPV', '0'))
NIL = int(os.environ.get('NIL', '2'))
SLOPES = [2.0 ** (-(h + 1)) for h in range(8)]


class _Stop(Exception):
    pass


class Dep:
    __slots__ = ("w", "rs")

    def __init__(self):
        self.w = None
        self.rs = {}


class _Eng:
    def __init__(self, name, eng, sem):
        self.name, self.eng, self.sem = name, eng, sem
        self.count = 0
        self.waited = {}


class _DmaSem:
    def __init__(self, key, sem):
        self.key, self.sem, self.total = key, sem, 0


class Kern:
    def __init__(self, nc, stack, n_dma_sems=96):
        self.nc = nc
        self.stack = stack
        self.E = {}
        for name, eng in (("pe", nc.tensor), ("act", nc.scalar), ("dve", nc.vector),
                          ("pool", nc.gpsimd), ("sp", nc.sync)):
            sem = stack.enter_context(nc.semaphore("s_" + name))
            self.E[name] = _Eng(name, eng, sem)
        self.dsems = [_DmaSem("d%d" % i, stack.enter_context(nc.semaphore("d%d" % i)))
                      for i in range(n_dma_sems)]
        self.dpool = {"hw": self.dsems[0:n_dma_sems // 2], "sw": self.dsems[n_dma_sems // 2:]}
        self.drr = {"hw": 0, "sw": 0}
        self.n_ins = 0
        self.uid = 0
        self._cur = None
        self._atomic = 0

    def sb(self, name, shape, dt):
        self.uid += 1
        return self.stack.enter_context(self.nc.sbuf_tensor("%s_%d" % (name, self.uid), list(shape), dt))

    def ps(self, name, shape, dt):
        return self.stack.enter_context(self.nc.psum_tensor(name, list(shape), dt))

    @contextmanager
    def scope(self):
        old = self.stack
        stopped = False
        with ExitStack() as st:
            self.stack = st
            try:
                yield
            except _Stop:
                stopped = True
            if not stopped:
                self.barrier()
        self.stack = old
        if stopped:
            raise _Stop()

    def _wait(self, es, ev):
        key, sem, val = ev
        if es.name == "pe" and key == "pe":
            return
        if es.waited.get(key, 0) >= val:
            return
        es.eng.wait_ge(sem, val)
        es.waited[key] = val

    def _pre(self, es, r, w):
        for d in r:
            if d.w is not None:
                self._wait(es, d.w)
        for d in w:
            if d.w is not None:
                self._wait(es, d.w)
            for ev in d.rs.values():
                self._wait(es, ev)

    def _post(self, ev, r, w):
        for d in r:
            d.rs[ev[0]] = ev
        for d in w:
            d.w = ev
            d.rs = {}

    def op(self, en, fn, r=(), w=()):
        es = self.E[en]
        self._pre(es, r, w)
        ins = fn(es.eng)
        es.count += 1
        ins.then_inc(es.sem, 1)
        self.n_ins += 1
        ev = (es.name, es.sem, es.count)
        self._post(ev, r, w)
        self._yield()
        return ev

    def dma(self, qn, out, in_, r=(), w=(), indirect=None, **kw):
        es = self.E[qn]
        self._pre(es, r, w)
        kind = "sw" if qn == "pool" else "hw"
        pool = self.dpool[kind]
        ds = pool[self.drr[kind]]
        self.drr[kind] = (self.drr[kind] + 1) % len(pool)
        if ds.total > 0:
            self._wait(es, (ds.key, ds.sem, ds.total))
        if indirect is not None:
            ins = es.eng.indirect_dma_start(out=out, in_=in_, **indirect)
        else:
            ins = es.eng.dma_start(out=out, in_=in_, **kw)
        ds.total += 16
        ins.then_inc(ds.sem, 16)
        self.n_ins += 1
        ev = (ds.key, ds.sem, ds.total)
        self._post(ev, r, w)
        self._yield()
        return ev

    def _yield(self):
        w = self._cur
        if w is None or self._atomic > 0:
            return
        self._sched_sem.release()
        w["go"].acquire()

    def interleave(self, fns, width=2):
        import threading
        if width <= 1 or len(fns) <= 1:
            for fn in fns:
                fn()
            return
        self._sched_sem = threading.Semaphore(0)
        pending = list(fns)
        active = []
        err = []

        def runner(w, fn):
            w["go"].acquire()
            try:
                fn()
            except BaseException as e:
                err.append(e)
            w["done"] = True
            self._sched_sem.release()
        while pending or active:
            while pending and len(active) < width:
                w = {"go": threading.Semaphore(0), "done": False}
                w["t"] = threading.Thread(target=runner, args=(w, pending.pop(0)), daemon=True)
                w["t"].start()
                active.append(w)
            for w in list(active):
                self._cur = w
                w["go"].release()
                self._sched_sem.acquire()
                self._cur = None
                if w["done"]:
                    active.remove(w)
                if err:
                    raise err[0]

    @contextmanager
    def atomic(self):
        self._atomic += 1
        try:
            yield
        finally:
            self._atomic -= 1

    def barrier(self):
        evs = [(e.name, e.sem, e.count) for e in self.E.values() if e.count > 0]
        evs += [(d.key, d.sem, d.total) for d in self.dsems if d.total > 0]
        for es in self.E.values():
            for ev in evs:
                if ev[0] == es.name and es.name == "pe":
                    continue
                self._wait(es, ev)


class Ring:
    def __init__(self, items):
        self.items = items
        self.i = 0

    def next(self):
        it = self.items[self.i]
        self.i = (self.i + 1) % len(self.items)
        return it


def build(TP, TO, CAP, dbg=False, stop=99):
    TA = TP + TO
    NTA, NTO, NTP = TA // 128, TO // 128, TP // 128
    NCH, CH0 = TA // 64, TP // 64
    NROW = NE * CAP + 128
    TRASH = NE * CAP
    nc = bass.Bass("TRN2", target_bir_lowering=False)

    def din(name, shape, dt=F32):
        return nc.dram_tensor(name, list(shape), dt, kind="ExternalInput").ap()

    x = din("x", [TA, D])
    w_in = din("w_in", [D, NIN])
    bfm_d = din("bfm", [128, 64]); bfmp_d = din("bfm_pre", [128, 64])
    btm_d = din("btm", [1, 3072]); btmp_d = din("btm_pre", [1, 3072])
    bif_d = din("bif", [4, 2]); bifp_d = din("bif_pre", [4, 2])
    valid_d = din("valid_tm", [128, NTA])
    cw_d = din("cw", [128, 16, 4]); cb_d = din("cb", [128, 16])
    ng_d = din("mlstm_norm_g", [1024]); dg_d = din("diff_norm_g", [128])
    lam_d = din("lamv", [4, 64])
    w_a = din("w_a", [1024, D]); w_b = din("w_b", [1024, D]); w_out = din("w_out", [D, D])
    ln1g_d = din("ln1_g", [D]); ln1b_d = din("ln1_b", [D]); ln2g_d = din("ln2_g", [D]); ln2b_d = din("ln2_b", [D])
    wr_d = din("w_r", [D, 36]); br_d = din("b_r", [1, 36])
    if stop > 6:
        w_gate = din("w_gate", [NE, D, 512]); w_up = din("w_up", [NE, D, 512]); w_down = din("w_down", [NE, 512, D])
    ident_d = din("ident", [128, 128]); mask64_d = din("mask64", [64, 64]); ustrict_d = din("ustrict", [128, 128])
    dgb_d = din("diagbias", [128, 8, 128]); abt_d = din("alibi", [128, 8, 32]); ecap_d = din("ecap", [128, 32])
    out_d = nc.dram_tensor("out", [TO, D], F32, kind="ExternalOutput").ap()

    def dscr(name, shape, dt):
        if dbg:
            return nc.dram_tensor(name, list(shape), dt, kind="ExternalOutput").ap()
        return nc.dram_tensor(name, list(shape), dt).ap()

    s_qkaT = dscr("s_qkaT", [2048, TA], BF16)
    s_qkbT = dscr("s_qkbT", [2048, TA], BF16)
    s_gT = dscr("s_gT", [4096, TO], BF16)
    s_va = dscr("s_va", [TA, 1024], BF16)
    s_vb = dscr("s_vb", [TA, 1024], BF16)
    s_oa = dscr("s_oa", [TO, 1024], BF16)
    s_seq = dscr("s_seq", [3, 4, TA], F32)
    s_dec = dscr("s_dec", [4, NCH], F32)
    s_h1 = dscr("s_h1", [TO, D], F32)
    s_Xg = dscr("s_Xg", [NROW, D], BF16)
    s_Yg = dscr("s_Yg", [NROW, D], BF16)
    s_haT = dscr("s_haT", [1024, TO], BF16)
    s_obT = dscr("s_obT", [1024, TO], BF16)
    s_mgT = dscr("s_mgT", [2048, TO], BF16)
    HB = min(512, TO)
    if dbg:
        s_TT = dscr("s_TT", [64, 3 * NCH * 4], F32)
        s_dtab = dscr("s_dtab", [128, NTO * 2], I32)
        s_wtab = dscr("s_wtab", [128, NTO * 2], F32)

    with ExitStack() as st0:
        k = Kern(nc, st0)
        try:
            banks = [(k.ps("pf%d" % i, [128, 512], F32), Dep()) for i in range(8)]
            pf = Ring(banks[0:6])
            pb = Ring([(banks[i][0].bitcast(BF16), banks[i][1]) for i in (6, 7)])
            d_c = Dep()
            identf = k.sb("identf", [128, 128], F32)
            identb = k.sb("identb", [128, 128], BF16)
            onesb = k.sb("onesb", [128, 128], BF16)
            onesf = k.sb("onesf", [128, 128], F32)
            mask64 = k.sb("mask64", [64, 64], F32)
            ustr = k.sb("ustr", [128, 128], BF16)
            dgb = k.sb("dgb", [128, 8, 128], BF16)
            abt = k.sb("abt", [128, 8, 32], F32)
            ecap = k.sb("ecap", [128, 32], F32)
            validb = k.sb("validb", [128, NTA], BF16)
            zero1 = k.sb("zero1", [128, 1], F32)
            epsc = k.sb("epsc", [128, 1], F32)
            k.dma("sp", identf[:], ident_d, w=[d_c])
            k.dma("pool", identb[:], ident_d, w=[d_c])
            k.dma("sp", mask64[:], mask64_d, w=[d_c])
            k.dma("pool", ustr[:], ustrict_d, w=[d_c])
            k.dma("pool", dgb[:], dgb_d, w=[d_c])
            k.dma("sp", abt[:], abt_d, w=[d_c])
            k.dma("sp", ecap[:], ecap_d, w=[d_c])
            k.dma("pool", validb[:], valid_d, w=[d_c])
            k.op("dve", lambda e: e.memset(onesb[:], 1.0), w=[d_c])
            k.op("dve", lambda e: e.memset(onesf[:], 1.0), w=[d_c])
            k.op("dve", lambda e: e.memset(zero1[:], 0.0), w=[d_c])
            k.op("dve", lambda e: e.memset(epsc[:], EPS), w=[d_c])
            d_zt, d_Xg, d_Yg = Dep(), Dep(), Dep()

            dtab = k.sb("dtab", [128, NTO, 2], I32)
            wtab = k.sb("wtab", [128, NTO, 2], F32)
            TT = k.sb("TT", [64, 3, NCH, 4], F32)
            decbc = k.sb("decbc", [128, 4 * NCH], F32)
            gscope = ExitStack()
            _old = k.stack
            k.stack = gscope
            Gi = k.sb("Gi", [4, TA], F32); Gf = k.sb("Gf", [4, TA], F32)
            k.stack = _old
            d_G = Dep()
            d_tab = Dep()
            d_h1 = Dep()
            d_haT, d_obT, d_mg = Dep(), Dep(), Dep()

            with k.scope():
                zt = k.sb("zt", [128, 4096], BF16)
                k.op("pool", lambda e: e.memset(zt[:], 0.0), w=[d_zt])
                r0 = 0
                while r0 < NROW:
                    nr = min(256, NROW - r0)
                    k.dma("sp", s_Xg[r0:r0 + nr, :].rearrange("(t p) d -> p t d", p=128),
                          zt[:, 0:(nr // 128) * D].rearrange("p (t d) -> p t d", d=D), r=[d_zt], w=[d_Xg])
                    r0 += nr
                k.dma("sp", s_Yg[TRASH:TRASH + 128, :], zt[:, 0:D], r=[d_zt], w=[d_Yg])
                TX = max(TP, TO)
                xT = k.sb("xT", [128, DC, TX], BF16)
                xb = Ring([(k.sb("xb", [128, D], BF16), Dep()) for _ in range(2)])
                wt = Ring([(k.sb("wt", [128, DC, 512], BF16), Dep()) for _ in range(2)])
                wif = k.sb("wif", [128, DC, 8], BF16)
                evf = Ring([(k.sb("evf", [128, 4, 512], BF16), Dep()) for _ in range(2)])
                evt = Ring([(k.sb("evt", [128, 512], BF16), Dep()) for _ in range(3)])
                bfm = k.sb("bfm", [128, 64], F32); bfmp = k.sb("bfmp", [128, 64], F32)
                btm = k.sb("btm", [1, 3072], BF16); btmp = k.sb("btmp", [1, 3072], BF16)
                bif = k.sb("bif", [4, 2], F32); bifp = k.sb("bifp", [4, 2], F32)
                d_b, d_wif = Dep(), Dep()
                k.dma("sp", bfm[:], bfm_d, w=[d_b]); k.dma("sp", bfmp[:], bfmp_d, w=[d_b])
                k.dma("pool", btm[:], btm_d, w=[d_b]); k.dma("pool", btmp[:], btmp_d, w=[d_b])
                k.dma("sp", bif[:], bif_d, w=[d_b]); k.dma("sp", bifp[:], bifp_d, w=[d_b])
                k.dma("pool", wif[:], w_in.rearrange("(c p) n -> p c n", p=128)[:, :, O_IA:O_IA + 8], w=[d_wif])
                d_scr = {"qka": Dep(), "qkb": Dep(), "g": Dep(), "va": Dep(), "vb": Dep(), "oa": Dep()}

                FM = []
                for j in range(2):
                    FM.append((O_QA + 512 * j, "qka", s_qkaT, 512 * j, AF.Identity, "q", 4 * j))
                    FM.append((O_KA + 512 * j, "qka", s_qkaT, 1024 + 512 * j, AF.Identity, "all", 8 + 4 * j))
                    FM.append((O_QB + 512 * j, "qkb", s_qkbT, 512 * j, AF.Identity, "own", 16 + 4 * j))
                    FM.append((O_KB + 512 * j, "qkb", s_qkbT, 1024 + 512 * j, AF.Identity, "all", 24 + 4 * j))
                for j in range(8):
                    FM.append((O_GA + 512 * j, "g", s_gT, 512 * j, AF.Sigmoid, "gate", 32 + 4 * j))
                TM = []
                for j in range(2):
                    TM.append((O_VA + 512 * j, "va", s_va, 512 * j, AF.Identity, "all", 512 * j))
                    TM.append((O_OA + 512 * j, "oa", s_oa, 512 * j, AF.Sigmoid, "own", 1024 + 512 * j))
                    TM.append((O_VB + 512 * j, "vb", s_vb, 512 * j, AF.Identity, "all", 2048 + 512 * j))

                for phase in ("pre", "own"):
                    t0, nt = (0, TP) if phase == "pre" else (TP, TO)
                    bfm_x, btm_x, bif_x = (bfmp, btmp, bifp) if phase == "pre" else (bfm, btm, bif)
                    d_xT = [Dep() for _ in range(nt // 128)]
                    for ti in range(nt // 128):
                        xb_t, xb_d = xb.next()
                        k.dma("pool", xb_t[:], x[t0 + ti * 128:t0 + (ti + 1) * 128, :], w=[xb_d])
                        for g in range(4):
                            pt, pd = pb.next()

                            def f(pe, g=g, pt=pt, xb_t=xb_t):
                                for j in range(4):
                                    c = g * 4 + j
                                    ins = pe.transpose(pt[:, j * 128:(j + 1) * 128], xb_t[:, c * 128:(c + 1) * 128], identb[:])
                                return ins
                            k.op("pe", f, r=[xb_d, d_c], w=[pd])
                            k.op("dve", lambda e, g=g, ti=ti, pt=pt: e.tensor_copy(
                                xT[:, g * 4:(g + 1) * 4, ti * 128:(ti + 1) * 128],
                                pt[:, 0:512].rearrange("p (j n) -> p j n", j=4)), r=[pd], w=[d_xT[ti]])
                    tb = 0
                    while tb < nt:
                        n = min(512, nt - tb)
                        dx = d_xT[tb // 128:(tb + n) // 128]
                        for gi, Gt in ((0, Gi), (1, Gf)):
                            bk, bd = pf.next()

                            def f(pe, gi=gi, bk=bk, tb=tb, n=n):
                                for c in range(DC):
                                    ins = pe.matmul(bk[0:4, 0:n], wif[:, c, gi * 4:(gi + 1) * 4], xT[:, c, tb:tb + n],
                                                    start=(c == 0), stop=(c == DC - 1))
                                return ins
                            k.op("pe", f, r=dx + [d_wif], w=[bd])
                            k.op("act", lambda e, gi=gi, Gt=Gt, bk=bk, tb=tb, n=n: e.activation(
                                Gt[0:4, t0 + tb:t0 + tb + n], bk[0:4, 0:n], AF.Identity, bias=bif_x[:, gi:gi + 1], scale=1.0),
                                r=[bd, d_b], w=[d_G])
                        tb += n
                    for (c0, dkey, dst, row0, func, which, fmc) in FM:
                        if phase == "pre":
                            if which in ("own", "gate"):
                                continue
                            tlo = (TP - 128) if which == "q" else 0
                        else:
                            tlo = 0
                        w_t, w_d = wt.next()
                        k.dma("pool", w_t[:], w_in.rearrange("(c p) n -> p c n", p=128)[:, :, c0:c0 + 512], w=[w_d])
                        tb = tlo
                        while tb < nt:
                            n = min(512, nt - tb)
                            dx = d_xT[tb // 128:(tb + n) // 128]
                            ev_t, ev_d = evf.next()
                            for g in range(4):
                                bk, bd = pf.next()

                                def f(pe, g=g, bk=bk, tb=tb, n=n, w_t=w_t):
                                    for c in range(DC):
                                        ins = pe.matmul(bk[:, 0:n], w_t[:, c, g * 128:(g + 1) * 128], xT[:, c, tb:tb + n],
                                                        start=(c == 0), stop=(c == DC - 1))
                                    return ins
                                k.op("pe", f, r=dx + [w_d], w=[bd])
                                k.op("act", lambda e, g=g, bk=bk, n=n, ev_t=ev_t, func=func, fmc=fmc: e.activation(
                                    ev_t[:, g, 0:n], bk[:, 0:n], func, bias=bfm_x[:, fmc + g:fmc + g + 1], scale=1.0),
                                    r=[bd, d_b], w=[ev_d])
                            tcol = (tb if which == "gate" else t0 + tb)
                            k.dma("sp", dst[row0:row0 + 512, tcol:tcol + n].rearrange("(g p) t -> p g t", p=128),
                                  ev_t[:, :, 0:n], r=[ev_d], w=[d_scr[dkey]])
                            tb += n
                    for (c0, dkey, dst, col0, func, which, bcol) in TM:
                        if phase == "pre" and which == "own":
                            continue
                        w_t, w_d = wt.next()
                        k.dma("pool", w_t[:], w_in.rearrange("(c p) n -> p c n", p=128)[:, :, c0:c0 + 512], w=[w_d])
                        for ti in range(nt // 128):
                            bk, bd = pf.next()

                            def f(pe, bk=bk, ti=ti, w_t=w_t, bcol=bcol):
                                for c in range(DC):
                                    pe.matmul(bk[:, :], xT[:, c, ti * 128:(ti + 1) * 128], w_t[:, c, :],
                                              start=(c == 0), stop=False)
                                return pe.matmul(bk[:, :], onesb[0:1, :], btm_x[0:1, bcol:bcol + 512], start=False, stop=True)
                            k.op("pe", f, r=[d_xT[ti], w_d, d_b, d_c], w=[bd])
                            e_t, e_d = evt.next()
                            k.op("act", lambda e, bk=bk, e_t=e_t, func=func: e.activation(e_t[:], bk[:, :], func),
                                 r=[bd], w=[e_d])
                            trow = (ti * 128 if which == "own" else t0 + ti * 128)
                            k.dma("sp", dst[trow:trow + 128, col0:col0 + 512], e_t[:], r=[e_d], w=[d_scr[dkey]])

            if True:
                if stop <= 1:
                    raise _Stop()
                with k.scope():
                    t1 = k.sb("t1", [4, TA], F32); t2 = k.sb("t2", [4, TA], F32)
                    mt = k.sb("mt", [4, NCH], F32); dec = k.sb("dec", [4, NCH], F32)
                    sq = k.sb("sq", [4, 3, TA], F32)
                    dq = Dep()
                    V = "dve"
                    k.op("act", lambda e: e.activation(t1[:], Gf[:], AF.Abs), r=[d_G], w=[dq])
                    k.op("act", lambda e: e.activation(t1[:], t1[:], AF.Exp, scale=-1.0), r=[dq], w=[dq])
                    k.op("act", lambda e: e.activation(t1[:], t1[:], AF.Ln, bias=1.0, scale=1.0), r=[dq], w=[dq])
                    k.op(V, lambda e: e.tensor_scalar_min(t2[:], Gf[:], 0.0), r=[d_G], w=[dq])
                    k.op(V, lambda e: e.tensor_sub(t2[:], t2[:], t1[:]), r=[dq], w=[dq])
                    k.op(V, lambda e: e.tensor_scalar_mul(t2[:], t2[:], 0.5), r=[dq], w=[dq])
                    k.op(V, lambda e: e.tensor_tensor_scan(t1[:], t2[:], t2[:], 0.0, ALU.add, ALU.add), r=[dq], w=[dq])
                    Bc = t1
                    k.op(V, lambda e: e.tensor_sub(Gi[:], Gi[:], Bc[:]), r=[dq, d_G], w=[dq, d_G])
                    at = Gi
                    k.op(V, lambda e: e.tensor_tensor_scan(t2[:], at[:], at[:], 0.0, ALU.max, ALU.max), r=[dq, d_G], w=[dq])
                    ut = t2
                    ut3 = ut[:].rearrange("p (c s) -> p c s", s=64)
                    at3 = at[:].rearrange("p (c s) -> p c s", s=64)
                    Bc3 = Bc[:].rearrange("p (c s) -> p c s", s=64)
                    k.op(V, lambda e: e.memset(mt[:, 0:1], 0.0), w=[dq])
                    if NCH > 1:
                        k.op(V, lambda e: e.tensor_copy(mt[:, 1:NCH], ut3[:, 0:NCH - 1, 63]), r=[dq], w=[dq])
                    uL = ut3[:, :, 63]
                    mtb = mt[:, :].unsqueeze(2).to_broadcast([4, NCH, 64])
                    k.op(V, lambda e: e.tensor_sub(dec[:], mt[:], uL), r=[dq], w=[dq])
                    k.op("act", lambda e: e.activation(dec[:], dec[:], AF.Exp), r=[dq], w=[dq])
                    sq0 = sq[:, 0, :].rearrange("p (c s) -> p c s", s=64)
                    sq1 = sq[:, 1, :].rearrange("p (c s) -> p c s", s=64)
                    sq2 = sq[:, 2, :].rearrange("p (c s) -> p c s", s=64)
                    k.op(V, lambda e: e.tensor_sub(sq0, at3, mtb), r=[dq, d_G], w=[dq])
                    k.op(V, lambda e: e.tensor_scalar(sq[:, 0, :], sq[:, 0, :], 80.0, -LN16, ALU.min, ALU.add), r=[dq], w=[dq])
                    k.op("act", lambda e: e.activation(sq[:, 0, :], sq[:, 0, :], AF.Exp), r=[dq], w=[dq])
                    k.op(V, lambda e: e.tensor_tensor(sq1, sq0, dec[:, :].unsqueeze(2).to_broadcast([4, NCH, 64]), ALU.mult),
                         r=[dq], w=[dq])
                    k.op(V, lambda e: e.tensor_tensor(sq2, Bc3, mtb, ALU.add), r=[dq], w=[dq])
                    k.op(V, lambda e: e.tensor_scalar(sq[:, 2, :], sq[:, 2, :], -1.0, 80.0, ALU.mult, ALU.min), r=[dq], w=[dq])
                    k.op("act", lambda e: e.activation(sq[:, 2, :], sq[:, 2, :], AF.Exp), r=[dq], w=[dq])
                    d_seq = Dep()
                    k.dma("sp", s_dec, dec[:], r=[dq], w=[d_seq])
                    d_TT = Dep()
                    for q in range(3):
                        c0 = 0
                        while c0 < NCH:
                            ncc = min(128, NCH - c0)
                            bk, bd = pf.next()

                            def f(pe, bk=bk, q=q, c0=c0, ncc=ncc):
                                for cc in range(ncc):
                                    c = c0 + cc
                                    ins = pe.transpose(bk[0:64, cc * 4:cc * 4 + 4], sq[0:4, q, c * 64:(c + 1) * 64], identf[0:4, 0:4])
                                return ins
                            k.op("pe", f, r=[dq, d_c], w=[bd])
                            k.op("act", lambda e, bk=bk, q=q, c0=c0, ncc=ncc: e.copy(
                                TT[:, q, c0:c0 + ncc, :], bk[0:64, 0:ncc * 4].rearrange("p (c h) -> p c h", h=4)), r=[bd], w=[d_TT])
                            c0 += ncc
                    k.dma("sp", decbc[:], s_dec.rearrange("h c -> (h c)").partition_broadcast(128), r=[d_seq], w=[d_TT])
                gscope.close()
                if dbg:
                    k.dma("sp", s_TT, TT[:].rearrange("p a b c -> p (a b c)"), r=[d_TT], w=[Dep()])
                if stop <= 2:
                    raise _Stop()
                with k.scope():
                    qT = k.sb("qT", [128, 8, TO], BF16)
                    kT = k.sb("kT", [128, 8, TA], BF16)
                    cw = k.sb("cw", [128, 16, 4], F32); cb = k.sb("cb", [128, 16], F32)
                    ngb = k.sb("ngb", [64, 1024], F32)
                    d_cw, d_qT, d_kT = Dep(), Dep(), Dep()
                    k.dma("sp", cw[:], cw_d, w=[d_cw]); k.dma("sp", cb[:], cb_d, w=[d_cw])
                    k.dma("sp", ngb[:], ng_d.partition_broadcast(64), w=[d_cw])
                    cscope = k.scope()
                    cscope.__enter__()
                    cin = Ring([(k.sb("cin", [128, 3 + TA], BF16), Dep()) for _ in range(3)])
                    Dg = k.sb("Dg", [128, 16, 4, 128], BF16)
                    d_Dg = Dep()
                    for fc in range(16):
                        for j in range(4):
                            k.op("dve", lambda e, fc=fc, j=j: e.tensor_scalar_mul(Dg[:, fc, j, :], identf[:], cw[:, fc, j:j + 1]),
                                 r=[d_cw, d_c], w=[d_Dg])
                    for fc in list(range(8, 16)) + list(range(8)):
                        isq = fc < 8
                        lo = (TP - 128) if isq else 0
                        o0 = TP if isq else 0
                        n = TA - o0
                        ci, cd = cin.next()
                        if not isq:
                            k.op("pool", lambda e, ci=ci: e.memset(ci[:, 0:3], 0.0), w=[cd])
                        k.dma("sp", ci[:, 3 + lo:3 + TA], s_qkaT[fc * 128:(fc + 1) * 128, lo:TA], r=[d_scr["qka"]], w=[cd])
                        tb = 0
                        while tb < n:
                            nn = min(512, n - tb)
                            bk, bd = pf.next()

                            def f(pe, bk=bk, ci=ci, fc=fc, o0=o0, tb=tb, nn=nn):
                                for j in range(4):
                                    ins = pe.matmul(bk[:, 0:nn], Dg[:, fc, j, :], ci[:, o0 + tb + j:o0 + tb + j + nn],
                                                    start=(j == 0), stop=(j == 3))
                                return ins
                            k.op("pe", f, r=[cd, d_Dg], w=[bd])
                            if isq:
                                k.op("act", lambda e, bk=bk, fc=fc, tb=tb, nn=nn: e.activation(
                                    qT[:, fc, tb:tb + nn], bk[:, 0:nn], AF.Silu, bias=cb[:, fc:fc + 1], scale=1.0),
                                    r=[bd, d_cw], w=[d_qT])
                            else:
                                k.op("act", lambda e, bk=bk, fc=fc, tb=tb, nn=nn: e.activation(
                                    kT[:, fc - 8, tb:tb + nn], bk[:, 0:nn], AF.Silu, bias=cb[:, fc:fc + 1], scale=1.0),
                                    r=[bd, d_cw], w=[d_kT])
                            tb += nn
                    cscope.__exit__(None, None, None)
                    a_S = Ring([banks[0], banks[1]])
                    bO, bOd = banks[2]
                    bO1, bO1d = banks[3]
                    m_U = Ring(banks[4:5])
                    bSN, bSNd = banks[5]
                    bNN, bNNd = banks[6]
                    pb_all = pb
                    pb = Ring([(banks[7][0].bitcast(BF16), banks[7][1])])
                    Cst = [k.sb("Cst", [128, 2, 257], F32) for _ in range(4)]
                    hblk = Ring([(k.sb("hblk", [128, 8, HB], BF16), Dep()) for _ in range(1)])
                    Cbf = [k.sb("Cbf", [128, 2, 257], BF16) for _ in range(4)]
                    d_C = [Dep() for _ in range(4)]
                    d_Cbf = [Dep() for _ in range(4)]
                    for h in range(4):
                        k.op("pool", lambda e, h=h: e.memset(Cst[h][:], 0.0), w=[d_C[h]])
                        k.op("pool", lambda e, h=h: e.memset(Cbf[h][:], 0.0), w=[d_Cbf[h]])
                    vch_items = []
                    for _ in range(3):
                        vt = k.sb("vch", [64, 4, 257], BF16)
                        vd = Dep()
                        k.op("pool", lambda e, vt=vt: e.memset(vt[:, :, 256:257], 1.0), w=[vd])
                        vch_items.append((vt, vd))
                    vch = Ring(vch_items)
                    sor = Ring([(k.sb("so", [64, 1024], BF16), Dep()) for _ in range(1)])
                    kwr = Ring([(k.sb("kw", [64, 256], BF16), Dep()) for _ in range(3)])
                    Wtr = Ring([(k.sb("Wt", [64, 64], BF16), Dep()) for _ in range(3)])
                    Nsr = Ring([(k.sb("Ns", [64, 4, 257], F32), Dep()) for _ in range(2)])
                    hgr = Ring([(k.sb("hg", [64, 4, 256], F32), Dep()) for _ in range(1)])
                    hbr = Ring([(k.sb("hb", [64, 1024], BF16), Dep()) for _ in range(2)])
                    smr = Ring([(k.sb("sm", [64, 64], F32), Dep()) for _ in range(2)])
                    lamt = k.sb("lamt", [128, 4, 64], F32)
                    lsm = k.sb("lsm", [128, 8], F32)
                    gnb = k.sb("gnb", [128, 128], F32)
                    d_l = Dep()
                    k.dma("sp", lamt[:], lam_d.rearrange("a b -> (a b)").partition_broadcast(128).rearrange("p (a b) -> p a b", a=4), w=[d_l])
                    k.dma("sp", gnb[:], dg_d.partition_broadcast(128), w=[d_l])
                    k.op("dve", lambda e: e.tensor_tensor(lamt[:, 0, :], lamt[:, 0, :], lamt[:, 1, :], ALU.mult), r=[d_l], w=[d_l])
                    k.op("dve", lambda e: e.tensor_tensor(lamt[:, 2, :], lamt[:, 2, :], lamt[:, 3, :], ALU.mult), r=[d_l], w=[d_l])
                    k.op("dve", lambda e: e.reduce_sum(lsm[:, 0:1], lamt[:, 0, :], AX.X), r=[d_l], w=[d_l])
                    k.op("dve", lambda e: e.reduce_sum(lsm[:, 1:2], lamt[:, 2, :], AX.X), r=[d_l], w=[d_l])
                    k.op("act", lambda e: e.activation(lsm[:, 2:4], lsm[:, 0:2], AF.Exp), r=[d_l], w=[d_l])
                    k.op("dve", lambda e: e.tensor_sub(lsm[:, 4:5], lsm[:, 3:4], lsm[:, 2:3]), r=[d_l], w=[d_l])
                    k.op("dve", lambda e: e.tensor_scalar_add(lsm[:, 5:6], lsm[:, 4:5], -0.2), r=[d_l], w=[d_l])
                    k.op("dve", lambda e: e.tensor_scalar_mul(gnb[:], gnb[:], 0.8), r=[d_l], w=[d_l])
                    neglam = lsm[:, 5:6]
                    kb_items = []
                    for _ in range(1):
                        kt_ = k.sb("kbT", [128, 2, TA], BF16)
                        kd_ = Dep()
                        k.op("pool", lambda e, kt_=kt_: e.memset(kt_[64:128, 0, :], 0.0), w=[kd_])
                        k.op("pool", lambda e, kt_=kt_: e.memset(kt_[0:64, 1, :], 0.0), w=[kd_])
                        kb_items.append((kt_, kd_))
                    kbr = Ring(kb_items)
                    qbr = Ring([(k.sb("qbT", [128, TO], BF16), Dep()) for _ in range(1)])
                    vb_items = []
                    for _ in range(1):
                        vt = k.sb("vbe", [128, NTA, 129], BF16)
                        vd = Dep()
                        k.op("dve", lambda e, vt=vt: e.tensor_copy(vt[:, :, 128], validb[:, :]), r=[d_c], w=[vd])
                        vb_items.append((vt, vd))
                    vbr = Ring(vb_items)
                    PTr = Ring([(k.sb("PT", [128, 256], BF16), Dep()) for _ in range(4)])
                    o1r = Ring([(k.sb("o1", [128, 128], F32), Dep()) for _ in range(2)])
                    o2r = Ring([(k.sb("o2", [128, 128], F32), Dep()) for _ in range(2)])
                    obr = Ring([(k.sb("ob", [128, 128], BF16), Dep()) for _ in range(4)])
                    s8r = Ring([(k.sb("s8", [128, 8], F32), Dep()) for _ in range(2)])
                    Osr = Ring([(k.sb("Os", [128, 2, 129], F32), Dep()) for _ in range(2)])
                    oblk = Ring([(k.sb("oblk", [128, HB], BF16), Dep()) for _ in range(2)])

                    def mlstm_gen():
                        st_m = {'hb': None, 'fin': None}
                        for c in range(NCH):
                            own = c >= CH0
                            tq = (c - CH0) * 64
                            v_t, v_d = vch.next()
                            k.dma("sp", v_t[:, :, 0:256], s_va[c * 64:(c + 1) * 64, :].rearrange("s (h d) -> s h d", h=4),
                                  r=[d_scr["va"]], w=[v_d])
                            if own:
                                so_t, so_d = sor.next()
                                k.dma("sp", so_t[:], s_oa[tq:tq + 64, :], r=[d_scr["oa"]], w=[so_d])
                                Ns_t, Ns_d = Nsr.next()
                            for h in range(4):
                                pt, pd = pb.next()

                                def f(pe, pt=pt, h=h, c=c):
                                    for j in range(2):
                                        ins = pe.transpose(pt[0:64, j * 128:(j + 1) * 128], kT[:, h * 2 + j, c * 64:(c + 1) * 64], identb[:])
                                    return ins
                                k.op("pe", f, r=[d_kT, d_c], w=[pd])
                                kw_t, kw_d = kwr.next()
                                k.op("act", lambda e, kw_t=kw_t, pt=pt, h=h, c=c: e.activation(
                                    kw_t[:], pt[0:64, 0:256], AF.Identity, scale=TT[:, 1, c, h:h + 1]), r=[pd, d_TT], w=[kw_d])
                                yield
                                bU, bUd = m_U.next()

                                def f(pe, bU=bU, kw_t=kw_t, v_t=v_t, h=h):
                                    for j in range(2):
                                        pe.matmul(bU[:, j * 256:(j + 1) * 256], kw_t[:, j * 128:(j + 1) * 128], v_t[:, h, 0:256],
                                                  start=True, stop=True)
                                    for j in range(2):
                                        ins = pe.matmul(bSN[:, 400 + j:401 + j], kw_t[:, j * 128:(j + 1) * 128], v_t[:, h, 256:257],
                                                        start=True, stop=True)
                                    return ins
                                k.op("pe", f, r=[kw_d, v_d], w=[bUd, bSNd])
                                if not own:
                                    yield
                                if own:
                                    def f(pe, h=h, c=c, tq=tq):
                                        for j in range(2):
                                            ins = pe.matmul(bSN[0:64, 320:384], kT[:, h * 2 + j, c * 64:(c + 1) * 64],
                                                            qT[:, h * 2 + j, tq:tq + 64], start=(j == 0), stop=(j == 1))
                                        return ins
                                    k.op("pe", f, r=[d_kT, d_qT], w=[bSNd])
                                    W_t, W_d = Wtr.next()
                                    k.op("dve", lambda e, W_t=W_t, h=h, c=c: e.scalar_tensor_tensor(
                                        W_t[:], bSN[0:64, 320:384], TT[:, 0, c, h:h + 1], mask64[:], ALU.mult, ALU.mult),
                                        r=[bSNd, d_TT, d_c], w=[W_d])
                                    yield

                                    def f(pe, W_t=W_t, v_t=v_t, h=h, tq=tq):
                                        for j in range(2):
                                            pe.matmul(bNN[0:64, 0:257], qT[:, h * 2 + j, tq:tq + 64], Cbf[h][:, j, :],
                                                      start=(j == 0), stop=False)
                                        return pe.matmul(bNN[0:64, 0:257], W_t[:], v_t[:, h, :], start=False, stop=True)
                                    k.op("pe", f, r=[d_qT, d_Cbf[h], W_d, v_d], w=[bNNd])
                                    k.op("act", lambda e, Ns_t=Ns_t, h=h: e.copy(Ns_t[:, h, :], bNN[0:64, 0:257]),
                                         r=[bNNd], w=[Ns_d])
                                    yield
                                dsc = decbc[:, h * NCH + c:h * NCH + c + 1]
                                k.op("dve", lambda e, h=h, bU=bU, dsc=dsc: e.scalar_tensor_tensor(
                                    Cst[h][:, :, 0:256], Cst[h][:, :, 0:256], dsc,
                                    bU[:, 0:512].rearrange("p (j n) -> p j n", j=2), ALU.mult, ALU.add),
                                    r=[bUd, d_TT], w=[d_C[h]])
                                k.op("dve", lambda e, h=h, dsc=dsc: e.scalar_tensor_tensor(
                                    Cst[h][:, :, 256:257], Cst[h][:, :, 256:257], dsc,
                                    bSN[:, 400:402].rearrange("p (j n) -> p j n", j=2), ALU.mult, ALU.add),
                                    r=[bSNd, d_TT], w=[d_C[h]])
                                k.op("act", lambda e, h=h: e.copy(Cbf[h][:], Cst[h][:]), r=[d_C[h]], w=[d_Cbf[h]])
                                if h == 1 and st_m['fin'] is not None:
                                    st_m['fin']()
                                    st_m['fin'] = None
                                yield
                            if own:
                                sm_t, sm_d = smr.next()
                                k.op("act", lambda e, sm_t=sm_t, Ns_t=Ns_t: e.activation(
                                    sm_t[:, 0:4], Ns_t[:, :, 256], AF.Abs), r=[Ns_d], w=[sm_d])
                                k.op("dve", lambda e, sm_t=sm_t, c=c: e.tensor_tensor(
                                    sm_t[:, 0:4], sm_t[:, 0:4], TT[:, 2, c, :], ALU.max), r=[sm_d, d_TT], w=[sm_d])
                                k.op("dve", lambda e, sm_t=sm_t: e.reciprocal(sm_t[:, 0:4], sm_t[:, 0:4]), r=[sm_d], w=[sm_d])
                                hg_t, hg_d = hgr.next()
                                k.op("dve", lambda e, hg_t=hg_t, Ns_t=Ns_t, sm_t=sm_t: e.tensor_tensor(
                                    hg_t[:], Ns_t[:, :, 0:256], sm_t[:, 0:4].unsqueeze(2).to_broadcast([64, 4, 256]), ALU.mult),
                                    r=[Ns_d, sm_d], w=[hg_d])
                                k.op("dve", lambda e, hg_t=hg_t, so_t=so_t: e.tensor_tensor(
                                    hg_t[:], hg_t[:], so_t[:].rearrange("s (h d) -> s h d", h=4), ALU.mult),
                                    r=[so_d, hg_d], w=[hg_d])

                                def f(e, hg_t=hg_t, sm_t=sm_t):
                                    for hh in range(4):
                                        ins = e.bn_stats(sm_t[:, 4 + 6 * hh:10 + 6 * hh], hg_t[:, hh, :])
                                    return ins
                                k.op("dve", f, r=[hg_d], w=[sm_d])

                                def f(e, sm_t=sm_t):
                                    for hh in range(4):
                                        ins = e.bn_aggr(sm_t[:, 28 + 2 * hh:30 + 2 * hh], sm_t[:, 4 + 6 * hh:10 + 6 * hh])
                                    return ins
                                k.op("dve", f, r=[sm_d], w=[sm_d])
                                mv = sm_t[:, 28:36].rearrange("s (h k) -> s h k", h=4)
                                k.op("act", lambda e, sm_t=sm_t, mv=mv: e.activation(
                                    sm_t[:, 36:40], mv[:, :, 1], AF.Ln, bias=epsc[0:64, 0:1], scale=1.0), r=[sm_d, d_c], w=[sm_d])
                                k.op("act", lambda e, sm_t=sm_t: e.activation(
                                    sm_t[:, 36:40], sm_t[:, 36:40], AF.Exp, scale=-0.5), r=[sm_d], w=[sm_d])
                                k.op("dve", lambda e, hg_t=hg_t, mv=mv: e.tensor_tensor(
                                    hg_t[:], hg_t[:], mv[:, :, 0:1].to_broadcast([64, 4, 256]), ALU.subtract),
                                    r=[sm_d, hg_d], w=[hg_d])
                                k.op("dve", lambda e, hg_t=hg_t, sm_t=sm_t: e.tensor_tensor(
                                    hg_t[:], hg_t[:], sm_t[:, 36:40].unsqueeze(2).to_broadcast([64, 4, 256]), ALU.mult),
                                    r=[sm_d, hg_d], w=[hg_d])
                                hb_t, hb_d = hbr.next()
                                k.op("dve", lambda e, hg_t=hg_t, hb_t=hb_t: e.tensor_tensor(
                                    hb_t[:], hg_t[:].rearrange("s h d -> s (h d)"), ngb[:], ALU.mult),
                                    r=[hg_d, d_cw], w=[hb_d])
                                def mfin(hb_t=hb_t, hb_d=hb_d, tq=tq):
                                    pt, pd = pb.next()

                                    def f(pe):
                                        for fc in range(8):
                                            ins = pe.transpose(pt[:, fc * 64:(fc + 1) * 64], hb_t[:, fc * 128:(fc + 1) * 128], identb[0:64, 0:64])
                                        return ins
                                    k.op("pe", f, r=[hb_d, d_c], w=[pd])
                                    if tq % HB == 0:
                                        st_m['hb'] = hblk.next()
                                    hk_t, hk_d = st_m['hb']
                                    k.op("act", lambda e: e.copy(
                                        hk_t[:, :, tq % HB:tq % HB + 64], pt[:, 0:512].rearrange("p (f s) -> p f s", f=8)), r=[pd], w=[hk_d])
                                    if (tq + 64) % HB == 0:
                                        tb0 = tq + 64 - HB
                                        k.dma("sp", s_haT[:, tb0:tb0 + HB].rearrange("(f p) t -> p f t", p=128), hk_t[:], r=[hk_d], w=[d_haT])
                                st_m['fin'] = mfin
                            yield
                        if st_m['fin'] is not None:
                            st_m['fin']()
                            st_m['fin'] = None
                        yield

                    def attn_gen():
                        st_a = {'ob': None}
                        deferred = []
                        for h in range(8):
                            kb_t, kb_d = kbr.next(); qb_t, qb_d = qbr.next(); vb_t, vb_d = vbr.next()
                            k.dma("sp", kb_t[0:64, 0, :], s_qkbT[1024 + h * 128:1024 + h * 128 + 64, :], r=[d_scr["qkb"]], w=[kb_d])
                            k.dma("sp", kb_t[64:128, 1, :], s_qkbT[1024 + h * 128 + 64:1024 + (h + 1) * 128, :], r=[d_scr["qkb"]], w=[kb_d])
                            k.dma("sp", qb_t[:], s_qkbT[h * 128:(h + 1) * 128, TP:TA], r=[d_scr["qkb"]], w=[qb_d])
                            for t8 in range(0, NTA, 8):
                                t9 = min(NTA, t8 + 8)
                                k.dma("sp", vb_t[:, t8:t9, 0:128],
                                      s_vb[t8 * 128:t9 * 128, h * 128:(h + 1) * 128].rearrange("(t p) d -> p t d", p=128),
                                      r=[d_scr["vb"]], w=[vb_d])
                            items = []
                            for qi in range(NTO):
                                qt = NTP + qi
                                kt_lo = 0
                                while kt_lo < qt and SLOPES[h] * (127 - 128 * (qt - kt_lo)) < -ACLAMP:
                                    kt_lo += 1
                                for kt in range(kt_lo, qt + 1):
                                    items.append((qi, qt, kt, kt_lo))

                            def emit_qk(it, h=h, kb_t=kb_t, qb_t=qb_t, kb_d=kb_d, qb_d=qb_d):
                                qi, qt, kt, kt_lo = it
                                bSt, sd = a_S.next()
                                off = 0
                                diag = (kt == qt)

                                def f(pe):
                                    for m in range(2):
                                        ins = pe.matmul(bSt[:, off + m * 128:off + (m + 1) * 128], kb_t[:, m, kt * 128:(kt + 1) * 128],
                                                        qb_t[:, qi * 128:(qi + 1) * 128], start=True, stop=(not diag))
                                        if diag:
                                            ins = pe.matmul(bSt[:, off + m * 128:off + (m + 1) * 128], identb[:], dgb[:, h, :], start=False, stop=True)
                                    return ins
                                k.op("pe", f, r=[kb_d, qb_d, d_c], w=[sd])
                                return bSt, sd
                            def do_pv(it, P_t, P_d, h=h, vb_t=vb_t, vb_d=vb_d):
                                qi, qt, kt, kt_lo = it

                                def f(pe, P_t=P_t, vb_t=vb_t, kt=kt, qt=qt, kt_lo=kt_lo):
                                    pe.matmul(bO[:, 0:129], P_t[:, 0:128], vb_t[:, kt, :], start=(kt == kt_lo), stop=(kt == qt))
                                    return pe.matmul(bO1[:, 0:129], P_t[:, 128:256], vb_t[:, kt, :], start=(kt == kt_lo), stop=(kt == qt))
                                k.op("pe", f, r=[P_d, vb_d], w=[bOd, bO1d])
                                if kt != qt:
                                    return
                                s8, s8d = s8r.next()
                                Os, Osd = Osr.next()
                                k.op("act", lambda e, Os=Os: e.copy(Os[:, 0, :], bO[:, 0:129]), r=[bOd], w=[Osd])
                                k.op("dve", lambda e, Os=Os: e.tensor_copy(Os[:, 1, :], bO1[:, 0:129]), r=[bO1d], w=[Osd])
                                k.op("dve", lambda e, s8=s8, Os=Os: e.reciprocal(s8[:, 0:2], Os[:, :, 128]), r=[Osd], w=[s8d])
                                k.op("dve", lambda e, s8=s8: e.tensor_tensor(s8[:, 2:3], s8[:, 1:2], neglam, ALU.mult),
                                     r=[s8d, d_l], w=[s8d])
                                o1, o1d = o1r.next(); o2, o2d = o2r.next()
                                k.op("dve", lambda e, o1=o1, s8=s8, Os=Os: e.tensor_scalar_mul(o1[:], Os[:, 0, 0:128], s8[:, 0:1]),
                                     r=[Osd, s8d], w=[o1d])
                                k.op("dve", lambda e, o1=o1, s8=s8, Os=Os: e.scalar_tensor_tensor(
                                    o1[:], Os[:, 1, 0:128], s8[:, 2:3], o1[:], ALU.mult, ALU.add), r=[Osd, s8d], w=[o1d])
                                k.op("pool", lambda e, o1=o1, o2=o2: e.tensor_tensor(o2[:], o1[:], o1[:], ALU.mult), r=[o1d], w=[o2d])
                                k.op("dve", lambda e, o2=o2, s8=s8: e.reduce_sum(s8[:, 3:4], o2[:], AX.X), r=[o2d], w=[s8d])
                                k.op("dve", lambda e, s8=s8: e.tensor_scalar(s8[:, 4:5], s8[:, 3:4], 1.0 / 128.0, EPS, ALU.mult, ALU.add),
                                     r=[s8d], w=[s8d])
                                k.op("act", lambda e, s8=s8: e.activation(s8[:, 5:6], s8[:, 4:5], AF.Ln), r=[s8d], w=[s8d])
                                k.op("act", lambda e, s8=s8: e.activation(s8[:, 5:6], s8[:, 5:6], AF.Exp, scale=-0.5),
                                     r=[s8d], w=[s8d])
                                ob, obd = obr.next()
                                k.op("dve", lambda e, ob=ob, o1=o1, s8=s8: e.scalar_tensor_tensor(
                                    ob[:], o1[:], s8[:, 5:6], gnb[:], ALU.mult, ALU.mult), r=[o1d, s8d, d_l], w=[obd])
                                def fin(ob=ob, obd=obd, qi=qi, h=h):
                                    pt, pd = pb.next()
                                    k.op("pe", lambda pe: pe.transpose(pt[:, 0:128], ob[:], identb[:]), r=[obd, d_c], w=[pd])
                                    tq = qi * 128
                                    if tq % HB == 0:
                                        st_a['ob'] = oblk.next()
                                    ok_t, ok_d = st_a['ob']
                                    k.op("act", lambda e: e.copy(ok_t[:, tq % HB:tq % HB + 128], pt[:, 0:128]),
                                         r=[pd], w=[ok_d])
                                    if (tq + 128) % HB == 0:
                                        tb0 = tq + 128 - HB
                                        k.dma("sp", s_obT[h * 128:(h + 1) * 128, tb0:tb0 + HB], ok_t[:], r=[ok_d], w=[d_obT])
                                deferred.append([3, fin])
                            nxt = emit_qk(items[0])
                            pend = None
                            for j, it in enumerate(items):
                                qi, qt, kt, kt_lo = it
                                bSt, sd = nxt
                                off = 0
                                if j + 1 < len(items):
                                    nxt = emit_qk(items[j + 1])
                                diag = (kt == qt)
                                P_t, P_d = PTr.next()
                                bias_ap = zero1[:, 0:1] if diag else abt[:, h, qt - kt:qt - kt + 1]
                                k.op("act", lambda e, P_t=P_t, off=off, bias_ap=bias_ap, bSt=bSt: e.activation(
                                    P_t[:], bSt[:, off:off + 256], AF.Exp, bias=bias_ap, scale=0.125), r=[sd, d_c], w=[P_d])

                                if pend is not None:
                                    do_pv(*pend)
                                pend = (it, P_t, P_d)
                                for dfr in list(deferred):
                                    dfr[0] -= 1
                                    if dfr[0] <= 0:
                                        deferred.remove(dfr)
                                        dfr[1]()
                                yield
                            if pend is not None:
                                do_pv(*pend)
                            yield
                        for dfr in list(deferred):
                            dfr[1]()
                        deferred.clear()
                        yield

                    gm = mlstm_gen()
                    ga_ = attn_gen()
                    if stop <= 3:
                        for _ in gm:
                            pass
                        raise _Stop()
                    if stop <= 3.5:
                        for _ in ga_:
                            pass
                        raise _Stop()
                    n_m = (CH0 * (4 * 3 + 1)) + ((NCH - CH0) * (4 * 4 + 1))
                    n_a = 0
                    for h_ in range(8):
                        for qi_ in range(NTO):
                            qt_ = NTP + qi_
                            lo_ = 0
                            while lo_ < qt_ and SLOPES[h_] * (127 - 128 * (qt_ - lo_)) < -ACLAMP:
                                lo_ += 1
                            n_a += qt_ + 1 - lo_
                    done_m = done_a = 0
                    m_alive = a_alive = True
                    while m_alive or a_alive:
                        if m_alive:
                            try:
                                next(gm); done_m += 1
                            except StopIteration:
                                m_alive = False
                        if ILV == 0:
                            tgt = n_a + 10 if not m_alive else 0
                        else:
                            tgt = n_a + 10 if not m_alive else (done_m * n_a) // n_m
                        while a_alive and done_a < tgt:
                            try:
                                next(ga_); done_a += 1
                            except StopIteration:
                                a_alive = False
                    for g_ in (gm, ga_):
                        for _ in g_:
                            pass
                    pb = pb_all
                if stop <= 4:
                    raise _Stop()
                with k.scope():
                    war = Ring([(k.sb("wa", [128, 8, 512], BF16), Dep()) for _ in range(2)])
                    wbr = Ring([(k.sb("wb", [128, 8, 512], BF16), Dep()) for _ in range(2)])
                    gar = Ring([(k.sb("ga", [128, 512], BF16), Dep()) for _ in range(2)])
                    gbr = Ring([(k.sb("gb", [128, 512], BF16), Dep()) for _ in range(2)])
                    m1r = Ring([(k.sb("m1", [128, 512], F32), Dep()) for _ in range(2)])
                    m2r = Ring([(k.sb("m2", [128, 512], F32), Dep()) for _ in range(2)])
                    hkr = Ring([(k.sb("hk", [128, 8, HB], BF16), Dep()) for _ in range(2)])
                    okr = Ring([(k.sb("ok", [128, 8, HB], BF16), Dep()) for _ in range(2)])
                    mgr = Ring([(k.sb("mgo", [128, 512], BF16), Dep()) for _ in range(3)])
                    st_e1 = {"db": None, "tb": None}

                    def e1_unit(db, tb, n, g):
                        with k.atomic():
                            if st_e1["db"] != db:
                                wa_t, wa_d = war.next(); wb_t, wb_d = wbr.next()
                                k.dma("pool", wa_t[:], w_a.rearrange("(c p) n -> p c n", p=128)[:, :, db * 512:(db + 1) * 512], w=[wa_d])
                                k.dma("pool", wb_t[:], w_b.rearrange("(c p) n -> p c n", p=128)[:, :, db * 512:(db + 1) * 512], w=[wb_d])
                                st_e1["db"] = db
                                st_e1["w"] = (wa_t, wa_d, wb_t, wb_d)
                            if st_e1["tb"] != (db, tb):
                                hk, hkd = hkr.next(); ok, okd = okr.next()
                                k.dma("sp", hk[:, :, 0:n], s_haT[:, tb:tb + n].rearrange("(f p) t -> p f t", p=128), r=[d_haT], w=[hkd])
                                k.dma("sp", ok[:, :, 0:n], s_obT[:, tb:tb + n].rearrange("(f p) t -> p f t", p=128), r=[d_obT], w=[okd])
                                st_e1["tb"] = (db, tb)
                                st_e1["h"] = (hk, hkd, ok, okd)
                        wa_t, wa_d, wb_t, wb_d = st_e1["w"]
                        hk, hkd, ok, okd = st_e1["h"]
                        dc = db * 4 + g
                        ga_t, ga_d = gar.next(); gb_t, gb_d = gbr.next()
                        k.dma("sp", ga_t[:, 0:n], s_gT[dc * 128:(dc + 1) * 128, tb:tb + n], r=[d_scr["g"]], w=[ga_d])
                        k.dma("sp", gb_t[:, 0:n], s_gT[2048 + dc * 128:2048 + (dc + 1) * 128, tb:tb + n], r=[d_scr["g"]], w=[gb_d])
                        bA, bAd = pf.next(); bB, bBd = pf.next()

                        def f(pe):
                            for fc in range(8):
                                ins = pe.matmul(bA[:, 0:n], wa_t[:, fc, g * 128:(g + 1) * 128], hk[:, fc, 0:n],
                                                start=(fc == 0), stop=(fc == 7))
                            return ins
                        k.op("pe", f, r=[wa_d, hkd], w=[bAd])

                        def f(pe):
                            for fc in range(8):
                                ins = pe.matmul(bB[:, 0:n], wb_t[:, fc, g * 128:(g + 1) * 128], ok[:, fc, 0:n],
                                                start=(fc == 0), stop=(fc == 7))
                            return ins
                        k.op("pe", f, r=[wb_d, okd], w=[bBd])
                        m1, m1d = m1r.next(); m2, m2d = m2r.next()
                        k.op("dve", lambda e: e.tensor_tensor(m1[:, 0:n], bA[:, 0:n], ga_t[:, 0:n], ALU.mult),
                             r=[bAd, ga_d], w=[m1d])
                        k.op("dve", lambda e: e.tensor_tensor(m2[:, 0:n], bB[:, 0:n], gb_t[:, 0:n], ALU.mult),
                             r=[bBd, gb_d], w=[m2d])
                        mg_t, mg_dd = mgr.next()
                        k.op("pool", lambda e: e.tensor_tensor(mg_t[:, 0:n], m1[:, 0:n], m2[:, 0:n], ALU.add), r=[m1d, m2d], w=[mg_dd])
                        k.dma("sp", s_mgT[dc * 128:(dc + 1) * 128, tb:tb + n], mg_t[:, 0:n], r=[mg_dd], w=[d_mg])
                    units = []
                    for db in range(4):
                        tb = 0
                        while tb < TO:
                            n = min(512, TO - tb)
                            for g in range(4):
                                units.append(lambda db=db, tb=tb, n=n, g=g: e1_unit(db, tb, n, g))
                            tb += n
                    k.interleave(units, width=NIL)
            if stop <= 5:
                raise _Stop()
            with k.scope():
                wo = k.sb("wo", [128, DC, D], BF16)
                wr = k.sb("wr", [128, DC, 36], F32)
                br = k.sb("br", [1, 36], F32)
                l1g = k.sb("l1g", [128, D], F32); l1b = k.sb("l1b", [128, D], F32)
                cnt = k.sb("cnt", [128, 32], F32)
                d_wo, d_cnt = Dep(), Dep()
                for q4 in range(4):
                    k.dma("pool", wo[:, :, q4 * 512:(q4 + 1) * 512],
                          w_out.rearrange("(c p) n -> p c n", p=128)[:, :, q4 * 512:(q4 + 1) * 512], w=[d_wo])
                k.dma("sp", wr[:], wr_d.rearrange("(c p) n -> p c n", p=128), w=[d_wo])
                k.dma("sp", br[:], br_d, w=[d_wo])
                k.dma("sp", l1g[:], ln1g_d.partition_broadcast(128), w=[d_wo])
                k.dma("sp", l1b[:], ln1b_d.partition_broadcast(128), w=[d_wo])
                k.op("dve", lambda e: e.memset(cnt[:], 0.0), w=[d_cnt])
                xtr = Ring([(k.sb("xt", [128, D], F32), Dep()) for _ in range(2)])
                x1r = Ring([(k.sb("x1", [128, D], F32), Dep()) for _ in range(2)])
                hbr2 = Ring([(k.sb("hb2", [128, D], BF16), Dep()) for _ in range(2)])
                hTr = Ring([(k.sb("hT", [128, DC, 128], F32), Dep()) for _ in range(2)])
                rsr = Ring([(k.sb("rs", [128, 256], F32), Dep()) for _ in range(2)])
                mkr = Ring([(k.sb("mk", [128, 32], BF16), Dep()) for _ in range(2)])
                mbr = Ring([(k.sb("mgb", [128, DC, HB], BF16), Dep()) for _ in range(2)])
                st_e2 = {"mb": None}

                def e2_tile(ti):
                    if (ti * 128) % HB == 0:
                        with k.atomic():
                            st_e2["mb"] = mbr.next()
                            k.dma("sp", st_e2["mb"][0][:], s_mgT[:, ti * 128:ti * 128 + HB].rearrange("(c p) t -> p c t", p=128),
                                  r=[d_mg], w=[st_e2["mb"][1]])
                    mgb, mgbd = st_e2["mb"]
                    tloc = (ti * 128) % HB
                    xt, xtd = xtr.next(); x1, x1d = x1r.next()
                    k.dma("sp", xt[:], x[TP + ti * 128:TP + (ti + 1) * 128, :], w=[xtd])
                    for db in range(4):
                        bk, bd = pf.next()

                        def f(pe, bk=bk, mgb=mgb, tloc=tloc, db=db):
                            for c in range(DC):
                                ins = pe.matmul(bk[:, :], mgb[:, c, tloc:tloc + 128], wo[:, c, db * 512:(db + 1) * 512],
                                                start=(c == 0), stop=(c == DC - 1))
                            return ins
                        k.op("pe", f, r=[mgbd, d_wo], w=[bd])
                        k.op("dve", lambda e, x1=x1, xt=xt, bk=bk, db=db: e.scalar_tensor_tensor(
                            x1[:, db * 512:(db + 1) * 512], xt[:, db * 512:(db + 1) * 512], ALPHA, bk[:, :], ALU.mult, ALU.add),
                            r=[xtd, bd], w=[x1d])
                    rs, rsd = rsr.next()

                    def layer_norm(xx, xd, rs, rsd, g_t, b_t, gdep):
                        st = rs[:, 0:24].rearrange("p (g k) -> p g k", g=4)
                        def fbn(e):
                            for gg in range(4):
                                ins = e.bn_stats(rs[:, 6 * gg:6 * gg + 6], xx[:, gg * 512:(gg + 1) * 512])
                            return ins
                        k.op("dve", fbn, r=[xd], w=[rsd])
                        k.op("dve", lambda e: e.bn_aggr(rs[:, 24:26], rs[:, 0:24]), r=[rsd], w=[rsd])
                        k.op("act", lambda e: e.activation(rs[:, 26:27], rs[:, 25:26], AF.Ln, bias=epsc[:, 0:1], scale=1.0), r=[rsd, d_c], w=[rsd])
                        k.op("act", lambda e: e.activation(rs[:, 26:27], rs[:, 26:27], AF.Exp, scale=-0.5), r=[rsd], w=[rsd])
                        k.op("dve", lambda e: e.tensor_scalar(xx[:], xx[:], rs[:, 24:25], rs[:, 26:27], ALU.subtract, ALU.mult),
                             r=[rsd, xd], w=[xd])
                        k.op("pool", lambda e: e.tensor_tensor(xx[:], xx[:], g_t[:], ALU.mult), r=[xd, gdep], w=[xd])
                        k.op("pool", lambda e: e.tensor_tensor(xx[:], xx[:], b_t[:], ALU.add), r=[xd, gdep], w=[xd])
                    layer_norm(x1, x1d, rs, rsd, l1g, l1b, d_wo)
                    k.dma("sp", s_h1[ti * 128:(ti + 1) * 128, :], x1[:], r=[x1d], w=[d_h1])
                    hb2, hb2d = hbr2.next()
                    k.op("act", lambda e, hb2=hb2, x1=x1: e.copy(hb2[:], x1[:]), r=[x1d], w=[hb2d])
                    hT, hTd = hTr.next()
                    for g in range(4):
                        bk, bd = pf.next()

                        def f(pe, bk=bk, x1=x1, g=g):
                            for j in range(4):
                                c = g * 4 + j
                                ins = pe.transpose(bk[:, j * 128:(j + 1) * 128], x1[:, c * 128:(c + 1) * 128], identf[:])
                            return ins
                        k.op("pe", f, r=[x1d, d_c], w=[bd])
                        k.op("act", lambda e, hT=hT, bk=bk, g=g: e.copy(
                            hT[:, g * 4:(g + 1) * 4, :], bk[:, :].rearrange("p (j n) -> p j n", j=4)), r=[bd], w=[hTd])
                    bk, bd = pf.next()

                    def f(pe, bk=bk, hT=hT):
                        for c in range(DC):
                            pe.matmul(bk[:, 0:36], hT[:, c, :], wr[:, c, :], start=(c == 0), stop=False)
                        return pe.matmul(bk[:, 0:36], onesf[0:1, :], br[0:1, :], start=False, stop=True)
                    k.op("pe", f, r=[hTd, d_wo, d_c], w=[bd])
                    V = "dve"
                    lg = rs[:, 32:68]
                    k.op(V, lambda e, bk=bk, lg=lg: e.tensor_copy(lg, bk[:, 0:36]), r=[bd], w=[rsd])
                    g4 = rs[:, 32:36]; e32 = rs[:, 36:68]
                    gmx = rs[:, 68:69]; ngm = rs[:, 69:70]; ohg = rs[:, 70:74]; eg = rs[:, 74:78]; sg = rs[:, 78:79]; gp = rs[:, 79:80]
                    pen = rs[:, 80:84]; msk = rs[:, 84:116]; top8 = rs[:, 116:124]; oh1 = rs[:, 124:156]; oh2 = rs[:, 156:188]
                    dd = rs[:, 188:189]; p1 = rs[:, 189:190]; p2 = rs[:, 190:191]; tmp = rs[:, 192:224]
                    pk = rs[:, 224:225]; ek = rs[:, 225:226]; okk = rs[:, 226:227]; dsf = rs[:, 227:228]; posg = rs[:, 228:260 - 4]
                    k.op(V, lambda e: e.reduce_max(gmx, g4, AX.X), r=[rsd], w=[rsd])
                    k.op(V, lambda e: e.tensor_scalar_mul(ngm, gmx, -1.0), r=[rsd], w=[rsd])
                    k.op(V, lambda e: e.tensor_scalar(ohg, g4, gmx, None, ALU.is_equal), r=[rsd], w=[rsd])
                    k.op("act", lambda e: e.activation(eg, g4, AF.Exp, bias=ngm, scale=1.0), r=[rsd], w=[rsd])
                    k.op(V, lambda e: e.reduce_sum(sg, eg, AX.X), r=[rsd], w=[rsd])
                    k.op(V, lambda e: e.reciprocal(gp, sg), r=[rsd], w=[rsd])
                    k.op(V, lambda e: e.tensor_scalar(pen, ohg, BIG, -BIG, ALU.mult, ALU.add), r=[rsd], w=[rsd])
                    k.op(V, lambda e: e.tensor_tensor(msk.rearrange("p (g j) -> p g j", g=4), e32.rearrange("p (g j) -> p g j", g=4),
                                                      pen.unsqueeze(2).to_broadcast([128, 4, 8]), ALU.add), r=[rsd], w=[rsd])
                    k.op(V, lambda e: e.max(top8, msk), r=[rsd], w=[rsd])
                    k.op(V, lambda e: e.tensor_scalar(oh1, msk, top8[:, 0:1], None, ALU.is_equal), r=[rsd], w=[rsd])
                    k.op(V, lambda e: e.tensor_scalar(oh2, msk, top8[:, 1:2], None, ALU.is_equal), r=[rsd], w=[rsd])
                    k.op(V, lambda e: e.tensor_sub(dd, top8[:, 0:1], top8[:, 1:2]), r=[rsd], w=[rsd])
                    k.op("act", lambda e: e.activation(p1, dd, AF.Sigmoid), r=[rsd], w=[rsd])
                    k.op("act", lambda e: e.activation(p2, dd, AF.Sigmoid, scale=-1.0), r=[rsd], w=[rsd])
                    k.op(V, lambda e, ti=ti: e.tensor_tensor(wtab[:, ti, 0:1], p1, gp, ALU.mult), r=[rsd], w=[d_tab])
                    k.op(V, lambda e, ti=ti: e.tensor_tensor(wtab[:, ti, 1:2], p2, gp, ALU.mult), r=[rsd], w=[d_tab])
                    mk, mkd = mkr.next()
                    k.op(V, lambda e, mk=mk: e.tensor_tensor(mk[:], oh1, oh2, ALU.add), r=[rsd], w=[mkd])
                    bk2, bd2 = pf.next()

                    def f(pe, bk2=bk2, mk=mk):
                        pe.matmul(bk2[:, 0:32], ustr[:], mk[:], start=True, stop=True)
                        return pe.matmul(bk2[:, 32:64], onesb[:], mk[:], start=True, stop=True)
                    k.op("pe", f, r=[mkd, d_c], w=[bd2])
                    posg = rs[:, 224:256]
                    pk = rs[:, 28:29]; ek = rs[:, 29:30]; okk = rs[:, 30:31]; dsf = rs[:, 31:32]
                    with k.atomic():
                        k.op(V, lambda e, bk2=bk2: e.tensor_tensor(posg, bk2[:, 0:32], cnt[:], ALU.add), r=[bd2, d_cnt, rsd], w=[rsd])
                        k.op(V, lambda e, bk2=bk2: e.tensor_tensor(cnt[:], cnt[:], bk2[:, 32:64], ALU.add), r=[bd2, rsd], w=[d_cnt])
                    for kk, oh in ((0, oh1), (1, oh2)):
                        k.op(V, lambda e, oh=oh: e.tensor_tensor(tmp, oh, posg, ALU.mult), r=[rsd], w=[rsd])
                        k.op(V, lambda e: e.reduce_sum(pk, tmp, AX.X), r=[rsd], w=[rsd])
                        k.op(V, lambda e, oh=oh: e.tensor_tensor(tmp, oh, ecap[:], ALU.mult), r=[rsd, d_c], w=[rsd])
                        k.op(V, lambda e: e.reduce_sum(ek, tmp, AX.X), r=[rsd], w=[rsd])
                        k.op(V, lambda e: e.tensor_scalar(okk, pk, float(CAP), None, ALU.is_lt), r=[rsd], w=[rsd])
                        k.op(V, lambda e: e.tensor_tensor(dsf, ek, pk, ALU.add), r=[rsd], w=[rsd])
                        k.op(V, lambda e: e.tensor_scalar_add(dsf, dsf, -float(TRASH)), r=[rsd], w=[rsd])
                        k.op(V, lambda e: e.tensor_tensor(dsf, dsf, okk, ALU.mult), r=[rsd], w=[rsd])
                        k.op(V, lambda e: e.tensor_scalar_add(dsf, dsf, float(TRASH)), r=[rsd], w=[rsd])
                        k.op(V, lambda e, ti=ti, kk=kk: e.tensor_copy(dtab[:, ti, kk:kk + 1], dsf), r=[rsd], w=[d_tab])
                        k.dma("pool", s_Xg, hb2[:], r=[hb2d, d_tab, d_Xg], w=[d_Xg],
                              indirect=dict(out_offset=bass.IndirectOffsetOnAxis(ap=dtab[:, ti, kk:kk + 1], axis=0), in_offset=None))
                k.interleave([(lambda ti=ti: e2_tile(ti)) for ti in range(NTO)], width=NIL)

            if dbg:
                k.dma("sp", s_dtab, dtab[:].rearrange("p a b -> p (a b)"), r=[d_tab], w=[Dep()])
                k.dma("sp", s_wtab, wtab[:].rearrange("p a b -> p (a b)"), r=[d_tab], w=[Dep()])
            if stop <= 6:
                raise _Stop()
            NCT = CAP // 128
            with k.scope():
                wgr = Ring([(k.sb("wg", [128, DC, 512], BF16), Dep()) for _ in range(3)])
                wur = Ring([(k.sb("wu", [128, DC, 512], BF16), Dep()) for _ in range(3)])
                wdr = Ring([(k.sb("wd", [128, 4, D], BF16), Dep()) for _ in range(3)])
                Xer = Ring([(k.sb("Xe", [128, NCT, D], BF16), Dep()) for _ in range(2)])
                XTr = Ring([(k.sb("XT", [128, DC, CAP], BF16), Dep()) for _ in range(2)])
                ATr = Ring([(k.sb("AT", [128, 4, CAP], BF16), Dep()) for _ in range(2)])
                sgr = Ring([(k.sb("sgt", [128, CAP], F32), Dep()) for _ in range(2)])
                Ysr = Ring([(k.sb("Ys", [128, D], BF16), Dep()) for _ in range(2)])
                def expert_fn(e_):
                    wg, wgd = wgr.next(); wu, wud = wur.next(); wd, wdd = wdr.next()
                    k.dma("pool", wg[:], w_gate[e_].rearrange("(c p) n -> p c n", p=128), w=[wgd])
                    k.dma("pool", wu[:], w_up[e_].rearrange("(c p) n -> p c n", p=128), w=[wud])
                    k.dma("pool", wd[:], w_down[e_].rearrange("(c p) n -> p c n", p=128), w=[wdd])
                    Xe, Xed = Xer.next(); XT, XTd = XTr.next(); AT, ATd = ATr.next()
                    k.dma("sp", Xe[:], s_Xg[e_ * CAP:(e_ + 1) * CAP, :].rearrange("(t p) d -> p t d", p=128), r=[d_Xg], w=[Xed])
                    for t in range(NCT):
                        for g in range(4):
                            pt, pd = pb.next()

                            def f(pe, pt=pt, Xe=Xe, t=t, g=g):
                                for j in range(4):
                                    c = g * 4 + j
                                    ins = pe.transpose(pt[:, j * 128:(j + 1) * 128], Xe[:, t, c * 128:(c + 1) * 128], identb[:])
                                return ins
                            k.op("pe", f, r=[Xed, d_c], w=[pd])
                            eng = "dve" if (g % 2 == 0) else "act"
                            if eng == "dve":
                                k.op("dve", lambda e, XT=XT, pt=pt, g=g, t=t: e.tensor_copy(
                                    XT[:, g * 4:(g + 1) * 4, t * 128:(t + 1) * 128], pt[:, 0:512].rearrange("p (j n) -> p j n", j=4)),
                                    r=[pd], w=[XTd])
                            else:
                                k.op("act", lambda e, XT=XT, pt=pt, g=g, t=t: e.copy(
                                    XT[:, g * 4:(g + 1) * 4, t * 128:(t + 1) * 128], pt[:, 0:512].rearrange("p (j n) -> p j n", j=4)),
                                    r=[pd], w=[XTd])
                    for fcx in range(4):
                        bG, bGd = pf.next(); bU, bUd = pf.next()

                        def f(pe, bG=bG, wg=wg, XT=XT, fcx=fcx):
                            for c in range(DC):
                                ins = pe.matmul(bG[:, 0:CAP], wg[:, c, fcx * 128:(fcx + 1) * 128], XT[:, c, :], start=(c == 0), stop=(c == DC - 1))
                            return ins
                        k.op("pe", f, r=[wgd, XTd], w=[bGd])

                        def f(pe, bU=bU, wu=wu, XT=XT, fcx=fcx):
                            for c in range(DC):
                                ins = pe.matmul(bU[:, 0:CAP], wu[:, c, fcx * 128:(fcx + 1) * 128], XT[:, c, :], start=(c == 0), stop=(c == DC - 1))
                            return ins
                        k.op("pe", f, r=[wud, XTd], w=[bUd])
                        sg_t, sg_d = sgr.next()
                        k.op("act", lambda e, sg_t=sg_t, bG=bG: e.activation(sg_t[:], bG[:, 0:CAP], AF.Silu), r=[bGd], w=[sg_d])
                        k.op("dve", lambda e, AT=AT, sg_t=sg_t, bU=bU, fcx=fcx: e.tensor_tensor(AT[:, fcx, :], sg_t[:], bU[:, 0:CAP], ALU.mult),
                             r=[sg_d, bUd], w=[ATd])
                    for t in range(NCT):
                        Ys, Ysd = Ysr.next()
                        for db in range(4):
                            bk, bd = pf.next()

                            def f(pe, bk=bk, AT=AT, wd=wd, t=t, db=db):
                                for fcx in range(4):
                                    ins = pe.matmul(bk[:, :], AT[:, fcx, t * 128:(t + 1) * 128], wd[:, fcx, db * 512:(db + 1) * 512],
                                                    start=(fcx == 0), stop=(fcx == 3))
                                return ins
                            k.op("pe", f, r=[ATd, wdd], w=[bd])
                            if db % 2 == 0:
                                k.op("act", lambda e, Ys=Ys, bk=bk, db=db: e.copy(Ys[:, db * 512:(db + 1) * 512], bk[:, :]), r=[bd], w=[Ysd])
                            else:
                                k.op("dve", lambda e, Ys=Ys, bk=bk, db=db: e.tensor_copy(Ys[:, db * 512:(db + 1) * 512], bk[:, :]), r=[bd], w=[Ysd])
                        r0 = e_ * CAP + t * 128
                        k.dma("sp", s_Yg[r0:r0 + 128, :], Ys[:], r=[Ysd], w=[d_Yg])
                k.interleave([(lambda e_=e_: expert_fn(e_)) for e_ in range(NE)], width=NIL)

            if stop <= 7:
                raise _Stop()
            d_out = Dep()
            with k.scope():
                l2g = k.sb("l2g", [128, D], F32); l2b = k.sb("l2b", [128, D], F32)
                d_l2 = Dep()
                k.dma("sp", l2g[:], ln2g_d.partition_broadcast(128), w=[d_l2])
                k.dma("sp", l2b[:], ln2b_d.partition_broadcast(128), w=[d_l2])
                h1r = Ring([(k.sb("h1", [128, D], F32), Dep()) for _ in range(3)])
                y1r = Ring([(k.sb("y1", [128, D], BF16), Dep()) for _ in range(3)])
                y2r = Ring([(k.sb("y2", [128, D], BF16), Dep()) for _ in range(3)])
                rsr = Ring([(k.sb("rs2", [128, 32], F32), Dep()) for _ in range(3)])
                def g_tile(ti):
                    h1, h1d = h1r.next(); y1, y1d = y1r.next(); y2, y2d = y2r.next()
                    k.dma("sp", h1[:], s_h1[ti * 128:(ti + 1) * 128, :], r=[d_h1], w=[h1d])
                    k.dma("pool", y1[:], s_Yg, r=[d_Yg, d_tab], w=[y1d],
                          indirect=dict(out_offset=None, in_offset=bass.IndirectOffsetOnAxis(ap=dtab[:, ti, 0:1], axis=0)))
                    k.dma("pool", y2[:], s_Yg, r=[d_Yg, d_tab], w=[y2d],
                          indirect=dict(out_offset=None, in_offset=bass.IndirectOffsetOnAxis(ap=dtab[:, ti, 1:2], axis=0)))
                    k.op("act", lambda e, h1=h1: e.mul(h1[:], h1[:], ALPHA), r=[h1d], w=[h1d])
                    k.op("dve", lambda e, h1=h1, y1=y1, ti=ti: e.scalar_tensor_tensor(
                        h1[:], y1[:], wtab[:, ti, 0:1], h1[:], ALU.mult, ALU.add), r=[y1d, d_tab, h1d], w=[h1d])
                    k.op("dve", lambda e, h1=h1, y2=y2, ti=ti: e.scalar_tensor_tensor(
                        h1[:], y2[:], wtab[:, ti, 1:2], h1[:], ALU.mult, ALU.add), r=[y2d, d_tab, h1d], w=[h1d])
                    rs, rsd = rsr.next()
                    st = rs[:, 0:24].rearrange("p (g k) -> p g k", g=4)
                    def fbn(e, rs=rs, h1=h1):
                        for gg in range(4):
                            ins = e.bn_stats(rs[:, 6 * gg:6 * gg + 6], h1[:, gg * 512:(gg + 1) * 512])
                        return ins
                    k.op("dve", fbn, r=[h1d], w=[rsd])
                    k.op("dve", lambda e, rs=rs: e.bn_aggr(rs[:, 24:26], rs[:, 0:24]), r=[rsd], w=[rsd])
                    k.op("act", lambda e, rs=rs: e.activation(rs[:, 26:27], rs[:, 25:26], AF.Ln, bias=epsc[:, 0:1], scale=1.0), r=[rsd, d_c], w=[rsd])
                    k.op("act", lambda e, rs=rs: e.activation(rs[:, 26:27], rs[:, 26:27], AF.Exp, scale=-0.5), r=[rsd], w=[rsd])
                    k.op("dve", lambda e, rs=rs, h1=h1: e.tensor_scalar(h1[:], h1[:], rs[:, 24:25], rs[:, 26:27], ALU.subtract, ALU.mult),
                         r=[rsd, h1d], w=[h1d])
                    k.op("pool", lambda e, h1=h1: e.tensor_tensor(h1[:], h1[:], l2g[:], ALU.mult), r=[h1d, d_l2], w=[h1d])
                    k.op("pool", lambda e, h1=h1: e.tensor_tensor(h1[:], h1[:], l2b[:], ALU.add), r=[h1d, d_l2], w=[h1d])
                    k.dma("sp", out_d[ti * 128:(ti + 1) * 128, :], h1[:], r=[h1d], w=[d_out])
                k.interleave([(lambda ti=ti: g_tile(ti)) for ti in range(NTO)], width=3)
        except _Stop:
            gscope.close()
        k.barrier()
        build.stats = (k.n_ins, {n: e.count for n, e in k.E.items()})
    return nc


def _consts(TP, TO, CAP):
    TA = TP + TO
    c = {}
    c["ident"] = np.eye(128, dtype=np.float32)
    s = np.arange(64)
    c["mask64"] = (s[:, None] <= s[None, :]).astype(np.float32)
    p = np.arange(128)
    c["ustrict"] = (p[:, None] < p[None, :]).astype(np.float32)
    slopes = 2.0 ** (-8.0 * np.arange(1, 9) / 8.0)
    kl = p[:, None]; ql = p[None, :]
    vis = (kl // 64) <= (ql // 64)
    dg = np.zeros((128, 8, 128), np.float32)
    for h in range(8):
        b = np.where(kl <= ql, slopes[h] * kl, slopes[h] * (2 * ql - kl))
        dg[:, h, :] = np.where(vis, b, -BIG) * 8.0
    c["diagbias"] = dg
    ab = np.zeros((128, 8, 32), np.float32)
    for h in range(8):
        for dlt in range(32):
            ab[:, h, dlt] = np.maximum(slopes[h] * (p - 128 * dlt), -ACLAMP)
    c["alibi"] = ab
    c["ecap"] = np.tile((np.arange(32) * CAP).astype(np.float32)[None, :], (128, 1))
    return c


def _prep_shared(inp):
    f = lambda a: np.ascontiguousarray(np.asarray(a, dtype=np.float32))
    b_in = f(inp["b_in"][0])
    sh = {}
    sh["w_in"] = f(inp["w_in"][0])

    def fm_bias(b):
        cols = np.concatenate([b[O_QA:O_QA + 1024], b[O_KA:O_KA + 1024], b[O_QB:O_QB + 1024], b[O_KB:O_KB + 1024],
                               b[O_GA:O_GA + 2048], b[O_GB:O_GB + 2048]])
        return np.ascontiguousarray(cols.reshape(64, 128).T)

    def tm_bias(b):
        return np.ascontiguousarray(np.concatenate([b[O_VA:O_VA + 1024], b[O_OA:O_OA + 1024], b[O_VB:O_VB + 1024]])[None, :])

    def if_bias(b):
        return np.ascontiguousarray(np.stack([b[O_IA:O_IA + 4], b[O_FA:O_FA + 4]], axis=1))
    b_mask = np.zeros_like(b_in)
    b_mask[O_IA:O_IA + 4] = -BIG
    b_mask[O_FA:O_FA + 4] = BIG
    sh["bias_real"] = (fm_bias(b_in), tm_bias(b_in), if_bias(b_in))
    sh["bias_mask"] = (fm_bias(b_mask), tm_bias(b_mask), if_bias(b_mask))
    cw = f(inp["conv_w"][0])
    sh["cw"] = np.ascontiguousarray(cw.T.reshape(16, 128, 4).transpose(1, 0, 2))
    sh["cb"] = np.ascontiguousarray(f(inp["conv_b"][0]).reshape(16, 128).T)
    sh["mlstm_norm_g"] = f(inp["mlstm_norm_g"][0])
    sh["diff_norm_g"] = f(inp["diff_norm_g"][0])
    sh["lamv"] = np.ascontiguousarray(np.stack([f(inp["lambda_q1"][0]), f(inp["lambda_k1"][0]),
                                                f(inp["lambda_q2"][0]), f(inp["lambda_k2"][0])]))
    for n in ("w_a", "w_b", "w_out", "ln1_g", "ln1_b", "ln2_g", "ln2_b", "w_gate", "w_up", "w_down"):
        sh[n] = f(inp[n][0])
    sh["w_r"] = np.ascontiguousarray(np.concatenate([f(inp["w_grp"][0]), f(inp["w_exp"][0])], axis=1))
    sh["b_r"] = np.ascontiguousarray(np.concatenate([f(inp["b_grp"][0]), f(inp["b_exp"][0])])[None, :])
    return sh


def make_in_maps(inp, TP, TO, CAP):
    x = np.asarray(inp["x"], dtype=np.float32)
    B, S, _ = x.shape
    assert S == TP + TO or S == 2 * TO
    sh = _prep_shared(inp)
    cs = _consts(TP, TO, CAP)
    TA = TP + TO
    maps = []
    for b in range(B):
        for half in range(2):
            m = {}
            if half == 0:
                xa = np.zeros((TA, D), np.float32)
                xa[TP:] = x[b, 0:TO]
                valid = np.concatenate([np.zeros(TP, np.float32), np.ones(TO, np.float32)])
                pre = sh["bias_mask"]
            else:
                xa = np.ascontiguousarray(x[b, TO - TP:2 * TO])
                valid = np.ones(TA, np.float32)
                pre = sh["bias_real"]
            m["x"] = xa
            m["valid_tm"] = np.ascontiguousarray(valid.reshape(TA // 128, 128).T)
            m["bfm"], m["btm"], m["bif"] = sh["bias_real"]
            m["bfm_pre"], m["btm_pre"], m["bif_pre"] = pre
            for n in ("w_in", "cw", "cb", "mlstm_norm_g", "diff_norm_g", "lamv", "w_a", "w_b", "w_out", "ln1_g", "ln1_b",
                      "ln2_g", "ln2_b", "w_gate", "w_up", "w_down", "w_r", "b_r"):
                m[n] = sh[n]
            m.update(cs)
            maps.append(m)
    return maps


_TP, _TO, _CAP = 2048, 2048, 256


def kernel(**inputs):
    x = np.asarray(inputs["x"])
    B, S, _ = x.shape
    maps = make_in_maps(inputs, _TP, _TO, _CAP)
    nc = build(_TP, _TO, _CAP)
    res = run_bass_kernel_spmd(nc, maps, core_ids=list(range(len(maps))))
    out = np.zeros((B, S, D), np.float32)
    i = 0
    for b in range(B):
        for half in range(2):
            out[b, half * _TO:(half + 1) * _TO] = res.results[i]["out"]
            i += 1
    return out
```

```python
import math
from contextlib import ExitStack, contextmanager
import numpy as np
import concourse.bass as bass
import concourse.mybir as mybir
from concourse.bass_utils import run_bass_kernel_spmd

F32 = mybir.dt.float32
BF16 = mybir.dt.bfloat16
I32 = mybir.dt.int32
AF = mybir.ActivationFunctionType
ALU = mybir.AluOpType
AX = mybir.AxisListType

D = 2048
DC = 16
NIN = 11272
O_QA, O_KA, O_VA, O_OA, O_IA, O_FA, O_QB, O_KB, O_VB, O_GA, O_GB = (
    0, 1024, 2048, 3072, 4096, 4100, 4104, 5128, 6152, 7176, 9224)
ALPHA = 2.0 ** 0.25
EPS = 1e-5
NE = 32
BIG = 30000.0
LN16 = math.log(16.0)
ACLAMP = 40.0
import os
ILV = int(os.environ.get('ILV', '1'))
EXPV = int(os.environ.get('EXPV', '0'))
NIL = int(os.environ.get('NIL', '2'))
SLOPES = [2.0 ** (-(h + 1)) for h in range(8)]


class _Stop(Exception):
    pass


class Dep:
    __slots__ = ("w", "rs")

    def __init__(self):
        self.w = None
        self.rs = {}


class _Eng:
    def __init__(self, name, eng, sem):
        self.name, self.eng, self.sem = name, eng, sem
        self.count = 0
        self.waited = {}


class _DmaSem:
    def __init__(self, key, sem):
        self.key, self.sem, self.total = key, sem, 0


class Kern:
    def __init__(self, nc, stack, n_dma_sems=48):
        self.nc = nc
        self.stack = stack
        self.E = {}
        for name, eng in (("pe", nc.tensor), ("act", nc.scalar), ("dve", nc.vector),
                          ("pool", nc.gpsimd), ("sp", nc.sync)):
            sem = stack.enter_context(nc.semaphore("s_" + name))
            self.E[name] = _Eng(name, eng, sem)
        self.dsems = [_DmaSem("d%d" % i, stack.enter_context(nc.semaphore("d%d" % i)))
                      for i in range(n_dma_sems)]
        self.dpool = {"hw": self.dsems[0:n_dma_sems // 2], "sw": self.dsems[n_dma_sems // 2:]}
        self.drr = {"hw": 0, "sw": 0}
        self.n_ins = 0
        self.uid = 0
        self._cur = None
        self._atomic = 0

    def sb(self, name, shape, dt):
        self.uid += 1
        return self.stack.enter_context(self.nc.sbuf_tensor("%s_%d" % (name, self.uid), list(shape), dt))

    def ps(self, name, shape, dt):
        return self.stack.enter_context(self.nc.psum_tensor(name, list(shape), dt))

    @contextmanager
    def scope(self):
        old = self.stack
        stopped = False
        with ExitStack() as st:
            self.stack = st
            try:
                yield
            except _Stop:
                stopped = True
            if not stopped:
                self.barrier()
        self.stack = old
        if stopped:
            raise _Stop()

    def _wait(self, es, ev):
        key, sem, val = ev
        if es.name == "pe" and key == "pe":
            return
        if es.waited.get(key, 0) >= val:
            return
        es.eng.wait_ge(sem, val)
        es.waited[key] = val

    def _pre(self, es, r, w):
        for d in r:
            if d.w is not None:
                self._wait(es, d.w)
        for d in w:
            if d.w is not None:
                self._wait(es, d.w)
            for ev in d.rs.values():
                self._wait(es, ev)

    def _post(self, ev, r, w):
        for d in r:
            d.rs[ev[0]] = ev
        for d in w:
            d.w = ev
            d.rs = {}

    def op(self, en, fn, r=(), w=()):
        es = self.E[en]
        self._pre(es, r, w)
        ins = fn(es.eng)
        es.count += 1
        ins.then_inc(es.sem, 1)
        self.n_ins += 1
        ev = (es.name, es.sem, es.count)
        self._post(ev, r, w)
        self._yield()
        return ev

    def dma(self, qn, out, in_, r=(), w=(), indirect=None, **kw):
        es = self.E[qn]
        self._pre(es, r, w)
        kind = "sw" if qn == "pool" else "hw"
        pool = self.dpool[kind]
        ds = pool[self.drr[kind]]
        self.drr[kind] = (self.drr[kind] + 1) % len(pool)
        if ds.total > 0:
            self._wait(es, (ds.key, ds.sem, ds.total))
        if indirect is not None:
            ins = es.eng.indirect_dma_start(out=out, in_=in_, **indirect)
        else:
            ins = es.eng.dma_start(out=out, in_=in_, **kw)
        ds.total += 16
        ins.then_inc(ds.sem, 16)
        self.n_ins += 1
        ev = (ds.key, ds.sem, ds.total)
        self._post(ev, r, w)
        self._yield()
        return ev

    def _yield(self):
        w = self._cur
        if w is None or self._atomic > 0:
            return
        self._sched_sem.release()
        w["go"].acquire()

    def interleave(self, fns, width=2):
        import threading
        if width <= 1 or len(fns) <= 1:
            for fn in fns:
                fn()
            return
        self._sched_sem = threading.Semaphore(0)
        pending = list(fns)
        active = []
        err = []

        def runner(w, fn):
            w["go"].acquire()
            try:
                fn()
            except BaseException as e:
                err.append(e)
            w["done"] = True
            self._sched_sem.release()
        while pending or active:
            while pending and len(active) < width:
                w = {"go": threading.Semaphore(0), "done": False}
                w["t"] = threading.Thread(target=runner, args=(w, pending.pop(0)), daemon=True)
                w["t"].start()
                active.append(w)
            for w in list(active):
                self._cur = w
                w["go"].release()
                self._sched_sem.acquire()
                self._cur = None
                if w["done"]:
                    active.remove(w)
                if err:
                    raise err[0]

    @contextmanager
    def atomic(self):
        self._atomic += 1
        try:
            yield
        finally:
            self._atomic -= 1

    def barrier(self):
        evs = [(e.name, e.sem, e.count) for e in self.E.values() if e.count > 0]
        evs += [(d.key, d.sem, d.total) for d in self.dsems if d.total > 0]
        for es in self.E.values():
            for ev in evs:
                if ev[0] == es.name and es.name == "pe":
                    continue
                self._wait(es, ev)


class Ring:
    def __init__(self, items):
        self.items = items
        self.i = 0

    def next(self):
        it = self.items[self.i]
        self.i = (self.i + 1) % len(self.items)
        return it


def build(TP, TO, CAP, dbg=False, stop=99):
    TA = TP + TO
    NTA, NTO, NTP = TA // 128, TO // 128, TP // 128
    NCH, CH0 = TA // 64, TP // 64
    NROW = NE * CAP + 128
    TRASH = NE * CAP
    nc = bass.Bass("TRN2", target_bir_lowering=False)

    def din(name, shape, dt=F32):
        return nc.dram_tensor(name, list(shape), dt, kind="ExternalInput").ap()

    x = din("x", [TA, D])
    w_in = din("w_in", [D, NIN])
    bfm_d = din("bfm", [128, 64]); bfmp_d = din("bfm_pre", [128, 64])
    btm_d = din("btm", [1, 3072]); btmp_d = din("btm_pre", [1, 3072])
    bif_d = din("bif", [4, 2]); bifp_d = din("bif_pre", [4, 2])
    valid_d = din("valid_tm", [128, NTA])
    cw_d = din("cw", [128, 16, 4]); cb_d = din("cb", [128, 16])
    ng_d = din("mlstm_norm_g", [1024]); dg_d = din("diff_norm_g", [128])
    lam_d = din("lamv", [4, 64])
    w_a = din("w_a", [1024, D]); w_b = din("w_b", [1024, D]); w_out = din("w_out", [D, D])
    ln1g_d = din("ln1_g", [D]); ln1b_d = din("ln1_b", [D]); ln2g_d = din("ln2_g", [D]); ln2b_d = din("ln2_b", [D])
    wr_d = din("w_r", [D, 36]); br_d = din("b_r", [1, 36])
    if stop > 6:
        w_gate = din("w_gate", [NE, D, 512]); w_up = din("w_up", [NE, D, 512]); w_down = din("w_down", [NE, 512, D])
    ident_d = din("ident", [128, 128]); mask64_d = din("mask64", [64, 64]); ustrict_d = din("ustrict", [128, 128])
    dgb_d = din("diagbias", [128, 8, 128]); abt_d = din("alibi", [128, 8, 32]); ecap_d = din("ecap", [128, 32])
    out_d = nc.dram_tensor("out", [TO, D], F32, kind="ExternalOutput").ap()

    def dscr(name, shape, dt):
        if dbg:
            return nc.dram_tensor(name, list(shape), dt, kind="ExternalOutput").ap()
        return nc.dram_tensor(name, list(shape), dt).ap()

    s_qkaT = dscr("s_qkaT", [2048, TA], BF16)
    s_qkbT = dscr("s_qkbT", [2048, TA], BF16)
    s_gT = dscr("s_gT", [4096, TO], BF16)
    s_va = dscr("s_va", [TA, 1024], BF16)
    s_vb = dscr("s_vb", [TA, 1024], BF16)
    s_oa = dscr("s_oa", [TO, 1024], BF16)
    s_seq = dscr("s_seq", [3, 4, TA], F32)
    s_dec = dscr("s_dec", [4, NCH], F32)
    s_h1 = dscr("s_h1", [TO, D], F32)
    s_Xg = dscr("s_Xg", [NROW, D], BF16)
    s_Yg = dscr("s_Yg", [NROW, D], BF16)
    s_haT = dscr("s_haT", [1024, TO], BF16)
    s_obT = dscr("s_obT", [1024, TO], BF16)
    s_mgT = dscr("s_mgT", [2048, TO], BF16)
    HB = min(512, TO)
    if dbg:
        s_TT = dscr("s_TT", [64, 3 * NCH * 4], F32)
        s_dtab = dscr("s_dtab", [128, NTO * 2], I32)
        s_wtab = dscr("s_wtab", [128, NTO * 2], F32)

    with ExitStack() as st0:
        k = Kern(nc, st0)
        try:
            banks = [(k.ps("pf%d" % i, [128, 512], F32), Dep()) for i in range(8)]
            pf = Ring(banks[0:6])
            pb = Ring([(banks[i][0].bitcast(BF16), banks[i][1]) for i in (6, 7)])
            d_c = Dep()
            identf = k.sb("identf", [128, 128], F32)
            identb = k.sb("identb", [128, 128], BF16)
            onesb = k.sb("onesb", [128, 128], BF16)
            onesf = k.sb("onesf", [128, 128], F32)
            mask64 = k.sb("mask64", [64, 64], F32)
            ustr = k.sb("ustr", [128, 128], BF16)
            dgb = k.sb("dgb", [128, 8, 128], BF16)
            abt = k.sb("abt", [128, 8, 32], F32)
            ecap = k.sb("ecap", [128, 32], F32)
            validb = k.sb("validb", [128, NTA], BF16)
            zero1 = k.sb("zero1", [128, 1], F32)
            epsc = k.sb("epsc", [128, 1], F32)
            k.dma("sp", identf[:], ident_d, w=[d_c])
            k.dma("pool", identb[:], ident_d, w=[d_c])
            k.dma("sp", mask64[:], mask64_d, w=[d_c])
            k.dma("pool", ustr[:], ustrict_d, w=[d_c])
            k.dma("pool", dgb[:], dgb_d, w=[d_c])
            k.dma("sp", abt[:], abt_d, w=[d_c])
            k.dma("sp", ecap[:], ecap_d, w=[d_c])
            k.dma("pool", validb[:], valid_d, w=[d_c])
            k.op("dve", lambda e: e.memset(onesb[:], 1.0), w=[d_c])
            k.op("dve", lambda e: e.memset(onesf[:], 1.0), w=[d_c])
            k.op("dve", lambda e: e.memset(zero1[:], 0.0), w=[d_c])
            k.op("dve", lambda e: e.memset(epsc[:], EPS), w=[d_c])
            d_zt, d_Xg, d_Yg = Dep(), Dep(), Dep()

            dtab = k.sb("dtab", [128, NTO, 2], I32)
            wtab = k.sb("wtab", [128, NTO, 2], F32)
            TT = k.sb("TT", [64, 3, NCH, 4], F32)
            decbc = k.sb("decbc", [128, 4 * NCH], F32)
            gscope = ExitStack()
            _old = k.stack
            k.stack = gscope
            Gi = k.sb("Gi", [4, TA], F32); Gf = k.sb("Gf", [4, TA], F32)
            k.stack = _old
            d_G = Dep()
            d_tab = Dep()
            d_h1 = Dep()
            d_haT, d_obT, d_mg = Dep(), Dep(), Dep()

            with k.scope():
                zt = k.sb("zt", [128, 4096], BF16)
                k.op("pool", lambda e: e.memset(zt[:], 0.0), w=[d_zt])
                r0 = 0
                while r0 < NROW:
                    nr = min(256, NROW - r0)
                    k.dma("sp", s_Xg[r0:r0 + nr, :].rearrange("(t p) d -> p t d", p=128),
                          zt[:, 0:(nr // 128) * D].rearrange("p (t d) -> p t d", d=D), r=[d_zt], w=[d_Xg])
                    r0 += nr
                k.dma("sp", s_Yg[TRASH:TRASH + 128, :], zt[:, 0:D], r=[d_zt], w=[d_Yg])
                TX = max(TP, TO)
                xT = k.sb("xT", [128, DC, TX], BF16)
                xb = Ring([(k.sb("xb", [128, D], BF16), Dep()) for _ in range(2)])
                wt = Ring([(k.sb("wt", [128, DC, 512], BF16), Dep()) for _ in range(2)])
                wif = k.sb("wif", [128, DC, 8], BF16)
                evf = Ring([(k.sb("evf", [128, 4, 512], BF16), Dep()) for _ in range(2)])
                evt = Ring([(k.sb("evt", [128, 512], BF16), Dep()) for _ in range(3)])
                bfm = k.sb("bfm", [128, 64], F32); bfmp = k.sb("bfmp", [128, 64], F32)
                btm = k.sb("btm", [1, 3072], BF16); btmp = k.sb("btmp", [1, 3072], BF16)
                bif = k.sb("bif", [4, 2], F32); bifp = k.sb("bifp", [4, 2], F32)
                d_b, d_wif = Dep(), Dep()
                k.dma("sp", bfm[:], bfm_d, w=[d_b]); k.dma("sp", bfmp[:], bfmp_d, w=[d_b])
                k.dma("pool", btm[:], btm_d, w=[d_b]); k.dma("pool", btmp[:], btmp_d, w=[d_b])
                k.dma("sp", bif[:], bif_d, w=[d_b]); k.dma("sp", bifp[:], bifp_d, w=[d_b])
                k.dma("pool", wif[:], w_in.rearrange("(c p) n -> p c n", p=128)[:, :, O_IA:O_IA + 8], w=[d_wif])
                d_scr = {"qka": Dep(), "qkb": Dep(), "g": Dep(), "va": Dep(), "vb": Dep(), "oa": Dep()}

                FM = []
                for j in range(2):
                    FM.append((O_QA + 512 * j, "qka", s_qkaT, 512 * j, AF.Identity, "q", 4 * j))
                    FM.append((O_KA + 512 * j, "qka", s_qkaT, 1024 + 512 * j, AF.Identity, "all", 8 + 4 * j))
                    FM.append((O_QB + 512 * j, "qkb", s_qkbT, 512 * j, AF.Identity, "own", 16 + 4 * j))
                    FM.append((O_KB + 512 * j, "qkb", s_qkbT, 1024 + 512 * j, AF.Identity, "all", 24 + 4 * j))
                for j in range(8):
                    FM.append((O_GA + 512 * j, "g", s_gT, 512 * j, AF.Sigmoid, "gate", 32 + 4 * j))
                TM = []
                for j in range(2):
                    TM.append((O_VA + 512 * j, "va", s_va, 512 * j, AF.Identity, "all", 512 * j))
                    TM.append((O_OA + 512 * j, "oa", s_oa, 512 * j, AF.Sigmoid, "own", 1024 + 512 * j))
                    TM.append((O_VB + 512 * j, "vb", s_vb, 512 * j, AF.Identity, "all", 2048 + 512 * j))

                for phase in ("pre", "own"):
                    t0, nt = (0, TP) if phase == "pre" else (TP, TO)
                    bfm_x, btm_x, bif_x = (bfmp, btmp, bifp) if phase == "pre" else (bfm, btm, bif)
                    d_xT = [Dep() for _ in range(nt // 128)]
                    for ti in range(nt // 128):
                        xb_t, xb_d = xb.next()
                        k.dma("pool", xb_t[:], x[t0 + ti * 128:t0 + (ti + 1) * 128, :], w=[xb_d])
                        for g in range(4):
                            pt, pd = pb.next()

                            def f(pe, g=g, pt=pt, xb_t=xb_t):
                                for j in range(4):
                                    c = g * 4 + j
                                    ins = pe.transpose(pt[:, j * 128:(j + 1) * 128], xb_t[:, c * 128:(c + 1) * 128], identb[:])
                                return ins
                            k.op("pe", f, r=[xb_d, d_c], w=[pd])
                            k.op("dve", lambda e, g=g, ti=ti, pt=pt: e.tensor_copy(
                                xT[:, g * 4:(g + 1) * 4, ti * 128:(ti + 1) * 128],
                                pt[:, 0:512].rearrange("p (j n) -> p j n", j=4)), r=[pd], w=[d_xT[ti]])
                    tb = 0
                    while tb < nt:
                        n = min(512, nt - tb)
                        dx = d_xT[tb // 128:(tb + n) // 128]
                        for gi, Gt in ((0, Gi), (1, Gf)):
                            bk, bd = pf.next()

                            def f(pe, gi=gi, bk=bk, tb=tb, n=n):
                                for c in range(DC):
                                    ins = pe.matmul(bk[0:4, 0:n], wif[:, c, gi * 4:(gi + 1) * 4], xT[:, c, tb:tb + n],
                                                    start=(c == 0), stop=(c == DC - 1))
                                return ins
                            k.op("pe", f, r=dx + [d_wif], w=[bd])
                            k.op("act", lambda e, gi=gi, Gt=Gt, bk=bk, tb=tb, n=n: e.activation(
                                Gt[0:4, t0 + tb:t0 + tb + n], bk[0:4, 0:n], AF.Identity, bias=bif_x[:, gi:gi + 1], scale=1.0),
                                r=[bd, d_b], w=[d_G])
                        tb += n
                    for (c0, dkey, dst, row0, func, which, fmc) in FM:
                        if phase == "pre":
                            if which in ("own", "gate"):
                                continue
                            tlo = (TP - 128) if which == "q" else 0
                        else:
                            tlo = 0
                        w_t, w_d = wt.next()
                        k.dma("pool", w_t[:], w_in.rearrange("(c p) n -> p c n", p=128)[:, :, c0:c0 + 512], w=[w_d])
                        tb = tlo
                        while tb < nt:
                            n = min(512, nt - tb)
                            dx = d_xT[tb // 128:(tb + n) // 128]
                            ev_t, ev_d = evf.next()
                            for g in range(4):
                                bk, bd = pf.next()

                                def f(pe, g=g, bk=bk, tb=tb, n=n, w_t=w_t):
                                    for c in range(DC):
                                        ins = pe.matmul(bk[:, 0:n], w_t[:, c, g * 128:(g + 1) * 128], xT[:, c, tb:tb + n],
                                                        start=(c == 0), stop=(c == DC - 1))
                                    return ins
                                k.op("pe", f, r=dx + [w_d], w=[bd])
                                k.op("act", lambda e, g=g, bk=bk, n=n, ev_t=ev_t, func=func, fmc=fmc: e.activation(
                                    ev_t[:, g, 0:n], bk[:, 0:n], func, bias=bfm_x[:, fmc + g:fmc + g + 1], scale=1.0),
                                    r=[bd, d_b], w=[ev_d])
                            tcol = (tb if which == "gate" else t0 + tb)
                            k.dma("sp", dst[row0:row0 + 512, tcol:tcol + n].rearrange("(g p) t -> p g t", p=128),
                                  ev_t[:, :, 0:n], r=[ev_d], w=[d_scr[dkey]])
                            tb += n
                    for (c0, dkey, dst, col0, func, which, bcol) in TM:
                        if phase == "pre" and which == "own":
                            continue
                        w_t, w_d = wt.next()
                        k.dma("pool", w_t[:], w_in.rearrange("(c p) n -> p c n", p=128)[:, :, c0:c0 + 512], w=[w_d])
                        for ti in range(nt // 128):
                            bk, bd = pf.next()

                            def f(pe, bk=bk, ti=ti, w_t=w_t, bcol=bcol):
                                for c in range(DC):
                                    pe.matmul(bk[:, :], xT[:, c, ti * 128:(ti + 1) * 128], w_t[:, c, :],
                                              start=(c == 0), stop=False)
                                return pe.matmul(bk[:, :], onesb[0:1, :], btm_x[0:1, bcol:bcol + 512], start=False, stop=True)
                            k.op("pe", f, r=[d_xT[ti], w_d, d_b, d_c], w=[bd])
                            e_t, e_d = evt.next()
                            k.op("act", lambda e, bk=bk, e_t=e_t, func=func: e.activation(e_t[:], bk[:, :], func),
                                 r=[bd], w=[e_d])
                            trow = (ti * 128 if which == "own" else t0 + ti * 128)
                            k.dma("sp", dst[trow:trow + 128, col0:col0 + 512], e_t[:], r=[e_d], w=[d_scr[dkey]])

            if True:
                if stop <= 1:
                    raise _Stop()
                with k.scope():
                    t1 = k.sb("t1", [4, TA], F32); t2 = k.sb("t2", [4, TA], F32)
                    mt = k.sb("mt", [4, NCH], F32); dec = k.sb("dec", [4, NCH], F32)
                    sq = k.sb("sq", [4, 3, TA], F32)
                    dq = Dep()
                    V = "dve"
                    k.op("act", lambda e: e.activation(t1[:], Gf[:], AF.Abs), r=[d_G], w=[dq])
                    k.op("act", lambda e: e.activation(t1[:], t1[:], AF.Exp, scale=-1.0), r=[dq], w=[dq])
                    k.op("act", lambda e: e.activation(t1[:], t1[:], AF.Ln, bias=1.0, scale=1.0), r=[dq], w=[dq])
                    k.op(V, lambda e: e.tensor_scalar_min(t2[:], Gf[:], 0.0), r=[d_G], w=[dq])
                    k.op(V, lambda e: e.tensor_sub(t2[:], t2[:], t1[:]), r=[dq], w=[dq])
                    k.op(V, lambda e: e.tensor_scalar_mul(t2[:], t2[:], 0.5), r=[dq], w=[dq])
                    k.op(V, lambda e: e.tensor_tensor_scan(t1[:], t2[:], t2[:], 0.0, ALU.add, ALU.add), r=[dq], w=[dq])
                    Bc = t1
                    k.op(V, lambda e: e.tensor_sub(Gi[:], Gi[:], Bc[:]), r=[dq, d_G], w=[dq, d_G])
                    at = Gi
                    k.op(V, lambda e: e.tensor_tensor_scan(t2[:], at[:], at[:], 0.0, ALU.max, ALU.max), r=[dq, d_G], w=[dq])
                    ut = t2
                    ut3 = ut[:].rearrange("p (c s) -> p c s", s=64)
                    at3 = at[:].rearrange("p (c s) -> p c s", s=64)
                    Bc3 = Bc[:].rearrange("p (c s) -> p c s", s=64)
                    k.op(V, lambda e: e.memset(mt[:, 0:1], 0.0), w=[dq])
                    if NCH > 1:
                        k.op(V, lambda e: e.tensor_copy(mt[:, 1:NCH], ut3[:, 0:NCH - 1, 63]), r=[dq], w=[dq])
                    uL = ut3[:, :, 63]
                    mtb = mt[:, :].unsqueeze(2).to_broadcast([4, NCH, 64])
                    k.op(V, lambda e: e.tensor_sub(dec[:], mt[:], uL), r=[dq], w=[dq])
                    k.op("act", lambda e: e.activation(dec[:], dec[:], AF.Exp), r=[dq], w=[dq])
                    sq0 = sq[:, 0, :].rearrange("p (c s) -> p c s", s=64)
                    sq1 = sq[:, 1, :].rearrange("p (c s) -> p c s", s=64)
                    sq2 = sq[:, 2, :].rearrange("p (c s) -> p c s", s=64)
                    k.op(V, lambda e: e.tensor_sub(sq0, at3, mtb), r=[dq, d_G], w=[dq])
                    k.op(V, lambda e: e.tensor_scalar(sq[:, 0, :], sq[:, 0, :], 80.0, -LN16, ALU.min, ALU.add), r=[dq], w=[dq])
                    k.op("act", lambda e: e.activation(sq[:, 0, :], sq[:, 0, :], AF.Exp), r=[dq], w=[dq])
                    k.op(V, lambda e: e.tensor_tensor(sq1, sq0, dec[:, :].unsqueeze(2).to_broadcast([4, NCH, 64]), ALU.mult),
                         r=[dq], w=[dq])
                    k.op(V, lambda e: e.tensor_tensor(sq2, Bc3, mtb, ALU.add), r=[dq], w=[dq])
                    k.op(V, lambda e: e.tensor_scalar(sq[:, 2, :], sq[:, 2, :], -1.0, 80.0, ALU.mult, ALU.min), r=[dq], w=[dq])
                    k.op("act", lambda e: e.activation(sq[:, 2, :], sq[:, 2, :], AF.Exp), r=[dq], w=[dq])
                    d_seq = Dep()
                    k.dma("sp", s_dec, dec[:], r=[dq], w=[d_seq])
                    d_TT = Dep()
                    for q in range(3):
                        c0 = 0
                        while c0 < NCH:
                            ncc = min(128, NCH - c0)
                            bk, bd = pf.next()

                            def f(pe, bk=bk, q=q, c0=c0, ncc=ncc):
                                for cc in range(ncc):
                                    c = c0 + cc
                                    ins = pe.transpose(bk[0:64, cc * 4:cc * 4 + 4], sq[0:4, q, c * 64:(c + 1) * 64], identf[0:4, 0:4])
                                return ins
                            k.op("pe", f, r=[dq, d_c], w=[bd])
                            k.op("act", lambda e, bk=bk, q=q, c0=c0, ncc=ncc: e.copy(
                                TT[:, q, c0:c0 + ncc, :], bk[0:64, 0:ncc * 4].rearrange("p (c h) -> p c h", h=4)), r=[bd], w=[d_TT])
                            c0 += ncc
                    k.dma("sp", decbc[:], s_dec.rearrange("h c -> (h c)").partition_broadcast(128), r=[d_seq], w=[d_TT])
                gscope.close()
                if dbg:
                    k.dma("sp", s_TT, TT[:].rearrange("p a b c -> p (a b c)"), r=[d_TT], w=[Dep()])
                if stop <= 2:
                    raise _Stop()
                with k.scope():
                    qT = k.sb("qT", [128, 8, TO], BF16)
                    kT = k.sb("kT", [128, 8, TA], BF16)
                    cw = k.sb("cw", [128, 16, 4], F32); cb = k.sb("cb", [128, 16], F32)
                    ngb = k.sb("ngb", [64, 1024], F32)
                    d_cw, d_qT, d_kT = Dep(), Dep(), Dep()
                    k.dma("sp", cw[:], cw_d, w=[d_cw]); k.dma("sp", cb[:], cb_d, w=[d_cw])
                    k.dma("sp", ngb[:], ng_d.partition_broadcast(64), w=[d_cw])
                    cscope = k.scope()
                    cscope.__enter__()
                    cin = Ring([(k.sb("cin", [128, 3 + TA], BF16), Dep()) for _ in range(3)])
                    Dg = k.sb("Dg", [128, 16, 4, 128], BF16)
                    d_Dg = Dep()
                    for fc in range(16):
                        for j in range(4):
                            k.op("dve", lambda e, fc=fc, j=j: e.tensor_scalar_mul(Dg[:, fc, j, :], identf[:], cw[:, fc, j:j + 1]),
                                 r=[d_cw, d_c], w=[d_Dg])
                    for fc in list(range(8, 16)) + list(range(8)):
                        isq = fc < 8
                        lo = (TP - 128) if isq else 0
                        o0 = TP if isq else 0
                        n = TA - o0
                        ci, cd = cin.next()
                        if not isq:
                            k.op("pool", lambda e, ci=ci: e.memset(ci[:, 0:3], 0.0), w=[cd])
                        k.dma("sp", ci[:, 3 + lo:3 + TA], s_qkaT[fc * 128:(fc + 1) * 128, lo:TA], r=[d_scr["qka"]], w=[cd])
                        tb = 0
                        while tb < n:
                            nn = min(512, n - tb)
                            bk, bd = pf.next()

                            def f(pe, bk=bk, ci=ci, fc=fc, o0=o0, tb=tb, nn=nn):
                                for j in range(4):
                                    ins = pe.matmul(bk[:, 0:nn], Dg[:, fc, j, :], ci[:, o0 + tb + j:o0 + tb + j + nn],
                                                    start=(j == 0), stop=(j == 3))
                                return ins
                            k.op("pe", f, r=[cd, d_Dg], w=[bd])
                            if isq:
                                k.op("act", lambda e, bk=bk, fc=fc, tb=tb, nn=nn: e.activation(
                                    qT[:, fc, tb:tb + nn], bk[:, 0:nn], AF.Silu, bias=cb[:, fc:fc + 1], scale=1.0),
                                    r=[bd, d_cw], w=[d_qT])
                            else:
                                k.op("act", lambda e, bk=bk, fc=fc, tb=tb, nn=nn: e.activation(
                                    kT[:, fc - 8, tb:tb + nn], bk[:, 0:nn], AF.Silu, bias=cb[:, fc:fc + 1], scale=1.0),
                                    r=[bd, d_cw], w=[d_kT])
                            tb += nn
                    cscope.__exit__(None, None, None)
                    a_S = Ring([banks[0], banks[1]])
                    bO, bOd = banks[2]
                    bO1, bO1d = banks[3]
                    m_U = Ring(banks[4:5])
                    bSN, bSNd = banks[5]
                    bNN, bNNd = banks[6]
                    pb_all = pb
                    pb = Ring([(banks[7][0].bitcast(BF16), banks[7][1])])
                    Cst = [k.sb("Cst", [128, 2, 257], F32) for _ in range(4)]
                    hblk = Ring([(k.sb("hblk", [128, 8, HB], BF16), Dep()) for _ in range(1)])
                    Cbf = [k.sb("Cbf", [128, 2, 257], BF16) for _ in range(4)]
                    d_C = [Dep() for _ in range(4)]
                    d_Cbf = [Dep() for _ in range(4)]
                    for h in range(4):
                        k.op("pool", lambda e, h=h: e.memset(Cst[h][:], 0.0), w=[d_C[h]])
                        k.op("pool", lambda e, h=h: e.memset(Cbf[h][:], 0.0), w=[d_Cbf[h]])
                    vch_items = []
                    for _ in range(3):
                        vt = k.sb("vch", [64, 4, 257], BF16)
                        vd = Dep()
                        k.op("pool", lambda e, vt=vt: e.memset(vt[:, :, 256:257], 1.0), w=[vd])
                        vch_items.append((vt, vd))
                    vch = Ring(vch_items)
                    sor = Ring([(k.sb("so", [64, 1024], BF16), Dep()) for _ in range(1)])
                    kwr = Ring([(k.sb("kw", [64, 256], BF16), Dep()) for _ in range(3)])
                    Wtr = Ring([(k.sb("Wt", [64, 64], BF16), Dep()) for _ in range(3)])
                    Nsr = Ring([(k.sb("Ns", [64, 4, 257], F32), Dep()) for _ in range(2)])
                    hgr = Ring([(k.sb("hg", [64, 4, 256], F32), Dep()) for _ in range(1)])
                    hbr = Ring([(k.sb("hb", [64, 1024], BF16), Dep()) for _ in range(2)])
                    smr = Ring([(k.sb("sm", [64, 64], F32), Dep()) for _ in range(2)])
                    lamt = k.sb("lamt", [128, 4, 64], F32)
                    lsm = k.sb("lsm", [128, 8], F32)
                    gnb = k.sb("gnb", [128, 128], F32)
                    d_l = Dep()
                    k.dma("sp", lamt[:], lam_d.rearrange("a b -> (a b)").partition_broadcast(128).rearrange("p (a b) -> p a b", a=4), w=[d_l])
                    k.dma("sp", gnb[:], dg_d.partition_broadcast(128), w=[d_l])
                    k.op("dve", lambda e: e.tensor_tensor(lamt[:, 0, :], lamt[:, 0, :], lamt[:, 1, :], ALU.mult), r=[d_l], w=[d_l])
                    k.op("dve", lambda e: e.tensor_tensor(lamt[:, 2, :], lamt[:, 2, :], lamt[:, 3, :], ALU.mult), r=[d_l], w=[d_l])
                    k.op("dve", lambda e: e.reduce_sum(lsm[:, 0:1], lamt[:, 0, :], AX.X), r=[d_l], w=[d_l])
                    k.op("dve", lambda e: e.reduce_sum(lsm[:, 1:2], lamt[:, 2, :], AX.X), r=[d_l], w=[d_l])
                    k.op("act", lambda e: e.activation(lsm[:, 2:4], lsm[:, 0:2], AF.Exp), r=[d_l], w=[d_l])
                    k.op("dve", lambda e: e.tensor_sub(lsm[:, 4:5], lsm[:, 3:4], lsm[:, 2:3]), r=[d_l], w=[d_l])
                    k.op("dve", lambda e: e.tensor_scalar_add(lsm[:, 5:6], lsm[:, 4:5], -0.2), r=[d_l], w=[d_l])
                    k.op("dve", lambda e: e.tensor_scalar_mul(gnb[:], gnb[:], 0.8), r=[d_l], w=[d_l])
                    neglam = lsm[:, 5:6]
                    kb_items = []
                    for _ in range(1):
                        kt_ = k.sb("kbT", [128, 2, TA], BF16)
                        kd_ = Dep()
                        k.op("pool", lambda e, kt_=kt_: e.memset(kt_[64:128, 0, :], 0.0), w=[kd_])
                        k.op("pool", lambda e, kt_=kt_: e.memset(kt_[0:64, 1, :], 0.0), w=[kd_])
                        kb_items.append((kt_, kd_))
                    kbr = Ring(kb_items)
                    qbr = Ring([(k.sb("qbT", [128, TO], BF16), Dep()) for _ in range(1)])
                    vb_items = []
                    for _ in range(1):
                        vt = k.sb("vbe", [128, NTA, 129], BF16)
                        vd = Dep()
                        k.op("dve", lambda e, vt=vt: e.tensor_copy(vt[:, :, 128], validb[:, :]), r=[d_c], w=[vd])
                        vb_items.append((vt, vd))
                    vbr = Ring(vb_items)
                    PTr = Ring([(k.sb("PT", [128, 256], BF16), Dep()) for _ in range(4)])
                    o1r = Ring([(k.sb("o1", [128, 128], F32), Dep()) for _ in range(2)])
                    o2r = Ring([(k.sb("o2", [128, 128], F32), Dep()) for _ in range(2)])
                    obr = Ring([(k.sb("ob", [128, 128], BF16), Dep()) for _ in range(4)])
                    s8r = Ring([(k.sb("s8", [128, 8], F32), Dep()) for _ in range(2)])
                    Osr = Ring([(k.sb("Os", [128, 2, 129], F32), Dep()) for _ in range(2)])
                    oblk = Ring([(k.sb("oblk", [128, HB], BF16), Dep()) for _ in range(2)])

                    def mlstm_gen():
                        st_m = {'hb': None, 'fin': None}
                        for c in range(NCH):
                            own = c >= CH0
                            tq = (c - CH0) * 64
                            v_t, v_d = vch.next()
                            k.dma("sp", v_t[:, :, 0:256], s_va[c * 64:(c + 1) * 64, :].rearrange("s (h d) -> s h d", h=4),
                                  r=[d_scr["va"]], w=[v_d])
                            if own:
                                so_t, so_d = sor.next()
                                k.dma("sp", so_t[:], s_oa[tq:tq + 64, :], r=[d_scr["oa"]], w=[so_d])
                                Ns_t, Ns_d = Nsr.next()
                            for h in range(4):
                                pt, pd = pb.next()

                                def f(pe, pt=pt, h=h, c=c):
                                    for j in range(2):
                                        ins = pe.transpose(pt[0:64, j * 128:(j + 1) * 128], kT[:, h * 2 + j, c * 64:(c + 1) * 64], identb[:])
                                    return ins
                                k.op("pe", f, r=[d_kT, d_c], w=[pd])
                                kw_t, kw_d = kwr.next()
                                k.op("act", lambda e, kw_t=kw_t, pt=pt, h=h, c=c: e.activation(
                                    kw_t[:], pt[0:64, 0:256], AF.Identity, scale=TT[:, 1, c, h:h + 1]), r=[pd, d_TT], w=[kw_d])
                                yield
                                bU, bUd = m_U.next()

                                def f(pe, bU=bU, kw_t=kw_t, v_t=v_t, h=h):
                                    for j in range(2):
                                        pe.matmul(bU[:, j * 256:(j + 1) * 256], kw_t[:, j * 128:(j + 1) * 128], v_t[:, h, 0:256],
                                                  start=True, stop=True)
                                    for j in range(2):
                                        ins = pe.matmul(bSN[:, 400 + j:401 + j], kw_t[:, j * 128:(j + 1) * 128], v_t[:, h, 256:257],
                                                        start=True, stop=True)
                                    return ins
                                k.op("pe", f, r=[kw_d, v_d], w=[bUd, bSNd])
                                if not own:
                                    yield
                                if own:
                                    def f(pe, h=h, c=c, tq=tq):
                                        for j in range(2):
                                            ins = pe.matmul(bSN[0:64, 320:384], kT[:, h * 2 + j, c * 64:(c + 1) * 64],
                                                            qT[:, h * 2 + j, tq:tq + 64], start=(j == 0), stop=(j == 1))
                                        return ins
                                    k.op("pe", f, r=[d_kT, d_qT], w=[bSNd])
                                    W_t, W_d = Wtr.next()
                                    k.op("dve", lambda e, W_t=W_t, h=h, c=c: e.scalar_tensor_tensor(
                                        W_t[:], bSN[0:64, 320:384], TT[:, 0, c, h:h + 1], mask64[:], ALU.mult, ALU.mult),
                                        r=[bSNd, d_TT, d_c], w=[W_d])
                                    yield

                                    def f(pe, W_t=W_t, v_t=v_t, h=h, tq=tq):
                                        for j in range(2):
                                            pe.matmul(bNN[0:64, 0:257], qT[:, h * 2 + j, tq:tq + 64], Cbf[h][:, j, :],
                                                      start=(j == 0), stop=False)
                                        return pe.matmul(bNN[0:64, 0:257], W_t[:], v_t[:, h, :], start=False, stop=True)
                                    k.op("pe", f, r=[d_qT, d_Cbf[h], W_d, v_d], w=[bNNd])
                                    k.op("act", lambda e, Ns_t=Ns_t, h=h: e.copy(Ns_t[:, h, :], bNN[0:64, 0:257]),
                                         r=[bNNd], w=[Ns_d])
                                    yield
                                dsc = decbc[:, h * NCH + c:h * NCH + c + 1]
                                k.op("dve", lambda e, h=h, bU=bU, dsc=dsc: e.scalar_tensor_tensor(
                                    Cst[h][:, :, 0:256], Cst[h][:, :, 0:256], dsc,
                                    bU[:, 0:512].rearrange("p (j n) -> p j n", j=2), ALU.mult, ALU.add),
                                    r=[bUd, d_TT], w=[d_C[h]])
                                k.op("dve", lambda e, h=h, dsc=dsc: e.scalar_tensor_tensor(
                                    Cst[h][:, :, 256:257], Cst[h][:, :, 256:257], dsc,
                                    bSN[:, 400:402].rearrange("p (j n) -> p j n", j=2), ALU.mult, ALU.add),
                                    r=[bSNd, d_TT], w=[d_C[h]])
                                if own or c == CH0 - 1:
                                    k.op("act", lambda e, h=h: e.copy(Cbf[h][:], Cst[h][:]), r=[d_C[h]], w=[d_Cbf[h]])
                                if h == 1 and st_m['fin'] is not None:
                                    st_m['fin']()
                                    st_m['fin'] = None
                                yield
                            if own:
                                sm_t, sm_d = smr.next()
                                k.op("act", lambda e, sm_t=sm_t, Ns_t=Ns_t: e.activation(
                                    sm_t[:, 0:4], Ns_t[:, :, 256], AF.Abs), r=[Ns_d], w=[sm_d])
                                k.op("dve", lambda e, sm_t=sm_t, c=c: e.tensor_tensor(
                                    sm_t[:, 0:4], sm_t[:, 0:4], TT[:, 2, c, :], ALU.max), r=[sm_d, d_TT], w=[sm_d])
                                k.op("dve", lambda e, sm_t=sm_t: e.reciprocal(sm_t[:, 0:4], sm_t[:, 0:4]), r=[sm_d], w=[sm_d])
                                hg_t, hg_d = hgr.next()
                                k.op("dve", lambda e, hg_t=hg_t, Ns_t=Ns_t, sm_t=sm_t: e.tensor_tensor(
                                    hg_t[:], Ns_t[:, :, 0:256], sm_t[:, 0:4].unsqueeze(2).to_broadcast([64, 4, 256]), ALU.mult),
                                    r=[Ns_d, sm_d], w=[hg_d])
                                k.op("dve", lambda e, hg_t=hg_t, so_t=so_t: e.tensor_tensor(
                                    hg_t[:], hg_t[:], so_t[:].rearrange("s (h d) -> s h d", h=4), ALU.mult),
                                    r=[so_d, hg_d], w=[hg_d])

                                def f(e, hg_t=hg_t, sm_t=sm_t):
                                    for hh in range(4):
                                        ins = e.bn_stats(sm_t[:, 4 + 6 * hh:10 + 6 * hh], hg_t[:, hh, :])
                                    return ins
                                k.op("dve", f, r=[hg_d], w=[sm_d])

                                def f(e, sm_t=sm_t):
                                    for hh in range(4):
                                        ins = e.bn_aggr(sm_t[:, 28 + 2 * hh:30 + 2 * hh], sm_t[:, 4 + 6 * hh:10 + 6 * hh])
                                    return ins
                                k.op("dve", f, r=[sm_d], w=[sm_d])
                                mv = sm_t[:, 28:36].rearrange("s (h k) -> s h k", h=4)
                                k.op("act", lambda e, sm_t=sm_t, mv=mv: e.activation(
                                    sm_t[:, 36:40], mv[:, :, 1], AF.Ln, bias=epsc[0:64, 0:1], scale=1.0), r=[sm_d, d_c], w=[sm_d])
                                k.op("act", lambda e, sm_t=sm_t: e.activation(
                                    sm_t[:, 36:40], sm_t[:, 36:40], AF.Exp, scale=-0.5), r=[sm_d], w=[sm_d])
                                k.op("dve", lambda e, hg_t=hg_t, mv=mv: e.tensor_tensor(
                                    hg_t[:], hg_t[:], mv[:, :, 0:1].to_broadcast([64, 4, 256]), ALU.subtract),
                                    r=[sm_d, hg_d], w=[hg_d])
                                k.op("dve", lambda e, hg_t=hg_t, sm_t=sm_t: e.tensor_tensor(
                                    hg_t[:], hg_t[:], sm_t[:, 36:40].unsqueeze(2).to_broadcast([64, 4, 256]), ALU.mult),
                                    r=[sm_d, hg_d], w=[hg_d])
                                hb_t, hb_d = hbr.next()
                                k.op("dve", lambda e, hg_t=hg_t, hb_t=hb_t: e.tensor_tensor(
                                    hb_t[:], hg_t[:].rearrange("s h d -> s (h d)"), ngb[:], ALU.mult),
                                    r=[hg_d, d_cw], w=[hb_d])
                                def mfin(hb_t=hb_t, hb_d=hb_d, tq=tq):
                                    pt, pd = pb.next()

                                    def f(pe):
                                        for fc in range(8):
                                            ins = pe.transpose(pt[:, fc * 64:(fc + 1) * 64], hb_t[:, fc * 128:(fc + 1) * 128], identb[0:64, 0:64])
                                        return ins
                                    k.op("pe", f, r=[hb_d, d_c], w=[pd])
                                    if tq % HB == 0:
                                        st_m['hb'] = hblk.next()
                                    hk_t, hk_d = st_m['hb']
                                    k.op("act", lambda e: e.copy(
                                        hk_t[:, :, tq % HB:tq % HB + 64], pt[:, 0:512].rearrange("p (f s) -> p f s", f=8)), r=[pd], w=[hk_d])
                                    if (tq + 64) % HB == 0:
                                        tb0 = tq + 64 - HB
                                        k.dma("sp", s_haT[:, tb0:tb0 + HB].rearrange("(f p) t -> p f t", p=128), hk_t[:], r=[hk_d], w=[d_haT])
                                st_m['fin'] = mfin
                            yield
                        if st_m['fin'] is not None:
                            st_m['fin']()
                            st_m['fin'] = None
                        yield

                    def attn_gen():
                        st_a = {'ob': None}
                        deferred = []
                        for h in range(8):
                            kb_t, kb_d = kbr.next(); qb_t, qb_d = qbr.next(); vb_t, vb_d = vbr.next()
                            k.dma("sp", kb_t[0:64, 0, :], s_qkbT[1024 + h * 128:1024 + h * 128 + 64, :], r=[d_scr["qkb"]], w=[kb_d])
                            k.dma("sp", kb_t[64:128, 1, :], s_qkbT[1024 + h * 128 + 64:1024 + (h + 1) * 128, :], r=[d_scr["qkb"]], w=[kb_d])
                            k.dma("sp", qb_t[:], s_qkbT[h * 128:(h + 1) * 128, TP:TA], r=[d_scr["qkb"]], w=[qb_d])
                            for t8 in range(0, NTA, 8):
                                t9 = min(NTA, t8 + 8)
                                k.dma("sp", vb_t[:, t8:t9, 0:128],
                                      s_vb[t8 * 128:t9 * 128, h * 128:(h + 1) * 128].rearrange("(t p) d -> p t d", p=128),
                                      r=[d_scr["vb"]], w=[vb_d])
                            items = []
                            for qi in range(NTO):
                                qt = NTP + qi
                                kt_lo = 0
                                while kt_lo < qt and SLOPES[h] * (127 - 128 * (qt - kt_lo)) < -ACLAMP:
                                    kt_lo += 1
                                for kt in range(kt_lo, qt + 1):
                                    items.append((qi, qt, kt, kt_lo))

                            def emit_qk(it, h=h, kb_t=kb_t, qb_t=qb_t, kb_d=kb_d, qb_d=qb_d):
                                qi, qt, kt, kt_lo = it
                                bSt, sd = a_S.next()
                                off = 0
                                diag = (kt == qt)

                                def f(pe):
                                    for m in range(2):
                                        ins = pe.matmul(bSt[:, off + m * 128:off + (m + 1) * 128], kb_t[:, m, kt * 128:(kt + 1) * 128],
                                                        qb_t[:, qi * 128:(qi + 1) * 128], start=True, stop=(not diag))
                                        if diag:
                                            ins = pe.matmul(bSt[:, off + m * 128:off + (m + 1) * 128], identb[:], dgb[:, h, :], start=False, stop=True)
                                    return ins
                                k.op("pe", f, r=[kb_d, qb_d, d_c], w=[sd])
                                return bSt, sd
                            def do_pv(it, P_t, P_d, h=h, vb_t=vb_t, vb_d=vb_d):
                                qi, qt, kt, kt_lo = it

                                def f(pe, P_t=P_t, vb_t=vb_t, kt=kt, qt=qt, kt_lo=kt_lo):
                                    pe.matmul(bO[:, 0:129], P_t[:, 0:128], vb_t[:, kt, :], start=(kt == kt_lo), stop=(kt == qt))
                                    return pe.matmul(bO1[:, 0:129], P_t[:, 128:256], vb_t[:, kt, :], start=(kt == kt_lo), stop=(kt == qt))
                                k.op("pe", f, r=[P_d, vb_d], w=[bOd, bO1d])
                                if kt != qt:
                                    return
                                s8, s8d = s8r.next()
                                Os, Osd = Osr.next()
                                k.op("act", lambda e, Os=Os: e.copy(Os[:, 0, :], bO[:, 0:129]), r=[bOd], w=[Osd])
                                k.op("dve", lambda e, Os=Os: e.tensor_copy(Os[:, 1, :], bO1[:, 0:129]), r=[bO1d], w=[Osd])
                                k.op("dve", lambda e, s8=s8, Os=Os: e.reciprocal(s8[:, 0:2], Os[:, :, 128]), r=[Osd], w=[s8d])
                                k.op("dve", lambda e, s8=s8: e.tensor_tensor(s8[:, 2:3], s8[:, 1:2], neglam, ALU.mult),
                                     r=[s8d, d_l], w=[s8d])
                                o1, o1d = o1r.next(); o2, o2d = o2r.next()
                                k.op("dve", lambda e, o1=o1, s8=s8, Os=Os: e.tensor_scalar_mul(o1[:], Os[:, 0, 0:128], s8[:, 0:1]),
                                     r=[Osd, s8d], w=[o1d])
                                k.op("dve", lambda e, o1=o1, s8=s8, Os=Os: e.scalar_tensor_tensor(
                                    o1[:], Os[:, 1, 0:128], s8[:, 2:3], o1[:], ALU.mult, ALU.add), r=[Osd, s8d], w=[o1d])
                                k.op("pool", lambda e, o1=o1, o2=o2: e.tensor_tensor(o2[:], o1[:], o1[:], ALU.mult), r=[o1d], w=[o2d])
                                k.op("dve", lambda e, o2=o2, s8=s8: e.reduce_sum(s8[:, 3:4], o2[:], AX.X), r=[o2d], w=[s8d])
                                k.op("dve", lambda e, s8=s8: e.tensor_scalar(s8[:, 4:5], s8[:, 3:4], 1.0 / 128.0, EPS, ALU.mult, ALU.add),
                                     r=[s8d], w=[s8d])
                                k.op("act", lambda e, s8=s8: e.activation(s8[:, 5:6], s8[:, 4:5], AF.Ln), r=[s8d], w=[s8d])
                                k.op("act", lambda e, s8=s8: e.activation(s8[:, 5:6], s8[:, 5:6], AF.Exp, scale=-0.5),
                                     r=[s8d], w=[s8d])
                                ob, obd = obr.next()
                                k.op("dve", lambda e, ob=ob, o1=o1, s8=s8: e.scalar_tensor_tensor(
                                    ob[:], o1[:], s8[:, 5:6], gnb[:], ALU.mult, ALU.mult), r=[o1d, s8d, d_l], w=[obd])
                                def fin(ob=ob, obd=obd, qi=qi, h=h):
                                    pt, pd = pb.next()
                                    k.op("pe", lambda pe: pe.transpose(pt[:, 0:128], ob[:], identb[:]), r=[obd, d_c], w=[pd])
                                    tq = qi * 128
                                    if tq % HB == 0:
                                        st_a['ob'] = oblk.next()
                                    ok_t, ok_d = st_a['ob']
                                    k.op("act", lambda e: e.copy(ok_t[:, tq % HB:tq % HB + 128], pt[:, 0:128]),
                                         r=[pd], w=[ok_d])
                                    if (tq + 128) % HB == 0:
                                        tb0 = tq + 128 - HB
                                        k.dma("sp", s_obT[h * 128:(h + 1) * 128, tb0:tb0 + HB], ok_t[:], r=[ok_d], w=[d_obT])
                                deferred.append([3, fin])
                            nxt = emit_qk(items[0])
                            pend = None
                            for j, it in enumerate(items):
                                qi, qt, kt, kt_lo = it
                                bSt, sd = nxt
                                off = 0
                                if j + 1 < len(items):
                                    nxt = emit_qk(items[j + 1])
                                diag = (kt == qt)
                                P_t, P_d = PTr.next()
                                bias_ap = zero1[:, 0:1] if diag else abt[:, h, qt - kt:qt - kt + 1]
                                k.op("act", lambda e, P_t=P_t, off=off, bias_ap=bias_ap, bSt=bSt: e.activation(
                                    P_t[:], bSt[:, off:off + 256], AF.Exp, bias=bias_ap, scale=0.125), r=[sd, d_c], w=[P_d])

                                if pend is not None:
                                    do_pv(*pend)
                                pend = (it, P_t, P_d)
                                for dfr in list(deferred):
                                    dfr[0] -= 1
                                    if dfr[0] <= 0:
                                        deferred.remove(dfr)
                                        dfr[1]()
                                yield
                            if pend is not None:
                                do_pv(*pend)
                            yield
                        for dfr in list(deferred):
                            dfr[1]()
                        deferred.clear()
                        yield

                    gm = mlstm_gen()
                    ga_ = attn_gen()
                    if stop <= 3:
                        for _ in gm:
                            pass
                        raise _Stop()
                    if stop <= 3.5:
                        for _ in ga_:
                            pass
                        raise _Stop()
                    n_m = (CH0 * (4 * 3 + 1)) + ((NCH - CH0) * (4 * 4 + 1))
                    n_a = 0
                    for h_ in range(8):
                        for qi_ in range(NTO):
                            qt_ = NTP + qi_
                            lo_ = 0
                            while lo_ < qt_ and SLOPES[h_] * (127 - 128 * (qt_ - lo_)) < -ACLAMP:
                                lo_ += 1
                            n_a += qt_ + 1 - lo_
                    done_m = done_a = 0
                    m_alive = a_alive = True
                    while m_alive or a_alive:
                        if m_alive:
                            try:
                                next(gm); done_m += 1
                            except StopIteration:
                                m_alive = False
                        if ILV == 0:
                            tgt = n_a + 10 if not m_alive else 0
                        else:
                            tgt = n_a + 10 if not m_alive else (done_m * n_a) // n_m
                        while a_alive and done_a < tgt:
                            try:
                                next(ga_); done_a += 1
                            except StopIteration:
                                a_alive = False
                    for g_ in (gm, ga_):
                        for _ in g_:
                            pass
                    pb = pb_all
                if stop <= 4:
                    raise _Stop()
                with k.scope():
                    war = Ring([(k.sb("wa", [128, 8, 512], BF16), Dep()) for _ in range(2)])
                    wbr = Ring([(k.sb("wb", [128, 8, 512], BF16), Dep()) for _ in range(2)])
                    gar = Ring([(k.sb("ga", [128, 512], BF16), Dep()) for _ in range(3)])
                    gbr = Ring([(k.sb("gb", [128, 512], BF16), Dep()) for _ in range(3)])
                    m1r = Ring([(k.sb("m1", [128, 512], F32), Dep()) for _ in range(3)])
                    m2r = Ring([(k.sb("m2", [128, 512], F32), Dep()) for _ in range(3)])
                    hkr = Ring([(k.sb("hk", [128, 8, HB], BF16), Dep()) for _ in range(2)])
                    okr = Ring([(k.sb("ok", [128, 8, HB], BF16), Dep()) for _ in range(2)])
                    mgr = Ring([(k.sb("mgo", [128, 512], BF16), Dep()) for _ in range(3)])
                    st_e1 = {"db": None, "tb": None}

                    def e1_unit(db, tb, n, g):
                        with k.atomic():
                            if st_e1["db"] != db:
                                wa_t, wa_d = war.next(); wb_t, wb_d = wbr.next()
                                k.dma("pool", wa_t[:], w_a.rearrange("(c p) n -> p c n", p=128)[:, :, db * 512:(db + 1) * 512], w=[wa_d])
                                k.dma("pool", wb_t[:], w_b.rearrange("(c p) n -> p c n", p=128)[:, :, db * 512:(db + 1) * 512], w=[wb_d])
                                st_e1["db"] = db
                                st_e1["w"] = (wa_t, wa_d, wb_t, wb_d)
                            if st_e1["tb"] != (db, tb):
                                hk, hkd = hkr.next(); ok, okd = okr.next()
                                k.dma("sp", hk[:, :, 0:n], s_haT[:, tb:tb + n].rearrange("(f p) t -> p f t", p=128), r=[d_haT], w=[hkd])
                                k.dma("sp", ok[:, :, 0:n], s_obT[:, tb:tb + n].rearrange("(f p) t -> p f t", p=128), r=[d_obT], w=[okd])
                                st_e1["tb"] = (db, tb)
                                st_e1["h"] = (hk, hkd, ok, okd)
                        wa_t, wa_d, wb_t, wb_d = st_e1["w"]
                        hk, hkd, ok, okd = st_e1["h"]
                        dc = db * 4 + g
                        ga_t, ga_d = gar.next(); gb_t, gb_d = gbr.next()
                        k.dma("sp", ga_t[:, 0:n], s_gT[dc * 128:(dc + 1) * 128, tb:tb + n], r=[d_scr["g"]], w=[ga_d])
                        k.dma("sp", gb_t[:, 0:n], s_gT[2048 + dc * 128:2048 + (dc + 1) * 128, tb:tb + n], r=[d_scr["g"]], w=[gb_d])
                        bA, bAd = pf.next(); bB, bBd = pf.next()

                        def f(pe):
                            for fc in range(8):
                                ins = pe.matmul(bA[:, 0:n], wa_t[:, fc, g * 128:(g + 1) * 128], hk[:, fc, 0:n],
                                                start=(fc == 0), stop=(fc == 7))
                            return ins
                        k.op("pe", f, r=[wa_d, hkd], w=[bAd])

                        def f(pe):
                            for fc in range(8):
                                ins = pe.matmul(bB[:, 0:n], wb_t[:, fc, g * 128:(g + 1) * 128], ok[:, fc, 0:n],
                                                start=(fc == 0), stop=(fc == 7))
                            return ins
                        k.op("pe", f, r=[wb_d, okd], w=[bBd])
                        m1, m1d = m1r.next(); m2, m2d = m2r.next()
                        k.op("dve", lambda e: e.tensor_tensor(m1[:, 0:n], bA[:, 0:n], ga_t[:, 0:n], ALU.mult),
                             r=[bAd, ga_d], w=[m1d])
                        k.op("dve", lambda e: e.tensor_tensor(m2[:, 0:n], bB[:, 0:n], gb_t[:, 0:n], ALU.mult),
                             r=[bBd, gb_d], w=[m2d])
                        mg_t, mg_dd = mgr.next()
                        k.op("pool", lambda e: e.tensor_tensor(mg_t[:, 0:n], m1[:, 0:n], m2[:, 0:n], ALU.add), r=[m1d, m2d], w=[mg_dd])
                        k.dma("sp", s_mgT[dc * 128:(dc + 1) * 128, tb:tb + n], mg_t[:, 0:n], r=[mg_dd], w=[d_mg])
                    units = []
                    for db in range(4):
                        tb = 0
                        while tb < TO:
                            n = min(512, TO - tb)
                            for g in range(4):
                                units.append(lambda db=db, tb=tb, n=n, g=g: e1_unit(db, tb, n, g))
                            tb += n
                    k.interleave(units, width=3)
            if stop <= 5:
                raise _Stop()
            with k.scope():
                wo = k.sb("wo", [128, DC, D], BF16)
                wr = k.sb("wr", [128, DC, 36], F32)
                br = k.sb("br", [1, 36], F32)
                l1g = k.sb("l1g", [128, D], F32); l1b = k.sb("l1b", [128, D], F32)
                cnt = k.sb("cnt", [128, 32], F32)
                d_wo, d_cnt = Dep(), Dep()
                for q4 in range(4):
                    k.dma("pool", wo[:, :, q4 * 512:(q4 + 1) * 512],
                          w_out.rearrange("(c p) n -> p c n", p=128)[:, :, q4 * 512:(q4 + 1) * 512], w=[d_wo])
                k.dma("sp", wr[:], wr_d.rearrange("(c p) n -> p c n", p=128), w=[d_wo])
                k.dma("sp", br[:], br_d, w=[d_wo])
                k.dma("sp", l1g[:], ln1g_d.partition_broadcast(128), w=[d_wo])
                k.dma("sp", l1b[:], ln1b_d.partition_broadcast(128), w=[d_wo])
                k.op("dve", lambda e: e.memset(cnt[:], 0.0), w=[d_cnt])
                xtr = Ring([(k.sb("xt", [128, D], F32), Dep()) for _ in range(2)])
                x1r = Ring([(k.sb("x1", [128, D], F32), Dep()) for _ in range(2)])
                hbr2 = Ring([(k.sb("hb2", [128, D], BF16), Dep()) for _ in range(2)])
                hTr = Ring([(k.sb("hT", [128, DC, 128], F32), Dep()) for _ in range(2)])
                rsr = Ring([(k.sb("rs", [128, 256], F32), Dep()) for _ in range(2)])
                mkr = Ring([(k.sb("mk", [128, 32], BF16), Dep()) for _ in range(2)])
                mbr = Ring([(k.sb("mgb", [128, DC, HB], BF16), Dep()) for _ in range(2)])
                st_e2 = {"mb": None}

                def e2_tile(ti):
                    if (ti * 128) % HB == 0:
                        with k.atomic():
                            st_e2["mb"] = mbr.next()
                            k.dma("sp", st_e2["mb"][0][:], s_mgT[:, ti * 128:ti * 128 + HB].rearrange("(c p) t -> p c t", p=128),
                                  r=[d_mg], w=[st_e2["mb"][1]])
                    mgb, mgbd = st_e2["mb"]
                    tloc = (ti * 128) % HB
                    xt, xtd = xtr.next(); x1, x1d = x1r.next()
                    k.dma("sp", xt[:], x[TP + ti * 128:TP + (ti + 1) * 128, :], w=[xtd])
                    for db in range(4):
                        bk, bd = pf.next()

                        def f(pe, bk=bk, mgb=mgb, tloc=tloc, db=db):
                            for c in range(DC):
                                ins = pe.matmul(bk[:, :], mgb[:, c, tloc:tloc + 128], wo[:, c, db * 512:(db + 1) * 512],
                                                start=(c == 0), stop=(c == DC - 1))
                            return ins
                        k.op("pe", f, r=[mgbd, d_wo], w=[bd])
                        k.op("dve", lambda e, x1=x1, xt=xt, bk=bk, db=db: e.scalar_tensor_tensor(
                            x1[:, db * 512:(db + 1) * 512], xt[:, db * 512:(db + 1) * 512], ALPHA, bk[:, :], ALU.mult, ALU.add),
                            r=[xtd, bd], w=[x1d])
                    rs, rsd = rsr.next()

                    def layer_norm(xx, xd, rs, rsd, g_t, b_t, gdep):
                        st = rs[:, 0:24].rearrange("p (g k) -> p g k", g=4)
                        def fbn(e):
                            for gg in range(4):
                                ins = e.bn_stats(rs[:, 6 * gg:6 * gg + 6], xx[:, gg * 512:(gg + 1) * 512])
                            return ins
                        k.op("dve", fbn, r=[xd], w=[rsd])
                        k.op("dve", lambda e: e.bn_aggr(rs[:, 24:26], rs[:, 0:24]), r=[rsd], w=[rsd])
                        k.op("act", lambda e: e.activation(rs[:, 26:27], rs[:, 25:26], AF.Ln, bias=epsc[:, 0:1], scale=1.0), r=[rsd, d_c], w=[rsd])
                        k.op("act", lambda e: e.activation(rs[:, 26:27], rs[:, 26:27], AF.Exp, scale=-0.5), r=[rsd], w=[rsd])
                        k.op("dve", lambda e: e.tensor_scalar(xx[:], xx[:], rs[:, 24:25], rs[:, 26:27], ALU.subtract, ALU.mult),
                             r=[rsd, xd], w=[xd])
                        k.op("pool", lambda e: e.tensor_tensor(xx[:], xx[:], g_t[:], ALU.mult), r=[xd, gdep], w=[xd])
                        k.op("pool", lambda e: e.tensor_tensor(xx[:], xx[:], b_t[:], ALU.add), r=[xd, gdep], w=[xd])
                    layer_norm(x1, x1d, rs, rsd, l1g, l1b, d_wo)
                    k.dma("sp", s_h1[ti * 128:(ti + 1) * 128, :], x1[:], r=[x1d], w=[d_h1])
                    hb2, hb2d = hbr2.next()
                    k.op("act", lambda e, hb2=hb2, x1=x1: e.copy(hb2[:], x1[:]), r=[x1d], w=[hb2d])
                    hT, hTd = hTr.next()
                    for g in range(4):
                        bk, bd = pf.next()

                        def f(pe, bk=bk, x1=x1, g=g):
                            for j in range(4):
                                c = g * 4 + j
                                ins = pe.transpose(bk[:, j * 128:(j + 1) * 128], x1[:, c * 128:(c + 1) * 128], identf[:])
                            return ins
                        k.op("pe", f, r=[x1d, d_c], w=[bd])
                        k.op("act", lambda e, hT=hT, bk=bk, g=g: e.copy(
                            hT[:, g * 4:(g + 1) * 4, :], bk[:, :].rearrange("p (j n) -> p j n", j=4)), r=[bd], w=[hTd])
                    bk, bd = pf.next()

                    def f(pe, bk=bk, hT=hT):
                        for c in range(DC):
                            pe.matmul(bk[:, 0:36], hT[:, c, :], wr[:, c, :], start=(c == 0), stop=False)
                        return pe.matmul(bk[:, 0:36], onesf[0:1, :], br[0:1, :], start=False, stop=True)
                    k.op("pe", f, r=[hTd, d_wo, d_c], w=[bd])
                    V = "dve"
                    lg = rs[:, 32:68]
                    k.op(V, lambda e, bk=bk, lg=lg: e.tensor_copy(lg, bk[:, 0:36]), r=[bd], w=[rsd])
                    g4 = rs[:, 32:36]; e32 = rs[:, 36:68]
                    gmx = rs[:, 68:69]; ngm = rs[:, 69:70]; ohg = rs[:, 70:74]; eg = rs[:, 74:78]; sg = rs[:, 78:79]; gp = rs[:, 79:80]
                    pen = rs[:, 80:84]; msk = rs[:, 84:116]; top8 = rs[:, 116:124]; oh1 = rs[:, 124:156]; oh2 = rs[:, 156:188]
                    dd = rs[:, 188:189]; p1 = rs[:, 189:190]; p2 = rs[:, 190:191]; tmp = rs[:, 192:224]
                    pk = rs[:, 224:225]; ek = rs[:, 225:226]; okk = rs[:, 226:227]; dsf = rs[:, 227:228]; posg = rs[:, 228:260 - 4]
                    k.op(V, lambda e: e.reduce_max(gmx, g4, AX.X), r=[rsd], w=[rsd])
                    k.op(V, lambda e: e.tensor_scalar_mul(ngm, gmx, -1.0), r=[rsd], w=[rsd])
                    k.op(V, lambda e: e.tensor_scalar(ohg, g4, gmx, None, ALU.is_equal), r=[rsd], w=[rsd])
                    k.op("act", lambda e: e.activation(eg, g4, AF.Exp, bias=ngm, scale=1.0), r=[rsd], w=[rsd])
                    k.op(V, lambda e: e.reduce_sum(sg, eg, AX.X), r=[rsd], w=[rsd])
                    k.op(V, lambda e: e.reciprocal(gp, sg), r=[rsd], w=[rsd])
                    k.op(V, lambda e: e.tensor_scalar(pen, ohg, BIG, -BIG, ALU.mult, ALU.add), r=[rsd], w=[rsd])
                    k.op(V, lambda e: e.tensor_tensor(msk.rearrange("p (g j) -> p g j", g=4), e32.rearrange("p (g j) -> p g j", g=4),
                                                      pen.unsqueeze(2).to_broadcast([128, 4, 8]), ALU.add), r=[rsd], w=[rsd])
                    k.op(V, lambda e: e.max(top8, msk), r=[rsd], w=[rsd])
                    k.op(V, lambda e: e.tensor_scalar(oh1, msk, top8[:, 0:1], None, ALU.is_equal), r=[rsd], w=[rsd])
                    k.op(V, lambda e: e.tensor_scalar(oh2, msk, top8[:, 1:2], None, ALU.is_equal), r=[rsd], w=[rsd])
                    k.op(V, lambda e: e.tensor_sub(dd, top8[:, 0:1], top8[:, 1:2]), r=[rsd], w=[rsd])
                    k.op("act", lambda e: e.activation(p1, dd, AF.Sigmoid), r=[rsd], w=[rsd])
                    k.op("act", lambda e: e.activation(p2, dd, AF.Sigmoid, scale=-1.0), r=[rsd], w=[rsd])
                    k.op(V, lambda e, ti=ti: e.tensor_tensor(wtab[:, ti, 0:1], p1, gp, ALU.mult), r=[rsd], w=[d_tab])
                    k.op(V, lambda e, ti=ti: e.tensor_tensor(wtab[:, ti, 1:2], p2, gp, ALU.mult), r=[rsd], w=[d_tab])
                    mk, mkd = mkr.next()
                    k.op(V, lambda e, mk=mk: e.tensor_tensor(mk[:], oh1, oh2, ALU.add), r=[rsd], w=[mkd])
                    bk2, bd2 = pf.next()

                    def f(pe, bk2=bk2, mk=mk):
                        pe.matmul(bk2[:, 0:32], ustr[:], mk[:], start=True, stop=True)
                        return pe.matmul(bk2[:, 32:64], onesb[:], mk[:], start=True, stop=True)
                    k.op("pe", f, r=[mkd, d_c], w=[bd2])
                    posg = rs[:, 224:256]
                    pk = rs[:, 28:29]; ek = rs[:, 29:30]; okk = rs[:, 30:31]; dsf = rs[:, 31:32]
                    with k.atomic():
                        k.op(V, lambda e, bk2=bk2: e.tensor_tensor(posg, bk2[:, 0:32], cnt[:], ALU.add), r=[bd2, d_cnt, rsd], w=[rsd])
                        k.op(V, lambda e, bk2=bk2: e.tensor_tensor(cnt[:], cnt[:], bk2[:, 32:64], ALU.add), r=[bd2, rsd], w=[d_cnt])
                    for kk, oh in ((0, oh1), (1, oh2)):
                        k.op(V, lambda e, oh=oh: e.tensor_tensor(tmp, oh, posg, ALU.mult), r=[rsd], w=[rsd])
                        k.op(V, lambda e: e.reduce_sum(pk, tmp, AX.X), r=[rsd], w=[rsd])
                        k.op(V, lambda e, oh=oh: e.tensor_tensor(tmp, oh, ecap[:], ALU.mult), r=[rsd, d_c], w=[rsd])
                        k.op(V, lambda e: e.reduce_sum(ek, tmp, AX.X), r=[rsd], w=[rsd])
                        k.op(V, lambda e: e.tensor_scalar(okk, pk, float(CAP), None, ALU.is_lt), r=[rsd], w=[rsd])
                        k.op(V, lambda e: e.tensor_tensor(dsf, ek, pk, ALU.add), r=[rsd], w=[rsd])
                        k.op(V, lambda e: e.tensor_scalar_add(dsf, dsf, -float(TRASH)), r=[rsd], w=[rsd])
                        k.op(V, lambda e: e.tensor_tensor(dsf, dsf, okk, ALU.mult), r=[rsd], w=[rsd])
                        k.op(V, lambda e: e.tensor_scalar_add(dsf, dsf, float(TRASH)), r=[rsd], w=[rsd])
                        k.op(V, lambda e, ti=ti, kk=kk: e.tensor_copy(dtab[:, ti, kk:kk + 1], dsf), r=[rsd], w=[d_tab])
                        k.dma("pool", s_Xg, hb2[:], r=[hb2d, d_tab, d_Xg], w=[d_Xg],
                              indirect=dict(out_offset=bass.IndirectOffsetOnAxis(ap=dtab[:, ti, kk:kk + 1], axis=0), in_offset=None))
                k.interleave([(lambda ti=ti: e2_tile(ti)) for ti in range(NTO)], width=NIL)

            if dbg:
                k.dma("sp", s_dtab, dtab[:].rearrange("p a b -> p (a b)"), r=[d_tab], w=[Dep()])
                k.dma("sp", s_wtab, wtab[:].rearrange("p a b -> p (a b)"), r=[d_tab], w=[Dep()])
            if stop <= 6:
                raise _Stop()
            NCT = CAP // 128
            with k.scope():
                wgr = Ring([(k.sb("wg", [128, DC, 512], BF16), Dep()) for _ in range(3)])
                wur = Ring([(k.sb("wu", [128, DC, 512], BF16), Dep()) for _ in range(3)])
                wdr = Ring([(k.sb("wd", [128, 4, D], BF16), Dep()) for _ in range(3)])
                Xer = Ring([(k.sb("Xe", [128, NCT, D], BF16), Dep()) for _ in range(2)])
                XTr = Ring([(k.sb("XT", [128, DC, CAP], BF16), Dep()) for _ in range(2)])
                ATr = Ring([(k.sb("AT", [128, 4, CAP], BF16), Dep()) for _ in range(2)])
                sgr = Ring([(k.sb("sgt", [128, CAP], F32), Dep()) for _ in range(2)])
                Ysr = Ring([(k.sb("Ys", [128, D], BF16), Dep()) for _ in range(2)])
                def expert_fn(e_):
                    wg, wgd = wgr.next(); wu, wud = wur.next(); wd, wdd = wdr.next()
                    k.dma("pool", wg[:], w_gate[e_].rearrange("(c p) n -> p c n", p=128), w=[wgd])
                    k.dma("pool", wu[:], w_up[e_].rearrange("(c p) n -> p c n", p=128), w=[wud])
                    k.dma("pool", wd[:], w_down[e_].rearrange("(c p) n -> p c n", p=128), w=[wdd])
                    Xe, Xed = Xer.next(); XT, XTd = XTr.next(); AT, ATd = ATr.next()
                    k.dma("sp", Xe[:], s_Xg[e_ * CAP:(e_ + 1) * CAP, :].rearrange("(t p) d -> p t d", p=128), r=[d_Xg], w=[Xed])
                    for t in range(NCT):
                        for g in range(4):
                            pt, pd = pb.next()

                            def f(pe, pt=pt, Xe=Xe, t=t, g=g):
                                for j in range(4):
                                    c = g * 4 + j
                                    ins = pe.transpose(pt[:, j * 128:(j + 1) * 128], Xe[:, t, c * 128:(c + 1) * 128], identb[:])
                                return ins
                            k.op("pe", f, r=[Xed, d_c], w=[pd])
                            eng = "dve" if (g % 2 == 0) else "act"
                            if eng == "dve":
                                k.op("dve", lambda e, XT=XT, pt=pt, g=g, t=t: e.tensor_copy(
                                    XT[:, g * 4:(g + 1) * 4, t * 128:(t + 1) * 128], pt[:, 0:512].rearrange("p (j n) -> p j n", j=4)),
                                    r=[pd], w=[XTd])
                            else:
                                k.op("act", lambda e, XT=XT, pt=pt, g=g, t=t: e.copy(
                                    XT[:, g * 4:(g + 1) * 4, t * 128:(t + 1) * 128], pt[:, 0:512].rearrange("p (j n) -> p j n", j=4)),
                                    r=[pd], w=[XTd])
                    for fcx in range(4):
                        bG, bGd = pf.next(); bU, bUd = pf.next()

                        def f(pe, bG=bG, wg=wg, XT=XT, fcx=fcx):
                            for c in range(DC):
                                ins = pe.matmul(bG[:, 0:CAP], wg[:, c, fcx * 128:(fcx + 1) * 128], XT[:, c, :], start=(c == 0), stop=(c == DC - 1))
                            return ins
                        k.op("pe", f, r=[wgd, XTd], w=[bGd])

                        def f(pe, bU=bU, wu=wu, XT=XT, fcx=fcx):
                            for c in range(DC):
                                ins = pe.matmul(bU[:, 0:CAP], wu[:, c, fcx * 128:(fcx + 1) * 128], XT[:, c, :], start=(c == 0), stop=(c == DC - 1))
                            return ins
                        k.op("pe", f, r=[wud, XTd], w=[bUd])
                        sg_t, sg_d = sgr.next()
                        k.op("act", lambda e, sg_t=sg_t, bG=bG: e.activation(sg_t[:], bG[:, 0:CAP], AF.Silu), r=[bGd], w=[sg_d])
                        k.op("dve", lambda e, AT=AT, sg_t=sg_t, bU=bU, fcx=fcx: e.tensor_tensor(AT[:, fcx, :], sg_t[:], bU[:, 0:CAP], ALU.mult),
                             r=[sg_d, bUd], w=[ATd])
                    for t in range(NCT):
                        Ys, Ysd = Ysr.next()
                        for db in range(4):
                            bk, bd = pf.next()

                            def f(pe, bk=bk, AT=AT, wd=wd, t=t, db=db):
                                for fcx in range(4):
                                    ins = pe.matmul(bk[:, :], AT[:, fcx, t * 128:(t + 1) * 128], wd[:, fcx, db * 512:(db + 1) * 512],
                                                    start=(fcx == 0), stop=(fcx == 3))
                                return ins
                            k.op("pe", f, r=[ATd, wdd], w=[bd])
                            if db % 2 == 0:
                                k.op("act", lambda e, Ys=Ys, bk=bk, db=db: e.copy(Ys[:, db * 512:(db + 1) * 512], bk[:, :]), r=[bd], w=[Ysd])
                            else:
                                k.op("dve", lambda e, Ys=Ys, bk=bk, db=db: e.tensor_copy(Ys[:, db * 512:(db + 1) * 512], bk[:, :]), r=[bd], w=[Ysd])
                        r0 = e_ * CAP + t * 128
                        k.dma("sp", s_Yg[r0:r0 + 128, :], Ys[:], r=[Ysd], w=[d_Yg])
                k.interleave([(lambda e_=e_: expert_fn(e_)) for e_ in range(NE)], width=NIL)

            if stop <= 7:
                raise _Stop()
            d_out = Dep()
            with k.scope():
                l2g = k.sb("l2g", [128, D], F32); l2b = k.sb("l2b", [128, D], F32)
                d_l2 = Dep()
                k.dma("sp", l2g[:], ln2g_d.partition_broadcast(128), w=[d_l2])
                k.dma("sp", l2b[:], ln2b_d.partition_broadcast(128), w=[d_l2])
                h1r = Ring([(k.sb("h1", [128, D], F32), Dep()) for _ in range(3)])
                y1r = Ring([(k.sb("y1", [128, D], BF16), Dep()) for _ in range(3)])
                y2r = Ring([(k.sb("y2", [128, D], BF16), Dep()) for _ in range(3)])
                rsr = Ring([(k.sb("rs2", [128, 32], F32), Dep()) for _ in range(3)])
                def g_tile(ti):
                    h1, h1d = h1r.next(); y1, y1d = y1r.next(); y2, y2d = y2r.next()
                    k.dma("sp", h1[:], s_h1[ti * 128:(ti + 1) * 128, :], r=[d_h1], w=[h1d])
                    k.dma("pool", y1[:], s_Yg, r=[d_Yg, d_tab], w=[y1d],
                          indirect=dict(out_offset=None, in_offset=bass.IndirectOffsetOnAxis(ap=dtab[:, ti, 0:1], axis=0)))
                    k.dma("pool", y2[:], s_Yg, r=[d_Yg, d_tab], w=[y2d],
                          indirect=dict(out_offset=None, in_offset=bass.IndirectOffsetOnAxis(ap=dtab[:, ti, 1:2], axis=0)))
                    k.op("act", lambda e, h1=h1: e.mul(h1[:], h1[:], ALPHA), r=[h1d], w=[h1d])
                    k.op("dve", lambda e, h1=h1, y1=y1, ti=ti: e.scalar_tensor_tensor(
                        h1[:], y1[:], wtab[:, ti, 0:1], h1[:], ALU.mult, ALU.add), r=[y1d, d_tab, h1d], w=[h1d])
                    k.op("dve", lambda e, h1=h1, y2=y2, ti=ti: e.scalar_tensor_tensor(
                        h1[:], y2[:], wtab[:, ti, 1:2], h1[:], ALU.mult, ALU.add), r=[y2d, d_tab, h1d], w=[h1d])
                    rs, rsd = rsr.next()
                    st = rs[:, 0:24].rearrange("p (g k) -> p g k", g=4)
                    def fbn(e, rs=rs, h1=h1):
                        for gg in range(4):
                            ins = e.bn_stats(rs[:, 6 * gg:6 * gg + 6], h1[:, gg * 512:(gg + 1) * 512])
                        return ins
                    k.op("dve", fbn, r=[h1d], w=[rsd])
                    k.op("dve", lambda e, rs=rs: e.bn_aggr(rs[:, 24:26], rs[:, 0:24]), r=[rsd], w=[rsd])
                    k.op("act", lambda e, rs=rs: e.activation(rs[:, 26:27], rs[:, 25:26], AF.Ln, bias=epsc[:, 0:1], scale=1.0), r=[rsd, d_c], w=[rsd])
                    k.op("act", lambda e, rs=rs: e.activation(rs[:, 26:27], rs[:, 26:27], AF.Exp, scale=-0.5), r=[rsd], w=[rsd])
                    k.op("dve", lambda e, rs=rs, h1=h1: e.tensor_scalar(h1[:], h1[:], rs[:, 24:25], rs[:, 26:27], ALU.subtract, ALU.mult),
                         r=[rsd, h1d], w=[h1d])
                    k.op("pool", lambda e, h1=h1: e.tensor_tensor(h1[:], h1[:], l2g[:], ALU.mult), r=[h1d, d_l2], w=[h1d])
                    k.op("pool", lambda e, h1=h1: e.tensor_tensor(h1[:], h1[:], l2b[:], ALU.add), r=[h1d, d_l2], w=[h1d])
                    k.dma("sp", out_d[ti * 128:(ti + 1) * 128, :], h1[:], r=[h1d], w=[d_out])
                k.interleave([(lambda ti=ti: g_tile(ti)) for ti in range(NTO)], width=3)
        except _Stop:
            gscope.close()
        k.barrier()
        build.stats = (k.n_ins, {n: e.count for n, e in k.E.items()})
    return nc


def _consts(TP, TO, CAP):
    TA = TP + TO
    c = {}
    c["ident"] = np.eye(128, dtype=np.float32)
    s = np.arange(64)
    c["mask64"] = (s[:, None] <= s[None, :]).astype(np.float32)
    p = np.arange(128)
    c["ustrict"] = (p[:, None] < p[None, :]).astype(np.float32)
    slopes = 2.0 ** (-8.0 * np.arange(1, 9) / 8.0)
    kl = p[:, None]; ql = p[None, :]
    vis = (kl // 64) <= (ql // 64)
    dg = np.zeros((128, 8, 128), np.float32)
    for h in range(8):
        b = np.where(kl <= ql, slopes[h] * kl, slopes[h] * (2 * ql - kl))
        dg[:, h, :] = np.where(vis, b, -BIG) * 8.0
    c["diagbias"] = dg
    ab = np.zeros((128, 8, 32), np.float32)
    for h in range(8):
        for dlt in range(32):
            ab[:, h, dlt] = np.maximum(slopes[h] * (p - 128 * dlt), -ACLAMP)
    c["alibi"] = ab
    c["ecap"] = np.tile((np.arange(32) * CAP).astype(np.float32)[None, :], (128, 1))
    return c


def _prep_shared(inp):
    f = lambda a: np.ascontiguousarray(np.asarray(a, dtype=np.float32))
    b_in = f(inp["b_in"][0])
    sh = {}
    sh["w_in"] = f(inp["w_in"][0])

    def fm_bias(b):
        cols = np.concatenate([b[O_QA:O_QA + 1024], b[O_KA:O_KA + 1024], b[O_QB:O_QB + 1024], b[O_KB:O_KB + 1024],
                               b[O_GA:O_GA + 2048], b[O_GB:O_GB + 2048]])
        return np.ascontiguousarray(cols.reshape(64, 128).T)

    def tm_bias(b):
        return np.ascontiguousarray(np.concatenate([b[O_VA:O_VA + 1024], b[O_OA:O_OA + 1024], b[O_VB:O_VB + 1024]])[None, :])

    def if_bias(b):
        return np.ascontiguousarray(np.stack([b[O_IA:O_IA + 4], b[O_FA:O_FA + 4]], axis=1))
    b_mask = np.zeros_like(b_in)
    b_mask[O_IA:O_IA + 4] = -BIG
    b_mask[O_FA:O_FA + 4] = BIG
    sh["bias_real"] = (fm_bias(b_in), tm_bias(b_in), if_bias(b_in))
    sh["bias_mask"] = (fm_bias(b_mask), tm_bias(b_mask), if_bias(b_mask))
    cw = f(inp["conv_w"][0])
    sh["cw"] = np.ascontiguousarray(cw.T.reshape(16, 128, 4).transpose(1, 0, 2))
    sh["cb"] = np.ascontiguousarray(f(inp["conv_b"][0]).reshape(16, 128).T)
    sh["mlstm_norm_g"] = f(inp["mlstm_norm_g"][0])
    sh["diff_norm_g"] = f(inp["diff_norm_g"][0])
    sh["lamv"] = np.ascontiguousarray(np.stack([f(inp["lambda_q1"][0]), f(inp["lambda_k1"][0]),
                                                f(inp["lambda_q2"][0]), f(inp["lambda_k2"][0])]))
    for n in ("w_a", "w_b", "w_out", "ln1_g", "ln1_b", "ln2_g", "ln2_b", "w_gate", "w_up", "w_down"):
        sh[n] = f(inp[n][0])
    sh["w_r"] = np.ascontiguousarray(np.concatenate([f(inp["w_grp"][0]), f(inp["w_exp"][0])], axis=1))
    sh["b_r"] = np.ascontiguousarray(np.concatenate([f(inp["b_grp"][0]), f(inp["b_exp"][0])])[None, :])
    return sh


def make_in_maps(inp, TP, TO, CAP):
    x = np.asarray(inp["x"], dtype=np.float32)
    B, S, _ = x.shape
    assert S == TP + TO or S == 2 * TO
    sh = _prep_shared(inp)
    cs = _consts(TP, TO, CAP)
    TA = TP + TO
    maps = []
    for b in range(B):
        for half in range(2):
            m = {}
            if half == 0:
                xa = np.zeros((TA, D), np.float32)
                xa[TP:] = x[b, 0:TO]
                valid = np.concatenate([np.zeros(TP, np.float32), np.ones(TO, np.float32)])
                pre = sh["bias_mask"]
            else:
                xa = np.ascontiguousarray(x[b, TO - TP:2 * TO])
                valid = np.ones(TA, np.float32)
                pre = sh["bias_real"]
            m["x"] = xa
            m["valid_tm"] = np.ascontiguousarray(valid.reshape(TA // 128, 128).T)
            m["bfm"], m["btm"], m["bif"] = sh["bias_real"]
            m["bfm_pre"], m["btm_pre"], m["bif_pre"] = pre
            for n in ("w_in", "cw", "cb", "mlstm_norm_g", "diff_norm_g", "lamv", "w_a", "w_b", "w_out", "ln1_g", "ln1_b",
                      "ln2_g", "ln2_b", "w_gate", "w_up", "w_down", "w_r", "b_r"):
                m[n] = sh[n]
            m.update(cs)
            maps.append(m)
    return maps


_TP, _TO, _CAP = 2048, 2048, 256


def kernel(**inputs):
    x = np.asarray(inputs["x"])
    B, S, _ = x.shape
    maps = make_in_maps(inputs, _TP, _TO, _CAP)
    nc = build(_TP, _TO, _CAP)
    res = run_bass_kernel_spmd(nc, maps, core_ids=list(range(len(maps))))
    out = np.zeros((B, S, D), np.float32)
    i = 0
    for b in range(B):
        for half in range(2):
            out[b, half * _TO:(half + 1) * _TO] = res.results[i]["out"]
            i += 1
    return out
```

```python
import math
from contextlib import ExitStack, contextmanager
import numpy as np
import concourse.bass as bass
import concourse.mybir as mybir
from concourse.bass_utils import run_bass_kernel_spmd

F32 = mybir.dt.float32
BF16 = mybir.dt.bfloat16
I32 = mybir.dt.int32
AF = mybir.ActivationFunctionType
ALU = mybir.AluOpType
AX = mybir.AxisListType

D = 2048
DC = 16
NIN = 11272
O_QA, O_KA, O_VA, O_OA, O_IA, O_FA, O_QB, O_KB, O_VB, O_GA, O_GB = (
    0, 1024, 2048, 3072, 4096, 4100, 4104, 5128, 6152, 7176, 9224)
ALPHA = 2.0 ** 0.25
EPS = 1e-5
NE = 32
BIG = 30000.0
LN16 = math.log(16.0)
ACLAMP = 40.0
import os
ILV = int(os.environ.get('ILV', '1'))
EXPV = int(os.environ.get('EXPV', '0'))
NIL = int(os.environ.get('NIL', '2'))
SLOPES = [2.0 ** (-(h + 1)) for h in range(8)]


class _Stop(Exception):
    pass


class Dep:
    __slots__ = ("w", "rs")

    def __init__(self):
        self.w = None
        self.rs = {}


class _Eng:
    def __init__(self, name, eng, sem):
        self.name, self.eng, self.sem = name, eng, sem
        self.count = 0
        self.waited = {}


class _DmaSem:
    def __init__(self, key, sem):
        self.key, self.sem, self.total = key, sem, 0


class Kern:
    def __init__(self, nc, stack, n_dma_sems=48):
        self.nc = nc
        self.stack = stack
        self.E = {}
        for name, eng in (("pe", nc.tensor), ("act", nc.scalar), ("dve", nc.vector),
                          ("pool", nc.gpsimd), ("sp", nc.sync)):
            sem = stack.enter_context(nc.semaphore("s_" + name))
            self.E[name] = _Eng(name, eng, sem)
        self.dsems = [_DmaSem("d%d" % i, stack.enter_context(nc.semaphore("d%d" % i)))
                      for i in range(n_dma_sems)]
        self.dpool = {"hw": self.dsems[0:n_dma_sems // 2], "sw": self.dsems[n_dma_sems // 2:]}
        self.drr = {"hw": 0, "sw": 0}
        self.n_ins = 0
        self.uid = 0
        self._cur = None
        self._atomic = 0

    def sb(self, name, shape, dt):
        self.uid += 1
        return self.stack.enter_context(self.nc.sbuf_tensor("%s_%d" % (name, self.uid), list(shape), dt))

    def ps(self, name, shape, dt):
        return self.stack.enter_context(self.nc.psum_tensor(name, list(shape), dt))

    @contextmanager
    def scope(self):
        old = self.stack
        stopped = False
        with ExitStack() as st:
            self.stack = st
            try:
                yield
            except _Stop:
                stopped = True
            if not stopped:
                self.barrier()
        self.stack = old
        if stopped:
            raise _Stop()

    def _wait(self, es, ev):
        key, sem, val = ev
        if es.name == "pe" and key == "pe":
            return
        if es.waited.get(key, 0) >= val:
            return
        es.eng.wait_ge(sem, val)
        es.waited[key] = val

    def _pre(self, es, r, w):
        for d in r:
            if d.w is not None:
                self._wait(es, d.w)
        for d in w:
            if d.w is not None:
                self._wait(es, d.w)
            for ev in d.rs.values():
                self._wait(es, ev)

    def _post(self, ev, r, w):
        for d in r:
            d.rs[ev[0]] = ev
        for d in w:
            d.w = ev
            d.rs = {}

    def op(self, en, fn, r=(), w=()):
        es = self.E[en]
        self._pre(es, r, w)
        ins = fn(es.eng)
        es.count += 1
        ins.then_inc(es.sem, 1)
        self.n_ins += 1
        ev = (es.name, es.sem, es.count)
        self._post(ev, r, w)
        self._yield()
        return ev

    def dma(self, qn, out, in_, r=(), w=(), indirect=None, **kw):
        es = self.E[qn]
        self._pre(es, r, w)
        kind = "sw" if qn == "pool" else "hw"
        pool = self.dpool[kind]
        ds = pool[self.drr[kind]]
        self.drr[kind] = (self.drr[kind] + 1) % len(pool)
        if ds.total > 0:
            self._wait(es, (ds.key, ds.sem, ds.total))
        if indirect is not None:
            ins = es.eng.indirect_dma_start(out=out, in_=in_, **indirect)
        else:
            ins = es.eng.dma_start(out=out, in_=in_, **kw)
        ds.total += 16
        ins.then_inc(ds.sem, 16)
        self.n_ins += 1
        ev = (ds.key, ds.sem, ds.total)
        self._post(ev, r, w)
        self._yield()
        return ev

    def _yield(self):
        w = self._cur
        if w is None or self._atomic > 0:
            return
        self._sched_sem.release()
        w["go"].acquire()

    def interleave(self, fns, width=2):
        import threading
        if width <= 1 or len(fns) <= 1:
            for fn in fns:
                fn()
            return
        self._sched_sem = threading.Semaphore(0)
        pending = list(fns)
        active = []
        err = []

        def runner(w, fn):
            w["go"].acquire()
            try:
                fn()
            except BaseException as e:
                err.append(e)
            w["done"] = True
            self._sched_sem.release()
        while pending or active:
            while pending and len(active) < width:
                w = {"go": threading.Semaphore(0), "done": False}
                w["t"] = threading.Thread(target=runner, args=(w, pending.pop(0)), daemon=True)
                w["t"].start()
                active.append(w)
            for w in list(active):
                self._cur = w
                w["go"].release()
                self._sched_sem.acquire()
                self._cur = None
                if w["done"]:
                    active.remove(w)
                if err:
                    raise err[0]

    @contextmanager
    def atomic(self):
        self._atomic += 1
        try:
            yield
        finally:
            self._atomic -= 1

    def barrier(self):
        evs = [(e.name, e.sem, e.count) for e in self.E.values() if e.count > 0]
        evs += [(d.key, d.sem, d.total) for d in self.dsems if d.total > 0]
        for es in self.E.values():
            for ev in evs:
                if ev[0] == es.name and es.name == "pe":
                    continue
                self._wait(es, ev)


class Ring:
    def __init__(self, items):
        self.items = items
        self.i = 0

    def next(self):
        it = self.items[self.i]
        self.i = (self.i + 1) % len(self.items)
        return it


def build(TP, TO, CAP, dbg=False, stop=99):
    TA = TP + TO
    NTA, NTO, NTP = TA // 128, TO // 128, TP // 128
    NCH, CH0 = TA // 64, TP // 64
    NROW = NE * CAP + 128
    TRASH = NE * CAP
    nc = bass.Bass("TRN2", target_bir_lowering=False)

    def din(name, shape, dt=F32):
        return nc.dram_tensor(name, list(shape), dt, kind="ExternalInput").ap()

    x = din("x", [TA, D])
    w_in = din("w_in", [D, NIN])
    bfm_d = din("bfm", [128, 64]); bfmp_d = din("bfm_pre", [128, 64])
    btm_d = din("btm", [1, 3072]); btmp_d = din("btm_pre", [1, 3072])
    bif_d = din("bif", [4, 2]); bifp_d = din("bif_pre", [4, 2])
    valid_d = din("valid_tm", [128, NTA])
    cw_d = din("cw", [128, 16, 4]); cb_d = din("cb", [128, 16])
    ng_d = din("mlstm_norm_g", [1024]); dg_d = din("diff_norm_g", [128])
    lam_d = din("lamv", [4, 64])
    w_a = din("w_a", [1024, D]); w_b = din("w_b", [1024, D]); w_out = din("w_out", [D, D])
    ln1g_d = din("ln1_g", [D]); ln1b_d = din("ln1_b", [D]); ln2g_d = din("ln2_g", [D]); ln2b_d = din("ln2_b", [D])
    wr_d = din("w_r", [D, 36]); br_d = din("b_r", [1, 36])
    if stop > 6:
        w_gate = din("w_gate", [NE, D, 512]); w_up = din("w_up", [NE, D, 512]); w_down = din("w_down", [NE, 512, D])
    ident_d = din("ident", [128, 128]); mask64_d = din("mask64", [64, 64]); ustrict_d = din("ustrict", [128, 128])
    dgb_d = din("diagbias", [128, 8, 128]); abt_d = din("alibi", [128, 8, 32]); ecap_d = din("ecap", [128, 32])
    out_d = nc.dram_tensor("out", [TO, D], F32, kind="ExternalOutput").ap()

    def dscr(name, shape, dt):
        if dbg:
            return nc.dram_tensor(name, list(shape), dt, kind="ExternalOutput").ap()
        return nc.dram_tensor(name, list(shape), dt).ap()

    s_qkaT = dscr("s_qkaT", [2048, TA], BF16)
    s_qkbT = dscr("s_qkbT", [2048, TA], BF16)
    s_gT = dscr("s_gT", [4096, TO], BF16)
    s_va = dscr("s_va", [TA, 1024], BF16)
    s_vb = dscr("s_vb", [TA, 1024], BF16)
    s_oa = dscr("s_oa", [TO, 1024], BF16)
    s_seq = dscr("s_seq", [3, 4, TA], F32)
    s_dec = dscr("s_dec", [4, NCH], F32)
    s_h1 = dscr("s_h1", [TO, D], F32)
    s_Xg = dscr("s_Xg", [NROW, D], BF16)
    s_Yg = dscr("s_Yg", [NROW, D], BF16)
    s_haT = dscr("s_haT", [1024, TO], BF16)
    s_obT = dscr("s_obT", [1024, TO], BF16)
    s_mgT = dscr("s_mgT", [2048, TO], BF16)
    HB = min(512, TO)
    if dbg:
        s_TT = dscr("s_TT", [64, 3 * NCH * 4], F32)
        s_dtab = dscr("s_dtab", [128, NTO * 2], I32)
        s_wtab = dscr("s_wtab", [128, NTO * 2], F32)

    with ExitStack() as st0:
        k = Kern(nc, st0)
        try:
            banks = [(k.ps("pf%d" % i, [128, 512], F32), Dep()) for i in range(8)]
            pf = Ring(banks[0:6])
            pb = Ring([(banks[i][0].bitcast(BF16), banks[i][1]) for i in (6, 7)])
            d_c = Dep()
            identf = k.sb("identf", [128, 128], F32)
            identb = k.sb("identb", [128, 128], BF16)
            onesb = k.sb("onesb", [128, 128], BF16)
            onesf = k.sb("onesf", [128, 128], F32)
            mask64 = k.sb("mask64", [64, 64], F32)
            ustr = k.sb("ustr", [128, 128], BF16)
            dgb = k.sb("dgb", [128, 8, 128], BF16)
            abt = k.sb("abt", [128, 8, 32], F32)
            ecap = k.sb("ecap", [128, 32], F32)
            validb = k.sb("validb", [128, NTA], BF16)
            zero1 = k.sb("zero1", [128, 1], F32)
            epsc = k.sb("epsc", [128, 1], F32)
            k.dma("sp", identf[:], ident_d, w=[d_c])
            k.dma("pool", identb[:], ident_d, w=[d_c])
            k.dma("sp", mask64[:], mask64_d, w=[d_c])
            k.dma("pool", ustr[:], ustrict_d, w=[d_c])
            k.dma("pool", dgb[:], dgb_d, w=[d_c])
            k.dma("sp", abt[:], abt_d, w=[d_c])
            k.dma("sp", ecap[:], ecap_d, w=[d_c])
            k.dma("pool", validb[:], valid_d, w=[d_c])
            k.op("dve", lambda e: e.memset(onesb[:], 1.0), w=[d_c])
            k.op("dve", lambda e: e.memset(onesf[:], 1.0), w=[d_c])
            k.op("dve", lambda e: e.memset(zero1[:], 0.0), w=[d_c])
            k.op("dve", lambda e: e.memset(epsc[:], EPS), w=[d_c])
            d_zt, d_Xg, d_Yg = Dep(), Dep(), Dep()

            dtab = k.sb("dtab", [128, NTO, 2], I32)
            wtab = k.sb("wtab", [128, NTO, 2], F32)
            TT = k.sb("TT", [64, 3, NCH, 4], F32)
            decbc = k.sb("decbc", [128, 4 * NCH], F32)
            gscope = ExitStack()
            _old = k.stack
            k.stack = gscope
            Gi = k.sb("Gi", [4, TA], F32); Gf = k.sb("Gf", [4, TA], F32)
            k.stack = _old
            d_G = Dep()
            d_tab = Dep()
            d_h1 = Dep()
            d_haT, d_obT, d_mg = Dep(), Dep(), Dep()

            with k.scope():
                zt = k.sb("zt", [128, 4096], BF16)
                k.op("pool", lambda e: e.memset(zt[:], 0.0), w=[d_zt])
                r0 = 0
                while r0 < NROW:
                    nr = min(256, NROW - r0)
                    k.dma("sp", s_Xg[r0:r0 + nr, :].rearrange("(t p) d -> p t d", p=128),
                          zt[:, 0:(nr // 128) * D].rearrange("p (t d) -> p t d", d=D), r=[d_zt], w=[d_Xg])
                    r0 += nr
                k.dma("sp", s_Yg[TRASH:TRASH + 128, :], zt[:, 0:D], r=[d_zt], w=[d_Yg])
                TX = max(TP, TO)
                xT = k.sb("xT", [128, DC, TX], BF16)
                xb = Ring([(k.sb("xb", [128, D], BF16), Dep()) for _ in range(2)])
                wt = Ring([(k.sb("wt", [128, DC, 512], BF16), Dep()) for _ in range(2)])
                wif = k.sb("wif", [128, DC, 8], BF16)
                evf = Ring([(k.sb("evf", [128, 4, 512], BF16), Dep()) for _ in range(2)])
                evt = Ring([(k.sb("evt", [128, 512], BF16), Dep()) for _ in range(3)])
                bfm = k.sb("bfm", [128, 64], F32); bfmp = k.sb("bfmp", [128, 64], F32)
                btm = k.sb("btm", [1, 3072], BF16); btmp = k.sb("btmp", [1, 3072], BF16)
                bif = k.sb("bif", [4, 2], F32); bifp = k.sb("bifp", [4, 2], F32)
                d_b, d_wif = Dep(), Dep()
                k.dma("sp", bfm[:], bfm_d, w=[d_b]); k.dma("sp", bfmp[:], bfmp_d, w=[d_b])
                k.dma("pool", btm[:], btm_d, w=[d_b]); k.dma("pool", btmp[:], btmp_d, w=[d_b])
                k.dma("sp", bif[:], bif_d, w=[d_b]); k.dma("sp", bifp[:], bifp_d, w=[d_b])
                k.dma("pool", wif[:], w_in.rearrange("(c p) n -> p c n", p=128)[:, :, O_IA:O_IA + 8], w=[d_wif])
                d_scr = {"qka": Dep(), "qkb": Dep(), "g": Dep(), "va": Dep(), "vb": Dep(), "oa": Dep()}

                FM = []
                for j in range(2):
                    FM.append((O_QA + 512 * j, "qka", s_qkaT, 512 * j, AF.Identity, "q", 4 * j))
                    FM.append((O_KA + 512 * j, "qka", s_qkaT, 1024 + 512 * j, AF.Identity, "all", 8 + 4 * j))
                    FM.append((O_QB + 512 * j, "qkb", s_qkbT, 512 * j, AF.Identity, "own", 16 + 4 * j))
                    FM.append((O_KB + 512 * j, "qkb", s_qkbT, 1024 + 512 * j, AF.Identity, "all", 24 + 4 * j))
                for j in range(8):
                    FM.append((O_GA + 512 * j, "g", s_gT, 512 * j, AF.Sigmoid, "gate", 32 + 4 * j))
                TM = []
                for j in range(2):
                    TM.append((O_VA + 512 * j, "va", s_va, 512 * j, AF.Identity, "all", 512 * j))
                    TM.append((O_OA + 512 * j, "oa", s_oa, 512 * j, AF.Sigmoid, "own", 1024 + 512 * j))
                    TM.append((O_VB + 512 * j, "vb", s_vb, 512 * j, AF.Identity, "all", 2048 + 512 * j))

                for phase in ("pre", "own"):
                    t0, nt = (0, TP) if phase == "pre" else (TP, TO)
                    bfm_x, btm_x, bif_x = (bfmp, btmp, bifp) if phase == "pre" else (bfm, btm, bif)
                    d_xT = [Dep() for _ in range(nt // 128)]
                    for ti in range(nt // 128):
                        xb_t, xb_d = xb.next()
                        k.dma("pool", xb_t[:], x[t0 + ti * 128:t0 + (ti + 1) * 128, :], w=[xb_d])
                        for g in range(4):
                            pt, pd = pb.next()

                            def f(pe, g=g, pt=pt, xb_t=xb_t):
                                for j in range(4):
                                    c = g * 4 + j
                                    ins = pe.transpose(pt[:, j * 128:(j + 1) * 128], xb_t[:, c * 128:(c + 1) * 128], identb[:])
                                return ins
                            k.op("pe", f, r=[xb_d, d_c], w=[pd])
                            k.op("dve", lambda e, g=g, ti=ti, pt=pt: e.tensor_copy(
                                xT[:, g * 4:(g + 1) * 4, ti * 128:(ti + 1) * 128],
                                pt[:, 0:512].rearrange("p (j n) -> p j n", j=4)), r=[pd], w=[d_xT[ti]])
                    tb = 0
                    while tb < nt:
                        n = min(512, nt - tb)
                        dx = d_xT[tb // 128:(tb + n) // 128]
                        for gi, Gt in ((0, Gi), (1, Gf)):
                            bk, bd = pf.next()

                            def f(pe, gi=gi, bk=bk, tb=tb, n=n):
                                for c in range(DC):
                                    ins = pe.matmul(bk[0:4, 0:n], wif[:, c, gi * 4:(gi + 1) * 4], xT[:, c, tb:tb + n],
                                                    start=(c == 0), stop=(c == DC - 1))
                                return ins
                            k.op("pe", f, r=dx + [d_wif], w=[bd])
                            k.op("act", lambda e, gi=gi, Gt=Gt, bk=bk, tb=tb, n=n: e.activation(
                                Gt[0:4, t0 + tb:t0 + tb + n], bk[0:4, 0:n], AF.Identity, bias=bif_x[:, gi:gi + 1], scale=1.0),
                                r=[bd, d_b], w=[d_G])
                        tb += n
                    for (c0, dkey, dst, row0, func, which, fmc) in FM:
                        if phase == "pre":
                            if which in ("own", "gate"):
                                continue
                            tlo = (TP - 128) if which == "q" else 0
                        else:
                            tlo = 0
                        w_t, w_d = wt.next()
                        k.dma("pool", w_t[:], w_in.rearrange("(c p) n -> p c n", p=128)[:, :, c0:c0 + 512], w=[w_d])
                        tb = tlo
                        while tb < nt:
                            n = min(512, nt - tb)
                            dx = d_xT[tb // 128:(tb + n) // 128]
                            ev_t, ev_d = evf.next()
                            for g in range(4):
                                bk, bd = pf.next()

                                def f(pe, g=g, bk=bk, tb=tb, n=n, w_t=w_t):
                                    for c in range(DC):
                                        ins = pe.matmul(bk[:, 0:n], w_t[:, c, g * 128:(g + 1) * 128], xT[:, c, tb:tb + n],
                                                        start=(c == 0), stop=(c == DC - 1))
                                    return ins
                                k.op("pe", f, r=dx + [w_d], w=[bd])
                                k.op("act", lambda e, g=g, bk=bk, n=n, ev_t=ev_t, func=func, fmc=fmc: e.activation(
                                    ev_t[:, g, 0:n], bk[:, 0:n], func, bias=bfm_x[:, fmc + g:fmc + g + 1], scale=1.0),
                                    r=[bd, d_b], w=[ev_d])
                            tcol = (tb if which == "gate" else t0 + tb)
                            k.dma("sp", dst[row0:row0 + 512, tcol:tcol + n].rearrange("(g p) t -> p g t", p=128),
                                  ev_t[:, :, 0:n], r=[ev_d], w=[d_scr[dkey]])
                            tb += n
                    for (c0, dkey, dst, col0, func, which, bcol) in TM:
                        if phase == "pre" and which == "own":
                            continue
                        w_t, w_d = wt.next()
                        k.dma("pool", w_t[:], w_in.rearrange("(c p) n -> p c n", p=128)[:, :, c0:c0 + 512], w=[w_d])
                        for ti in range(nt // 128):
                            bk, bd = pf.next()

                            def f(pe, bk=bk, ti=ti, w_t=w_t, bcol=bcol):
                                for c in range(DC):
                                    pe.matmul(bk[:, :], xT[:, c, ti * 128:(ti + 1) * 128], w_t[:, c, :],
                                              start=(c == 0), stop=False)
                                return pe.matmul(bk[:, :], onesb[0:1, :], btm_x[0:1, bcol:bcol + 512], start=False, stop=True)
                            k.op("pe", f, r=[d_xT[ti], w_d, d_b, d_c], w=[bd])
                            e_t, e_d = evt.next()
                            k.op("act", lambda e, bk=bk, e_t=e_t, func=func: e.activation(e_t[:], bk[:, :], func),
                                 r=[bd], w=[e_d])
                            trow = (ti * 128 if which == "own" else t0 + ti * 128)
                            k.dma("sp", dst[trow:trow + 128, col0:col0 + 512], e_t[:], r=[e_d], w=[d_scr[dkey]])

            if True:
                if stop <= 1:
                    raise _Stop()
                with k.scope():
                    t1 = k.sb("t1", [4, TA], F32); t2 = k.sb("t2", [4, TA], F32)
                    mt = k.sb("mt", [4, NCH], F32); dec = k.sb("dec", [4, NCH], F32)
                    sq = k.sb("sq", [4, 3, TA], F32)
                    dq = Dep()
                    V = "dve"
                    k.op("act", lambda e: e.activation(t1[:], Gf[:], AF.Abs), r=[d_G], w=[dq])
                    k.op("act", lambda e: e.activation(t1[:], t1[:], AF.Exp, scale=-1.0), r=[dq], w=[dq])
                    k.op("act", lambda e: e.activation(t1[:], t1[:], AF.Ln, bias=1.0, scale=1.0), r=[dq], w=[dq])
                    k.op(V, lambda e: e.tensor_scalar_min(t2[:], Gf[:], 0.0), r=[d_G], w=[dq])
                    k.op(V, lambda e: e.tensor_sub(t2[:], t2[:], t1[:]), r=[dq], w=[dq])
                    k.op(V, lambda e: e.tensor_scalar_mul(t2[:], t2[:], 0.5), r=[dq], w=[dq])
                    k.op(V, lambda e: e.tensor_tensor_scan(t1[:], t2[:], t2[:], 0.0, ALU.add, ALU.add), r=[dq], w=[dq])
                    Bc = t1
                    k.op(V, lambda e: e.tensor_sub(Gi[:], Gi[:], Bc[:]), r=[dq, d_G], w=[dq, d_G])
                    at = Gi
                    k.op(V, lambda e: e.tensor_tensor_scan(t2[:], at[:], at[:], 0.0, ALU.max, ALU.max), r=[dq, d_G], w=[dq])
                    ut = t2
                    ut3 = ut[:].rearrange("p (c s) -> p c s", s=64)
                    at3 = at[:].rearrange("p (c s) -> p c s", s=64)
                    Bc3 = Bc[:].rearrange("p (c s) -> p c s", s=64)
                    k.op(V, lambda e: e.memset(mt[:, 0:1], 0.0), w=[dq])
                    if NCH > 1:
                        k.op(V, lambda e: e.tensor_copy(mt[:, 1:NCH], ut3[:, 0:NCH - 1, 63]), r=[dq], w=[dq])
                    uL = ut3[:, :, 63]
                    mtb = mt[:, :].unsqueeze(2).to_broadcast([4, NCH, 64])
                    k.op(V, lambda e: e.tensor_sub(dec[:], mt[:], uL), r=[dq], w=[dq])
                    k.op("act", lambda e: e.activation(dec[:], dec[:], AF.Exp), r=[dq], w=[dq])
                    sq0 = sq[:, 0, :].rearrange("p (c s) -> p c s", s=64)
                    sq1 = sq[:, 1, :].rearrange("p (c s) -> p c s", s=64)
                    sq2 = sq[:, 2, :].rearrange("p (c s) -> p c s", s=64)
                    k.op(V, lambda e: e.tensor_sub(sq0, at3, mtb), r=[dq, d_G], w=[dq])
                    k.op(V, lambda e: e.tensor_scalar(sq[:, 0, :], sq[:, 0, :], 80.0, -LN16, ALU.min, ALU.add), r=[dq], w=[dq])
                    k.op("act", lambda e: e.activation(sq[:, 0, :], sq[:, 0, :], AF.Exp), r=[dq], w=[dq])
                    k.op(V, lambda e: e.tensor_tensor(sq1, sq0, dec[:, :].unsqueeze(2).to_broadcast([4, NCH, 64]), ALU.mult),
                         r=[dq], w=[dq])
                    k.op(V, lambda e: e.tensor_tensor(sq2, Bc3, mtb, ALU.add), r=[dq], w=[dq])
                    k.op(V, lambda e: e.tensor_scalar(sq[:, 2, :], sq[:, 2, :], -1.0, 80.0, ALU.mult, ALU.min), r=[dq], w=[dq])
                    k.op("act", lambda e: e.activation(sq[:, 2, :], sq[:, 2, :], AF.Exp), r=[dq], w=[dq])
                    d_seq = Dep()
                    k.dma("sp", s_dec, dec[:], r=[dq], w=[d_seq])
                    d_TT = Dep()
                    for q in range(3):
                        c0 = 0
                        while c0 < NCH:
                            ncc = min(128, NCH - c0)
                            bk, bd = pf.next()

                            def f(pe, bk=bk, q=q, c0=c0, ncc=ncc):
                                for cc in range(ncc):
                                    c = c0 + cc
                                    ins = pe.transpose(bk[0:64, cc * 4:cc * 4 + 4], sq[0:4, q, c * 64:(c + 1) * 64], identf[0:4, 0:4])
                                return ins
                            k.op("pe", f, r=[dq, d_c], w=[bd])
                            k.op("act", lambda e, bk=bk, q=q, c0=c0, ncc=ncc: e.copy(
                                TT[:, q, c0:c0 + ncc, :], bk[0:64, 0:ncc * 4].rearrange("p (c h) -> p c h", h=4)), r=[bd], w=[d_TT])
                            c0 += ncc
                    k.dma("sp", decbc[:], s_dec.rearrange("h c -> (h c)").partition_broadcast(128), r=[d_seq], w=[d_TT])
                gscope.close()
                if dbg:
                    k.dma("sp", s_TT, TT[:].rearrange("p a b c -> p (a b c)"), r=[d_TT], w=[Dep()])
                if stop <= 2:
                    raise _Stop()
                with k.scope():
                    qT = k.sb("qT", [128, 8, TO], BF16)
                    kT = k.sb("kT", [128, 8, TA], BF16)
                    cw = k.sb("cw", [128, 16, 4], F32); cb = k.sb("cb", [128, 16], F32)
                    ngb = k.sb("ngb", [64, 1024], F32)
                    d_cw, d_qT, d_kT = Dep(), Dep(), Dep()
                    k.dma("sp", cw[:], cw_d, w=[d_cw]); k.dma("sp", cb[:], cb_d, w=[d_cw])
                    k.dma("sp", ngb[:], ng_d.partition_broadcast(64), w=[d_cw])
                    cscope = k.scope()
                    cscope.__enter__()
                    cin = Ring([(k.sb("cin", [128, 3 + TA], BF16), Dep()) for _ in range(3)])
                    Dg = k.sb("Dg", [128, 16, 4, 128], BF16)
                    d_Dg = Dep()
                    for fc in range(16):
                        for j in range(4):
                            k.op("dve", lambda e, fc=fc, j=j: e.tensor_scalar_mul(Dg[:, fc, j, :], identf[:], cw[:, fc, j:j + 1]),
                                 r=[d_cw, d_c], w=[d_Dg])
                    for fc in list(range(8, 16)) + list(range(8)):
                        isq = fc < 8
                        lo = (TP - 128) if isq else 0
                        o0 = TP if isq else 0
                        n = TA - o0
                        ci, cd = cin.next()
                        if not isq:
                            k.op("pool", lambda e, ci=ci: e.memset(ci[:, 0:3], 0.0), w=[cd])
                        k.dma("sp", ci[:, 3 + lo:3 + TA], s_qkaT[fc * 128:(fc + 1) * 128, lo:TA], r=[d_scr["qka"]], w=[cd])
                        tb = 0
                        while tb < n:
                            nn = min(512, n - tb)
                            bk, bd = pf.next()

                            def f(pe, bk=bk, ci=ci, fc=fc, o0=o0, tb=tb, nn=nn):
                                for j in range(4):
                                    ins = pe.matmul(bk[:, 0:nn], Dg[:, fc, j, :], ci[:, o0 + tb + j:o0 + tb + j + nn],
                                                    start=(j == 0), stop=(j == 3))
                                return ins
                            k.op("pe", f, r=[cd, d_Dg], w=[bd])
                            if isq:
                                k.op("act", lambda e, bk=bk, fc=fc, tb=tb, nn=nn: e.activation(
                                    qT[:, fc, tb:tb + nn], bk[:, 0:nn], AF.Silu, bias=cb[:, fc:fc + 1], scale=1.0),
                                    r=[bd, d_cw], w=[d_qT])
                            else:
                                k.op("act", lambda e, bk=bk, fc=fc, tb=tb, nn=nn: e.activation(
                                    kT[:, fc - 8, tb:tb + nn], bk[:, 0:nn], AF.Silu, bias=cb[:, fc:fc + 1], scale=1.0),
                                    r=[bd, d_cw], w=[d_kT])
                            tb += nn
                    cscope.__exit__(None, None, None)
                    a_S = Ring([banks[0], banks[1]])
                    bO, bOd = banks[2]
                    bO1, bO1d = banks[3]
                    m_U = Ring(banks[4:5])
                    bSN, bSNd = banks[5]
                    bNN, bNNd = banks[6]
                    pb_all = pb
                    pb = Ring([(banks[7][0].bitcast(BF16), banks[7][1])])
                    Cst = [k.sb("Cst", [128, 2, 257], F32) for _ in range(4)]
                    hblk = Ring([(k.sb("hblk", [128, 8, HB], BF16), Dep()) for _ in range(1)])
                    Cbf = [k.sb("Cbf", [128, 2, 257], BF16) for _ in range(4)]
                    d_C = [Dep() for _ in range(4)]
                    d_Cbf = [Dep() for _ in range(4)]
                    for h in range(4):
                        k.op("pool", lambda e, h=h: e.memset(Cst[h][:], 0.0), w=[d_C[h]])
                        k.op("pool", lambda e, h=h: e.memset(Cbf[h][:], 0.0), w=[d_Cbf[h]])
                    vch_items = []
                    for _ in range(3):
                        vt = k.sb("vch", [64, 4, 257], BF16)
                        vd = Dep()
                        k.op("pool", lambda e, vt=vt: e.memset(vt[:, :, 256:257], 1.0), w=[vd])
                        vch_items.append((vt, vd))
                    vch = Ring(vch_items)
                    sor = Ring([(k.sb("so", [64, 1024], BF16), Dep()) for _ in range(1)])
                    kwr = Ring([(k.sb("kw", [64, 256], BF16), Dep()) for _ in range(3)])
                    Wtr = Ring([(k.sb("Wt", [64, 64], BF16), Dep()) for _ in range(3)])
                    Nsr = Ring([(k.sb("Ns", [64, 4, 257], F32), Dep()) for _ in range(2)])
                    hgr = Ring([(k.sb("hg", [64, 4, 256], F32), Dep()) for _ in range(1)])
                    hbr = Ring([(k.sb("hb", [64, 1024], BF16), Dep()) for _ in range(2)])
                    smr = Ring([(k.sb("sm", [64, 64], F32), Dep()) for _ in range(2)])
                    lamt = k.sb("lamt", [128, 4, 64], F32)
                    lsm = k.sb("lsm", [128, 8], F32)
                    gnb = k.sb("gnb", [128, 128], F32)
                    d_l = Dep()
                    k.dma("sp", lamt[:], lam_d.rearrange("a b -> (a b)").partition_broadcast(128).rearrange("p (a b) -> p a b", a=4), w=[d_l])
                    k.dma("sp", gnb[:], dg_d.partition_broadcast(128), w=[d_l])
                    k.op("dve", lambda e: e.tensor_tensor(lamt[:, 0, :], lamt[:, 0, :], lamt[:, 1, :], ALU.mult), r=[d_l], w=[d_l])
                    k.op("dve", lambda e: e.tensor_tensor(lamt[:, 2, :], lamt[:, 2, :], lamt[:, 3, :], ALU.mult), r=[d_l], w=[d_l])
                    k.op("dve", lambda e: e.reduce_sum(lsm[:, 0:1], lamt[:, 0, :], AX.X), r=[d_l], w=[d_l])
                    k.op("dve", lambda e: e.reduce_sum(lsm[:, 1:2], lamt[:, 2, :], AX.X), r=[d_l], w=[d_l])
                    k.op("act", lambda e: e.activation(lsm[:, 2:4], lsm[:, 0:2], AF.Exp), r=[d_l], w=[d_l])
                    k.op("dve", lambda e: e.tensor_sub(lsm[:, 4:5], lsm[:, 3:4], lsm[:, 2:3]), r=[d_l], w=[d_l])
                    k.op("dve", lambda e: e.tensor_scalar_add(lsm[:, 5:6], lsm[:, 4:5], -0.2), r=[d_l], w=[d_l])
                    k.op("dve", lambda e: e.tensor_scalar_mul(gnb[:], gnb[:], 0.8), r=[d_l], w=[d_l])
                    neglam = lsm[:, 5:6]
                    kb_items = []
                    for _ in range(1):
                        kt_ = k.sb("kbT", [128, 2, TA], BF16)
                        kd_ = Dep()
                        k.op("pool", lambda e, kt_=kt_: e.memset(kt_[64:128, 0, :], 0.0), w=[kd_])
                        k.op("pool", lambda e, kt_=kt_: e.memset(kt_[0:64, 1, :], 0.0), w=[kd_])
                        kb_items.append((kt_, kd_))
                    kbr = Ring(kb_items)
                    qbr = Ring([(k.sb("qbT", [128, TO], BF16), Dep()) for _ in range(1)])
                    vb_items = []
                    for _ in range(1):
                        vt = k.sb("vbe", [128, NTA, 129], BF16)
                        vd = Dep()
                        k.op("dve", lambda e, vt=vt: e.tensor_copy(vt[:, :, 128], validb[:, :]), r=[d_c], w=[vd])
                        vb_items.append((vt, vd))
                    vbr = Ring(vb_items)
                    PTr = Ring([(k.sb("PT", [128, 256], BF16), Dep()) for _ in range(4)])
                    o1r = Ring([(k.sb("o1", [128, 128], F32), Dep()) for _ in range(2)])
                    o2r = Ring([(k.sb("o2", [128, 128], F32), Dep()) for _ in range(2)])
                    obr = Ring([(k.sb("ob", [128, 128], BF16), Dep()) for _ in range(6)])
                    s8r = Ring([(k.sb("s8", [128, 8], F32), Dep()) for _ in range(2)])
                    Osr = Ring([(k.sb("Os", [128, 2, 129], F32), Dep()) for _ in range(2)])
                    oblk = Ring([(k.sb("oblk", [128, HB], BF16), Dep()) for _ in range(2)])

                    def mlstm_gen():
                        st_m = {'hb': None, 'fin': None}
                        for c in range(NCH):
                            own = c >= CH0
                            tq = (c - CH0) * 64
                            v_t, v_d = vch.next()
                            k.dma("sp", v_t[:, :, 0:256], s_va[c * 64:(c + 1) * 64, :].rearrange("s (h d) -> s h d", h=4),
                                  r=[d_scr["va"]], w=[v_d])
                            if own:
                                so_t, so_d = sor.next()
                                k.dma("sp", so_t[:], s_oa[tq:tq + 64, :], r=[d_scr["oa"]], w=[so_d])
                                Ns_t, Ns_d = Nsr.next()
                            for h in range(4):
                                pt, pd = pb.next()

                                def f(pe, pt=pt, h=h, c=c):
                                    for j in range(2):
                                        ins = pe.transpose(pt[0:64, j * 128:(j + 1) * 128], kT[:, h * 2 + j, c * 64:(c + 1) * 64], identb[:])
                                    return ins
                                k.op("pe", f, r=[d_kT, d_c], w=[pd])
                                kw_t, kw_d = kwr.next()
                                k.op("act", lambda e, kw_t=kw_t, pt=pt, h=h, c=c: e.activation(
                                    kw_t[:], pt[0:64, 0:256], AF.Identity, scale=TT[:, 1, c, h:h + 1]), r=[pd, d_TT], w=[kw_d])
                                yield
                                bU, bUd = m_U.next()

                                def f(pe, bU=bU, kw_t=kw_t, v_t=v_t, h=h):
                                    for j in range(2):
                                        pe.matmul(bU[:, j * 256:(j + 1) * 256], kw_t[:, j * 128:(j + 1) * 128], v_t[:, h, 0:256],
                                                  start=True, stop=True)
                                    for j in range(2):
                                        ins = pe.matmul(bSN[:, 400 + j:401 + j], kw_t[:, j * 128:(j + 1) * 128], v_t[:, h, 256:257],
                                                        start=True, stop=True)
                                    return ins
                                k.op("pe", f, r=[kw_d, v_d], w=[bUd, bSNd])
                                if not own:
                                    yield
                                if own:
                                    def f(pe, h=h, c=c, tq=tq):
                                        for j in range(2):
                                            ins = pe.matmul(bSN[0:64, 320:384], kT[:, h * 2 + j, c * 64:(c + 1) * 64],
                                                            qT[:, h * 2 + j, tq:tq + 64], start=(j == 0), stop=(j == 1))
                                        return ins
                                    k.op("pe", f, r=[d_kT, d_qT], w=[bSNd])
                                    W_t, W_d = Wtr.next()
                                    k.op("dve", lambda e, W_t=W_t, h=h, c=c: e.scalar_tensor_tensor(
                                        W_t[:], bSN[0:64, 320:384], TT[:, 0, c, h:h + 1], mask64[:], ALU.mult, ALU.mult),
                                        r=[bSNd, d_TT, d_c], w=[W_d])
                                    yield

                                    def f(pe, W_t=W_t, v_t=v_t, h=h, tq=tq):
                                        for j in range(2):
                                            pe.matmul(bNN[0:64, 0:257], qT[:, h * 2 + j, tq:tq + 64], Cbf[h][:, j, :],
                                                      start=(j == 0), stop=False)
                                        return pe.matmul(bNN[0:64, 0:257], W_t[:], v_t[:, h, :], start=False, stop=True)
                                    k.op("pe", f, r=[d_qT, d_Cbf[h], W_d, v_d], w=[bNNd])
                                    k.op("act", lambda e, Ns_t=Ns_t, h=h: e.copy(Ns_t[:, h, :], bNN[0:64, 0:257]),
                                         r=[bNNd], w=[Ns_d])
                                    yield
                                dsc = decbc[:, h * NCH + c:h * NCH + c + 1]
                                k.op("dve", lambda e, h=h, bU=bU, dsc=dsc: e.scalar_tensor_tensor(
                                    Cst[h][:, :, 0:256], Cst[h][:, :, 0:256], dsc,
                                    bU[:, 0:512].rearrange("p (j n) -> p j n", j=2), ALU.mult, ALU.add),
                                    r=[bUd, d_TT], w=[d_C[h]])
                                k.op("dve", lambda e, h=h, dsc=dsc: e.scalar_tensor_tensor(
                                    Cst[h][:, :, 256:257], Cst[h][:, :, 256:257], dsc,
                                    bSN[:, 400:402].rearrange("p (j n) -> p j n", j=2), ALU.mult, ALU.add),
                                    r=[bSNd, d_TT], w=[d_C[h]])
                                if own or c == CH0 - 1:
                                    k.op("act", lambda e, h=h: e.copy(Cbf[h][:], Cst[h][:]), r=[d_C[h]], w=[d_Cbf[h]])
                                if h == 3 and st_m['fin'] is not None:
                                    st_m['fin']()
                                    st_m['fin'] = None
                                yield
                            if own:
                                sm_t, sm_d = smr.next()
                                k.op("act", lambda e, sm_t=sm_t, Ns_t=Ns_t: e.activation(
                                    sm_t[:, 0:4], Ns_t[:, :, 256], AF.Abs), r=[Ns_d], w=[sm_d])
                                k.op("dve", lambda e, sm_t=sm_t, c=c: e.tensor_tensor(
                                    sm_t[:, 0:4], sm_t[:, 0:4], TT[:, 2, c, :], ALU.max), r=[sm_d, d_TT], w=[sm_d])
                                k.op("dve", lambda e, sm_t=sm_t: e.reciprocal(sm_t[:, 0:4], sm_t[:, 0:4]), r=[sm_d], w=[sm_d])
                                hg_t, hg_d = hgr.next()
                                k.op("dve", lambda e, hg_t=hg_t, Ns_t=Ns_t, sm_t=sm_t: e.tensor_tensor(
                                    hg_t[:], Ns_t[:, :, 0:256], sm_t[:, 0:4].unsqueeze(2).to_broadcast([64, 4, 256]), ALU.mult),
                                    r=[Ns_d, sm_d], w=[hg_d])
                                k.op("dve", lambda e, hg_t=hg_t, so_t=so_t: e.tensor_tensor(
                                    hg_t[:], hg_t[:], so_t[:].rearrange("s (h d) -> s h d", h=4), ALU.mult),
                                    r=[so_d, hg_d], w=[hg_d])

                                def f(e, hg_t=hg_t, sm_t=sm_t):
                                    for hh in range(4):
                                        ins = e.bn_stats(sm_t[:, 4 + 6 * hh:10 + 6 * hh], hg_t[:, hh, :])
                                    return ins
                                k.op("dve", f, r=[hg_d], w=[sm_d])

                                def f(e, sm_t=sm_t):
                                    for hh in range(4):
                                        ins = e.bn_aggr(sm_t[:, 28 + 2 * hh:30 + 2 * hh], sm_t[:, 4 + 6 * hh:10 + 6 * hh])
                                    return ins
                                k.op("dve", f, r=[sm_d], w=[sm_d])
                                mv = sm_t[:, 28:36].rearrange("s (h k) -> s h k", h=4)
                                k.op("act", lambda e, sm_t=sm_t, mv=mv: e.activation(
                                    sm_t[:, 36:40], mv[:, :, 1], AF.Ln, bias=epsc[0:64, 0:1], scale=1.0), r=[sm_d, d_c], w=[sm_d])
                                k.op("act", lambda e, sm_t=sm_t: e.activation(
                                    sm_t[:, 36:40], sm_t[:, 36:40], AF.Exp, scale=-0.5), r=[sm_d], w=[sm_d])
                                k.op("dve", lambda e, hg_t=hg_t, mv=mv: e.tensor_tensor(
                                    hg_t[:], hg_t[:], mv[:, :, 0:1].to_broadcast([64, 4, 256]), ALU.subtract),
                                    r=[sm_d, hg_d], w=[hg_d])
                                k.op("dve", lambda e, hg_t=hg_t, sm_t=sm_t: e.tensor_tensor(
                                    hg_t[:], hg_t[:], sm_t[:, 36:40].unsqueeze(2).to_broadcast([64, 4, 256]), ALU.mult),
                                    r=[sm_d, hg_d], w=[hg_d])
                                hb_t, hb_d = hbr.next()
                                k.op("dve", lambda e, hg_t=hg_t, hb_t=hb_t: e.tensor_tensor(
                                    hb_t[:], hg_t[:].rearrange("s h d -> s (h d)"), ngb[:], ALU.mult),
                                    r=[hg_d, d_cw], w=[hb_d])
                                def mfin(hb_t=hb_t, hb_d=hb_d, tq=tq):
                                    pt, pd = pb.next()

                                    def f(pe):
                                        for fc in range(8):
                                            ins = pe.transpose(pt[:, fc * 64:(fc + 1) * 64], hb_t[:, fc * 128:(fc + 1) * 128], identb[0:64, 0:64])
                                        return ins
                                    k.op("pe", f, r=[hb_d, d_c], w=[pd])
                                    if tq % HB == 0:
                                        st_m['hb'] = hblk.next()
                                    hk_t, hk_d = st_m['hb']
                                    k.op("act", lambda e: e.copy(
                                        hk_t[:, :, tq % HB:tq % HB + 64], pt[:, 0:512].rearrange("p (f s) -> p f s", f=8)), r=[pd], w=[hk_d])
                                    if (tq + 64) % HB == 0:
                                        tb0 = tq + 64 - HB
                                        k.dma("sp", s_haT[:, tb0:tb0 + HB].rearrange("(f p) t -> p f t", p=128), hk_t[:], r=[hk_d], w=[d_haT])
                                st_m['fin'] = mfin
                            yield
                        if st_m['fin'] is not None:
                            st_m['fin']()
                            st_m['fin'] = None
                        yield

                    def attn_gen():
                        st_a = {'ob': None}
                        deferred = []
                        for h in range(8):
                            kb_t, kb_d = kbr.next(); qb_t, qb_d = qbr.next(); vb_t, vb_d = vbr.next()
                            k.dma("sp", kb_t[0:64, 0, :], s_qkbT[1024 + h * 128:1024 + h * 128 + 64, :], r=[d_scr["qkb"]], w=[kb_d])
                            k.dma("sp", kb_t[64:128, 1, :], s_qkbT[1024 + h * 128 + 64:1024 + (h + 1) * 128, :], r=[d_scr["qkb"]], w=[kb_d])
                            k.dma("sp", qb_t[:], s_qkbT[h * 128:(h + 1) * 128, TP:TA], r=[d_scr["qkb"]], w=[qb_d])
                            for t8 in range(0, NTA, 8):
                                t9 = min(NTA, t8 + 8)
                                k.dma("sp", vb_t[:, t8:t9, 0:128],
                                      s_vb[t8 * 128:t9 * 128, h * 128:(h + 1) * 128].rearrange("(t p) d -> p t d", p=128),
                                      r=[d_scr["vb"]], w=[vb_d])
                            items = []
                            for qi in range(NTO):
                                qt = NTP + qi
                                kt_lo = 0
                                while kt_lo < qt and SLOPES[h] * (127 - 128 * (qt - kt_lo)) < -ACLAMP:
                                    kt_lo += 1
                                for kt in range(kt_lo, qt + 1):
                                    items.append((qi, qt, kt, kt_lo))

                            def emit_qk(it, h=h, kb_t=kb_t, qb_t=qb_t, kb_d=kb_d, qb_d=qb_d):
                                qi, qt, kt, kt_lo = it
                                bSt, sd = a_S.next()
                                off = 0
                                diag = (kt == qt)

                                def f(pe):
                                    for m in range(2):
                                        ins = pe.matmul(bSt[:, off + m * 128:off + (m + 1) * 128], kb_t[:, m, kt * 128:(kt + 1) * 128],
                                                        qb_t[:, qi * 128:(qi + 1) * 128], start=True, stop=(not diag))
                                        if diag:
                                            ins = pe.matmul(bSt[:, off + m * 128:off + (m + 1) * 128], identb[:], dgb[:, h, :], start=False, stop=True)
                                    return ins
                                k.op("pe", f, r=[kb_d, qb_d, d_c], w=[sd])
                                return bSt, sd
                            def do_pv(it, P_t, P_d, h=h, vb_t=vb_t, vb_d=vb_d):
                                qi, qt, kt, kt_lo = it

                                def f(pe, P_t=P_t, vb_t=vb_t, kt=kt, qt=qt, kt_lo=kt_lo):
                                    pe.matmul(bO[:, 0:129], P_t[:, 0:128], vb_t[:, kt, :], start=(kt == kt_lo), stop=(kt == qt))
                                    return pe.matmul(bO1[:, 0:129], P_t[:, 128:256], vb_t[:, kt, :], start=(kt == kt_lo), stop=(kt == qt))
                                k.op("pe", f, r=[P_d, vb_d], w=[bOd, bO1d])
                                if kt != qt:
                                    return
                                s8, s8d = s8r.next()
                                Os, Osd = Osr.next()
                                k.op("act", lambda e, Os=Os: e.copy(Os[:, 0, :], bO[:, 0:129]), r=[bOd], w=[Osd])
                                k.op("dve", lambda e, Os=Os: e.tensor_copy(Os[:, 1, :], bO1[:, 0:129]), r=[bO1d], w=[Osd])
                                k.op("dve", lambda e, s8=s8, Os=Os: e.reciprocal(s8[:, 0:2], Os[:, :, 128]), r=[Osd], w=[s8d])
                                k.op("dve", lambda e, s8=s8: e.tensor_tensor(s8[:, 2:3], s8[:, 1:2], neglam, ALU.mult),
                                     r=[s8d, d_l], w=[s8d])
                                o1, o1d = o1r.next(); o2, o2d = o2r.next()
                                k.op("dve", lambda e, o1=o1, s8=s8, Os=Os: e.tensor_scalar_mul(o1[:], Os[:, 0, 0:128], s8[:, 0:1]),
                                     r=[Osd, s8d], w=[o1d])
                                k.op("dve", lambda e, o1=o1, s8=s8, Os=Os: e.scalar_tensor_tensor(
                                    o1[:], Os[:, 1, 0:128], s8[:, 2:3], o1[:], ALU.mult, ALU.add), r=[Osd, s8d], w=[o1d])
                                k.op("pool", lambda e, o1=o1, o2=o2: e.tensor_tensor(o2[:], o1[:], o1[:], ALU.mult), r=[o1d], w=[o2d])
                                k.op("dve", lambda e, o2=o2, s8=s8: e.reduce_sum(s8[:, 3:4], o2[:], AX.X), r=[o2d], w=[s8d])
                                k.op("dve", lambda e, s8=s8: e.tensor_scalar(s8[:, 4:5], s8[:, 3:4], 1.0 / 128.0, EPS, ALU.mult, ALU.add),
                                     r=[s8d], w=[s8d])
                                k.op("act", lambda e, s8=s8: e.activation(s8[:, 5:6], s8[:, 4:5], AF.Ln), r=[s8d], w=[s8d])
                                k.op("act", lambda e, s8=s8: e.activation(s8[:, 5:6], s8[:, 5:6], AF.Exp, scale=-0.5),
                                     r=[s8d], w=[s8d])
                                ob, obd = obr.next()
                                k.op("dve", lambda e, ob=ob, o1=o1, s8=s8: e.scalar_tensor_tensor(
                                    ob[:], o1[:], s8[:, 5:6], gnb[:], ALU.mult, ALU.mult), r=[o1d, s8d, d_l], w=[obd])
                                def fin(ob=ob, obd=obd, qi=qi, h=h):
                                    pt, pd = pb.next()
                                    k.op("pe", lambda pe: pe.transpose(pt[:, 0:128], ob[:], identb[:]), r=[obd, d_c], w=[pd])
                                    tq = qi * 128
                                    if tq % HB == 0:
                                        st_a['ob'] = oblk.next()
                                    ok_t, ok_d = st_a['ob']
                                    k.op("act", lambda e: e.copy(ok_t[:, tq % HB:tq % HB + 128], pt[:, 0:128]),
                                         r=[pd], w=[ok_d])
                                    if (tq + 128) % HB == 0:
                                        tb0 = tq + 128 - HB
                                        k.dma("sp", s_obT[h * 128:(h + 1) * 128, tb0:tb0 + HB], ok_t[:], r=[ok_d], w=[d_obT])
                                deferred.append([6, fin])
                            nxt = emit_qk(items[0])
                            pend = None
                            for j, it in enumerate(items):
                                qi, qt, kt, kt_lo = it
                                bSt, sd = nxt
                                off = 0
                                if j + 1 < len(items):
                                    nxt = emit_qk(items[j + 1])
                                diag = (kt == qt)
                                P_t, P_d = PTr.next()
                                bias_ap = zero1[:, 0:1] if diag else abt[:, h, qt - kt:qt - kt + 1]
                                k.op("act", lambda e, P_t=P_t, off=off, bias_ap=bias_ap, bSt=bSt: e.activation(
                                    P_t[:], bSt[:, off:off + 256], AF.Exp, bias=bias_ap, scale=0.125), r=[sd, d_c], w=[P_d])

                                if pend is not None:
                                    do_pv(*pend)
                                pend = (it, P_t, P_d)
                                for dfr in list(deferred):
                                    dfr[0] -= 1
                                    if dfr[0] <= 0:
                                        deferred.remove(dfr)
                                        dfr[1]()
                                yield
                            if pend is not None:
                                do_pv(*pend)
                            yield
                        for dfr in list(deferred):
                            dfr[1]()
                        deferred.clear()
                        yield

                    gm = mlstm_gen()
                    ga_ = attn_gen()
                    if stop <= 3:
                        for _ in gm:
                            pass
                        raise _Stop()
                    if stop <= 3.5:
                        for _ in ga_:
                            pass
                        raise _Stop()
                    n_m = (CH0 * (4 * 3 + 1)) + ((NCH - CH0) * (4 * 4 + 1))
                    n_a = 0
                    for h_ in range(8):
                        for qi_ in range(NTO):
                            qt_ = NTP + qi_
                            lo_ = 0
                            while lo_ < qt_ and SLOPES[h_] * (127 - 128 * (qt_ - lo_)) < -ACLAMP:
                                lo_ += 1
                            n_a += qt_ + 1 - lo_
                    done_m = done_a = 0
                    m_alive = a_alive = True
                    while m_alive or a_alive:
                        if m_alive:
                            try:
                                next(gm); done_m += 1
                            except StopIteration:
                                m_alive = False
                        if ILV == 0:
                            tgt = n_a + 10 if not m_alive else 0
                        else:
                            tgt = n_a + 10 if not m_alive else (done_m * n_a) // n_m
                        while a_alive and done_a < tgt:
                            try:
                                next(ga_); done_a += 1
                            except StopIteration:
                                a_alive = False
                    for g_ in (gm, ga_):
                        for _ in g_:
                            pass
                    pb = pb_all
                if stop <= 4:
                    raise _Stop()
                with k.scope():
                    war = Ring([(k.sb("wa", [128, 8, 512], BF16), Dep()) for _ in range(2)])
                    wbr = Ring([(k.sb("wb", [128, 8, 512], BF16), Dep()) for _ in range(2)])
                    gar = Ring([(k.sb("ga", [128, 512], BF16), Dep()) for _ in range(3)])
                    gbr = Ring([(k.sb("gb", [128, 512], BF16), Dep()) for _ in range(3)])
                    m1r = Ring([(k.sb("m1", [128, 512], F32), Dep()) for _ in range(3)])
                    m2r = Ring([(k.sb("m2", [128, 512], F32), Dep()) for _ in range(3)])
                    hkr = Ring([(k.sb("hk", [128, 8, HB], BF16), Dep()) for _ in range(2)])
                    okr = Ring([(k.sb("ok", [128, 8, HB], BF16), Dep()) for _ in range(2)])
                    mgr = Ring([(k.sb("mgo", [128, 512], BF16), Dep()) for _ in range(3)])
                    st_e1 = {"db": None, "tb": None}

                    def e1_unit(db, tb, n, g):
                        with k.atomic():
                            if st_e1["db"] != db:
                                wa_t, wa_d = war.next(); wb_t, wb_d = wbr.next()
                                k.dma("pool", wa_t[:], w_a.rearrange("(c p) n -> p c n", p=128)[:, :, db * 512:(db + 1) * 512], w=[wa_d])
                                k.dma("pool", wb_t[:], w_b.rearrange("(c p) n -> p c n", p=128)[:, :, db * 512:(db + 1) * 512], w=[wb_d])
                                st_e1["db"] = db
                                st_e1["w"] = (wa_t, wa_d, wb_t, wb_d)
                            if st_e1["tb"] != (db, tb):
                                hk, hkd = hkr.next(); ok, okd = okr.next()
                                k.dma("sp", hk[:, :, 0:n], s_haT[:, tb:tb + n].rearrange("(f p) t -> p f t", p=128), r=[d_haT], w=[hkd])
                                k.dma("sp", ok[:, :, 0:n], s_obT[:, tb:tb + n].rearrange("(f p) t -> p f t", p=128), r=[d_obT], w=[okd])
                                st_e1["tb"] = (db, tb)
                                st_e1["h"] = (hk, hkd, ok, okd)
                        wa_t, wa_d, wb_t, wb_d = st_e1["w"]
                        hk, hkd, ok, okd = st_e1["h"]
                        dc = db * 4 + g
                        ga_t, ga_d = gar.next(); gb_t, gb_d = gbr.next()
                        k.dma("sp", ga_t[:, 0:n], s_gT[dc * 128:(dc + 1) * 128, tb:tb + n], r=[d_scr["g"]], w=[ga_d])
                        k.dma("sp", gb_t[:, 0:n], s_gT[2048 + dc * 128:2048 + (dc + 1) * 128, tb:tb + n], r=[d_scr["g"]], w=[gb_d])
                        bA, bAd = pf.next(); bB, bBd = pf.next()

                        def f(pe):
                            for fc in range(8):
                                ins = pe.matmul(bA[:, 0:n], wa_t[:, fc, g * 128:(g + 1) * 128], hk[:, fc, 0:n],
                                                start=(fc == 0), stop=(fc == 7))
                            return ins
                        k.op("pe", f, r=[wa_d, hkd], w=[bAd])

                        def f(pe):
                            for fc in range(8):
                                ins = pe.matmul(bB[:, 0:n], wb_t[:, fc, g * 128:(g + 1) * 128], ok[:, fc, 0:n],
                                                start=(fc == 0), stop=(fc == 7))
                            return ins
                        k.op("pe", f, r=[wb_d, okd], w=[bBd])
                        m1, m1d = m1r.next(); m2, m2d = m2r.next()
                        k.op("dve", lambda e: e.tensor_tensor(m1[:, 0:n], bA[:, 0:n], ga_t[:, 0:n], ALU.mult),
                             r=[bAd, ga_d], w=[m1d])
                        k.op("dve", lambda e: e.tensor_tensor(m2[:, 0:n], bB[:, 0:n], gb_t[:, 0:n], ALU.mult),
                             r=[bBd, gb_d], w=[m2d])
                        mg_t, mg_dd = mgr.next()
                        k.op("pool", lambda e: e.tensor_tensor(mg_t[:, 0:n], m1[:, 0:n], m2[:, 0:n], ALU.add), r=[m1d, m2d], w=[mg_dd])
                        k.dma("sp", s_mgT[dc * 128:(dc + 1) * 128, tb:tb + n], mg_t[:, 0:n], r=[mg_dd], w=[d_mg])
                    units = []
                    for db in range(4):
                        tb = 0
                        while tb < TO:
                            n = min(512, TO - tb)
                            for g in range(4):
                                units.append(lambda db=db, tb=tb, n=n, g=g: e1_unit(db, tb, n, g))
                            tb += n
                    k.interleave(units, width=3)
            if stop <= 5:
                raise _Stop()
            with k.scope():
                wo = k.sb("wo", [128, DC, D], BF16)
                wr = k.sb("wr", [128, DC, 36], F32)
                br = k.sb("br", [1, 36], F32)
                l1g = k.sb("l1g", [128, D], F32); l1b = k.sb("l1b", [128, D], F32)
                cnt = k.sb("cnt", [128, 32], F32)
                d_wo, d_cnt = Dep(), Dep()
                for q4 in range(4):
                    k.dma("pool", wo[:, :, q4 * 512:(q4 + 1) * 512],
                          w_out.rearrange("(c p) n -> p c n", p=128)[:, :, q4 * 512:(q4 + 1) * 512], w=[d_wo])
                k.dma("sp", wr[:], wr_d.rearrange("(c p) n -> p c n", p=128), w=[d_wo])
                k.dma("sp", br[:], br_d, w=[d_wo])
                k.dma("sp", l1g[:], ln1g_d.partition_broadcast(128), w=[d_wo])
                k.dma("sp", l1b[:], ln1b_d.partition_broadcast(128), w=[d_wo])
                k.op("dve", lambda e: e.memset(cnt[:], 0.0), w=[d_cnt])
                xtr = Ring([(k.sb("xt", [128, D], F32), Dep()) for _ in range(2)])
                x1r = Ring([(k.sb("x1", [128, D], F32), Dep()) for _ in range(2)])
                hbr2 = Ring([(k.sb("hb2", [128, D], BF16), Dep()) for _ in range(2)])
                hTr = Ring([(k.sb("hT", [128, DC, 128], F32), Dep()) for _ in range(2)])
                rsr = Ring([(k.sb("rs", [128, 256], F32), Dep()) for _ in range(2)])
                mkr = Ring([(k.sb("mk", [128, 32], BF16), Dep()) for _ in range(2)])
                mbr = Ring([(k.sb("mgb", [128, DC, HB], BF16), Dep()) for _ in range(2)])
                st_e2 = {"mb": None}

                def e2_tile(ti):
                    if (ti * 128) % HB == 0:
                        with k.atomic():
                            st_e2["mb"] = mbr.next()
                            k.dma("sp", st_e2["mb"][0][:], s_mgT[:, ti * 128:ti * 128 + HB].rearrange("(c p) t -> p c t", p=128),
                                  r=[d_mg], w=[st_e2["mb"][1]])
                    mgb, mgbd = st_e2["mb"]
                    tloc = (ti * 128) % HB
                    xt, xtd = xtr.next(); x1, x1d = x1r.next()
                    k.dma("sp", xt[:], x[TP + ti * 128:TP + (ti + 1) * 128, :], w=[xtd])
                    for db in range(4):
                        bk, bd = pf.next()

                        def f(pe, bk=bk, mgb=mgb, tloc=tloc, db=db):
                            for c in range(DC):
                                ins = pe.matmul(bk[:, :], mgb[:, c, tloc:tloc + 128], wo[:, c, db * 512:(db + 1) * 512],
                                                start=(c == 0), stop=(c == DC - 1))
                            return ins
                        k.op("pe", f, r=[mgbd, d_wo], w=[bd])
                        k.op("dve", lambda e, x1=x1, xt=xt, bk=bk, db=db: e.scalar_tensor_tensor(
                            x1[:, db * 512:(db + 1) * 512], xt[:, db * 512:(db + 1) * 512], ALPHA, bk[:, :], ALU.mult, ALU.add),
                            r=[xtd, bd], w=[x1d])
                    rs, rsd = rsr.next()

                    def layer_norm(xx, xd, rs, rsd, g_t, b_t, gdep):
                        st = rs[:, 0:24].rearrange("p (g k) -> p g k", g=4)
                        def fbn(e):
                            for gg in range(4):
                                ins = e.bn_stats(rs[:, 6 * gg:6 * gg + 6], xx[:, gg * 512:(gg + 1) * 512])
                            return ins
                        k.op("dve", fbn, r=[xd], w=[rsd])
                        k.op("dve", lambda e: e.bn_aggr(rs[:, 24:26], rs[:, 0:24]), r=[rsd], w=[rsd])
                        k.op("act", lambda e: e.activation(rs[:, 26:27], rs[:, 25:26], AF.Ln, bias=epsc[:, 0:1], scale=1.0), r=[rsd, d_c], w=[rsd])
                        k.op("act", lambda e: e.activation(rs[:, 26:27], rs[:, 26:27], AF.Exp, scale=-0.5), r=[rsd], w=[rsd])
                        k.op("dve", lambda e: e.tensor_scalar(xx[:], xx[:], rs[:, 24:25], rs[:, 26:27], ALU.subtract, ALU.mult),
                             r=[rsd, xd], w=[xd])
                        k.op("pool", lambda e: e.tensor_tensor(xx[:], xx[:], g_t[:], ALU.mult), r=[xd, gdep], w=[xd])
                        k.op("pool", lambda e: e.tensor_tensor(xx[:], xx[:], b_t[:], ALU.add), r=[xd, gdep], w=[xd])
                    layer_norm(x1, x1d, rs, rsd, l1g, l1b, d_wo)
                    k.dma("sp", s_h1[ti * 128:(ti + 1) * 128, :], x1[:], r=[x1d], w=[d_h1])
                    hb2, hb2d = hbr2.next()
                    k.op("act", lambda e, hb2=hb2, x1=x1: e.copy(hb2[:], x1[:]), r=[x1d], w=[hb2d])
                    hT, hTd = hTr.next()
                    for g in range(4):
                        bk, bd = pf.next()

                        def f(pe, bk=bk, x1=x1, g=g):
                            for j in range(4):
                                c = g * 4 + j
                                ins = pe.transpose(bk[:, j * 128:(j + 1) * 128], x1[:, c * 128:(c + 1) * 128], identf[:])
                            return ins
                        k.op("pe", f, r=[x1d, d_c], w=[bd])
                        k.op("act", lambda e, hT=hT, bk=bk, g=g: e.copy(
                            hT[:, g * 4:(g + 1) * 4, :], bk[:, :].rearrange("p (j n) -> p j n", j=4)), r=[bd], w=[hTd])
                    bk, bd = pf.next()

                    def f(pe, bk=bk, hT=hT):
                        for c in range(DC):
                            pe.matmul(bk[:, 0:36], hT[:, c, :], wr[:, c, :], start=(c == 0), stop=False)
                        return pe.matmul(bk[:, 0:36], onesf[0:1, :], br[0:1, :], start=False, stop=True)
                    k.op("pe", f, r=[hTd, d_wo, d_c], w=[bd])
                    V = "dve"
                    lg = rs[:, 32:68]
                    k.op(V, lambda e, bk=bk, lg=lg: e.tensor_copy(lg, bk[:, 0:36]), r=[bd], w=[rsd])
                    g4 = rs[:, 32:36]; e32 = rs[:, 36:68]
                    gmx = rs[:, 68:69]; ngm = rs[:, 69:70]; ohg = rs[:, 70:74]; eg = rs[:, 74:78]; sg = rs[:, 78:79]; gp = rs[:, 79:80]
                    pen = rs[:, 80:84]; msk = rs[:, 84:116]; top8 = rs[:, 116:124]; oh1 = rs[:, 124:156]; oh2 = rs[:, 156:188]
                    dd = rs[:, 188:189]; p1 = rs[:, 189:190]; p2 = rs[:, 190:191]; tmp = rs[:, 192:224]
                    pk = rs[:, 224:225]; ek = rs[:, 225:226]; okk = rs[:, 226:227]; dsf = rs[:, 227:228]; posg = rs[:, 228:260 - 4]
                    k.op(V, lambda e: e.reduce_max(gmx, g4, AX.X), r=[rsd], w=[rsd])
                    k.op(V, lambda e: e.tensor_scalar_mul(ngm, gmx, -1.0), r=[rsd], w=[rsd])
                    k.op(V, lambda e: e.tensor_scalar(ohg, g4, gmx, None, ALU.is_equal), r=[rsd], w=[rsd])
                    k.op("act", lambda e: e.activation(eg, g4, AF.Exp, bias=ngm, scale=1.0), r=[rsd], w=[rsd])
                    k.op(V, lambda e: e.reduce_sum(sg, eg, AX.X), r=[rsd], w=[rsd])
                    k.op(V, lambda e: e.reciprocal(gp, sg), r=[rsd], w=[rsd])
                    k.op(V, lambda e: e.tensor_scalar(pen, ohg, BIG, -BIG, ALU.mult, ALU.add), r=[rsd], w=[rsd])
                    k.op(V, lambda e: e.tensor_tensor(msk.rearrange("p (g j) -> p g j", g=4), e32.rearrange("p (g j) -> p g j", g=4),
                                                      pen.unsqueeze(2).to_broadcast([128, 4, 8]), ALU.add), r=[rsd], w=[rsd])
                    k.op(V, lambda e: e.max(top8, msk), r=[rsd], w=[rsd])
                    k.op(V, lambda e: e.tensor_scalar(oh1, msk, top8[:, 0:1], None, ALU.is_equal), r=[rsd], w=[rsd])
                    k.op(V, lambda e: e.tensor_scalar(oh2, msk, top8[:, 1:2], None, ALU.is_equal), r=[rsd], w=[rsd])
                    k.op(V, lambda e: e.tensor_sub(dd, top8[:, 0:1], top8[:, 1:2]), r=[rsd], w=[rsd])
                    k.op("act", lambda e: e.activation(p1, dd, AF.Sigmoid), r=[rsd], w=[rsd])
                    k.op("act", lambda e: e.activation(p2, dd, AF.Sigmoid, scale=-1.0), r=[rsd], w=[rsd])
                    k.op(V, lambda e, ti=ti: e.tensor_tensor(wtab[:, ti, 0:1], p1, gp, ALU.mult), r=[rsd], w=[d_tab])
                    k.op(V, lambda e, ti=ti: e.tensor_tensor(wtab[:, ti, 1:2], p2, gp, ALU.mult), r=[rsd], w=[d_tab])
                    mk, mkd = mkr.next()
                    k.op(V, lambda e, mk=mk: e.tensor_tensor(mk[:], oh1, oh2, ALU.add), r=[rsd], w=[mkd])
                    bk2, bd2 = pf.next()

                    def f(pe, bk2=bk2, mk=mk):
                        pe.matmul(bk2[:, 0:32], ustr[:], mk[:], start=True, stop=True)
                        return pe.matmul(bk2[:, 32:64], onesb[:], mk[:], start=True, stop=True)
                    k.op("pe", f, r=[mkd, d_c], w=[bd2])
                    posg = rs[:, 224:256]
                    pk = rs[:, 28:29]; ek = rs[:, 29:30]; okk = rs[:, 30:31]; dsf = rs[:, 31:32]
                    with k.atomic():
                        k.op(V, lambda e, bk2=bk2: e.tensor_tensor(posg, bk2[:, 0:32], cnt[:], ALU.add), r=[bd2, d_cnt, rsd], w=[rsd])
                        k.op(V, lambda e, bk2=bk2: e.tensor_tensor(cnt[:], cnt[:], bk2[:, 32:64], ALU.add), r=[bd2, rsd], w=[d_cnt])
                    for kk, oh in ((0, oh1), (1, oh2)):
                        k.op(V, lambda e, oh=oh: e.tensor_tensor(tmp, oh, posg, ALU.mult), r=[rsd], w=[rsd])
                        k.op(V, lambda e: e.reduce_sum(pk, tmp, AX.X), r=[rsd], w=[rsd])
                        k.op(V, lambda e, oh=oh: e.tensor_tensor(tmp, oh, ecap[:], ALU.mult), r=[rsd, d_c], w=[rsd])
                        k.op(V, lambda e: e.reduce_sum(ek, tmp, AX.X), r=[rsd], w=[rsd])
                        k.op(V, lambda e: e.tensor_scalar(okk, pk, float(CAP), None, ALU.is_lt), r=[rsd], w=[rsd])
                        k.op(V, lambda e: e.tensor_tensor(dsf, ek, pk, ALU.add), r=[rsd], w=[rsd])
                        k.op(V, lambda e: e.tensor_scalar_add(dsf, dsf, -float(TRASH)), r=[rsd], w=[rsd])
                        k.op(V, lambda e: e.tensor_tensor(dsf, dsf, okk, ALU.mult), r=[rsd], w=[rsd])
                        k.op(V, lambda e: e.tensor_scalar_add(dsf, dsf, float(TRASH)), r=[rsd], w=[rsd])
                        k.op(V, lambda e, ti=ti, kk=kk: e.tensor_copy(dtab[:, ti, kk:kk + 1], dsf), r=[rsd], w=[d_tab])
                        k.dma("pool", s_Xg, hb2[:], r=[hb2d, d_tab, d_Xg], w=[d_Xg],
                              indirect=dict(out_offset=bass.IndirectOffsetOnAxis(ap=dtab[:, ti, kk:kk + 1], axis=0), in_offset=None))
                k.interleave([(lambda ti=ti: e2_tile(ti)) for ti in range(NTO)], width=NIL)

            if dbg:
                k.dma("sp", s_dtab, dtab[:].rearrange("p a b -> p (a b)"), r=[d_tab], w=[Dep()])
                k.dma("sp", s_wtab, wtab[:].rearrange("p a b -> p (a b)"), r=[d_tab], w=[Dep()])
            if stop <= 6:
                raise _Stop()
            NCT = CAP // 128
            with k.scope():
                wgr = Ring([(k.sb("wg", [128, DC, 512], BF16), Dep()) for _ in range(3)])
                wur = Ring([(k.sb("wu", [128, DC, 512], BF16), Dep()) for _ in range(3)])
                wdr = Ring([(k.sb("wd", [128, 4, D], BF16), Dep()) for _ in range(3)])
                Xer = Ring([(k.sb("Xe", [128, NCT, D], BF16), Dep()) for _ in range(2)])
                XTr = Ring([(k.sb("XT", [128, DC, CAP], BF16), Dep()) for _ in range(2)])
                ATr = Ring([(k.sb("AT", [128, 4, CAP], BF16), Dep()) for _ in range(2)])
                sgr = Ring([(k.sb("sgt", [128, CAP], F32), Dep()) for _ in range(2)])
                Ysr = Ring([(k.sb("Ys", [128, D], BF16), Dep()) for _ in range(2)])
                def expert_fn(e_):
                    wg, wgd = wgr.next(); wu, wud = wur.next(); wd, wdd = wdr.next()
                    k.dma("pool", wg[:], w_gate[e_].rearrange("(c p) n -> p c n", p=128), w=[wgd])
                    k.dma("pool", wu[:], w_up[e_].rearrange("(c p) n -> p c n", p=128), w=[wud])
                    k.dma("pool", wd[:], w_down[e_].rearrange("(c p) n -> p c n", p=128), w=[wdd])
                    Xe, Xed = Xer.next(); XT, XTd = XTr.next(); AT, ATd = ATr.next()
                    k.dma("sp", Xe[:], s_Xg[e_ * CAP:(e_ + 1) * CAP, :].rearrange("(t p) d -> p t d", p=128), r=[d_Xg], w=[Xed])
                    for t in range(NCT):
                        for g in range(4):
                            pt, pd = pb.next()

                            def f(pe, pt=pt, Xe=Xe, t=t, g=g):
                                for j in range(4):
                                    c = g * 4 + j
                                    ins = pe.transpose(pt[:, j * 128:(j + 1) * 128], Xe[:, t, c * 128:(c + 1) * 128], identb[:])
                                return ins
                            k.op("pe", f, r=[Xed, d_c], w=[pd])
                            eng = "dve" if (g % 2 == 0) else "act"
                            if eng == "dve":
                                k.op("dve", lambda e, XT=XT, pt=pt, g=g, t=t: e.tensor_copy(
                                    XT[:, g * 4:(g + 1) * 4, t * 128:(t + 1) * 128], pt[:, 0:512].rearrange("p (j n) -> p j n", j=4)),
                                    r=[pd], w=[XTd])
                            else:
                                k.op("act", lambda e, XT=XT, pt=pt, g=g, t=t: e.copy(
                                    XT[:, g * 4:(g + 1) * 4, t * 128:(t + 1) * 128], pt[:, 0:512].rearrange("p (j n) -> p j n", j=4)),
                                    r=[pd], w=[XTd])
                    for fcx in range(4):
                        bG, bGd = pf.next(); bU, bUd = pf.next()

                        def f(pe, bG=bG, wg=wg, XT=XT, fcx=fcx):
                            for c in range(DC):
                                ins = pe.matmul(bG[:, 0:CAP], wg[:, c, fcx * 128:(fcx + 1) * 128], XT[:, c, :], start=(c == 0), stop=(c == DC - 1))
                            return ins
                        k.op("pe", f, r=[wgd, XTd], w=[bGd])

                        def f(pe, bU=bU, wu=wu, XT=XT, fcx=fcx):
                            for c in range(DC):
                                ins = pe.matmul(bU[:, 0:CAP], wu[:, c, fcx * 128:(fcx + 1) * 128], XT[:, c, :], start=(c == 0), stop=(c == DC - 1))
                            return ins
                        k.op("pe", f, r=[wud, XTd], w=[bUd])
                        sg_t, sg_d = sgr.next()
                        k.op("act", lambda e, sg_t=sg_t, bG=bG: e.activation(sg_t[:], bG[:, 0:CAP], AF.Silu), r=[bGd], w=[sg_d])
                        k.op("dve", lambda e, AT=AT, sg_t=sg_t, bU=bU, fcx=fcx: e.tensor_tensor(AT[:, fcx, :], sg_t[:], bU[:, 0:CAP], ALU.mult),
                             r=[sg_d, bUd], w=[ATd])
                    for t in range(NCT):
                        Ys, Ysd = Ysr.next()
                        for db in range(4):
                            bk, bd = pf.next()

                            def f(pe, bk=bk, AT=AT, wd=wd, t=t, db=db):
                                for fcx in range(4):
                                    ins = pe.matmul(bk[:, :], AT[:, fcx, t * 128:(t + 1) * 128], wd[:, fcx, db * 512:(db + 1) * 512],
                                                    start=(fcx == 0), stop=(fcx == 3))
                                return ins
                            k.op("pe", f, r=[ATd, wdd], w=[bd])
                            if db % 2 == 0:
                                k.op("act", lambda e, Ys=Ys, bk=bk, db=db: e.copy(Ys[:, db * 512:(db + 1) * 512], bk[:, :]), r=[bd], w=[Ysd])
                            else:
                                k.op("dve", lambda e, Ys=Ys, bk=bk, db=db: e.tensor_copy(Ys[:, db * 512:(db + 1) * 512], bk[:, :]), r=[bd], w=[Ysd])
                        r0 = e_ * CAP + t * 128
                        k.dma("sp", s_Yg[r0:r0 + 128, :], Ys[:], r=[Ysd], w=[d_Yg])
                k.interleave([(lambda e_=e_: expert_fn(e_)) for e_ in range(NE)], width=NIL)

            if stop <= 7:
                raise _Stop()
            d_out = Dep()
            with k.scope():
                l2g = k.sb("l2g", [128, D], F32); l2b = k.sb("l2b", [128, D], F32)
                d_l2 = Dep()
                k.dma("sp", l2g[:], ln2g_d.partition_broadcast(128), w=[d_l2])
                k.dma("sp", l2b[:], ln2b_d.partition_broadcast(128), w=[d_l2])
                h1r = Ring([(k.sb("h1", [128, D], F32), Dep()) for _ in range(3)])
                y1r = Ring([(k.sb("y1", [128, D], BF16), Dep()) for _ in range(3)])
                y2r = Ring([(k.sb("y2", [128, D], BF16), Dep()) for _ in range(3)])
                rsr = Ring([(k.sb("rs2", [128, 32], F32), Dep()) for _ in range(3)])
                def g_tile(ti):
                    h1, h1d = h1r.next(); y1, y1d = y1r.next(); y2, y2d = y2r.next()
                    k.dma("sp", h1[:], s_h1[ti * 128:(ti + 1) * 128, :], r=[d_h1], w=[h1d])
                    k.dma("pool", y1[:], s_Yg, r=[d_Yg, d_tab], w=[y1d],
                          indirect=dict(out_offset=None, in_offset=bass.IndirectOffsetOnAxis(ap=dtab[:, ti, 0:1], axis=0)))
                    k.dma("pool", y2[:], s_Yg, r=[d_Yg, d_tab], w=[y2d],
                          indirect=dict(out_offset=None, in_offset=bass.IndirectOffsetOnAxis(ap=dtab[:, ti, 1:2], axis=0)))
                    k.op("act", lambda e, h1=h1: e.mul(h1[:], h1[:], ALPHA), r=[h1d], w=[h1d])
                    k.op("dve", lambda e, h1=h1, y1=y1, ti=ti: e.scalar_tensor_tensor(
                        h1[:], y1[:], wtab[:, ti, 0:1], h1[:], ALU.mult, ALU.add), r=[y1d, d_tab, h1d], w=[h1d])
                    k.op("dve", lambda e, h1=h1, y2=y2, ti=ti: e.scalar_tensor_tensor(
                        h1[:], y2[:], wtab[:, ti, 1:2], h1[:], ALU.mult, ALU.add), r=[y2d, d_tab, h1d], w=[h1d])
                    rs, rsd = rsr.next()
                    st = rs[:, 0:24].rearrange("p (g k) -> p g k", g=4)
                    def fbn(e, rs=rs, h1=h1):
                        for gg in range(4):
                            ins = e.bn_stats(rs[:, 6 * gg:6 * gg + 6], h1[:, gg * 512:(gg + 1) * 512])
                        return ins
                    k.op("dve", fbn, r=[h1d], w=[rsd])
                    k.op("dve", lambda e, rs=rs: e.bn_aggr(rs[:, 24:26], rs[:, 0:24]), r=[rsd], w=[rsd])
                    k.op("act", lambda e, rs=rs: e.activation(rs[:, 26:27], rs[:, 25:26], AF.Ln, bias=epsc[:, 0:1], scale=1.0), r=[rsd, d_c], w=[rsd])
                    k.op("act", lambda e, rs=rs: e.activation(rs[:, 26:27], rs[:, 26:27], AF.Exp, scale=-0.5), r=[rsd], w=[rsd])
                    k.op("dve", lambda e, rs=rs, h1=h1: e.tensor_scalar(h1[:], h1[:], rs[:, 24:25], rs[:, 26:27], ALU.subtract, ALU.mult),
                         r=[rsd, h1d], w=[h1d])
                    k.op("pool", lambda e, h1=h1: e.tensor_tensor(h1[:], h1[:], l2g[:], ALU.mult), r=[h1d, d_l2], w=[h1d])
                    k.op("pool", lambda e, h1=h1: e.tensor_tensor(h1[:], h1[:], l2b[:], ALU.add), r=[h1d, d_l2], w=[h1d])
                    k.dma("sp", out_d[ti * 128:(ti + 1) * 128, :], h1[:], r=[h1d], w=[d_out])
                k.interleave([(lambda ti=ti: g_tile(ti)) for ti in range(NTO)], width=3)
        except _Stop:
            gscope.close()
        k.barrier()
        build.stats = (k.n_ins, {n: e.count for n, e in k.E.items()})
    return nc


def _consts(TP, TO, CAP):
    TA = TP + TO
    c = {}
    c["ident"] = np.eye(128, dtype=np.float32)
    s = np.arange(64)
    c["mask64"] = (s[:, None] <= s[None, :]).astype(np.float32)
    p = np.arange(128)
    c["ustrict"] = (p[:, None] < p[None, :]).astype(np.float32)
    slopes = 2.0 ** (-8.0 * np.arange(1, 9) / 8.0)
    kl = p[:, None]; ql = p[None, :]
    vis = (kl // 64) <= (ql // 64)
    dg = np.zeros((128, 8, 128), np.float32)
    for h in range(8):
        b = np.where(kl <= ql, slopes[h] * kl, slopes[h] * (2 * ql - kl))
        dg[:, h, :] = np.where(vis, b, -BIG) * 8.0
    c["diagbias"] = dg
    ab = np.zeros((128, 8, 32), np.float32)
    for h in range(8):
        for dlt in range(32):
            ab[:, h, dlt] = np.maximum(slopes[h] * (p - 128 * dlt), -ACLAMP)
    c["alibi"] = ab
    c["ecap"] = np.tile((np.arange(32) * CAP).astype(np.float32)[None, :], (128, 1))
    return c


def _prep_shared(inp):
    f = lambda a: np.ascontiguousarray(np.asarray(a, dtype=np.float32))
    b_in = f(inp["b_in"][0])
    sh = {}
    sh["w_in"] = f(inp["w_in"][0])

    def fm_bias(b):
        cols = np.concatenate([b[O_QA:O_QA + 1024], b[O_KA:O_KA + 1024], b[O_QB:O_QB + 1024], b[O_KB:O_KB + 1024],
                               b[O_GA:O_GA + 2048], b[O_GB:O_GB + 2048]])
        return np.ascontiguousarray(cols.reshape(64, 128).T)

    def tm_bias(b):
        return np.ascontiguousarray(np.concatenate([b[O_VA:O_VA + 1024], b[O_OA:O_OA + 1024], b[O_VB:O_VB + 1024]])[None, :])

    def if_bias(b):
        return np.ascontiguousarray(np.stack([b[O_IA:O_IA + 4], b[O_FA:O_FA + 4]], axis=1))
    b_mask = np.zeros_like(b_in)
    b_mask[O_IA:O_IA + 4] = -BIG
    b_mask[O_FA:O_FA + 4] = BIG
    sh["bias_real"] = (fm_bias(b_in), tm_bias(b_in), if_bias(b_in))
    sh["bias_mask"] = (fm_bias(b_mask), tm_bias(b_mask), if_bias(b_mask))
    cw = f(inp["conv_w"][0])
    sh["cw"] = np.ascontiguousarray(cw.T.reshape(16, 128, 4).transpose(1, 0, 2))
    sh["cb"] = np.ascontiguousarray(f(inp["conv_b"][0]).reshape(16, 128).T)
    sh["mlstm_norm_g"] = f(inp["mlstm_norm_g"][0])
    sh["diff_norm_g"] = f(inp["diff_norm_g"][0])
    sh["lamv"] = np.ascontiguousarray(np.stack([f(inp["lambda_q1"][0]), f(inp["lambda_k1"][0]),
                                                f(inp["lambda_q2"][0]), f(inp["lambda_k2"][0])]))
    for n in ("w_a", "w_b", "w_out", "ln1_g", "ln1_b", "ln2_g", "ln2_b", "w_gate", "w_up", "w_down"):
        sh[n] = f(inp[n][0])
    sh["w_r"] = np.ascontiguousarray(np.concatenate([f(inp["w_grp"][0]), f(inp["w_exp"][0])], axis=1))
    sh["b_r"] = np.ascontiguousarray(np.concatenate([f(inp["b_grp"][0]), f(inp["b_exp"][0])])[None, :])
    return sh


def make_in_maps(inp, TP, TO, CAP):
    x = np.asarray(inp["x"], dtype=np.float32)
    B, S, _ = x.shape
    assert S == TP + TO or S == 2 * TO
    sh = _prep_shared(inp)
    cs = _consts(TP, TO, CAP)
    TA = TP + TO
    maps = []
    for b in range(B):
        for half in range(2):
            m = {}
            if half == 0:
                xa = np.zeros((TA, D), np.float32)
                xa[TP:] = x[b, 0:TO]
                valid = np.concatenate([np.zeros(TP, np.float32), np.ones(TO, np.float32)])
                pre = sh["bias_mask"]
            else:
                xa = np.ascontiguousarray(x[b, TO - TP:2 * TO])
                valid = np.ones(TA, np.float32)
                pre = sh["bias_real"]
            m["x"] = xa
            m["valid_tm"] = np.ascontiguousarray(valid.reshape(TA // 128, 128).T)
            m["bfm"], m["btm"], m["bif"] = sh["bias_real"]
            m["bfm_pre"], m["btm_pre"], m["bif_pre"] = pre
            for n in ("w_in", "cw", "cb", "mlstm_norm_g", "diff_norm_g", "lamv", "w_a", "w_b", "w_out", "ln1_g", "ln1_b",
                      "ln2_g", "ln2_b", "w_gate", "w_up", "w_down", "w_r", "b_r"):
                m[n] = sh[n]
            m.update(cs)
            maps.append(m)
    return maps


_TP, _TO, _CAP = 2048, 2048, 256


def kernel(**inputs):
    x = np.asarray(inputs["x"])
    B, S, _ = x.shape
    maps = make_in_maps(inputs, _TP, _TO, _CAP)
    nc = build(_TP, _TO, _CAP)
    res = run_bass_kernel_spmd(nc, maps, core_ids=list(range(len(maps))))
    out = np.zeros((B, S, D), np.float32)
    i = 0
    for b in range(B):
        for half in range(2):
            out[b, half * _TO:(half + 1) * _TO] = res.results[i]["out"]
            i += 1
    return out
```

```python
import math
from contextlib import ExitStack, contextmanager
import numpy as np
import concourse.bass as bass
import concourse.mybir as mybir
from concourse.bass_utils import run_bass_kernel_spmd

F32 = mybir.dt.float32
BF16 = mybir.dt.bfloat16
I32 = mybir.dt.int32
AF = mybir.ActivationFunctionType
ALU = mybir.AluOpType
AX = mybir.AxisListType

D = 2048
DC = 16
NIN = 11272
O_QA, O_KA, O_VA, O_OA, O_IA, O_FA, O_QB, O_KB, O_VB, O_GA, O_GB = (
    0, 1024, 2048, 3072, 4096, 4100, 4104, 5128, 6152, 7176, 9224)
ALPHA = 2.0 ** 0.25
EPS = 1e-5
NE = 32
BIG = 30000.0
LN16 = math.log(16.0)
ACLAMP = 40.0
import os
ILV = int(os.environ.get('ILV', '1'))
EXPV = int(os.environ.get('EXPV', '0'))
NIL = int(os.environ.get('NIL', '2'))
SLOPES = [2.0 ** (-(h + 1)) for h in range(8)]


class _Stop(Exception):
    pass


class Dep:
    __slots__ = ("w", "rs")

    def __init__(self):
        self.w = None
        self.rs = {}


class _Eng:
    def __init__(self, name, eng, sem):
        self.name, self.eng, self.sem = name, eng, sem
        self.count = 0
        self.waited = {}


class _DmaSem:
    def __init__(self, key, sem):
        self.key, self.sem, self.total = key, sem, 0


class Kern:
    def __init__(self, nc, stack, n_dma_sems=48):
        self.nc = nc
        self.stack = stack
        self.E = {}
        for name, eng in (("pe", nc.tensor), ("act", nc.scalar), ("dve", nc.vector),
                          ("pool", nc.gpsimd), ("sp", nc.sync)):
            sem = stack.enter_context(nc.semaphore("s_" + name))
            self.E[name] = _Eng(name, eng, sem)
        self.dsems = [_DmaSem("d%d" % i, stack.enter_context(nc.semaphore("d%d" % i)))
                      for i in range(n_dma_sems)]
        self.dpool = {"hw": self.dsems[0:n_dma_sems // 2], "sw": self.dsems[n_dma_sems // 2:]}
        self.drr = {"hw": 0, "sw": 0}
        self.n_ins = 0
        self.uid = 0
        self._cur = None
        self._atomic = 0

    def sb(self, name, shape, dt):
        self.uid += 1
        return self.stack.enter_context(self.nc.sbuf_tensor("%s_%d" % (name, self.uid), list(shape), dt))

    def ps(self, name, shape, dt):
        return self.stack.enter_context(self.nc.psum_tensor(name, list(shape), dt))

    @contextmanager
    def scope(self):
        old = self.stack
        stopped = False
        with ExitStack() as st:
            self.stack = st
            try:
                yield
            except _Stop:
                stopped = True
            if not stopped:
                self.barrier()
        self.stack = old
        if stopped:
            raise _Stop()

    def _wait(self, es, ev):
        key, sem, val = ev
        if es.name == "pe" and key == "pe":
            return
        if es.waited.get(key, 0) >= val:
            return
        es.eng.wait_ge(sem, val)
        es.waited[key] = val

    def _pre(self, es, r, w):
        for d in r:
            if d.w is not None:
                self._wait(es, d.w)
        for d in w:
            if d.w is not None:
                self._wait(es, d.w)
            for ev in d.rs.values():
                self._wait(es, ev)

    def _post(self, ev, r, w):
        for d in r:
            d.rs[ev[0]] = ev
        for d in w:
            d.w = ev
            d.rs = {}

    def op(self, en, fn, r=(), w=()):
        es = self.E[en]
        self._pre(es, r, w)
        ins = fn(es.eng)
        es.count += 1
        ins.then_inc(es.sem, 1)
        self.n_ins += 1
        ev = (es.name, es.sem, es.count)
        self._post(ev, r, w)
        self._yield()
        return ev

    def dma(self, qn, out, in_, r=(), w=(), indirect=None, **kw):
        es = self.E[qn]
        self._pre(es, r, w)
        kind = "sw" if qn == "pool" else "hw"
        pool = self.dpool[kind]
        ds = pool[self.drr[kind]]
        self.drr[kind] = (self.drr[kind] + 1) % len(pool)
        if ds.total > 0:
            self._wait(es, (ds.key, ds.sem, ds.total))
        if indirect is not None:
            ins = es.eng.indirect_dma_start(out=out, in_=in_, **indirect)
        else:
            ins = es.eng.dma_start(out=out, in_=in_, **kw)
        ds.total += 16
        ins.then_inc(ds.sem, 16)
        self.n_ins += 1
        ev = (ds.key, ds.sem, ds.total)
        self._post(ev, r, w)
        self._yield()
        return ev

    def _yield(self):
        w = self._cur
        if w is None or self._atomic > 0:
            return
        self._sched_sem.release()
        w["go"].acquire()

    def interleave(self, fns, width=2):
        import threading
        if width <= 1 or len(fns) <= 1:
            for fn in fns:
                fn()
            return
        self._sched_sem = threading.Semaphore(0)
        pending = list(fns)
        active = []
        err = []

        def runner(w, fn):
            w["go"].acquire()
            try:
                fn()
            except BaseException as e:
                err.append(e)
            w["done"] = True
            self._sched_sem.release()
        while pending or active:
            while pending and len(active) < width:
                w = {"go": threading.Semaphore(0), "done": False}
                w["t"] = threading.Thread(target=runner, args=(w, pending.pop(0)), daemon=True)
                w["t"].start()
                active.append(w)
            for w in list(active):
                self._cur = w
                w["go"].release()
                self._sched_sem.acquire()
                self._cur = None
                if w["done"]:
                    active.remove(w)
                if err:
                    raise err[0]

    @contextmanager
    def atomic(self):
        self._atomic += 1
        try:
            yield
        finally:
            self._atomic -= 1

    def barrier(self):
        evs = [(e.name, e.sem, e.count) for e in self.E.values() if e.count > 0]
        evs += [(d.key, d.sem, d.total) for d in self.dsems if d.total > 0]
        for es in self.E.values():
            for ev in evs:
                if ev[0] == es.name and es.name == "pe":
                    continue
                self._wait(es, ev)


class Ring:
    def __init__(self, items):
        self.items = items
        self.i = 0

    def next(self):
        it = self.items[self.i]
        self.i = (self.i + 1) % len(self.items)
        return it


def build(TP, TO, CAP, dbg=False, stop=99):
    TA = TP + TO
    NTA, NTO, NTP = TA // 128, TO // 128, TP // 128
    NCH, CH0 = TA // 64, TP // 64
    NROW = NE * CAP + 128
    TRASH = NE * CAP
    nc = bass.Bass("TRN2", target_bir_lowering=False)

    def din(name, shape, dt=F32):
        return nc.dram_tensor(name, list(shape), dt, kind="ExternalInput").ap()

    x = din("x", [TA, D])
    w_in = din("w_in", [D, NIN])
    bfm_d = din("bfm", [128, 64]); bfmp_d = din("bfm_pre", [128, 64])
    btm_d = din("btm", [1, 3072]); btmp_d = din("btm_pre", [1, 3072])
    bif_d = din("bif", [4, 2]); bifp_d = din("bif_pre", [4, 2])
    valid_d = din("valid_tm", [128, NTA])
    cw_d = din("cw", [128, 16, 4]); cb_d = din("cb", [128, 16])
    ng_d = din("mlstm_norm_g", [1024]); dg_d = din("diff_norm_g", [128])
    lam_d = din("lamv", [4, 64])
    w_a = din("w_a", [1024, D]); w_b = din("w_b", [1024, D]); w_out = din("w_out", [D, D])
    ln1g_d = din("ln1_g", [D]); ln1b_d = din("ln1_b", [D]); ln2g_d = din("ln2_g", [D]); ln2b_d = din("ln2_b", [D])
    wr_d = din("w_r", [D, 36]); br_d = din("b_r", [1, 36])
    if stop > 6:
        w_gate = din("w_gate", [NE, D, 512]); w_up = din("w_up", [NE, D, 512]); w_down = din("w_down", [NE, 512, D])
    ident_d = din("ident", [128, 128]); mask64_d = din("mask64", [64, 64]); ustrict_d = din("ustrict", [128, 128])
    dgb_d = din("diagbias", [128, 8, 128]); abt_d = din("alibi", [128, 8, 32]); ecap_d = din("ecap", [128, 32])
    out_d = nc.dram_tensor("out", [TO, D], F32, kind="ExternalOutput").ap()

    def dscr(name, shape, dt):
        if dbg:
            return nc.dram_tensor(name, list(shape), dt, kind="ExternalOutput").ap()
        return nc.dram_tensor(name, list(shape), dt).ap()

    s_qkaT = dscr("s_qkaT", [2048, TA], BF16)
    s_qkbT = dscr("s_qkbT", [2048, TA], BF16)
    s_gT = dscr("s_gT", [4096, TO], BF16)
    s_va = dscr("s_va", [TA, 1024], BF16)
    s_vb = dscr("s_vb", [TA, 1024], BF16)
    s_oa = dscr("s_oa", [TO, 1024], BF16)
    s_seq = dscr("s_seq", [3, 4, TA], F32)
    s_dec = dscr("s_dec", [4, NCH], F32)
    s_h1 = dscr("s_h1", [TO, D], F32)
    s_Xg = dscr("s_Xg", [NROW, D], BF16)
    s_Yg = dscr("s_Yg", [NROW, D], BF16)
    s_haT = dscr("s_haT", [1024, TO], BF16)
    s_obT = dscr("s_obT", [1024, TO], BF16)
    s_mgT = dscr("s_mgT", [2048, TO], BF16)
    HB = min(512, TO)
    if dbg:
        s_TT = dscr("s_TT", [64, 3 * NCH * 4], F32)
        s_dtab = dscr("s_dtab", [128, NTO * 2], I32)
        s_wtab = dscr("s_wtab", [128, NTO * 2], F32)

    with ExitStack() as st0:
        k = Kern(nc, st0)
        try:
            banks = [(k.ps("pf%d" % i, [128, 512], F32), Dep()) for i in range(8)]
            pf = Ring(banks[0:6])
            pb = Ring([(banks[i][0].bitcast(BF16), banks[i][1]) for i in (6, 7)])
            d_c = Dep()
            identf = k.sb("identf", [128, 128], F32)
            identb = k.sb("identb", [128, 128], BF16)
            onesb = k.sb("onesb", [128, 128], BF16)
            onesf = k.sb("onesf", [128, 128], F32)
            mask64 = k.sb("mask64", [64, 64], F32)
            ustr = k.sb("ustr", [128, 128], BF16)
            dgb = k.sb("dgb", [128, 8, 128], BF16)
            abt = k.sb("abt", [128, 8, 32], F32)
            ecap = k.sb("ecap", [128, 32], F32)
            validb = k.sb("validb", [128, NTA], BF16)
            zero1 = k.sb("zero1", [128, 1], F32)
            epsc = k.sb("epsc", [128, 1], F32)
            k.dma("sp", identf[:], ident_d, w=[d_c])
            k.dma("pool", identb[:], ident_d, w=[d_c])
            k.dma("sp", mask64[:], mask64_d, w=[d_c])
            k.dma("pool", ustr[:], ustrict_d, w=[d_c])
            k.dma("pool", dgb[:], dgb_d, w=[d_c])
            k.dma("sp", abt[:], abt_d, w=[d_c])
            k.dma("sp", ecap[:], ecap_d, w=[d_c])
            k.dma("pool", validb[:], valid_d, w=[d_c])
            k.op("dve", lambda e: e.memset(onesb[:], 1.0), w=[d_c])
            k.op("dve", lambda e: e.memset(onesf[:], 1.0), w=[d_c])
            k.op("dve", lambda e: e.memset(zero1[:], 0.0), w=[d_c])
            k.op("dve", lambda e: e.memset(epsc[:], EPS), w=[d_c])
            d_zt, d_Xg, d_Yg = Dep(), Dep(), Dep()

            dtab = k.sb("dtab", [128, NTO, 2], I32)
            wtab = k.sb("wtab", [128, NTO, 2], F32)
            TT = k.sb("TT", [64, 3, NCH, 4], F32)
            decbc = k.sb("decbc", [128, 4 * NCH], F32)
            gscope = ExitStack()
            _old = k.stack
            k.stack = gscope
            Gi = k.sb("Gi", [4, TA], F32); Gf = k.sb("Gf", [4, TA], F32)
            k.stack = _old
            d_G = Dep()
            d_tab = Dep()
            d_h1 = Dep()
            d_haT, d_obT, d_mg = Dep(), Dep(), Dep()

            with k.scope():
                zt = k.sb("zt", [128, 4096], BF16)
                k.op("pool", lambda e: e.memset(zt[:], 0.0), w=[d_zt])
                r0 = 0
                while r0 < NROW:
                    nr = min(256, NROW - r0)
                    k.dma("sp", s_Xg[r0:r0 + nr, :].rearrange("(t p) d -> p t d", p=128),
                          zt[:, 0:(nr // 128) * D].rearrange("p (t d) -> p t d", d=D), r=[d_zt], w=[d_Xg])
                    r0 += nr
                k.dma("sp", s_Yg[TRASH:TRASH + 128, :], zt[:, 0:D], r=[d_zt], w=[d_Yg])
                TX = max(TP, TO)
                xT = k.sb("xT", [128, DC, TX], BF16)
                xb = Ring([(k.sb("xb", [128, D], BF16), Dep()) for _ in range(2)])
                wt = Ring([(k.sb("wt", [128, DC, 512], BF16), Dep()) for _ in range(2)])
                wif = k.sb("wif", [128, DC, 8], BF16)
                evf = Ring([(k.sb("evf", [128, 4, 512], BF16), Dep()) for _ in range(2)])
                evt = Ring([(k.sb("evt", [128, 512], BF16), Dep()) for _ in range(3)])
                bfm = k.sb("bfm", [128, 64], F32); bfmp = k.sb("bfmp", [128, 64], F32)
                btm = k.sb("btm", [1, 3072], BF16); btmp = k.sb("btmp", [1, 3072], BF16)
                bif = k.sb("bif", [4, 2], F32); bifp = k.sb("bifp", [4, 2], F32)
                d_b, d_wif = Dep(), Dep()
                k.dma("sp", bfm[:], bfm_d, w=[d_b]); k.dma("sp", bfmp[:], bfmp_d, w=[d_b])
                k.dma("pool", btm[:], btm_d, w=[d_b]); k.dma("pool", btmp[:], btmp_d, w=[d_b])
                k.dma("sp", bif[:], bif_d, w=[d_b]); k.dma("sp", bifp[:], bifp_d, w=[d_b])
                k.dma("pool", wif[:], w_in.rearrange("(c p) n -> p c n", p=128)[:, :, O_IA:O_IA + 8], w=[d_wif])
                d_scr = {"qka": Dep(), "qkb": Dep(), "g": Dep(), "va": Dep(), "vb": Dep(), "oa": Dep()}

                FM = []
                for j in range(2):
                    FM.append((O_QA + 512 * j, "qka", s_qkaT, 512 * j, AF.Identity, "q", 4 * j))
                    FM.append((O_KA + 512 * j, "qka", s_qkaT, 1024 + 512 * j, AF.Identity, "all", 8 + 4 * j))
                    FM.append((O_QB + 512 * j, "qkb", s_qkbT, 512 * j, AF.Identity, "own", 16 + 4 * j))
                    FM.append((O_KB + 512 * j, "qkb", s_qkbT, 1024 + 512 * j, AF.Identity, "all", 24 + 4 * j))
                for j in range(8):
                    FM.append((O_GA + 512 * j, "g", s_gT, 512 * j, AF.Sigmoid, "gate", 32 + 4 * j))
                TM = []
                for j in range(2):
                    TM.append((O_VA + 512 * j, "va", s_va, 512 * j, AF.Identity, "all", 512 * j))
                    TM.append((O_OA + 512 * j, "oa", s_oa, 512 * j, AF.Sigmoid, "own", 1024 + 512 * j))
                    TM.append((O_VB + 512 * j, "vb", s_vb, 512 * j, AF.Identity, "all", 2048 + 512 * j))

                for phase in ("pre", "own"):
                    t0, nt = (0, TP) if phase == "pre" else (TP, TO)
                    bfm_x, btm_x, bif_x = (bfmp, btmp, bifp) if phase == "pre" else (bfm, btm, bif)
                    d_xT = [Dep() for _ in range(nt // 128)]
                    for ti in range(nt // 128):
                        xb_t, xb_d = xb.next()
                        k.dma("pool", xb_t[:], x[t0 + ti * 128:t0 + (ti + 1) * 128, :], w=[xb_d])
                        for g in range(4):
                            pt, pd = pb.next()

                            def f(pe, g=g, pt=pt, xb_t=xb_t):
                                for j in range(4):
                                    c = g * 4 + j
                                    ins = pe.transpose(pt[:, j * 128:(j + 1) * 128], xb_t[:, c * 128:(c + 1) * 128], identb[:])
                                return ins
                            k.op("pe", f, r=[xb_d, d_c], w=[pd])
                            k.op("dve", lambda e, g=g, ti=ti, pt=pt: e.tensor_copy(
                                xT[:, g * 4:(g + 1) * 4, ti * 128:(ti + 1) * 128],
                                pt[:, 0:512].rearrange("p (j n) -> p j n", j=4)), r=[pd], w=[d_xT[ti]])
                    tb = 0
                    while tb < nt:
                        n = min(512, nt - tb)
                        dx = d_xT[tb // 128:(tb + n) // 128]
                        for gi, Gt in ((0, Gi), (1, Gf)):
                            bk, bd = pf.next()

                            def f(pe, gi=gi, bk=bk, tb=tb, n=n):
                                for c in range(DC):
                                    ins = pe.matmul(bk[0:4, 0:n], wif[:, c, gi * 4:(gi + 1) * 4], xT[:, c, tb:tb + n],
                                                    start=(c == 0), stop=(c == DC - 1))
                                return ins
                            k.op("pe", f, r=dx + [d_wif], w=[bd])
                            k.op("act", lambda e, gi=gi, Gt=Gt, bk=bk, tb=tb, n=n: e.activation(
                                Gt[0:4, t0 + tb:t0 + tb + n], bk[0:4, 0:n], AF.Identity, bias=bif_x[:, gi:gi + 1], scale=1.0),
                                r=[bd, d_b], w=[d_G])
                        tb += n
                    for (c0, dkey, dst, row0, func, which, fmc) in FM:
                        if phase == "pre":
                            if which in ("own", "gate"):
                                continue
                            tlo = (TP - 128) if which == "q" else 0
                        else:
                            tlo = 0
                        w_t, w_d = wt.next()
                        k.dma("pool", w_t[:], w_in.rearrange("(c p) n -> p c n", p=128)[:, :, c0:c0 + 512], w=[w_d])
                        tb = tlo
                        while tb < nt:
                            n = min(512, nt - tb)
                            dx = d_xT[tb // 128:(tb + n) // 128]
                            ev_t, ev_d = evf.next()
                            for g in range(4):
                                bk, bd = pf.next()

                                def f(pe, g=g, bk=bk, tb=tb, n=n, w_t=w_t):
                                    for c in range(DC):
                                        ins = pe.matmul(bk[:, 0:n], w_t[:, c, g * 128:(g + 1) * 128], xT[:, c, tb:tb + n],
                                                        start=(c == 0), stop=(c == DC - 1))
                                    return ins
                                k.op("pe", f, r=dx + [w_d], w=[bd])
                                k.op("act", lambda e, g=g, bk=bk, n=n, ev_t=ev_t, func=func, fmc=fmc: e.activation(
                                    ev_t[:, g, 0:n], bk[:, 0:n], func, bias=bfm_x[:, fmc + g:fmc + g + 1], scale=1.0),
                                    r=[bd, d_b], w=[ev_d])
                            tcol = (tb if which == "gate" else t0 + tb)
                            k.dma("sp", dst[row0:row0 + 512, tcol:tcol + n].rearrange("(g p) t -> p g t", p=128),
                                  ev_t[:, :, 0:n], r=[ev_d], w=[d_scr[dkey]])
                            tb += n
                    for (c0, dkey, dst, col0, func, which, bcol) in TM:
                        if phase == "pre" and which == "own":
                            continue
                        w_t, w_d = wt.next()
                        k.dma("pool", w_t[:], w_in.rearrange("(c p) n -> p c n", p=128)[:, :, c0:c0 + 512], w=[w_d])
                        for ti in range(nt // 128):
                            bk, bd = pf.next()

                            def f(pe, bk=bk, ti=ti, w_t=w_t, bcol=bcol):
                                for c in range(DC):
                                    pe.matmul(bk[:, :], xT[:, c, ti * 128:(ti + 1) * 128], w_t[:, c, :],
                                              start=(c == 0), stop=False)
                                return pe.matmul(bk[:, :], onesb[0:1, :], btm_x[0:1, bcol:bcol + 512], start=False, stop=True)
                            k.op("pe", f, r=[d_xT[ti], w_d, d_b, d_c], w=[bd])
                            e_t, e_d = evt.next()
                            k.op("act", lambda e, bk=bk, e_t=e_t, func=func: e.activation(e_t[:], bk[:, :], func),
                                 r=[bd], w=[e_d])
                            trow = (ti * 128 if which == "own" else t0 + ti * 128)
                            k.dma("sp", dst[trow:trow + 128, col0:col0 + 512], e_t[:], r=[e_d], w=[d_scr[dkey]])

            if True:
                if stop <= 1:
                    raise _Stop()
                with k.scope():
                    t1 = k.sb("t1", [4, TA], F32); t2 = k.sb("t2", [4, TA], F32)
                    mt = k.sb("mt", [4, NCH], F32); dec = k.sb("dec", [4, NCH], F32)
                    sq = k.sb("sq", [4, 3, TA], F32)
                    dq = Dep()
                    V = "dve"
                    k.op("act", lambda e: e.activation(t1[:], Gf[:], AF.Abs), r=[d_G], w=[dq])
                    k.op("act", lambda e: e.activation(t1[:], t1[:], AF.Exp, scale=-1.0), r=[dq], w=[dq])
                    k.op("act", lambda e: e.activation(t1[:], t1[:], AF.Ln, bias=1.0, scale=1.0), r=[dq], w=[dq])
                    k.op(V, lambda e: e.tensor_scalar_min(t2[:], Gf[:], 0.0), r=[d_G], w=[dq])
                    k.op(V, lambda e: e.tensor_sub(t2[:], t2[:], t1[:]), r=[dq], w=[dq])
                    k.op(V, lambda e: e.tensor_scalar_mul(t2[:], t2[:], 0.5), r=[dq], w=[dq])
                    k.op(V, lambda e: e.tensor_tensor_scan(t1[:], t2[:], t2[:], 0.0, ALU.add, ALU.add), r=[dq], w=[dq])
                    Bc = t1
                    k.op(V, lambda e: e.tensor_sub(Gi[:], Gi[:], Bc[:]), r=[dq, d_G], w=[dq, d_G])
                    at = Gi
                    k.op(V, lambda e: e.tensor_tensor_scan(t2[:], at[:], at[:], 0.0, ALU.max, ALU.max), r=[dq, d_G], w=[dq])
                    ut = t2
                    ut3 = ut[:].rearrange("p (c s) -> p c s", s=64)
                    at3 = at[:].rearrange("p (c s) -> p c s", s=64)
                    Bc3 = Bc[:].rearrange("p (c s) -> p c s", s=64)
                    k.op(V, lambda e: e.memset(mt[:, 0:1], 0.0), w=[dq])
                    if NCH > 1:
                        k.op(V, lambda e: e.tensor_copy(mt[:, 1:NCH], ut3[:, 0:NCH - 1, 63]), r=[dq], w=[dq])
                    uL = ut3[:, :, 63]
                    mtb = mt[:, :].unsqueeze(2).to_broadcast([4, NCH, 64])
                    k.op(V, lambda e: e.tensor_sub(dec[:], mt[:], uL), r=[dq], w=[dq])
                    k.op("act", lambda e: e.activation(dec[:], dec[:], AF.Exp), r=[dq], w=[dq])
                    sq0 = sq[:, 0, :].rearrange("p (c s) -> p c s", s=64)
                    sq1 = sq[:, 1, :].rearrange("p (c s) -> p c s", s=64)
                    sq2 = sq[:, 2, :].rearrange("p (c s) -> p c s", s=64)
                    k.op(V, lambda e: e.tensor_sub(sq0, at3, mtb), r=[dq, d_G], w=[dq])
                    k.op(V, lambda e: e.tensor_scalar(sq[:, 0, :], sq[:, 0, :], 80.0, -LN16, ALU.min, ALU.add), r=[dq], w=[dq])
                    k.op("act", lambda e: e.activation(sq[:, 0, :], sq[:, 0, :], AF.Exp), r=[dq], w=[dq])
                    k.op(V, lambda e: e.tensor_tensor(sq1, sq0, dec[:, :].unsqueeze(2).to_broadcast([4, NCH, 64]), ALU.mult),
                         r=[dq], w=[dq])
                    k.op(V, lambda e: e.tensor_tensor(sq2, Bc3, mtb, ALU.add), r=[dq], w=[dq])
                    k.op(V, lambda e: e.tensor_scalar(sq[:, 2, :], sq[:, 2, :], -1.0, 80.0, ALU.mult, ALU.min), r=[dq], w=[dq])
                    k.op("act", lambda e: e.activation(sq[:, 2, :], sq[:, 2, :], AF.Exp), r=[dq], w=[dq])
                    d_seq = Dep()
                    k.dma("sp", s_dec, dec[:], r=[dq], w=[d_seq])
                    d_TT = Dep()
                    for q in range(3):
                        c0 = 0
                        while c0 < NCH:
                            ncc = min(128, NCH - c0)
                            bk, bd = pf.next()

                            def f(pe, bk=bk, q=q, c0=c0, ncc=ncc):
                                for cc in range(ncc):
                                    c = c0 + cc
                                    ins = pe.transpose(bk[0:64, cc * 4:cc * 4 + 4], sq[0:4, q, c * 64:(c + 1) * 64], identf[0:4, 0:4])
                                return ins
                            k.op("pe", f, r=[dq, d_c], w=[bd])
                            k.op("act", lambda e, bk=bk, q=q, c0=c0, ncc=ncc: e.copy(
                                TT[:, q, c0:c0 + ncc, :], bk[0:64, 0:ncc * 4].rearrange("p (c h) -> p c h", h=4)), r=[bd], w=[d_TT])
                            c0 += ncc
                    k.dma("sp", decbc[:], s_dec.rearrange("h c -> (h c)").partition_broadcast(128), r=[d_seq], w=[d_TT])
                gscope.close()
                if dbg:
                    k.dma("sp", s_TT, TT[:].rearrange("p a b c -> p (a b c)"), r=[d_TT], w=[Dep()])
                if stop <= 2:
                    raise _Stop()
                with k.scope():
                    qT = k.sb("qT", [128, 8, TO], BF16)
                    kT = k.sb("kT", [128, 8, TA], BF16)
                    cw = k.sb("cw", [128, 16, 4], F32); cb = k.sb("cb", [128, 16], F32)
                    ngb = k.sb("ngb", [64, 1024], F32)
                    d_cw, d_qT, d_kT = Dep(), Dep(), Dep()
                    k.dma("sp", cw[:], cw_d, w=[d_cw]); k.dma("sp", cb[:], cb_d, w=[d_cw])
                    k.dma("sp", ngb[:], ng_d.partition_broadcast(64), w=[d_cw])
                    cscope = k.scope()
                    cscope.__enter__()
                    cin = Ring([(k.sb("cin", [128, 3 + TA], BF16), Dep()) for _ in range(3)])
                    Dg = k.sb("Dg", [128, 16, 4, 128], BF16)
                    d_Dg = Dep()
                    for fc in range(16):
                        for j in range(4):
                            k.op("dve", lambda e, fc=fc, j=j: e.tensor_scalar_mul(Dg[:, fc, j, :], identf[:], cw[:, fc, j:j + 1]),
                                 r=[d_cw, d_c], w=[d_Dg])
                    for fc in list(range(8, 16)) + list(range(8)):
                        isq = fc < 8
                        lo = (TP - 128) if isq else 0
                        o0 = TP if isq else 0
                        n = TA - o0
                        ci, cd = cin.next()
                        if not isq:
                            k.op("pool", lambda e, ci=ci: e.memset(ci[:, 0:3], 0.0), w=[cd])
                        k.dma("sp", ci[:, 3 + lo:3 + TA], s_qkaT[fc * 128:(fc + 1) * 128, lo:TA], r=[d_scr["qka"]], w=[cd])
                        tb = 0
                        while tb < n:
                            nn = min(512, n - tb)
                            bk, bd = pf.next()

                            def f(pe, bk=bk, ci=ci, fc=fc, o0=o0, tb=tb, nn=nn):
                                for j in range(4):
                                    ins = pe.matmul(bk[:, 0:nn], Dg[:, fc, j, :], ci[:, o0 + tb + j:o0 + tb + j + nn],
                                                    start=(j == 0), stop=(j == 3))
                                return ins
                            k.op("pe", f, r=[cd, d_Dg], w=[bd])
                            if isq:
                                k.op("act", lambda e, bk=bk, fc=fc, tb=tb, nn=nn: e.activation(
                                    qT[:, fc, tb:tb + nn], bk[:, 0:nn], AF.Silu, bias=cb[:, fc:fc + 1], scale=1.0),
                                    r=[bd, d_cw], w=[d_qT])
                            else:
                                k.op("act", lambda e, bk=bk, fc=fc, tb=tb, nn=nn: e.activation(
                                    kT[:, fc - 8, tb:tb + nn], bk[:, 0:nn], AF.Silu, bias=cb[:, fc:fc + 1], scale=1.0),
                                    r=[bd, d_cw], w=[d_kT])
                            tb += nn
                    cscope.__exit__(None, None, None)
                    a_S = Ring([banks[0], banks[1]])
                    bO, bOd = banks[2]
                    bO1, bO1d = banks[3]
                    m_U = Ring(banks[4:5])
                    bSN, bSNd = banks[5]
                    bNN, bNNd = banks[6]
                    pb_all = pb
                    pb = Ring([(banks[7][0].bitcast(BF16), banks[7][1])])
                    Cst = [k.sb("Cst", [128, 2, 257], F32) for _ in range(4)]
                    hblk = Ring([(k.sb("hblk", [128, 8, HB], BF16), Dep()) for _ in range(1)])
                    Cbf = [k.sb("Cbf", [128, 2, 257], BF16) for _ in range(4)]
                    d_C = [Dep() for _ in range(4)]
                    d_Cbf = [Dep() for _ in range(4)]
                    for h in range(4):
                        k.op("pool", lambda e, h=h: e.memset(Cst[h][:], 0.0), w=[d_C[h]])
                        k.op("pool", lambda e, h=h: e.memset(Cbf[h][:], 0.0), w=[d_Cbf[h]])
                    vch_items = []
                    for _ in range(3):
                        vt = k.sb("vch", [64, 4, 257], BF16)
                        vd = Dep()
                        k.op("pool", lambda e, vt=vt: e.memset(vt[:, :, 256:257], 1.0), w=[vd])
                        vch_items.append((vt, vd))
                    vch = Ring(vch_items)
                    sor = Ring([(k.sb("so", [64, 1024], BF16), Dep()) for _ in range(1)])
                    kwr = Ring([(k.sb("kw", [64, 256], BF16), Dep()) for _ in range(3)])
                    Wtr = Ring([(k.sb("Wt", [64, 64], BF16), Dep()) for _ in range(3)])
                    Nsr = Ring([(k.sb("Ns", [64, 4, 257], F32), Dep()) for _ in range(2)])
                    hgr = Ring([(k.sb("hg", [64, 4, 256], F32), Dep()) for _ in range(1)])
                    hbr = Ring([(k.sb("hb", [64, 1024], BF16), Dep()) for _ in range(2)])
                    smr = Ring([(k.sb("sm", [64, 64], F32), Dep()) for _ in range(2)])
                    lamt = k.sb("lamt", [128, 4, 64], F32)
                    lsm = k.sb("lsm", [128, 8], F32)
                    gnb = k.sb("gnb", [128, 128], F32)
                    d_l = Dep()
                    k.dma("sp", lamt[:], lam_d.rearrange("a b -> (a b)").partition_broadcast(128).rearrange("p (a b) -> p a b", a=4), w=[d_l])
                    k.dma("sp", gnb[:], dg_d.partition_broadcast(128), w=[d_l])
                    k.op("dve", lambda e: e.tensor_tensor(lamt[:, 0, :], lamt[:, 0, :], lamt[:, 1, :], ALU.mult), r=[d_l], w=[d_l])
                    k.op("dve", lambda e: e.tensor_tensor(lamt[:, 2, :], lamt[:, 2, :], lamt[:, 3, :], ALU.mult), r=[d_l], w=[d_l])
                    k.op("dve", lambda e: e.reduce_sum(lsm[:, 0:1], lamt[:, 0, :], AX.X), r=[d_l], w=[d_l])
                    k.op("dve", lambda e: e.reduce_sum(lsm[:, 1:2], lamt[:, 2, :], AX.X), r=[d_l], w=[d_l])
                    k.op("act", lambda e: e.activation(lsm[:, 2:4], lsm[:, 0:2], AF.Exp), r=[d_l], w=[d_l])
                    k.op("dve", lambda e: e.tensor_sub(lsm[:, 4:5], lsm[:, 3:4], lsm[:, 2:3]), r=[d_l], w=[d_l])
                    k.op("dve", lambda e: e.tensor_scalar_add(lsm[:, 5:6], lsm[:, 4:5], -0.2), r=[d_l], w=[d_l])
                    k.op("dve", lambda e: e.tensor_scalar_mul(gnb[:], gnb[:], 0.8), r=[d_l], w=[d_l])
                    neglam = lsm[:, 5:6]
                    kb_items = []
                    for _ in range(1):
                        kt_ = k.sb("kbT", [128, 2, TA], BF16)
                        kd_ = Dep()
                        k.op("pool", lambda e, kt_=kt_: e.memset(kt_[64:128, 0, :], 0.0), w=[kd_])
                        k.op("pool", lambda e, kt_=kt_: e.memset(kt_[0:64, 1, :], 0.0), w=[kd_])
                        kb_items.append((kt_, kd_))
                    kbr = Ring(kb_items)
                    qbr = Ring([(k.sb("qbT", [128, TO], BF16), Dep()) for _ in range(1)])
                    vb_items = []
                    for _ in range(1):
                        vt = k.sb("vbe", [128, NTA, 129], BF16)
                        vd = Dep()
                        k.op("dve", lambda e, vt=vt: e.tensor_copy(vt[:, :, 128], validb[:, :]), r=[d_c], w=[vd])
                        vb_items.append((vt, vd))
                    vbr = Ring(vb_items)
                    PTr = Ring([(k.sb("PT", [128, 256], BF16), Dep()) for _ in range(4)])
                    o1r = Ring([(k.sb("o1", [128, 128], F32), Dep()) for _ in range(2)])
                    o2r = Ring([(k.sb("o2", [128, 128], F32), Dep()) for _ in range(2)])
                    obr = Ring([(k.sb("ob", [128, 128], BF16), Dep()) for _ in range(4)])
                    s8r = Ring([(k.sb("s8", [128, 8], F32), Dep()) for _ in range(2)])
                    Osr = Ring([(k.sb("Os", [128, 2, 129], F32), Dep()) for _ in range(2)])
                    oblk = Ring([(k.sb("oblk", [128, HB], BF16), Dep()) for _ in range(2)])

                    def mlstm_gen():
                        st_m = {'hb': None, 'fin': None}
                        for c in range(NCH):
                            own = c >= CH0
                            tq = (c - CH0) * 64
                            v_t, v_d = vch.next()
                            k.dma("sp", v_t[:, :, 0:256], s_va[c * 64:(c + 1) * 64, :].rearrange("s (h d) -> s h d", h=4),
                                  r=[d_scr["va"]], w=[v_d])
                            if own:
                                so_t, so_d = sor.next()
                                k.dma("sp", so_t[:], s_oa[tq:tq + 64, :], r=[d_scr["oa"]], w=[so_d])
                                Ns_t, Ns_d = Nsr.next()
                            for h in range(4):
                                pt, pd = pb.next()

                                def f(pe, pt=pt, h=h, c=c):
                                    for j in range(2):
                                        ins = pe.transpose(pt[0:64, j * 128:(j + 1) * 128], kT[:, h * 2 + j, c * 64:(c + 1) * 64], identb[:])
                                    return ins
                                k.op("pe", f, r=[d_kT, d_c], w=[pd])
                                kw_t, kw_d = kwr.next()
                                k.op("act", lambda e, kw_t=kw_t, pt=pt, h=h, c=c: e.activation(
                                    kw_t[:], pt[0:64, 0:256], AF.Identity, scale=TT[:, 1, c, h:h + 1]), r=[pd, d_TT], w=[kw_d])
                                yield
                                bU, bUd = m_U.next()

                                def f(pe, bU=bU, kw_t=kw_t, v_t=v_t, h=h):
                                    for j in range(2):
                                        pe.matmul(bU[:, j * 256:(j + 1) * 256], kw_t[:, j * 128:(j + 1) * 128], v_t[:, h, 0:256],
                                                  start=True, stop=True)
                                    for j in range(2):
                                        ins = pe.matmul(bSN[:, 400 + j:401 + j], kw_t[:, j * 128:(j + 1) * 128], v_t[:, h, 256:257],
                                                        start=True, stop=True)
                                    return ins
                                k.op("pe", f, r=[kw_d, v_d], w=[bUd, bSNd])
                                if not own:
                                    yield
                                if own:
                                    def f(pe, h=h, c=c, tq=tq):
                                        for j in range(2):
                                            ins = pe.matmul(bSN[0:64, 320:384], kT[:, h * 2 + j, c * 64:(c + 1) * 64],
                                                            qT[:, h * 2 + j, tq:tq + 64], start=(j == 0), stop=(j == 1))
                                        return ins
                                    k.op("pe", f, r=[d_kT, d_qT], w=[bSNd])
                                    W_t, W_d = Wtr.next()
                                    k.op("dve", lambda e, W_t=W_t, h=h, c=c: e.scalar_tensor_tensor(
                                        W_t[:], bSN[0:64, 320:384], TT[:, 0, c, h:h + 1], mask64[:], ALU.mult, ALU.mult),
                                        r=[bSNd, d_TT, d_c], w=[W_d])
                                    yield

                                    def f(pe, W_t=W_t, v_t=v_t, h=h, tq=tq):
                                        for j in range(2):
                                            pe.matmul(bNN[0:64, 0:257], qT[:, h * 2 + j, tq:tq + 64], Cbf[h][:, j, :],
                                                      start=(j == 0), stop=False)
                                        return pe.matmul(bNN[0:64, 0:257], W_t[:], v_t[:, h, :], start=False, stop=True)
                                    k.op("pe", f, r=[d_qT, d_Cbf[h], W_d, v_d], w=[bNNd])
                                    k.op("act", lambda e, Ns_t=Ns_t, h=h: e.copy(Ns_t[:, h, :], bNN[0:64, 0:257]),
                                         r=[bNNd], w=[Ns_d])
                                    yield
                                dsc = decbc[:, h * NCH + c:h * NCH + c + 1]
                                k.op("dve", lambda e, h=h, bU=bU, dsc=dsc: e.scalar_tensor_tensor(
                                    Cst[h][:, :, 0:256], Cst[h][:, :, 0:256], dsc,
                                    bU[:, 0:512].rearrange("p (j n) -> p j n", j=2), ALU.mult, ALU.add),
                                    r=[bUd, d_TT], w=[d_C[h]])
                                k.op("dve", lambda e, h=h, dsc=dsc: e.scalar_tensor_tensor(
                                    Cst[h][:, :, 256:257], Cst[h][:, :, 256:257], dsc,
                                    bSN[:, 400:402].rearrange("p (j n) -> p j n", j=2), ALU.mult, ALU.add),
                                    r=[bSNd, d_TT], w=[d_C[h]])
                                if own or c == CH0 - 1:
                                    k.op("act", lambda e, h=h: e.copy(Cbf[h][:], Cst[h][:]), r=[d_C[h]], w=[d_Cbf[h]])
                                if h == 1 and st_m['fin'] is not None:
                                    st_m['fin']()
                                    st_m['fin'] = None
                                yield
                            if own:
                                sm_t, sm_d = smr.next()
                                k.op("act", lambda e, sm_t=sm_t, Ns_t=Ns_t: e.activation(
                                    sm_t[:, 0:4], Ns_t[:, :, 256], AF.Abs), r=[Ns_d], w=[sm_d])
                                k.op("dve", lambda e, sm_t=sm_t, c=c: e.tensor_tensor(
                                    sm_t[:, 0:4], sm_t[:, 0:4], TT[:, 2, c, :], ALU.max), r=[sm_d, d_TT], w=[sm_d])
                                k.op("dve", lambda e, sm_t=sm_t: e.reciprocal(sm_t[:, 0:4], sm_t[:, 0:4]), r=[sm_d], w=[sm_d])
                                hg_t, hg_d = hgr.next()
                                k.op("dve", lambda e, hg_t=hg_t, Ns_t=Ns_t, sm_t=sm_t: e.tensor_tensor(
                                    hg_t[:], Ns_t[:, :, 0:256], sm_t[:, 0:4].unsqueeze(2).to_broadcast([64, 4, 256]), ALU.mult),
                                    r=[Ns_d, sm_d], w=[hg_d])
                                k.op("dve", lambda e, hg_t=hg_t, so_t=so_t: e.tensor_tensor(
                                    hg_t[:], hg_t[:], so_t[:].rearrange("s (h d) -> s h d", h=4), ALU.mult),
                                    r=[so_d, hg_d], w=[hg_d])

                                def f(e, hg_t=hg_t, sm_t=sm_t):
                                    for hh in range(4):
                                        ins = e.bn_stats(sm_t[:, 4 + 6 * hh:10 + 6 * hh], hg_t[:, hh, :])
                                    return ins
                                k.op("dve", f, r=[hg_d], w=[sm_d])

                                def f(e, sm_t=sm_t):
                                    for hh in range(4):
                                        ins = e.bn_aggr(sm_t[:, 28 + 2 * hh:30 + 2 * hh], sm_t[:, 4 + 6 * hh:10 + 6 * hh])
                                    return ins
                                k.op("dve", f, r=[sm_d], w=[sm_d])
                                mv = sm_t[:, 28:36].rearrange("s (h k) -> s h k", h=4)
                                k.op("act", lambda e, sm_t=sm_t, mv=mv: e.activation(
                                    sm_t[:, 36:40], mv[:, :, 1], AF.Ln, bias=epsc[0:64, 0:1], scale=1.0), r=[sm_d, d_c], w=[sm_d])
                                k.op("act", lambda e, sm_t=sm_t: e.activation(
                                    sm_t[:, 36:40], sm_t[:, 36:40], AF.Exp, scale=-0.5), r=[sm_d], w=[sm_d])
                                k.op("dve", lambda e, hg_t=hg_t, mv=mv: e.tensor_tensor(
                                    hg_t[:], hg_t[:], mv[:, :, 0:1].to_broadcast([64, 4, 256]), ALU.subtract),
                                    r=[sm_d, hg_d], w=[hg_d])
                                k.op("dve", lambda e, hg_t=hg_t, sm_t=sm_t: e.tensor_tensor(
                                    hg_t[:], hg_t[:], sm_t[:, 36:40].unsqueeze(2).to_broadcast([64, 4, 256]), ALU.mult),
                                    r=[sm_d, hg_d], w=[hg_d])
                                hb_t, hb_d = hbr.next()
                                k.op("dve", lambda e, hg_t=hg_t, hb_t=hb_t: e.tensor_tensor(
                                    hb_t[:], hg_t[:].rearrange("s h d -> s (h d)"), ngb[:], ALU.mult),
                                    r=[hg_d, d_cw], w=[hb_d])
                                def mfin(hb_t=hb_t, hb_d=hb_d, tq=tq):
                                    pt, pd = pb.next()

                                    def f(pe):
                                        for fc in range(8):
                                            ins = pe.transpose(pt[:, fc * 64:(fc + 1) * 64], hb_t[:, fc * 128:(fc + 1) * 128], identb[0:64, 0:64])
                                        return ins
                                    k.op("pe", f, r=[hb_d, d_c], w=[pd])
                                    if tq % HB == 0:
                                        st_m['hb'] = hblk.next()
                                    hk_t, hk_d = st_m['hb']
                                    k.op("act", lambda e: e.copy(
                                        hk_t[:, :, tq % HB:tq % HB + 64], pt[:, 0:512].rearrange("p (f s) -> p f s", f=8)), r=[pd], w=[hk_d])
                                    if (tq + 64) % HB == 0:
                                        tb0 = tq + 64 - HB
                                        k.dma("sp", s_haT[:, tb0:tb0 + HB].rearrange("(f p) t -> p f t", p=128), hk_t[:], r=[hk_d], w=[d_haT])
                                st_m['fin'] = mfin
                            yield
                        if st_m['fin'] is not None:
                            st_m['fin']()
                            st_m['fin'] = None
                        yield

                    def attn_gen():
                        st_a = {'ob': None}
                        deferred = []
                        for h in range(8):
                            kb_t, kb_d = kbr.next(); qb_t, qb_d = qbr.next(); vb_t, vb_d = vbr.next()
                            k.dma("sp", kb_t[0:64, 0, :], s_qkbT[1024 + h * 128:1024 + h * 128 + 64, :], r=[d_scr["qkb"]], w=[kb_d])
                            k.dma("sp", kb_t[64:128, 1, :], s_qkbT[1024 + h * 128 + 64:1024 + (h + 1) * 128, :], r=[d_scr["qkb"]], w=[kb_d])
                            k.dma("sp", qb_t[:], s_qkbT[h * 128:(h + 1) * 128, TP:TA], r=[d_scr["qkb"]], w=[qb_d])
                            for t8 in range(0, NTA, 8):
                                t9 = min(NTA, t8 + 8)
                                k.dma("sp", vb_t[:, t8:t9, 0:128],
                                      s_vb[t8 * 128:t9 * 128, h * 128:(h + 1) * 128].rearrange("(t p) d -> p t d", p=128),
                                      r=[d_scr["vb"]], w=[vb_d])
                            items = []
                            for qi in range(NTO):
                                qt = NTP + qi
                                kt_lo = 0
                                while kt_lo < qt and SLOPES[h] * (127 - 128 * (qt - kt_lo)) < -ACLAMP:
                                    kt_lo += 1
                                for kt in range(kt_lo, qt + 1):
                                    items.append((qi, qt, kt, kt_lo))

                            def emit_qk(it, h=h, kb_t=kb_t, qb_t=qb_t, kb_d=kb_d, qb_d=qb_d):
                                qi, qt, kt, kt_lo = it
                                bSt, sd = a_S.next()
                                off = 0
                                diag = (kt == qt)

                                def f(pe):
                                    for m in range(2):
                                        ins = pe.matmul(bSt[:, off + m * 128:off + (m + 1) * 128], kb_t[:, m, kt * 128:(kt + 1) * 128],
                                                        qb_t[:, qi * 128:(qi + 1) * 128], start=True, stop=(not diag))
                                        if diag:
                                            ins = pe.matmul(bSt[:, off + m * 128:off + (m + 1) * 128], identb[:], dgb[:, h, :], start=False, stop=True)
                                    return ins
                                k.op("pe", f, r=[kb_d, qb_d, d_c], w=[sd])
                                return bSt, sd
                            def do_pv(it, P_t, P_d, h=h, vb_t=vb_t, vb_d=vb_d):
                                qi, qt, kt, kt_lo = it

                                def f(pe, P_t=P_t, vb_t=vb_t, kt=kt, qt=qt, kt_lo=kt_lo):
                                    pe.matmul(bO[:, 0:129], P_t[:, 0:128], vb_t[:, kt, :], start=(kt == kt_lo), stop=(kt == qt))
                                    return pe.matmul(bO1[:, 0:129], P_t[:, 128:256], vb_t[:, kt, :], start=(kt == kt_lo), stop=(kt == qt))
                                k.op("pe", f, r=[P_d, vb_d], w=[bOd, bO1d])
                                if kt != qt:
                                    return
                                s8, s8d = s8r.next()
                                Os, Osd = Osr.next()
                                k.op("act", lambda e, Os=Os: e.copy(Os[:, 0, :], bO[:, 0:129]), r=[bOd], w=[Osd])
                                k.op("dve", lambda e, Os=Os: e.tensor_copy(Os[:, 1, :], bO1[:, 0:129]), r=[bO1d], w=[Osd])
                                k.op("dve", lambda e, s8=s8, Os=Os: e.reciprocal(s8[:, 0:2], Os[:, :, 128]), r=[Osd], w=[s8d])
                                k.op("dve", lambda e, s8=s8: e.tensor_tensor(s8[:, 2:3], s8[:, 1:2], neglam, ALU.mult),
                                     r=[s8d, d_l], w=[s8d])
                                o1, o1d = o1r.next(); o2, o2d = o2r.next()
                                k.op("dve", lambda e, o1=o1, s8=s8, Os=Os: e.tensor_scalar_mul(o1[:], Os[:, 0, 0:128], s8[:, 0:1]),
                                     r=[Osd, s8d], w=[o1d])
                                k.op("dve", lambda e, o1=o1, s8=s8, Os=Os: e.scalar_tensor_tensor(
                                    o1[:], Os[:, 1, 0:128], s8[:, 2:3], o1[:], ALU.mult, ALU.add), r=[Osd, s8d], w=[o1d])
                                k.op("pool", lambda e, o1=o1, o2=o2: e.tensor_tensor(o2[:], o1[:], o1[:], ALU.mult), r=[o1d], w=[o2d])
                                k.op("dve", lambda e, o2=o2, s8=s8: e.reduce_sum(s8[:, 3:4], o2[:], AX.X), r=[o2d], w=[s8d])
                                k.op("dve", lambda e, s8=s8: e.tensor_scalar(s8[:, 4:5], s8[:, 3:4], 1.0 / 128.0, EPS, ALU.mult, ALU.add),
                                     r=[s8d], w=[s8d])
                                k.op("act", lambda e, s8=s8: e.activation(s8[:, 5:6], s8[:, 4:5], AF.Ln), r=[s8d], w=[s8d])
                                k.op("act", lambda e, s8=s8: e.activation(s8[:, 5:6], s8[:, 5:6], AF.Exp, scale=-0.5),
                                     r=[s8d], w=[s8d])
                                ob, obd = obr.next()
                                k.op("dve", lambda e, ob=ob, o1=o1, s8=s8: e.scalar_tensor_tensor(
                                    ob[:], o1[:], s8[:, 5:6], gnb[:], ALU.mult, ALU.mult), r=[o1d, s8d, d_l], w=[obd])
                                def fin(ob=ob, obd=obd, qi=qi, h=h):
                                    pt, pd = pb.next()
                                    k.op("pe", lambda pe: pe.transpose(pt[:, 0:128], ob[:], identb[:]), r=[obd, d_c], w=[pd])
                                    tq = qi * 128
                                    if tq % HB == 0:
                                        st_a['ob'] = oblk.next()
                                    ok_t, ok_d = st_a['ob']
                                    k.op("act", lambda e: e.copy(ok_t[:, tq % HB:tq % HB + 128], pt[:, 0:128]),
                                         r=[pd], w=[ok_d])
                                    if (tq + 128) % HB == 0:
                                        tb0 = tq + 128 - HB
                                        k.dma("sp", s_obT[h * 128:(h + 1) * 128, tb0:tb0 + HB], ok_t[:], r=[ok_d], w=[d_obT])
                                deferred.append([3, fin])
                            nxt = emit_qk(items[0])
                            pend = None
                            for j, it in enumerate(items):
                                qi, qt, kt, kt_lo = it
                                bSt, sd = nxt
                                off = 0
                                if j + 1 < len(items):
                                    nxt = emit_qk(items[j + 1])
                                diag = (kt == qt)
                                P_t, P_d = PTr.next()
                                bias_ap = zero1[:, 0:1] if diag else abt[:, h, qt - kt:qt - kt + 1]
                                k.op("act", lambda e, P_t=P_t, off=off, bias_ap=bias_ap, bSt=bSt: e.activation(
                                    P_t[:], bSt[:, off:off + 256], AF.Exp, bias=bias_ap, scale=0.125), r=[sd, d_c], w=[P_d])

                                if pend is not None:
                                    do_pv(*pend)
                                pend = (it, P_t, P_d)
                                for dfr in list(deferred):
                                    dfr[0] -= 1
                                    if dfr[0] <= 0:
                                        deferred.remove(dfr)
                                        dfr[1]()
                                yield
                            if pend is not None:
                                do_pv(*pend)
                            yield
                        for dfr in list(deferred):
                            dfr[1]()
                        deferred.clear()
                        yield

                    gm = mlstm_gen()
                    ga_ = attn_gen()
                    if stop <= 3:
                        for _ in gm:
                            pass
                        raise _Stop()
                    if stop <= 3.5:
                        for _ in ga_:
                            pass
                        raise _Stop()
                    n_m = (CH0 * (4 * 3 + 1)) + ((NCH - CH0) * (4 * 4 + 1))
                    n_a = 0
                    for h_ in range(8):
                        for qi_ in range(NTO):
                            qt_ = NTP + qi_
                            lo_ = 0
                            while lo_ < qt_ and SLOPES[h_] * (127 - 128 * (qt_ - lo_)) < -ACLAMP:
                                lo_ += 1
                            n_a += qt_ + 1 - lo_
                    done_m = done_a = 0
                    m_alive = a_alive = True
                    while m_alive or a_alive:
                        if m_alive:
                            try:
                                next(gm); done_m += 1
                            except StopIteration:
                                m_alive = False
                        if ILV == 0:
                            tgt = n_a + 10 if not m_alive else 0
                        else:
                            tgt = n_a + 10 if not m_alive else (done_m * n_a) // n_m
                        while a_alive and done_a < tgt:
                            try:
                                next(ga_); done_a += 1
                            except StopIteration:
                                a_alive = False
                    for g_ in (gm, ga_):
                        for _ in g_:
                            pass
                    pb = pb_all
                if stop <= 4:
                    raise _Stop()
                with k.scope():
                    war = Ring([(k.sb("wa", [128, 8, 512], BF16), Dep()) for _ in range(2)])
                    wbr = Ring([(k.sb("wb", [128, 8, 512], BF16), Dep()) for _ in range(2)])
                    gar = Ring([(k.sb("ga", [128, 512], BF16), Dep()) for _ in range(3)])
                    gbr = Ring([(k.sb("gb", [128, 512], BF16), Dep()) for _ in range(3)])
                    m1r = Ring([(k.sb("m1", [128, 512], F32), Dep()) for _ in range(3)])
                    m2r = Ring([(k.sb("m2", [128, 512], F32), Dep()) for _ in range(3)])
                    hkr = Ring([(k.sb("hk", [128, 8, HB], BF16), Dep()) for _ in range(2)])
                    okr = Ring([(k.sb("ok", [128, 8, HB], BF16), Dep()) for _ in range(2)])
                    mgr = Ring([(k.sb("mgo", [128, 512], BF16), Dep()) for _ in range(3)])
                    st_e1 = {"db": None, "tb": None}

                    def e1_unit(db, tb, n, g):
                        with k.atomic():
                            if st_e1["db"] != db:
                                wa_t, wa_d = war.next(); wb_t, wb_d = wbr.next()
                                k.dma("pool", wa_t[:], w_a.rearrange("(c p) n -> p c n", p=128)[:, :, db * 512:(db + 1) * 512], w=[wa_d])
                                k.dma("pool", wb_t[:], w_b.rearrange("(c p) n -> p c n", p=128)[:, :, db * 512:(db + 1) * 512], w=[wb_d])
                                st_e1["db"] = db
                                st_e1["w"] = (wa_t, wa_d, wb_t, wb_d)
                            if st_e1["tb"] != (db, tb):
                                hk, hkd = hkr.next(); ok, okd = okr.next()
                                k.dma("sp", hk[:, :, 0:n], s_haT[:, tb:tb + n].rearrange("(f p) t -> p f t", p=128), r=[d_haT], w=[hkd])
                                k.dma("sp", ok[:, :, 0:n], s_obT[:, tb:tb + n].rearrange("(f p) t -> p f t", p=128), r=[d_obT], w=[okd])
                                st_e1["tb"] = (db, tb)
                                st_e1["h"] = (hk, hkd, ok, okd)
                        wa_t, wa_d, wb_t, wb_d = st_e1["w"]
                        hk, hkd, ok, okd = st_e1["h"]
                        dc = db * 4 + g
                        ga_t, ga_d = gar.next(); gb_t, gb_d = gbr.next()
                        k.dma("sp", ga_t[:, 0:n], s_gT[dc * 128:(dc + 1) * 128, tb:tb + n], r=[d_scr["g"]], w=[ga_d])
                        k.dma("sp", gb_t[:, 0:n], s_gT[2048 + dc * 128:2048 + (dc + 1) * 128, tb:tb + n], r=[d_scr["g"]], w=[gb_d])
                        bA, bAd = pf.next(); bB, bBd = pf.next()

                        def f(pe):
                            for fc in range(8):
                                ins = pe.matmul(bA[:, 0:n], wa_t[:, fc, g * 128:(g + 1) * 128], hk[:, fc, 0:n],
                                                start=(fc == 0), stop=(fc == 7))
                            return ins
                        k.op("pe", f, r=[wa_d, hkd], w=[bAd])

                        def f(pe):
                            for fc in range(8):
                                ins = pe.matmul(bB[:, 0:n], wb_t[:, fc, g * 128:(g + 1) * 128], ok[:, fc, 0:n],
                                                start=(fc == 0), stop=(fc == 7))
                            return ins
                        k.op("pe", f, r=[wb_d, okd], w=[bBd])
                        m1, m1d = m1r.next(); m2, m2d = m2r.next()
                        k.op("dve", lambda e: e.tensor_tensor(m1[:, 0:n], bA[:, 0:n], ga_t[:, 0:n], ALU.mult),
                             r=[bAd, ga_d], w=[m1d])
                        k.op("dve", lambda e: e.tensor_tensor(m2[:, 0:n], bB[:, 0:n], gb_t[:, 0:n], ALU.mult),
                             r=[bBd, gb_d], w=[m2d])
                        mg_t, mg_dd = mgr.next()
                        k.op("pool", lambda e: e.tensor_tensor(mg_t[:, 0:n], m1[:, 0:n], m2[:, 0:n], ALU.add), r=[m1d, m2d], w=[mg_dd])
                        k.dma("sp", s_mgT[dc * 128:(dc + 1) * 128, tb:tb + n], mg_t[:, 0:n], r=[mg_dd], w=[d_mg])
                    units = []
                    for db in range(4):
                        tb = 0
                        while tb < TO:
                            n = min(512, TO - tb)
                            for g in range(4):
                                units.append(lambda db=db, tb=tb, n=n, g=g: e1_unit(db, tb, n, g))
                            tb += n
                    k.interleave(units, width=3)
            if stop <= 5:
                raise _Stop()
            with k.scope():
                wo = k.sb("wo", [128, DC, D], BF16)
                wr = k.sb("wr", [128, DC, 36], F32)
                br = k.sb("br", [1, 36], F32)
                l1g = k.sb("l1g", [128, D], F32); l1b = k.sb("l1b", [128, D], F32)
                cnt = k.sb("cnt", [128, 32], F32)
                d_wo, d_cnt = Dep(), Dep()
                for q4 in range(4):
                    k.dma("pool", wo[:, :, q4 * 512:(q4 + 1) * 512],
                          w_out.rearrange("(c p) n -> p c n", p=128)[:, :, q4 * 512:(q4 + 1) * 512], w=[d_wo])
                k.dma("sp", wr[:], wr_d.rearrange("(c p) n -> p c n", p=128), w=[d_wo])
                k.dma("sp", br[:], br_d, w=[d_wo])
                k.dma("sp", l1g[:], ln1g_d.partition_broadcast(128), w=[d_wo])
                k.dma("sp", l1b[:], ln1b_d.partition_broadcast(128), w=[d_wo])
                k.op("dve", lambda e: e.memset(cnt[:], 0.0), w=[d_cnt])
                xtr = Ring([(k.sb("xt", [128, D], F32), Dep()) for _ in range(2)])
                x1r = Ring([(k.sb("x1", [128, D], F32), Dep()) for _ in range(2)])
                hbr2 = Ring([(k.sb("hb2", [128, D], BF16), Dep()) for _ in range(2)])
                hTr = Ring([(k.sb("hT", [128, DC, 128], F32), Dep()) for _ in range(2)])
                rsr = Ring([(k.sb("rs", [128, 256], F32), Dep()) for _ in range(2)])
                mkr = Ring([(k.sb("mk", [128, 32], BF16), Dep()) for _ in range(2)])
                mbr = Ring([(k.sb("mgb", [128, DC, HB], BF16), Dep()) for _ in range(2)])
                st_e2 = {"mb": None}

                def e2_tile(ti):
                    if (ti * 128) % HB == 0:
                        with k.atomic():
                            st_e2["mb"] = mbr.next()
                            k.dma("sp", st_e2["mb"][0][:], s_mgT[:, ti * 128:ti * 128 + HB].rearrange("(c p) t -> p c t", p=128),
                                  r=[d_mg], w=[st_e2["mb"][1]])
                    mgb, mgbd = st_e2["mb"]
                    tloc = (ti * 128) % HB
                    xt, xtd = xtr.next(); x1, x1d = x1r.next()
                    k.dma("sp", xt[:], x[TP + ti * 128:TP + (ti + 1) * 128, :], w=[xtd])
                    for db in range(4):
                        bk, bd = pf.next()

                        def f(pe, bk=bk, mgb=mgb, tloc=tloc, db=db):
                            for c in range(DC):
                                ins = pe.matmul(bk[:, :], mgb[:, c, tloc:tloc + 128], wo[:, c, db * 512:(db + 1) * 512],
                                                start=(c == 0), stop=(c == DC - 1))
                            return ins
                        k.op("pe", f, r=[mgbd, d_wo], w=[bd])
                        k.op("dve", lambda e, x1=x1, xt=xt, bk=bk, db=db: e.scalar_tensor_tensor(
                            x1[:, db * 512:(db + 1) * 512], xt[:, db * 512:(db + 1) * 512], ALPHA, bk[:, :], ALU.mult, ALU.add),
                            r=[xtd, bd], w=[x1d])
                    rs, rsd = rsr.next()

                    def layer_norm(xx, xd, rs, rsd, g_t, b_t, gdep):
                        st = rs[:, 0:24].rearrange("p (g k) -> p g k", g=4)
                        def fbn(e):
                            for gg in range(4):
                                ins = e.bn_stats(rs[:, 6 * gg:6 * gg + 6], xx[:, gg * 512:(gg + 1) * 512])
                            return ins
                        k.op("dve", fbn, r=[xd], w=[rsd])
                        k.op("dve", lambda e: e.bn_aggr(rs[:, 24:26], rs[:, 0:24]), r=[rsd], w=[rsd])
                        k.op("act", lambda e: e.activation(rs[:, 26:27], rs[:, 25:26], AF.Ln, bias=epsc[:, 0:1], scale=1.0), r=[rsd, d_c], w=[rsd])
                        k.op("act", lambda e: e.activation(rs[:, 26:27], rs[:, 26:27], AF.Exp, scale=-0.5), r=[rsd], w=[rsd])
                        k.op("dve", lambda e: e.scalar_tensor_tensor(xx[:], xx[:], rs[:, 24:25], g_t[:], ALU.subtract, ALU.mult),
                             r=[rsd, xd, gdep], w=[xd])
                        k.op("dve", lambda e: e.scalar_tensor_tensor(xx[:], xx[:], rs[:, 26:27], b_t[:], ALU.mult, ALU.add),
                             r=[rsd, xd, gdep], w=[xd])
                    layer_norm(x1, x1d, rs, rsd, l1g, l1b, d_wo)
                    k.dma("sp", s_h1[ti * 128:(ti + 1) * 128, :], x1[:], r=[x1d], w=[d_h1])
                    hb2, hb2d = hbr2.next()
                    k.op("act", lambda e, hb2=hb2, x1=x1: e.copy(hb2[:], x1[:]), r=[x1d], w=[hb2d])
                    hT, hTd = hTr.next()
                    for g in range(4):
                        bk, bd = pf.next()

                        def f(pe, bk=bk, x1=x1, g=g):
                            for j in range(4):
                                c = g * 4 + j
                                ins = pe.transpose(bk[:, j * 128:(j + 1) * 128], x1[:, c * 128:(c + 1) * 128], identf[:])
                            return ins
                        k.op("pe", f, r=[x1d, d_c], w=[bd])
                        k.op("act", lambda e, hT=hT, bk=bk, g=g: e.copy(
                            hT[:, g * 4:(g + 1) * 4, :], bk[:, :].rearrange("p (j n) -> p j n", j=4)), r=[bd], w=[hTd])
                    bk, bd = pf.next()

                    def f(pe, bk=bk, hT=hT):
                        for c in range(DC):
                            pe.matmul(bk[:, 0:36], hT[:, c, :], wr[:, c, :], start=(c == 0), stop=False)
                        return pe.matmul(bk[:, 0:36], onesf[0:1, :], br[0:1, :], start=False, stop=True)
                    k.op("pe", f, r=[hTd, d_wo, d_c], w=[bd])
                    V = "dve"
                    lg = rs[:, 32:68]
                    k.op(V, lambda e, bk=bk, lg=lg: e.tensor_copy(lg, bk[:, 0:36]), r=[bd], w=[rsd])
                    g4 = rs[:, 32:36]; e32 = rs[:, 36:68]
                    gmx = rs[:, 68:69]; ngm = rs[:, 69:70]; ohg = rs[:, 70:74]; eg = rs[:, 74:78]; sg = rs[:, 78:79]; gp = rs[:, 79:80]
                    pen = rs[:, 80:84]; msk = rs[:, 84:116]; top8 = rs[:, 116:124]; oh1 = rs[:, 124:156]; oh2 = rs[:, 156:188]
                    dd = rs[:, 188:189]; p1 = rs[:, 189:190]; p2 = rs[:, 190:191]; tmp = rs[:, 192:224]
                    pk = rs[:, 224:225]; ek = rs[:, 225:226]; okk = rs[:, 226:227]; dsf = rs[:, 227:228]; posg = rs[:, 228:260 - 4]
                    k.op(V, lambda e: e.reduce_max(gmx, g4, AX.X), r=[rsd], w=[rsd])
                    k.op(V, lambda e: e.tensor_scalar_mul(ngm, gmx, -1.0), r=[rsd], w=[rsd])
                    k.op(V, lambda e: e.tensor_scalar(ohg, g4, gmx, None, ALU.is_equal), r=[rsd], w=[rsd])
                    k.op("act", lambda e: e.activation(eg, g4, AF.Exp, bias=ngm, scale=1.0), r=[rsd], w=[rsd])
                    k.op(V, lambda e: e.reduce_sum(sg, eg, AX.X), r=[rsd], w=[rsd])
                    k.op(V, lambda e: e.reciprocal(gp, sg), r=[rsd], w=[rsd])
                    k.op(V, lambda e: e.tensor_scalar(pen, ohg, BIG, -BIG, ALU.mult, ALU.add), r=[rsd], w=[rsd])
                    k.op(V, lambda e: e.tensor_tensor(msk.rearrange("p (g j) -> p g j", g=4), e32.rearrange("p (g j) -> p g j", g=4),
                                                      pen.unsqueeze(2).to_broadcast([128, 4, 8]), ALU.add), r=[rsd], w=[rsd])
                    k.op(V, lambda e: e.max(top8, msk), r=[rsd], w=[rsd])
                    k.op(V, lambda e: e.tensor_scalar(oh1, msk, top8[:, 0:1], None, ALU.is_equal), r=[rsd], w=[rsd])
                    k.op(V, lambda e: e.tensor_scalar(oh2, msk, top8[:, 1:2], None, ALU.is_equal), r=[rsd], w=[rsd])
                    k.op(V, lambda e: e.tensor_sub(dd, top8[:, 0:1], top8[:, 1:2]), r=[rsd], w=[rsd])
                    k.op("act", lambda e: e.activation(p1, dd, AF.Sigmoid), r=[rsd], w=[rsd])
                    k.op("act", lambda e: e.activation(p2, dd, AF.Sigmoid, scale=-1.0), r=[rsd], w=[rsd])
                    k.op(V, lambda e, ti=ti: e.tensor_tensor(wtab[:, ti, 0:1], p1, gp, ALU.mult), r=[rsd], w=[d_tab])
                    k.op(V, lambda e, ti=ti: e.tensor_tensor(wtab[:, ti, 1:2], p2, gp, ALU.mult), r=[rsd], w=[d_tab])
                    mk, mkd = mkr.next()
                    k.op(V, lambda e, mk=mk: e.tensor_tensor(mk[:], oh1, oh2, ALU.add), r=[rsd], w=[mkd])
                    bk2, bd2 = pf.next()

                    def f(pe, bk2=bk2, mk=mk):
                        pe.matmul(bk2[:, 0:32], ustr[:], mk[:], start=True, stop=True)
                        return pe.matmul(bk2[:, 32:64], onesb[:], mk[:], start=True, stop=True)
                    k.op("pe", f, r=[mkd, d_c], w=[bd2])
                    posg = rs[:, 224:256]
                    pk = rs[:, 28:29]; ek = rs[:, 29:30]; okk = rs[:, 30:31]; dsf = rs[:, 31:32]
                    with k.atomic():
                        k.op(V, lambda e, bk2=bk2: e.tensor_tensor(posg, bk2[:, 0:32], cnt[:], ALU.add), r=[bd2, d_cnt, rsd], w=[rsd])
                        k.op(V, lambda e, bk2=bk2: e.tensor_tensor(cnt[:], cnt[:], bk2[:, 32:64], ALU.add), r=[bd2, rsd], w=[d_cnt])
                    for kk, oh in ((0, oh1), (1, oh2)):
                        k.op(V, lambda e, oh=oh: e.tensor_tensor(tmp, oh, posg, ALU.mult), r=[rsd], w=[rsd])
                        k.op(V, lambda e: e.reduce_sum(pk, tmp, AX.X), r=[rsd], w=[rsd])
                        k.op(V, lambda e, oh=oh: e.tensor_tensor(tmp, oh, ecap[:], ALU.mult), r=[rsd, d_c], w=[rsd])
                        k.op(V, lambda e: e.reduce_sum(ek, tmp, AX.X), r=[rsd], w=[rsd])
                        k.op(V, lambda e: e.tensor_scalar(okk, pk, float(CAP), None, ALU.is_lt), r=[rsd], w=[rsd])
                        k.op(V, lambda e: e.tensor_tensor(dsf, ek, pk, ALU.add), r=[rsd], w=[rsd])
                        k.op(V, lambda e: e.tensor_scalar_add(dsf, dsf, -float(TRASH)), r=[rsd], w=[rsd])
                        k.op(V, lambda e: e.tensor_tensor(dsf, dsf, okk, ALU.mult), r=[rsd], w=[rsd])
                        k.op(V, lambda e: e.tensor_scalar_add(dsf, dsf, float(TRASH)), r=[rsd], w=[rsd])
                        k.op(V, lambda e, ti=ti, kk=kk: e.tensor_copy(dtab[:, ti, kk:kk + 1], dsf), r=[rsd], w=[d_tab])
                        k.dma("pool", s_Xg, hb2[:], r=[hb2d, d_tab, d_Xg], w=[d_Xg],
                              indirect=dict(out_offset=bass.IndirectOffsetOnAxis(ap=dtab[:, ti, kk:kk + 1], axis=0), in_offset=None))
                k.interleave([(lambda ti=ti: e2_tile(ti)) for ti in range(NTO)], width=NIL)

            if dbg:
                k.dma("sp", s_dtab, dtab[:].rearrange("p a b -> p (a b)"), r=[d_tab], w=[Dep()])
                k.dma("sp", s_wtab, wtab[:].rearrange("p a b -> p (a b)"), r=[d_tab], w=[Dep()])
            if stop <= 6:
                raise _Stop()
            NCT = CAP // 128
            with k.scope():
                wgr = Ring([(k.sb("wg", [128, DC, 512], BF16), Dep()) for _ in range(3)])
                wur = Ring([(k.sb("wu", [128, DC, 512], BF16), Dep()) for _ in range(3)])
                wdr = Ring([(k.sb("wd", [128, 4, D], BF16), Dep()) for _ in range(3)])
                Xer = Ring([(k.sb("Xe", [128, NCT, D], BF16), Dep()) for _ in range(2)])
                XTr = Ring([(k.sb("XT", [128, DC, CAP], BF16), Dep()) for _ in range(2)])
                ATr = Ring([(k.sb("AT", [128, 4, CAP], BF16), Dep()) for _ in range(2)])
                sgr = Ring([(k.sb("sgt", [128, CAP], F32), Dep()) for _ in range(2)])
                Ysr = Ring([(k.sb("Ys", [128, D], BF16), Dep()) for _ in range(2)])
                def expert_fn(e_):
                    wg, wgd = wgr.next(); wu, wud = wur.next(); wd, wdd = wdr.next()
                    k.dma("pool", wg[:], w_gate[e_].rearrange("(c p) n -> p c n", p=128), w=[wgd])
                    k.dma("pool", wu[:], w_up[e_].rearrange("(c p) n -> p c n", p=128), w=[wud])
                    k.dma("pool", wd[:], w_down[e_].rearrange("(c p) n -> p c n", p=128), w=[wdd])
                    Xe, Xed = Xer.next(); XT, XTd = XTr.next(); AT, ATd = ATr.next()
                    k.dma("sp", Xe[:], s_Xg[e_ * CAP:(e_ + 1) * CAP, :].rearrange("(t p) d -> p t d", p=128), r=[d_Xg], w=[Xed])
                    for t in range(NCT):
                        for g in range(4):
                            pt, pd = pb.next()

                            def f(pe, pt=pt, Xe=Xe, t=t, g=g):
                                for j in range(4):
                                    c = g * 4 + j
                                    ins = pe.transpose(pt[:, j * 128:(j + 1) * 128], Xe[:, t, c * 128:(c + 1) * 128], identb[:])
                                return ins
                            k.op("pe", f, r=[Xed, d_c], w=[pd])
                            eng = "dve" if (g % 2 == 0) else "act"
                            if eng == "dve":
                                k.op("dve", lambda e, XT=XT, pt=pt, g=g, t=t: e.tensor_copy(
                                    XT[:, g * 4:(g + 1) * 4, t * 128:(t + 1) * 128], pt[:, 0:512].rearrange("p (j n) -> p j n", j=4)),
                                    r=[pd], w=[XTd])
                            else:
                                k.op("act", lambda e, XT=XT, pt=pt, g=g, t=t: e.copy(
                                    XT[:, g * 4:(g + 1) * 4, t * 128:(t + 1) * 128], pt[:, 0:512].rearrange("p (j n) -> p j n", j=4)),
                                    r=[pd], w=[XTd])
                    for fcx in range(4):
                        bG, bGd = pf.next(); bU, bUd = pf.next()

                        def f(pe, bG=bG, wg=wg, XT=XT, fcx=fcx):
                            for c in range(DC):
                                ins = pe.matmul(bG[:, 0:CAP], wg[:, c, fcx * 128:(fcx + 1) * 128], XT[:, c, :], start=(c == 0), stop=(c == DC - 1))
                            return ins
                        k.op("pe", f, r=[wgd, XTd], w=[bGd])

                        def f(pe, bU=bU, wu=wu, XT=XT, fcx=fcx):
                            for c in range(DC):
                                ins = pe.matmul(bU[:, 0:CAP], wu[:, c, fcx * 128:(fcx + 1) * 128], XT[:, c, :], start=(c == 0), stop=(c == DC - 1))
                            return ins
                        k.op("pe", f, r=[wud, XTd], w=[bUd])
                        sg_t, sg_d = sgr.next()
                        k.op("act", lambda e, sg_t=sg_t, bG=bG: e.activation(sg_t[:], bG[:, 0:CAP], AF.Silu), r=[bGd], w=[sg_d])
                        k.op("dve", lambda e, AT=AT, sg_t=sg_t, bU=bU, fcx=fcx: e.tensor_tensor(AT[:, fcx, :], sg_t[:], bU[:, 0:CAP], ALU.mult),
                             r=[sg_d, bUd], w=[ATd])
                    for t in range(NCT):
                        Ys, Ysd = Ysr.next()
                        for db in range(4):
                            bk, bd = pf.next()

                            def f(pe, bk=bk, AT=AT, wd=wd, t=t, db=db):
                                for fcx in range(4):
                                    ins = pe.matmul(bk[:, :], AT[:, fcx, t * 128:(t + 1) * 128], wd[:, fcx, db * 512:(db + 1) * 512],
                                                    start=(fcx == 0), stop=(fcx == 3))
                                return ins
                            k.op("pe", f, r=[ATd, wdd], w=[bd])
                            if db % 2 == 0:
                                k.op("act", lambda e, Ys=Ys, bk=bk, db=db: e.copy(Ys[:, db * 512:(db + 1) * 512], bk[:, :]), r=[bd], w=[Ysd])
                            else:
                                k.op("dve", lambda e, Ys=Ys, bk=bk, db=db: e.tensor_copy(Ys[:, db * 512:(db + 1) * 512], bk[:, :]), r=[bd], w=[Ysd])
                        r0 = e_ * CAP + t * 128
                        k.dma("sp", s_Yg[r0:r0 + 128, :], Ys[:], r=[Ysd], w=[d_Yg])
                k.interleave([(lambda e_=e_: expert_fn(e_)) for e_ in range(NE)], width=NIL)

            if stop <= 7:
                raise _Stop()
            d_out = Dep()
            with k.scope():
                l2g = k.sb("l2g", [128, D], F32); l2b = k.sb("l2b", [128, D], F32)
                d_l2 = Dep()
                k.dma("sp", l2g[:], ln2g_d.partition_broadcast(128), w=[d_l2])
                k.dma("sp", l2b[:], ln2b_d.partition_broadcast(128), w=[d_l2])
                h1r = Ring([(k.sb("h1", [128, D], F32), Dep()) for _ in range(3)])
                y1r = Ring([(k.sb("y1", [128, D], BF16), Dep()) for _ in range(3)])
                y2r = Ring([(k.sb("y2", [128, D], BF16), Dep()) for _ in range(3)])
                rsr = Ring([(k.sb("rs2", [128, 32], F32), Dep()) for _ in range(3)])
                def g_tile(ti):
                    h1, h1d = h1r.next(); y1, y1d = y1r.next(); y2, y2d = y2r.next()
                    k.dma("sp", h1[:], s_h1[ti * 128:(ti + 1) * 128, :], r=[d_h1], w=[h1d])
                    k.dma("pool", y1[:], s_Yg, r=[d_Yg, d_tab], w=[y1d],
                          indirect=dict(out_offset=None, in_offset=bass.IndirectOffsetOnAxis(ap=dtab[:, ti, 0:1], axis=0)))
                    k.dma("pool", y2[:], s_Yg, r=[d_Yg, d_tab], w=[y2d],
                          indirect=dict(out_offset=None, in_offset=bass.IndirectOffsetOnAxis(ap=dtab[:, ti, 1:2], axis=0)))
                    k.op("act", lambda e, h1=h1: e.mul(h1[:], h1[:], ALPHA), r=[h1d], w=[h1d])
                    k.op("dve", lambda e, h1=h1, y1=y1, ti=ti: e.scalar_tensor_tensor(
                        h1[:], y1[:], wtab[:, ti, 0:1], h1[:], ALU.mult, ALU.add), r=[y1d, d_tab, h1d], w=[h1d])
                    k.op("dve", lambda e, h1=h1, y2=y2, ti=ti: e.scalar_tensor_tensor(
                        h1[:], y2[:], wtab[:, ti, 1:2], h1[:], ALU.mult, ALU.add), r=[y2d, d_tab, h1d], w=[h1d])
                    rs, rsd = rsr.next()
                    st = rs[:, 0:24].rearrange("p (g k) -> p g k", g=4)
                    def fbn(e, rs=rs, h1=h1):
                        for gg in range(4):
                            ins = e.bn_stats(rs[:, 6 * gg:6 * gg + 6], h1[:, gg * 512:(gg + 1) * 512])
                        return ins
                    k.op("dve", fbn, r=[h1d], w=[rsd])
                    k.op("dve", lambda e, rs=rs: e.bn_aggr(rs[:, 24:26], rs[:, 0:24]), r=[rsd], w=[rsd])
                    k.op("act", lambda e, rs=rs: e.activation(rs[:, 26:27], rs[:, 25:26], AF.Ln, bias=epsc[:, 0:1], scale=1.0), r=[rsd, d_c], w=[rsd])
                    k.op("act", lambda e, rs=rs: e.activation(rs[:, 26:27], rs[:, 26:27], AF.Exp, scale=-0.5), r=[rsd], w=[rsd])
                    k.op("dve", lambda e, rs=rs, h1=h1: e.scalar_tensor_tensor(h1[:], h1[:], rs[:, 24:25], l2g[:], ALU.subtract, ALU.mult),
                         r=[rsd, h1d, d_l2], w=[h1d])
                    k.op("dve", lambda e, rs=rs, h1=h1: e.scalar_tensor_tensor(h1[:], h1[:], rs[:, 26:27], l2b[:], ALU.mult, ALU.add),
                         r=[rsd, h1d, d_l2], w=[h1d])
                    k.dma("sp", out_d[ti * 128:(ti + 1) * 128, :], h1[:], r=[h1d], w=[d_out])
                k.interleave([(lambda ti=ti: g_tile(ti)) for ti in range(NTO)], width=3)
        except _Stop:
            gscope.close()
        k.barrier()
        build.stats = (k.n_ins, {n: e.count for n, e in k.E.items()})
    return nc


def _consts(TP, TO, CAP):
    TA = TP + TO
    c = {}
    c["ident"] = np.eye(128, dtype=np.float32)
    s = np.arange(64)
    c["mask64"] = (s[:, None] <= s[None, :]).astype(np.float32)
    p = np.arange(128)
    c["ustrict"] = (p[:, None] < p[None, :]).astype(np.float32)
    slopes = 2.0 ** (-8.0 * np.arange(1, 9) / 8.0)
    kl = p[:, None]; ql = p[None, :]
    vis = (kl // 64) <= (ql // 64)
    dg = np.zeros((128, 8, 128), np.float32)
    for h in range(8):
        b = np.where(kl <= ql, slopes[h] * kl, slopes[h] * (2 * ql - kl))
        dg[:, h, :] = np.where(vis, b, -BIG) * 8.0
    c["diagbias"] = dg
    ab = np.zeros((128, 8, 32), np.float32)
    for h in range(8):
        for dlt in range(32):
            ab[:, h, dlt] = np.maximum(slopes[h] * (p - 128 * dlt), -ACLAMP)
    c["alibi"] = ab
    c["ecap"] = np.tile((np.arange(32) * CAP).astype(np.float32)[None, :], (128, 1))
    return c


def _prep_shared(inp):
    f = lambda a: np.ascontiguousarray(np.asarray(a, dtype=np.float32))
    b_in = f(inp["b_in"][0])
    sh = {}
    sh["w_in"] = f(inp["w_in"][0])

    def fm_bias(b):
        cols = np.concatenate([b[O_QA:O_QA + 1024], b[O_KA:O_KA + 1024], b[O_QB:O_QB + 1024], b[O_KB:O_KB + 1024],
                               b[O_GA:O_GA + 2048], b[O_GB:O_GB + 2048]])
        return np.ascontiguousarray(cols.reshape(64, 128).T)

    def tm_bias(b):
        return np.ascontiguousarray(np.concatenate([b[O_VA:O_VA + 1024], b[O_OA:O_OA + 1024], b[O_VB:O_VB + 1024]])[None, :])

    def if_bias(b):
        return np.ascontiguousarray(np.stack([b[O_IA:O_IA + 4], b[O_FA:O_FA + 4]], axis=1))
    b_mask = np.zeros_like(b_in)
    b_mask[O_IA:O_IA + 4] = -BIG
    b_mask[O_FA:O_FA + 4] = BIG
    sh["bias_real"] = (fm_bias(b_in), tm_bias(b_in), if_bias(b_in))
    sh["bias_mask"] = (fm_bias(b_mask), tm_bias(b_mask), if_bias(b_mask))
    cw = f(inp["conv_w"][0])
    sh["cw"] = np.ascontiguousarray(cw.T.reshape(16, 128, 4).transpose(1, 0, 2))
    sh["cb"] = np.ascontiguousarray(f(inp["conv_b"][0]).reshape(16, 128).T)
    sh["mlstm_norm_g"] = f(inp["mlstm_norm_g"][0])
    sh["diff_norm_g"] = f(inp["diff_norm_g"][0])
    sh["lamv"] = np.ascontiguousarray(np.stack([f(inp["lambda_q1"][0]), f(inp["lambda_k1"][0]),
                                                f(inp["lambda_q2"][0]), f(inp["lambda_k2"][0])]))
    for n in ("w_a", "w_b", "w_out", "ln1_g", "ln1_b", "ln2_g", "ln2_b", "w_gate", "w_up", "w_down"):
        sh[n] = f(inp[n][0])
    sh["w_r"] = np.ascontiguousarray(np.concatenate([f(inp["w_grp"][0]), f(inp["w_exp"][0])], axis=1))
    sh["b_r"] = np.ascontiguousarray(np.concatenate([f(inp["b_grp"][0]), f(inp["b_exp"][0])])[None, :])
    return sh


def make_in_maps(inp, TP, TO, CAP):
    x = np.asarray(inp["x"], dtype=np.float32)
    B, S, _ = x.shape
    assert S == TP + TO or S == 2 * TO
    sh = _prep_shared(inp)
    cs = _consts(TP, TO, CAP)
    TA = TP + TO
    maps = []
    for b in range(B):
        for half in range(2):
            m = {}
            if half == 0:
                xa = np.zeros((TA, D), np.float32)
                xa[TP:] = x[b, 0:TO]
                valid = np.concatenate([np.zeros(TP, np.float32), np.ones(TO, np.float32)])
                pre = sh["bias_mask"]
            else:
                xa = np.ascontiguousarray(x[b, TO - TP:2 * TO])
                valid = np.ones(TA, np.float32)
                pre = sh["bias_real"]
            m["x"] = xa
            m["valid_tm"] = np.ascontiguousarray(valid.reshape(TA // 128, 128).T)
            m["bfm"], m["btm"], m["bif"] = sh["bias_real"]
            m["bfm_pre"], m["btm_pre"], m["bif_pre"] = pre
            for n in ("w_in", "cw", "cb", "mlstm_norm_g", "diff_norm_g", "lamv", "w_a", "w_b", "w_out", "ln1_g", "ln1_b",
                      "ln2_g", "ln2_b", "w_gate", "w_up", "w_down", "w_r", "b_r"):
                m[n] = sh[n]
            m.update(cs)
            maps.append(m)
    return maps


_TP, _TO, _CAP = 2048, 2048, 256


def kernel(**inputs):
    x = np.asarray(inputs["x"])
    B, S, _ = x.shape
    maps = make_in_maps(inputs, _TP, _TO, _CAP)
    nc = build(_TP, _TO, _CAP)
    res = run_bass_kernel_spmd(nc, maps, core_ids=list(range(len(maps))))
    out = np.zeros((B, S, D), np.float32)
    i = 0
    for b in range(B):
        for half in range(2):
            out[b, half * _TO:(half + 1) * _TO] = res.results[i]["out"]
            i += 1
    return out
```
